# Optimizing a Trainium2 kernel written in Bass

```python
import jax
import jax.numpy as jnp
from jax import lax
import numpy as np

D_MODEL = 1024
BATCH = 8
SEQ = 4096
DEPTH = 2

CTX_LEN = 256
GRID_W = 64
BRANCH_WIDTH = D_MODEL // 2
N_BRANCHES = 3
RET_HEADS = 4
RET_V_DIM = BRANCH_WIDTH // RET_HEADS
RET_QK_DIM = RET_V_DIM // 2
RET_CHUNK = 128
SGU_WIDTH = BRANCH_WIDTH
SGU_GROUPS = 4
SGU_CHUNK = 128
ATT_HEAD_DIM = 64
ATT_HEADS = BRANCH_WIDTH // ATT_HEAD_DIM
ATT_KV_HEADS = 2
ATT_GROUP = ATT_HEADS // ATT_KV_HEADS
WINDOW = 128
ATT_BLOCK = 128
ROPE_BASE = 10000.0
N_EXPERTS = 16
EXPERT_FF = D_MODEL
CAPACITY_FACTOR = 2
EPS = 1e-6
NEG_INF = -1e30
IN_LAYOUT = (
    ('ret_q', RET_HEADS * RET_QK_DIM),
    ('ret_k', RET_HEADS * RET_QK_DIM),
    ('ret_v', RET_HEADS * RET_V_DIM),
    ('ret_g', RET_HEADS * RET_V_DIM),
    ('sgu_u', SGU_WIDTH),
    ('sgu_v', SGU_WIDTH),
    ('att_q', ATT_HEADS * ATT_HEAD_DIM),
    ('att_k', ATT_KV_HEADS * ATT_HEAD_DIM),
    ('att_v', ATT_KV_HEADS * ATT_HEAD_DIM),
    ('gates', N_BRANCHES * D_MODEL),
)
IN_WIDTH = 2 * RET_HEADS * RET_QK_DIM + 2 * RET_HEADS * RET_V_DIM + 2 * SGU_WIDTH + (ATT_HEADS + 2 * ATT_KV_HEADS) * ATT_HEAD_DIM + N_BRANCHES * D_MODEL
CTX_KV_NAMES = ('ret_k', 'ret_v', 'att_k', 'att_v')

kernel_name = 'hybrid_retention_sgu_swa_ecmoe_dit'


def _rms_norm(x, gain):
    xf = x.astype(jnp.float32)
    y = xf * lax.rsqrt(jnp.mean(xf * xf, axis=-1, keepdims=True) + EPS)
    return y.astype(x.dtype) * gain


def _layer_norm(x):
    xf = x.astype(jnp.float32)
    xc = xf - jnp.mean(xf, axis=-1, keepdims=True)
    return (xc * lax.rsqrt(jnp.mean(xc * xc, axis=-1, keepdims=True) + EPS)).astype(x.dtype)


def _layout_ranges():
    ranges, start = {}, 0
    for name, width in IN_LAYOUT:
        ranges[name] = (start, start + width)
        start += width
    return ranges


def _project(h, w_in, names):
    ranges = _layout_ranges()
    w = jnp.concatenate([w_in[:, ranges[n][0]:ranges[n][1]] for n in names], axis=1)
    z = h @ w
    widths = [ranges[n][1] - ranges[n][0] for n in names]
    offs = np.cumsum(widths)[:-1].tolist()
    return dict(zip(names, jnp.split(z, offs, axis=-1)))


def _heads(t, n, d):
    return t.reshape(t.shape[:-1] + (n, d))


def _axial_rope(rows, head_dim, dtype):
    f32 = jnp.float32
    row = jnp.repeat(jnp.arange(rows), GRID_W).astype(f32)
    col = jnp.tile(jnp.arange(GRID_W), rows).astype(f32)
    n_freq = head_dim // 4
    inv = jnp.power(ROPE_BASE, -jnp.arange(n_freq, dtype=f32) / n_freq)
    ang = jnp.concatenate([row[:, None] * inv, col[:, None] * inv], axis=-1)
    return jnp.cos(ang).astype(dtype), jnp.sin(ang).astype(dtype)


def _apply_rope(x, cos, sin):
    half = x.shape[-1] // 2
    shape = (x.shape[1],) + (1,) * (x.ndim - 3) + (half,)
    cos = cos.reshape(shape)
    sin = sin.reshape(shape)
    x1, x2 = x[..., :half], x[..., half:]
    return jnp.concatenate([x1 * cos - x2 * sin, x1 * sin + x2 * cos], axis=-1)


def _retention_dir(q, k, v, log_gamma, state0, include_diag):
    f32 = jnp.float32
    Bn, T, H, dk = q.shape
    dv = v.shape[-1]
    C = RET_CHUNK
    N = T // C
    qc = q.astype(f32).reshape(Bn, N, C, H, dk)
    kc = k.astype(f32).reshape(Bn, N, C, H, dk)
    vc = v.astype(f32).reshape(Bn, N, C, H, dv)
    idx = jnp.arange(C, dtype=f32)
    diff = idx[:, None] - idx[None, :]
    mask = (diff >= 0) if include_diag else (diff > 0)
    dec = jnp.where(mask[None], jnp.exp(jnp.where(mask, diff, 0.0)[None] * log_gamma[:, None, None]), 0.0)
    scores = jnp.einsum('bnihd,bnjhd->bnhij', qc, kc) * dec[None, None]
    inner = jnp.einsum('bnhij,bnjhe->bnihe', scores, vc)
    w_k = jnp.exp((C - 1 - idx)[:, None] * log_gamma[None, :])
    upd = jnp.einsum('bnjhd,bnjhe->nbhde', kc * w_k[None, None, :, :, None], vc)
    chunk_decay = jnp.exp(C * log_gamma)[None, :, None, None]

    def step(s, u):
        return chunk_decay * s + u, s

    final, s_prev = lax.scan(step, state0, upd)
    q_dec = jnp.exp((idx + 1.0)[:, None] * log_gamma[None, :])
    cross = jnp.einsum('bnihd,nbhde->bnihe', qc * q_dec[None, None, :, :, None], s_prev)
    return (inner + cross).reshape(Bn, T, H, dv), final


def _context_state(k, v, log_gamma):
    T = k.shape[1]
    w = jnp.exp((T - 1 - jnp.arange(T, dtype=jnp.float32))[:, None] * log_gamma[None, :])
    return jnp.einsum('bthd,bthe->bhde', k.astype(jnp.float32) * w[None, :, :, None], v.astype(jnp.float32))


def _retention_out(o, g):
    Bn, T = o.shape[:2]
    y = _layer_norm(o).reshape(Bn, T, -1).astype(g.dtype)
    return y * jax.nn.silu(g)


def _sgu(zu, zv, w_s, b_s):
    Bn, T, _ = zu.shape
    u = jax.nn.gelu(zu)
    v = _layer_norm(jax.nn.gelu(zv))
    n = T // SGU_CHUNK
    vg = v.reshape(Bn, n, SGU_CHUNK, SGU_GROUPS, SGU_WIDTH // SGU_GROUPS)
    mixed = jnp.einsum('gij,bnjgc->bnigc', w_s, vg) + b_s.T[:, :, None]
    return u * mixed.reshape(Bn, T, SGU_WIDTH)


def _window_attention(q, k, v, kc, vc, sink):
    f32 = jnp.float32
    Bn, S = q.shape[:2]
    nb = S // ATT_BLOCK
    L = kc.shape[1]
    scale = ATT_HEAD_DIM ** -0.5
    qb = q.reshape(Bn, nb, ATT_BLOCK, ATT_KV_HEADS, ATT_GROUP, ATT_HEAD_DIM)

    def band(t):
        tp = jnp.pad(t, ((0, 0), (ATT_BLOCK, ATT_BLOCK), (0, 0), (0, 0)))
        tp = tp.reshape(Bn, nb + 2, ATT_BLOCK, ATT_KV_HEADS, ATT_HEAD_DIM)
        return jnp.concatenate([tp[:, :-2], tp[:, 1:-1], tp[:, 2:]], axis=2)

    kb, vb = band(k), band(v)
    s_loc = jnp.einsum('bnihgd,bnjhd->bhgnij', qb, kb).astype(f32) * scale
    blk = jnp.arange(nb)[:, None, None] * ATT_BLOCK
    q_pos = blk + jnp.arange(ATT_BLOCK)[None, :, None]
    k_pos = blk - ATT_BLOCK + jnp.arange(3 * ATT_BLOCK)[None, None, :]
    valid = (jnp.abs(q_pos - k_pos) <= WINDOW) & (k_pos >= 0) & (k_pos < S)
    s_loc = jnp.where(valid, s_loc, NEG_INF)
    s_ctx = jnp.einsum('bnihgd,bjhd->bhgnij', qb, kc).astype(f32) * scale
    s_sink = jnp.broadcast_to(sink.astype(f32).reshape(1, ATT_KV_HEADS, ATT_GROUP, 1, 1, 1), s_loc.shape[:-1] + (1,))
    probs = jax.nn.softmax(jnp.concatenate([s_loc, s_ctx, s_sink], axis=-1), axis=-1).astype(v.dtype)
    n_loc = 3 * ATT_BLOCK
    o = (jnp.einsum('bhgnij,bnjhd->bnihgd', probs[..., :n_loc], vb)
         + jnp.einsum('bhgnij,bjhd->bnihgd', probs[..., n_loc:n_loc + L], vc))
    return o.reshape(Bn, S, ATT_HEADS * ATT_HEAD_DIM)


def _context_attention(q, kc, vc, sink):
    f32 = jnp.float32
    Bn, L = q.shape[:2]
    s = jnp.einsum('bihgd,bjhd->bhgij', q, kc).astype(f32) * (ATT_HEAD_DIM ** -0.5)
    s_sink = jnp.broadcast_to(sink.astype(f32).reshape(1, ATT_KV_HEADS, ATT_GROUP, 1, 1), s.shape[:-1] + (1,))
    probs = jax.nn.softmax(jnp.concatenate([s, s_sink], axis=-1), axis=-1).astype(vc.dtype)
    o = jnp.einsum('bhgij,bjhd->bihgd', probs[..., :L], vc)
    return o.reshape(Bn, L, ATT_HEADS * ATT_HEAD_DIM)


def _merge(branches, gate_logits, w_branch, w_out):
    Bn, T, _ = gate_logits.shape
    gates = jax.nn.sigmoid(gate_logits.reshape(Bn, T, N_BRANCHES, D_MODEL))
    merged = gates[:, :, 0] * (branches[0] @ w_branch[0])
    for i in range(1, N_BRANCHES):
        merged = merged + gates[:, :, i] * (branches[i] @ w_branch[i])
    return merged @ w_out


def _expert_choice_ffn(h, w_router, w_gate, w_up, w_down):
    Bn, T, D = h.shape
    cap = CAPACITY_FACTOR * T // N_EXPERTS
    aff = jax.nn.softmax((h @ w_router).astype(jnp.float32), axis=-1)
    top_w, top_idx = lax.top_k(jnp.swapaxes(aff, 1, 2), cap)
    xs = jax.vmap(lambda hb, ib: hb[ib])(h, top_idx)
    a = jnp.einsum('becd,edf->becf', xs, w_gate)
    u = jnp.einsum('becd,edf->becf', xs, w_up)
    y = jnp.einsum('becf,efd->becd', jax.nn.silu(a) * u, w_down) * top_w[..., None].astype(h.dtype)
    return jax.vmap(lambda ib, yb: jnp.zeros((T, D), h.dtype).at[ib.reshape(-1)].add(yb.reshape(-1, D)))(top_idx, y)


def _layer(x, xc, mod_lat, mod_ctx, rope_ret, rope_att, p, last):
    f32 = jnp.float32
    Bn = x.shape[0]
    sh1, sc1, g1, sh2, sc2, g2 = jnp.split(mod_lat, 6, axis=-1)
    csh1, csc1, cg1, csh2, csc2, cg2 = jnp.split(mod_ctx, 6, axis=-1)
    h = _rms_norm(x, p['norm1_g']) * (1 + sc1) + sh1
    hc = _rms_norm(xc, p['norm1_g']) * (1 + csc1) + csh1
    all_names = tuple(name for name, _ in IN_LAYOUT)
    z = _project(h, p['w_in'], all_names)
    zc = _project(hc, p['w_in'], CTX_KV_NAMES if last else all_names)

    cos_r, sin_r = rope_ret
    lg_f = jax.nn.log_sigmoid(p['ret_decay_fwd'].astype(f32))
    lg_b = jax.nn.log_sigmoid(p['ret_decay_bwd'].astype(f32))
    k_scale = RET_QK_DIM ** -0.5
    rq = _apply_rope(_heads(z['ret_q'], RET_HEADS, RET_QK_DIM), cos_r, sin_r)
    rk = _apply_rope(_heads(z['ret_k'], RET_HEADS, RET_QK_DIM), cos_r, sin_r) * k_scale
    rv = _heads(z['ret_v'], RET_HEADS, RET_V_DIM)
    rkc = _heads(zc['ret_k'], RET_HEADS, RET_QK_DIM) * k_scale
    rvc = _heads(zc['ret_v'], RET_HEADS, RET_V_DIM)
    if last:
        s_f = _context_state(rkc, rvc, lg_f)
        s_b = _context_state(rkc[:, ::-1], rvc[:, ::-1], lg_b)
    else:
        rqc = _heads(zc['ret_q'], RET_HEADS, RET_QK_DIM)
        zero = jnp.zeros((Bn, RET_HEADS, RET_QK_DIM, RET_V_DIM), f32)
        oc_f, s_f = _retention_dir(rqc, rkc, rvc, lg_f, zero, True)
        oc_b, s_b = _retention_dir(rqc[:, ::-1], rkc[:, ::-1], rvc[:, ::-1], lg_b, zero, False)
        ret_c = _retention_out(oc_f + oc_b[:, ::-1], zc['ret_g'])
    o_f, _ = _retention_dir(rq, rk, rv, lg_f, s_f, True)
    o_b, _ = _retention_dir(rq[:, ::-1], rk[:, ::-1], rv[:, ::-1], lg_b, s_b, False)
    ret = _retention_out(o_f + o_b[:, ::-1], z['ret_g'])

    sgu = _sgu(z['sgu_u'], z['sgu_v'], p['sgu_w'], p['sgu_b'])

    cos_a, sin_a = rope_att
    aq = _rms_norm(_heads(z['att_q'], ATT_KV_HEADS, ATT_GROUP * ATT_HEAD_DIM).reshape(z['att_q'].shape[:2] + (ATT_KV_HEADS, ATT_GROUP, ATT_HEAD_DIM)), p['q_norm_g'])
    aq = _apply_rope(aq, cos_a, sin_a)
    ak = _apply_rope(_rms_norm(_heads(z['att_k'], ATT_KV_HEADS, ATT_HEAD_DIM), p['k_norm_g']), cos_a, sin_a)
    av = _heads(z['att_v'], ATT_KV_HEADS, ATT_HEAD_DIM)
    akc = _rms_norm(_heads(zc['att_k'], ATT_KV_HEADS, ATT_HEAD_DIM), p['k_norm_g'])
    avc = _heads(zc['att_v'], ATT_KV_HEADS, ATT_HEAD_DIM)
    att = _window_attention(aq, ak, av, akc, avc, p['attn_sink'])

    x = x + g1 * _merge((ret, sgu, att), z['gates'], p['w_branch'], p['w_out'])
    h2 = _rms_norm(x, p['norm2_g']) * (1 + sc2) + sh2
    x = x + g2 * _expert_choice_ffn(h2, p['w_router'], p['w_exp_gate'], p['w_exp_up'], p['w_exp_down'])
    if last:
        return x, None

    sgu_c = _sgu(zc['sgu_u'], zc['sgu_v'], p['sgu_w'], p['sgu_b'])
    aqc = _rms_norm(zc['att_q'].reshape(zc['att_q'].shape[:2] + (ATT_KV_HEADS, ATT_GROUP, ATT_HEAD_DIM)), p['q_norm_g'])
    att_c = _context_attention(aqc, akc, avc, p['attn_sink'])
    xc = xc + cg1 * _merge((ret_c, sgu_c, att_c), zc['gates'], p['w_branch'], p['w_out'])
    h2c = _rms_norm(xc, p['norm2_g']) * (1 + csc2) + csh2
    xc = xc + cg2 * _expert_choice_ffn(h2c, p['w_router'], p['w_exp_gate'], p['w_exp_up'], p['w_exp_down'])
    return x, xc


def setup_inputs(seed: int = 0) -> dict:
    key = jax.random.key(seed)
    ks = jax.random.split(key, 24)
    f32 = jnp.float32
    D = D_MODEL

    def nrm(k, shape, s):
        return jax.random.normal(k, shape, f32) * s

    hidx = jnp.arange(RET_HEADS, dtype=f32)
    base_logit = jnp.log(jnp.power(2.0, 5.0 + hidx) - 1.0)
    return {
        'x': nrm(ks[0], (BATCH, SEQ, D), 1.0),
        'c': nrm(ks[1], (BATCH, D), 1.0),
        'ctx': nrm(ks[2], (BATCH, CTX_LEN, D), 1.0),
        'c_ctx': nrm(ks[3], (D,), 1.0),
        'w_mod': nrm(ks[4], (DEPTH, D, 6 * D), 0.5 * D ** -0.5),
        'b_mod': nrm(ks[5], (DEPTH, 6 * D), 0.02),
        'norm1_g': 1.0 + nrm(ks[6], (DEPTH, D), 0.02),
        'norm2_g': 1.0 + nrm(ks[7], (DEPTH, D), 0.02),
        'w_in': nrm(ks[8], (DEPTH, D, IN_WIDTH), D ** -0.5),
        'ret_decay_fwd': base_logit + nrm(ks[9], (DEPTH, RET_HEADS), 0.05),
        'ret_decay_bwd': base_logit + nrm(ks[10], (DEPTH, RET_HEADS), 0.05),
        'sgu_w': nrm(ks[11], (DEPTH, SGU_GROUPS, SGU_CHUNK, SGU_CHUNK), SGU_CHUNK ** -0.5),
        'sgu_b': 1.0 + nrm(ks[12], (DEPTH, SGU_GROUPS, SGU_CHUNK), 0.02),
        'q_norm_g': 1.0 + nrm(ks[13], (DEPTH, ATT_HEAD_DIM), 0.02),
        'k_norm_g': 1.0 + nrm(ks[14], (DEPTH, ATT_HEAD_DIM), 0.02),
        'attn_sink': nrm(ks[15], (DEPTH, ATT_HEADS), 0.5),
        'w_branch': nrm(ks[16], (DEPTH, N_BRANCHES, BRANCH_WIDTH, D), BRANCH_WIDTH ** -0.5),
        'w_out': nrm(ks[17], (DEPTH, D, D), D ** -0.5),
        'w_router': nrm(ks[18], (DEPTH, D, N_EXPERTS), D ** -0.5),
        'w_exp_gate': nrm(ks[19], (DEPTH, N_EXPERTS, D, EXPERT_FF), D ** -0.5),
        'w_exp_up': nrm(ks[20], (DEPTH, N_EXPERTS, D, EXPERT_FF), D ** -0.5),
        'w_exp_down': nrm(ks[21], (DEPTH, N_EXPERTS, EXPERT_FF, D), EXPERT_FF ** -0.5),
    }


def reference(x, c, ctx, c_ctx, w_mod, b_mod, norm1_g, norm2_g, w_in, ret_decay_fwd, ret_decay_bwd, sgu_w, sgu_b, q_norm_g, k_norm_g, attn_sink, w_branch, w_out, w_router, w_exp_gate, w_exp_up, w_exp_down):
    rows = x.shape[1] // GRID_W
    rope_ret = _axial_rope(rows, RET_QK_DIM, x.dtype)
    rope_att = _axial_rope(rows, ATT_HEAD_DIM, x.dtype)
    c_act = jax.nn.silu(c)
    c_ctx_act = jax.nn.silu(c_ctx)
    xc = ctx
    for l in range(DEPTH):
        p = {
            'norm1_g': norm1_g[l], 'norm2_g': norm2_g[l], 'w_in': w_in[l],
            'ret_decay_fwd': ret_decay_fwd[l], 'ret_decay_bwd': ret_decay_bwd[l],
            'sgu_w': sgu_w[l], 'sgu_b': sgu_b[l],
            'q_norm_g': q_norm_g[l], 'k_norm_g': k_norm_g[l], 'attn_sink': attn_sink[l],
            'w_branch': w_branch[l], 'w_out': w_out[l], 'w_router': w_router[l],
            'w_exp_gate': w_exp_gate[l], 'w_exp_up': w_exp_up[l], 'w_exp_down': w_exp_down[l],
        }
        mod_lat = (c_act @ w_mod[l] + b_mod[l])[:, None, :]
        mod_ctx = (c_ctx_act @ w_mod[l] + b_mod[l])[None, None, :]
        x, xc = _layer(x, xc, mod_lat, mod_ctx, rope_ret, rope_att, p, l == DEPTH - 1)
    return x
```

```python
import contextlib
import numpy as np
import concourse.bass as bass
import concourse.mybir as mybir
from concourse.bass_utils import run_bass_kernel_spmd

F32 = mybir.dt.float32
BF16 = mybir.dt.bfloat16
I32 = mybir.dt.int32
AF = mybir.ActivationFunctionType
ALU = mybir.AluOpType
AX = mybir.AxisListType

D = 1024
SEQ = 4096
CTX = 256
NLAT = 32
NT = 34
DEPTH = 2
NEXP = 16
EPS = 1e-6
INW = 6400
NG = 3328

_DBG = {}


class Res:
    __slots__ = ("name", "w", "r", "excl")

    def __init__(self, name="", excl=False):
        self.name = name
        self.w = None
        self.r = []
        self.excl = excl


class _Op:
    __slots__ = ("eng", "fn", "reads", "writes", "dma", "deps", "inc", "val", "semid")

    def __init__(self, eng, fn, reads, writes, dma):
        self.eng = eng
        self.fn = fn
        self.reads = reads
        self.writes = writes
        self.dma = dma
        self.deps = ()
        self.inc = False
        self.val = 0
        self.semid = None


class Prog:
    NDMASEM = 48
    NHW = 32

    def __init__(self, nc, es):
        self.nc = nc
        self.ops = []
        self.done = 0
        self.engs = {"pe": nc.tensor, "act": nc.scalar, "dve": nc.vector,
                     "pool": nc.gpsimd, "sp": nc.sync}
        self.sems = {e: es.enter_context(nc.semaphore("sem_" + e)) for e in self.engs}
        self.dsems = [es.enter_context(nc.semaphore("dsem%d" % i)) for i in range(self.NDMASEM)]
        self.dlast = [None] * self.NDMASEM
        self.drr = 0
        self.drr_sw = 0
        self.ecount = {e: 0 for e in self.engs}
        self.dcount = [0] * self.NDMASEM
        self.waited = {e: {} for e in self.engs}
        self.nwait = 0
        self._cap = None
        self._unit = None

    def add(self, eng, fn, reads=(), writes=(), dma=False):
        reads = tuple(reads)
        writes = tuple(writes)
        ex = tuple(r for r in reads if r.excl)
        if ex:
            reads = tuple(r for r in reads if not r.excl)
            writes = writes + ex
        op = _Op(eng, fn, reads, writes, dma)
        if self._cap is not None:
            if self._unit is not None:
                self._unit.append(op)
            else:
                self._cap.append([op])
        else:
            self.ops.append(op)

    def capture_start(self):
        self._cap = []
        self._unit = None

    def capture_end(self):
        cap = self._cap
        self._cap = None
        return cap

    @contextlib.contextmanager
    def atomic(self):
        if self._cap is None or self._unit is not None:
            yield
            return
        self._unit = []
        try:
            yield
        finally:
            self._cap.append(self._unit)
            self._unit = None

    def emit_zip(self, chains):
        items = []
        for ci, ch in enumerate(chains):
            lo, hi = 0.0, 1.0
            if isinstance(ch, tuple):
                ch, lo, hi = ch
            n = len(ch)
            for ui, unit in enumerate(ch):
                items.append((lo + (hi - lo) * (ui + 0.5) / n, ci, ui, unit))
        items.sort(key=lambda t: (t[0], t[1]))
        for _, _, _, unit in items:
            self.ops.extend(unit)

    def pe(self, fn, reads=(), writes=()):
        self.add("pe", fn, reads, writes)

    def act(self, fn, reads=(), writes=()):
        self.add("act", fn, reads, writes)

    def dve(self, fn, reads=(), writes=()):
        self.add("dve", fn, reads, writes)

    def pool(self, fn, reads=(), writes=()):
        self.add("pool", fn, reads, writes)

    def dma(self, eng, fn, reads=(), writes=()):
        self.add(eng, fn, reads, writes, dma=True)

    def flush(self):
        ops = self.ops
        base = self.done
        n = len(ops)
        last_compute = {}
        for i in range(base, n):
            op = ops[i]
            raw = set()
            war = set()
            for r in op.reads:
                if r.w is not None:
                    raw.add(r.w)
            for w in op.writes:
                if w.w is not None:
                    raw.add(w.w)
                war.update(w.r)
            if op.dma:
                if op.eng == "pool":
                    s = self.NHW + self.drr_sw
                    self.drr_sw = (self.drr_sw + 1) % (self.NDMASEM - self.NHW)
                else:
                    s = self.drr
                    self.drr = (self.drr + 1) % self.NHW
                op.semid = s
                if self.dlast[s] is not None:
                    raw.add(self.dlast[s])
                self.dlast[s] = i
            else:
                last_compute[op.eng] = i
            deps = set()
            for j in raw:
                if j < base or j == i:
                    continue
                o = ops[j]
                if (not o.dma) and (not op.dma) and o.eng == op.eng and op.eng == "pe":
                    continue
                deps.add(j)
            for j in war:
                if j < base or j == i:
                    continue
                o = ops[j]
                if (not o.dma) and o.eng == op.eng and not op.dma and op.eng == "pe":
                    continue
                deps.add(j)
            for r in op.reads:
                r.r.append(i)
            for w in op.writes:
                w.w = i
                w.r = []
            best = {}
            for j in deps:
                o = ops[j]
                key = ("d", o.semid) if o.dma else ("e", o.eng)
                if key not in best or best[key] < j:
                    best[key] = j
            op.deps = tuple(best.values())
            for j in op.deps:
                ops[j].inc = True
        for e, i in last_compute.items():
            ops[i].inc = True
        for i in range(base, n):
            op = ops[i]
            if op.dma:
                self.dcount[op.semid] += 16
                op.val = self.dcount[op.semid]
            elif op.inc:
                self.ecount[op.eng] += 1
                op.val = self.ecount[op.eng]
        for i in range(base, n):
            op = ops[i]
            eng = self.engs[op.eng]
            wt = self.waited[op.eng]
            for j in op.deps:
                o = ops[j]
                if o.dma:
                    key, sem = ("d", o.semid), self.dsems[o.semid]
                else:
                    key, sem = ("e", o.eng), self.sems[o.eng]
                if wt.get(key, 0) >= o.val:
                    continue
                eng.wait_ge(sem, o.val)
                self.nwait += 1
                wt[key] = o.val
            inst = op.fn()
            if op.dma:
                inst.then_inc(self.dsems[op.semid], 16)
            elif op.inc:
                inst.then_inc(self.sems[op.eng], 1)
            op.fn = None
        for e, eng in self.engs.items():
            wt = self.waited[e]
            for x in self.engs:
                if x == e:
                    continue
                v = self.ecount[x]
                if v > wt.get(("e", x), 0):
                    eng.wait_ge(self.sems[x], v)
                    wt[("e", x)] = v
            for s in range(self.NDMASEM):
                v = self.dcount[s]
                if v > wt.get(("d", s), 0):
                    eng.wait_ge(self.dsems[s], v)
                    wt[("d", s)] = v
        self.done = n


def bc(ap, axis, n):
    s = list(ap.shape)
    s.insert(axis, n)
    return ap.unsqueeze(axis).to_broadcast(s)


def _rope_tables():
    f32 = np.float32
    rows = SEQ // 64
    row = np.repeat(np.arange(rows), 64).astype(f32)
    col = np.tile(np.arange(64), rows).astype(f32)
    nf = 16
    inv = np.power(f32(10000.0), -np.arange(nf, dtype=f32) / f32(nf)).astype(f32)
    ang = np.concatenate([row[:, None] * inv, col[:, None] * inv], axis=-1).astype(f32)
    cos = np.cos(ang).astype(f32)
    sin = np.sin(ang).astype(f32)
    tab = np.zeros((NT * 128, 128), f32)
    tab[:SEQ, 0:32] = cos
    tab[:SEQ, 32:64] = cos
    tab[:SEQ, 64:96] = -sin
    tab[:SEQ, 96:128] = sin
    tab[SEQ:, 0:64] = 1.0
    return tab.reshape(NT, 128, 128)


CT = {}


def _const_table():
    cols = []

    def put(name, arr):
        arr = np.asarray(arr, np.float32).reshape(128, -1)
        CT[name] = (sum(c.shape[1] for c in cols), arr.shape[1])
        cols.append(arr)

    j = np.arange(128)[:, None]
    i = np.arange(128)[None, :]
    put("ident", (j == i))
    put("ones", np.ones((128, 128)))
    put("PF", np.maximum(i - j, 0))
    put("PB", np.maximum(j - i, 0))
    put("MF", (i >= j))
    put("MB", (j > i))
    put("iota1", np.broadcast_to(i + 1, (128, 128)))
    put("cmi", np.broadcast_to(128 - i, (128, 128)))
    put("cjm", 127 - j)
    put("jcol", j)
    put("tri", (j < i))
    put("iota512", np.broadcast_to(np.arange(512)[None, :], (128, 512)))
    mp = np.where(j >= i, 0.0, -30000.0)
    mn = np.where(j <= i, 0.0, -30000.0)
    put("mprev", np.tile(mp, (1, 4)))
    put("mnext", np.tile(mn, (1, 4)))
    put("ttv", np.broadcast_to(np.arange(NLAT)[None, :], (128, NLAT)))
    put("eps", np.full((128, 1), EPS))
    return np.concatenate(cols, axis=1)


def build_program(stop_after=None, dbg_out=False):
    nc = bass.Bass("TRN2", target_bir_lowering=False)
    es = contextlib.ExitStack()
    with es:
        _build(nc, es, stop_after, dbg_out)
    return nc


def _build(nc, es, stop_after, dbg_out):
    ctab_np = _const_table()
    NCT = ctab_np.shape[1]

    def din(name, shape, dt=F32):
        return nc.dram_tensor(name, list(shape), dt, kind="ExternalInput").ap()

    x_in = din("x", [SEQ, D])
    ctx_in = din("ctxin", [CTX, D])
    cT_d = din("cT", [128, 16])
    w_mod = din("w_mod", [DEPTH, D, 6 * D])
    bmT_d = din("bmT", [DEPTH, 128, 48])
    n1T_d = din("n1T", [DEPTH, 128, 8])
    n2T_d = din("n2T", [DEPTH, 128, 8])
    w_in = din("w_in", [DEPTH, D, INW])
    decbc_d = din("decbc", [DEPTH, 128, 8])
    dechp_d = din("dechp", [DEPTH, 128, 4])
    swT_d = din("sgu_wT", [DEPTH, 4, 128, 128])
    sbb_d = din("sgu_bbc", [DEPTH, 128, 512])
    qg_d = din("qg", [DEPTH, 128, 64])
    kg_d = din("kg", [DEPTH, 128, 64])
    sink_d = din("sink", [DEPTH, 128, 8])
    w_br = din("w_branch", [DEPTH, 3, 512, D])
    w_out = din("w_out", [DEPTH, D, D])
    w_rt = din("w_router", [DEPTH, D, NEXP])
    w_eg = din("w_exp_gate", [DEPTH, NEXP, D, D])
    w_eu = din("w_exp_up", [DEPTH, NEXP, D, D])
    w_ed = din("w_exp_down", [DEPTH, NEXP, D, D])
    rope_d = din("rope", [NT, 128, 128])
    ctab_d = din("ctab", [128, NCT])
    out = nc.dram_tensor("out", [SEQ, D], F32, kind="ExternalOutput").ap()
    xc = nc.dram_tensor("xc", [CTX, D], F32, kind="ExternalOutput").ap()
    BR = nc.dram_tensor("BR", [3, 512, NT * 128], BF16, kind="ExternalOutput").ap()
    H2 = nc.dram_tensor("H2", [NT * 128, D], BF16, kind="ExternalOutput").ap()

    P = Prog(nc, es)

    _uid = [0]

    def sb(name, shape, dt, stack=es):
        _uid[0] += 1
        return stack.enter_context(nc.sbuf_tensor("%s_%d" % (name, _uid[0]), list(shape), dt))

    bank = [es.enter_context(nc.psum_tensor("bank%d" % i, [128, 512], F32)) for i in range(7)]
    bankT = es.enter_context(nc.psum_tensor("bankT", [128, 1024], BF16))
    rbank = [Res("bank%d" % i, excl=True) for i in range(7)]
    _rbT = Res("bankT", excl=True)
    rbT = [_rbT, _rbT]

    RS = [Res("S%d" % t) for t in range(NT)]
    RBR = [[Res() for _ in range(NT)] for _ in range(3)]
    RH2 = [Res() for _ in range(NT)]

    def srows(T):
        return out[T * 128:(T + 1) * 128, :] if T < NLAT else xc[(T - NLAT) * 128:(T - NLAT + 1) * 128, :]

    ctab = sb("ctab", [128, NCT], F32)
    rct = Res("ctab")
    P.dma("sp", lambda: nc.sync.dma_start(out=ctab[:], in_=ctab_d), writes=[rct])

    def ct(name):
        o, n = CT[name]
        return ctab[:, o:o + n]

    identb = sb("identb", [128, 128], BF16)
    onesb = sb("onesb", [128, 128], BF16)
    trib = sb("trib", [128, 128], BF16)
    mprevb = sb("mprevb", [128, 512], BF16)
    mnextb = sb("mnextb", [128, 512], BF16)
    rcb = Res("constb")
    for dst, nm in ((identb, "ident"), (onesb, "ones"), (trib, "tri"), (mprevb, "mprev"), (mnextb, "mnext")):
        (lambda dst, nm: P.dve(lambda: nc.vector.tensor_copy(out=dst[:], in_=ct(nm)), reads=[rct], writes=[rcb]))(dst, nm)
    epsc = ct("eps")

    for q in range(8):
        (lambda q: P.dma("sp", lambda: nc.sync.dma_start(out=out[q * 512:(q + 1) * 512, :], in_=x_in[q * 512:(q + 1) * 512, :]),
                         writes=RS[q * 4:(q + 1) * 4]))(q)
    P.dma("sp", lambda: nc.sync.dma_start(out=xc, in_=ctx_in), writes=RS[NLAT:NT])

    cact = sb("cact", [128, 8, 2], BF16)
    modT = sb("modT", [128, 48, 2], F32)
    A1 = sb("A1", [128, 8, 2], F32)
    A2 = sb("A2", [128, 8, 2], F32)
    rmod = Res("mod")
    rowbcA = None
    rowbcB = None
    rrowA = Res("rowA")
    rrowB = Res("rowB")
    dgt = [sb("dgt%d" % i, [128, 128], F32) for i in range(2)]
    rdg = [Res(), Res()]

    xt = [sb("xt%d" % i, [128, D], F32) for i in range(2)]
    rxt = [Res(), Res()]
    sqj = sb("sqj", [128, D], F32)
    rsq = Res()
    ssum = [sb("ssum%d" % i, [128, 4], F32) for i in range(2)]
    rss = [Res(), Res()]
    xn = [sb("xn%d" % i, [128, D], BF16) for i in range(2)]
    rxn = [Res(), Res()]
    ropet = [sb("ropet%d" % i, [128, 128], F32) for i in range(2)]
    rrope = [Res(), Res()]
    cnt = {"tile": 0, "ev": 0}

    def load_rope(T):
        i = T % 2
        P.dma("sp", lambda: nc.sync.dma_start(out=ropet[i][:], in_=rope_d[T]), writes=[rrope[i]])
        return ropet[i], rrope[i]

    def norm_tile(T, want_x=False):
        i = cnt["tile"] % 2
        cnt["tile"] += 1
        X, rX, S, rS, XN, rXN = xt[i], rxt[i], ssum[i], rss[i], xn[i], rxn[i]
        P.dma("sp", lambda: nc.sync.dma_start(out=X[:], in_=srows(T)), reads=[RS[T]], writes=[rX])
        P.dve(lambda: nc.vector.memset(S[:], 0.0), writes=[rS])
        P.act(lambda: nc.scalar.activation(out=sqj[:], in_=X[:], func=AF.Square, accum_out=S[:, 0:1]),
              reads=[rX], writes=[rsq, rS])
        P.act(lambda: nc.scalar.activation(out=S[:, 1:2], in_=S[:, 0:1], func=AF.Sqrt, scale=1.0 / D, bias=epsc),
              reads=[rS, rct], writes=[rS])
        P.dve(lambda: nc.vector.reciprocal(out=S[:, 2:3], in_=S[:, 1:2]), reads=[rS], writes=[rS])
        P.dve(lambda: nc.vector.tensor_scalar(out=XN[:], in0=X[:], scalar1=S[:, 2:3], scalar2=None, op0=ALU.mult),
              reads=[rX, rS], writes=[rXN])
        return X, rX, XN, rXN, S, rS

    def transpose_mod(XN, rXN, A, Bm, s, dst_fn, rdst):
        for half in range(2):
          with P.atomic():
            rb = rbT[half]
            for kk in range(4):
                k = half * 4 + kk
                (lambda k, kk, half: P.pe(lambda: nc.tensor.transpose(out=bankT[:, half * 512 + kk * 128: half * 512 + (kk + 1) * 128],
                                                                       in_=XN[:, k * 128:(k + 1) * 128], identity=identb[:]),
                                          reads=[rXN, rcb], writes=[rb]))(k, kk, half)
            for kk in range(4):
                k = half * 4 + kk
                src = bankT[:, half * 512 + kk * 128: half * 512 + (kk + 1) * 128]
                if kk % 2 == 0:
                    (lambda k, src: P.act(lambda: nc.scalar.activation(out=dst_fn(k), in_=src, func=AF.Identity,
                                                                       scale=A[:, k, s:s + 1], bias=Bm[:, k, s:s + 1]),
                                          reads=[rb, rmod], writes=[rdst]))(k, src)
                else:
                    (lambda k, src: P.dve(lambda: nc.vector.tensor_scalar(out=dst_fn(k), in0=src, scalar1=A[:, k, s:s + 1],
                                                                          scalar2=Bm[:, k, s:s + 1], op0=ALU.mult, op1=ALU.add),
                                          reads=[rb, rmod], writes=[rdst]))(k, src)

    def build_rowbc(dst, rdst, col_fn):
        n = 0
        for s in range(2):
            for hb in range(2):
                b = bank[hb]
                rb = rbank[hb]
                for cc in range(4):
                    c = hb * 4 + cc
                    g = n % 2
                    n += 1
                    (lambda c, cc, g, s, b, rb: (
                        P.dve(lambda: nc.vector.tensor_scalar(out=dgt[g][:], in0=ct("ident"), scalar1=col_fn(s, c), scalar2=None, op0=ALU.mult),
                              reads=[rct, rmod], writes=[rdg[g]]),
                        P.pe(lambda: nc.tensor.matmul(b[:, cc * 128:(cc + 1) * 128], lhsT=ct("ones"), rhs=dgt[g][:], start=True, stop=True),
                             reads=[rdg[g], rct], writes=[rb])))(c, cc, g, s, b, rb)
                (lambda s, hb, b, rb: P.act(lambda: nc.scalar.copy(out=dst[:, s, hb * 512:(hb + 1) * 512], in_=b[:]),
                                            reads=[rb], writes=[rdst]))(s, hb, b, rb)

    def mm(o, lhsT, rhs, start, stop, reads, writes, tp=None):
        kw = {} if (tp is None or _DBG.get("notp")) else {"tile_position": tp}
        P.pe(lambda: nc.tensor.matmul(o, lhsT=lhsT, rhs=rhs, start=start, stop=stop, **kw), reads, writes)

    def tr(o, in_, ident, reads, writes):
        P.pe(lambda: nc.tensor.transpose(out=o, in_=in_, identity=ident), reads, writes)

    def actf(o, in_, func, reads, writes, **kw):
        P.act(lambda: nc.scalar.activation(out=o, in_=in_, func=func, **kw), reads, writes)

    def cpa(o, in_, reads, writes):
        P.act(lambda: nc.scalar.copy(out=o, in_=in_), reads, writes)

    def cpv(o, in_, reads, writes, eng="dve"):
        if eng == "POOL":
            eng = "pool"
        elif not _DBG.get("usepool"):
            eng = "dve"
        e = nc.vector if eng == "dve" else nc.gpsimd
        P.add(eng, lambda: e.tensor_copy(out=o, in_=in_), reads, writes)

    def tt(o, a, b, op, reads, writes, eng="dve"):
        if eng == "POOL":
            eng = "pool"
        elif not _DBG.get("usepool"):
            eng = "dve"
        e = nc.vector if eng == "dve" else nc.gpsimd
        P.add(eng, lambda: e.tensor_tensor(out=o, in0=a, in1=b, op=op), reads, writes)

    def tsc(o, a, s1, s2, op0, op1, reads, writes, eng="dve"):
        if eng == "POOL":
            eng = "pool"
        elif not _DBG.get("usepool"):
            eng = "dve"
        e = nc.vector if eng == "dve" else nc.gpsimd
        if op1 is None:
            P.add(eng, lambda: e.tensor_scalar(out=o, in0=a, scalar1=s1, scalar2=None, op0=op0), reads, writes)
        else:
            P.add(eng, lambda: e.tensor_scalar(out=o, in0=a, scalar1=s1, scalar2=s2, op0=op0, op1=op1), reads, writes)

    def stt(o, a, s, b, op0, op1, reads, writes, eng="dve"):
        if eng == "POOL":
            eng = "pool"
        elif not _DBG.get("usepool"):
            eng = "dve"
        e = nc.vector if eng == "dve" else nc.gpsimd
        P.add(eng, lambda: e.scalar_tensor_tensor(out=o, in0=a, scalar=s, in1=b, op0=op0, op1=op1), reads, writes)

    def mset(o, v, writes, eng="dve"):
        if eng == "POOL":
            eng = "pool"
        elif not _DBG.get("usepool"):
            eng = "dve"
        e = nc.vector if eng == "dve" else nc.gpsimd
        P.add(eng, lambda: e.memset(o, v), (), writes)

    def dsp(o, in_, reads, writes):
        P.dma("sp", lambda: nc.sync.dma_start(out=o, in_=in_), reads, writes)

    def dpl(o, in_, reads, writes):
        P.dma("pool", lambda: nc.gpsimd.dma_start(out=o, in_=in_), reads, writes)

    def rsqrt_chain(dst_sd, dst_r, src, scale, reads_res, wres):
        n = src.shape[0]
        actf(dst_sd, src, AF.Sqrt, [reads_res, rct], [wres], scale=scale, bias=epsc[0:n, :])
        P.dve(lambda: nc.vector.reciprocal(out=dst_r, in_=dst_sd), [wres], [wres])

    def kwview(w):
        return w.rearrange("(k p) n -> p k n", p=128)

    def phase_B(stk, l, last, ST, rST, DTt, DfT, rdec):
        wng = sb("wng", [128, 8, NG], BF16, stk)
        rwng = Res()
        for q in range(4):
            c0, c1 = q * 832, (q + 1) * 832
            dpl(wng[:, :, c0:c1], kwview(w_in[l][:, c0:c1]), [], [rwng])
        wsT = sb("wsT", [128, 4, 128], BF16, stk)
        sbb = sb("sbb", [128, 512], F32, stk)
        qg = sb("qg", [128, 64], F32, stk)
        kg = sb("kg", [128, 64], F32, stk)
        esink = sb("esink", [128, 8], F32, stk)
        rsm = Res()
        dpl(wsT[:], swT_d[l].rearrange("g j i -> j g i"), [], [rsm])
        dsp(sbb[:], sbb_d[l], [], [rsm])
        dsp(qg[:], qg_d[l], [], [rsm])
        dsp(kg[:], kg_d[l], [], [rsm])
        dsp(esink[:], sink_d[l], [], [rsm])
        tsc(qg[:], qg[:], 0.125, None, ALU.mult, None, [rsm], [rsm])
        actf(esink[:], esink[:], AF.Exp, [rsm], [rsm])
        KT_all = sb("KT_all", [128, NT, 128], BF16, stk)
        V_all = sb("V_all", [128, NT, 2, 80], BF16, stk)
        rKV = [Res() for _ in range(NT)]
        mset(V_all[:], 1.0, rKV)
        hT = [sb("hTb%d" % i, [128, 8, 128], BF16, stk) for i in range(2)]
        rhT = [Res(), Res()]
        zqk = [sb("zqk%d" % i, [128, 512], F32, stk) for i in range(2)]
        vb = [sb("vb%d" % i, [128, 512], BF16, stk) for i in range(2)]
        sg = [sb("sg%d" % i, [128, 512], BF16, stk) for i in range(2)]
        uT = [sb("uT%d" % i, [128, 512], BF16, stk) for i in range(2)]
        gv = [sb("gv%d" % i, [128, 512], F32, stk) for i in range(2)]
        zq = [sb("zq%d" % i, [128, 512], F32, stk) for i in range(2)]
        zk = [sb("zk%d" % i, [128, 128], F32, stk) for i in range(2)]
        rzqk, rvb, rsg, ruT, rgv, rzq, rzk = [[Res(), Res()] for _ in range(7)]
        t1 = sb("t1", [128, 512], F32, stk)
        t2 = sb("t2", [128, 512], F32, stk)
        rtmp = Res()
        t1a = sb("t1a", [128, 512], F32, stk)
        t2a = sb("t2a", [128, 512], F32, stk)
        rtmpa = Res()
        qrb = sb("qrb", [128, 256], BF16, stk)
        krb = sb("krb", [128, 256], BF16, stk)
        rqk = Res()
        qT = sb("qT", [128, 2, 128], BF16, stk)
        qfT = sb("qfT", [128, 2, 2, 128], BF16, stk)
        kT = sb("kT", [128, 2, 128], BF16, stk)
        rqT = Res()
        SD = sb("SD", [128, 512], BF16, stk)
        rSD = Res()
        bst = sb("bst", [128, 4, 6], F32, stk)
        mv = sb("mv", [128, 4, 2], F32, stk)
        sdr = sb("sdr", [128, 8], F32, stk)
        rln = Res()
        yb = sb("yb", [128, 512], BF16, stk)
        retb = sb("retb", [128, 512], BF16, stk)
        ryb = Res()
        brT = [sb("brTo%d" % i, [128, 4, 128], BF16, stk) for i in range(3)]
        rbrT = [Res(), Res(), Res()]
        vn = sb("vn", [128, 512], BF16, stk)
        stmp = sb("stmp", [128, 512], F32, stk)
        rsg2 = Res()
        bst2 = sb("bst2", [128, 6], F32, stk)
        mv2 = sb("mv2", [128, 4], F32, stk)
        sq = sb("sq", [128, 512], F32, stk)
        qn = sb("qn", [128, 512], F32, stk)
        ssq = sb("ssq", [128, 24], F32, stk)
        ratt = Res()
        sqk = sb("sqk", [128, 128], F32, stk)
        kn = sb("kn", [128, 128], F32, stk)
        ssk = sb("ssk", [128, 8], F32, stk)
        rattk = Res()
        qperm = sb("qperm", [128, 4, 2, 64], BF16, stk)
        krb2 = sb("krb2", [128, 128], BF16, stk)
        qT2 = [sb("qT2_%d" % i, [128, 4, 128], BF16, stk) for i in range(2)]
        rqT2 = [Res(), Res()]
        PT = [sb("PT%d" % i, [128, 512], BF16, stk) for i in range(2)]
        rPT = [Res(), Res()]
        den = sb("den", [128, 16], F32, stk)
        attb = sb("attb", [128, 512], BF16, stk)
        rden = Res()
        BRv = [BR[i].rearrange("(c p) t -> p c t", p=128) for i in range(3)]
        pbc = {"n": 0}

        def pbank():
            i = pbc["n"] % 2
            pbc["n"] += 1
            return bank[i], rbank[i]

        def out_branch(i, T, srcb, rsrc):
            with P.atomic():
                for c in range(4):
                    tr(bankT[:, 512 + c * 128: 512 + (c + 1) * 128], srcb[:, c * 128:(c + 1) * 128], identb[:], [rsrc, rcb], [rbT[1]])
                cpa(brT[i][:], bankT[:, 512:1024].rearrange("p (c t) -> p c t", t=128), [rbT[1]], [rbrT[i]])
            dsp(BRv[i][:, :, T * 128:(T + 1) * 128], brT[i][:], [rbrT[i]], [RBR[i][T]])

        def Pst(T, full, par):
            s = 0 if T < NLAT else 1
            X, rX, XN, rXN, S, rS = norm_tile(T)
            load_rope(T)
            H, rH = hT[par], rhT[par]
            transpose_mod(XN, rXN, A1, B1m, s, lambda k: H[:, k, :], rH)

            def proj(c0, c1):
                b, rb = pbank()
                for k in range(8):
                    mm(b[:, 0:c1 - c0], H[:, k, :], wng[:, k, c0:c1], k == 0, k == 7, [rH, rwng], [rb])
                return b, rb

            if full:
                b, rb = proj(0, 512)
                cpa(zqk[par][:], b[:], [rb], [rzqk[par]])
                b, rb = proj(512, 1024)
                cpv(vb[par][:], b[:], [rb], [rvb[par]])
                b, rb = proj(1024, 1536)
                actf(sg[par][:], b[:], AF.Silu, [rb], [rsg[par]])
                b, rb = pbank()
                for c in range(4):
                    for k in range(8):
                        mm(b[:, c * 128:(c + 1) * 128], wng[:, k, 1536 + c * 128:1536 + (c + 1) * 128], H[:, k, :], k == 0, k == 7, [rH, rwng], [rb])
                actf(uT[par][:], b[:], AF.Gelu_apprx_tanh, [rb], [ruT[par]])
                b, rb = proj(2048, 2560)
                actf(gv[par][:], b[:], AF.Gelu_apprx_tanh, [rb], [rgv[par]])
                b, rb = proj(2560, 3072)
                cpv(zq[par][:], b[:], [rb], [rzq[par]])
            b, rb = proj(3072, 3328)
            cpv(zk[par][:], b[:, 0:128], [rb], [rzk[par]])
            cpa(V_all[:, T, :, 0:64], b[:, 128:256].rearrange("p (k d) -> p k d", d=64), [rb], [rKV[T]])

        def chainR(T, par):
            RT, rRT = ropet[T % 2], rrope[T % 2]
            if True:
                rope_apply(P, nc, zqk[par][:, 0:256], rzqk[par], RT, rRT, 4, t1, t2, qrb[:].rearrange("p (h d) -> p h d", d=64), rtmp, rqk)
                rope_apply(P, nc, zqk[par][:, 256:512], rzqk[par], RT, rRT, 4, t1, t2, krb[:].rearrange("p (h d) -> p h d", d=64), rtmp, rqk)
                with P.atomic():
                    for hb in range(2):
                        tr(bankT[:, hb * 128:(hb + 1) * 128], qrb[:, hb * 128:(hb + 1) * 128], identb[:], [rqk, rcb], [rbT[0]])
                        tr(bankT[:, 256 + hb * 128:256 + (hb + 1) * 128], krb[:, hb * 128:(hb + 1) * 128], identb[:], [rqk, rcb], [rbT[0]])
                    qv = bankT[:, 0:256].rearrange("p (b t) -> p b t", t=128)
                    cpa(qT[:], qv, [rbT[0]], [rqT])
                    for dr in range(2):
                        tt(qfT[:, dr, :, :], qv, DfT[:, dr, :, :], ALU.mult, [rbT[0], rdec], [rqT])
                    cpa(kT[:], bankT[:, 256:512].rearrange("p (b t) -> p b t", t=128), [rbT[0]], [rqT])
                for h in range(4):
                    hb, hl = h // 2, h % 2
                    bS = 2 + hl
                    mm(bank[bS][:, hb * 128:(hb + 1) * 128], kT[64 * hl:64 * hl + 64, hb, :], qT[64 * hl:64 * hl + 64, hb, :], True, True,
                       [rqT], [rbank[bS]], tp=(64 * hl, 0))
                for h in range(4):
                    hb, hl = h // 2, h % 2
                    bS = 2 + hl
                    tt(SD[:, h * 128:(h + 1) * 128], bank[bS][:, hb * 128:(hb + 1) * 128], DTt[:, h, :], ALU.mult, [rbank[bS], rdec], [rSD])
                for h in range(4):
                    hb, hl = h // 2, h % 2
                    o = bank[2][:, h * 128:(h + 1) * 128]
                    mm(o, qfT[64 * hl:64 * hl + 64, 0, hb, :], ST[64 * hl:64 * hl + 64, T, 0, hb, :], True, False, [rqT, rST], [rbank[2]], tp=(64 * hl, 0))
                    mm(o, qfT[64 * hl:64 * hl + 64, 1, hb, :], ST[64 * hl:64 * hl + 64, T, 1, hb, :], False, False, [rqT, rST], [rbank[2]], tp=(64 * hl, 0))
                    mm(o, SD[:, h * 128:(h + 1) * 128], vb[par][:, h * 128:(h + 1) * 128], False, True, [rSD, rvb[par]], [rbank[2]])
                cpa(t1[:], bank[2][:], [rbank[2]], [rtmp])
                for h in range(4):
                    (lambda h: P.dve(lambda: nc.vector.bn_stats(out=bst[:, h, :], in_=t1[:, h * 128:(h + 1) * 128]), [rtmp], [rln]))(h)
                    (lambda h: P.dve(lambda: nc.vector.bn_aggr(out=mv[:, h, :], in_=bst[:, h, :]), [rln], [rln]))(h)
                rsqrt_chain(sdr[:, 0:4], sdr[:, 4:8], mv[:, :, 1], 1.0, rln, rln)
                for h in range(4):
                    tsc(yb[:, h * 128:(h + 1) * 128], t1[:, h * 128:(h + 1) * 128], mv[:, h, 0:1], sdr[:, 4 + h:5 + h], ALU.subtract, ALU.mult,
                        [rtmp, rln], [ryb])
                tt(retb[:], yb[:], sg[par][:], ALU.mult, [ryb, rsg[par]], [ryb], eng="POOL")
                out_branch(0, T, retb, ryb)

        def chainS(T, par):
            if True:
                P.dve(lambda: nc.vector.bn_stats(out=bst2[:], in_=gv[par][:]), [rgv[par]], [rsg2])
                P.dve(lambda: nc.vector.bn_aggr(out=mv2[:, 0:2], in_=bst2[:]), [rsg2], [rsg2])
                rsqrt_chain(mv2[:, 2:3], mv2[:, 3:4], mv2[:, 1:2], 1.0, rsg2, rsg2)
                tsc(vn[:], gv[par][:], mv2[:, 0:1], mv2[:, 3:4], ALU.subtract, ALU.mult, [rsg2, rgv[par]], [rsg2])
                for g in range(4):
                    mm(bank[4][:, g * 128:(g + 1) * 128], vn[:, g * 128:(g + 1) * 128], wsT[:, g, :], True, True, [rsg2, rsm], [rbank[4]])
                tt(stmp[:], bank[4][:], sbb[:], ALU.add, [rbank[4], rsm], [rsg2])
                tt(brT[1][:].rearrange("p c t -> p (c t)"), stmp[:], uT[par][:], ALU.mult, [rsg2, ruT[par]], [rbrT[1]], eng="POOL")
                dsp(BRv[1][:, :, T * 128:(T + 1) * 128], brT[1][:], [rbrT[1]], [RBR[1][T]])

        def chainQ(T, par):
            RT, rRT = ropet[T % 2], rrope[T % 2]
            if True:
                zq3 = zq[par][:].rearrange("p (h d) -> p h d", d=64)
                qn3 = qn[:].rearrange("p (h d) -> p h d", d=64)
                actf(sq[:], zq[par][:], AF.Square, [rzq[par]], [ratt])
                P.dve(lambda: nc.vector.tensor_reduce(out=ssq[:, 0:8], in_=sq[:].rearrange("p (h d) -> p h d", d=64), axis=AX.X, op=ALU.add), [ratt], [ratt])
                rsqrt_chain(ssq[:, 8:16], ssq[:, 16:24], ssq[:, 0:8], 1.0 / 64, ratt, ratt)
                tt(qn3, zq3, bc(ssq[:, 16:24], 2, 64), ALU.mult, [rzq[par], ratt], [ratt])
                tt(qn3, qn3, bc(qg[:], 1, 8), ALU.mult, [ratt, rsm], [ratt], eng="POOL")
                for hk in range(2):
                    rope_apply(P, nc, qn[:, hk * 256:(hk + 1) * 256], ratt, RT, rRT, 4, t1a, t2a, qperm[:, :, hk, :], rtmpa, ratt, pool=True)
                p2 = T % 2
                with P.atomic():
                    for g in range(4):
                        tr(bankT[:, g * 128:(g + 1) * 128], qperm[:, g, :, :].rearrange("p k d -> p (k d)"), identb[:], [ratt, rcb], [rbT[0]])
                    cpa(qT2[p2][:], bankT[:, 0:512].rearrange("p (g t) -> p g t", t=128), [rbT[0]], [rqT2[p2]])

        def chainK(T, par):
            RT, rRT = ropet[T % 2], rrope[T % 2]
            zk3 = zk[par][:].rearrange("p (h d) -> p h d", d=64)
            kn3 = kn[:].rearrange("p (h d) -> p h d", d=64)
            actf(sqk[:], zk[par][:], AF.Square, [rzk[par]], [rattk])
            P.dve(lambda: nc.vector.tensor_reduce(out=ssk[:, 0:2], in_=sqk[:].rearrange("p (h d) -> p h d", d=64), axis=AX.X, op=ALU.add), [rattk], [rattk])
            rsqrt_chain(ssk[:, 2:4], ssk[:, 4:6], ssk[:, 0:2], 1.0 / 64, rattk, rattk)
            tt(kn3, zk3, bc(ssk[:, 4:6], 2, 64), ALU.mult, [rzk[par], rattk], [rattk])
            tt(kn3, kn3, bc(kg[:], 1, 2), ALU.mult, [rattk, rsm], [rattk])
            rope_apply(P, nc, kn[:], rattk, RT, rRT, 2, t1a, t2a, krb2[:].rearrange("p (h d) -> p h d", d=64), rtmpa, rattk)
            with P.atomic():
                tr(bankT[:, 512:640], krb2[:], identb[:], [rattk, rcb], [rbT[1]])
                cpa(KT_all[:, T, :], bankT[:, 512:640], [rbT[1]], [rKV[T]])

        stc = {"n": 0}

        def Yst(T):
            p2 = T % 2
            if T < NLAT:
                chunks = []
                if T > 0:
                    chunks.append((T - 1, mprevb))
                chunks.append((T, None))
                if T < NLAT - 1:
                    chunks.append((T + 1, mnextb))
                chunks += [(NLAT, None), (NLAT + 1, None)]
            else:
                chunks = [(NLAT, None), (NLAT + 1, None)]
            for hk in range(2):
                ob = bank[6]
                rob = rbank[6]
                for ci, (c, m) in enumerate(chunks):
                    i = stc["n"] % 2
                    stc["n"] += 1
                    sb_, rsb = bank[5], rbank[5]
                    mm(sb_[:, :], KT_all[64 * hk:64 * hk + 64, c, :], qT2[p2][64 * hk:64 * hk + 64, :, :].rearrange("p g t -> p (g t)"),
                       True, m is None, [rKV[c], rqT2[p2]], [rsb], tp=(64 * hk, 0))
                    if m is not None:
                        mm(sb_[:, :], identb[:], m[:], False, True, [rcb], [rsb])
                    actf(PT[i][:], sb_[:, :], AF.Exp, [rsb], [rPT[i]])
                    for g in range(4):
                        mm(ob[:, g * 128:g * 128 + 65], PT[i][:, g * 128:(g + 1) * 128], V_all[:, c, hk, 0:65], ci == 0 and g == 0,
                           ci == len(chunks) - 1 and g == 3, [rPT[i], rKV[c]], [rob])
                ov = ob[:, 0:512].rearrange("p (g e) -> p g e", e=128)
                tt(den[:, hk * 4:(hk + 1) * 4], ov[:, :, 64], esink[:, hk * 4:(hk + 1) * 4], ALU.add, [rob, rsm], [rden])
                (lambda hk: P.dve(lambda: nc.vector.reciprocal(out=den[:, 8 + hk * 4:8 + (hk + 1) * 4], in_=den[:, hk * 4:(hk + 1) * 4]), [rden], [rden]))(hk)
                tt(attb[:, hk * 256:(hk + 1) * 256].rearrange("p (g d) -> p g d", d=64), ov[:, :, 0:64], bc(den[:, 8 + hk * 4:8 + (hk + 1) * 4], 2, 64),
                   ALU.mult, [rob, rden], [rden])
            out_branch(2, T, attb, rden)

        L = [NLAT, NLAT + 1] + list(range(NLAT))
        full = [(not last) if T >= NLAT else True for T in L]
        Pst(L[0], full[0], 0)
        for i, T in enumerate(L):
            par = i % 2
            chainK(T, par)
            chains = []
            if i + 1 < len(L):
                P.capture_start()
                Pst(L[i + 1], full[i + 1], (i + 1) % 2)
                chains.append(P.capture_end())
            if full[i]:
                for ch in (chainR, chainS, chainQ):
                    P.capture_start()
                    ch(T, par)
                    chains.append(P.capture_end())
            if T < NLAT and T >= 1:
                P.capture_start()
                Yst(T - 1)
                chains.append(P.capture_end())
            P.emit_zip(chains)
            if T == NLAT + 1 and not last:
                Yst(NLAT)
                Yst(NLAT + 1)
            if T == NLAT - 1:
                Yst(T)

    def phase_merge(stk, l, last):
        wgt = sb("wgt", [128, 8, 3072], BF16, stk)
        wbr = sb("wbr", [128, 3, 4, D], BF16, stk)
        wo = sb("wo", [128, 8, D], BF16, stk)
        rw = Res()
        for q in range(3):
            dpl(wgt[:, :, q * 1024:(q + 1) * 1024], kwview(w_in[l][:, NG + q * 1024:NG + (q + 1) * 1024]), [], [rw])
            dpl(wbr[:, q, :, :], kwview(w_br[l][q]), [], [rw])
        dpl(wo[:], kwview(w_out[l]), [], [rw])
        hTg = [sb("hTg%d" % i, [128, 8, 512], BF16, stk) for i in range(2)]
        rhTg = [Res(), Res()]
        brTi = [sb("brTi%d" % i, [128, 3, 4, 512], BF16, stk) for i in range(2)]
        rbrTi = [Res(), Res()]
        mT = sb("mT", [128, 8, 512], BF16, stk)
        rmT = Res()
        sgm = [sb("sgm%d" % i, [128, 512], F32, stk) for i in range(2)]
        rsgm = [Res(), Res()]
        acc = sb("acc", [128, 512], F32, stk)
        tmp = sb("tmpm", [128, 512], F32, stk)
        racc = Res()
        rtmpm = Res()
        xo = [sb("xo%d" % i, [128, D], F32, stk) for i in range(2)]
        rxo = [Res(), Res()]
        xr = [sb("xr%d" % i, [128, D], F32, stk) for i in range(2)]
        rxr = [Res(), Res()]
        BRv = [BR[i].rearrange("(c p) t -> p c t", p=128) for i in range(3)]
        groups = [list(range(g * 4, g * 4 + 4)) for g in range(8)]
        if not last:
            groups = [[NLAT, NLAT + 1]] + groups
        cnts = {"pc": 0, "xc": 0}

        def prologue(gi):
            grp = groups[gi]
            gp = gi % 2
            s_ = 0 if grp[0] < NLAT else 1
            ntok = 128 * len(grp)
            T0 = grp[0]
            for ti, T in enumerate(grp):
                X, rX, XN, rXN, S, rS = norm_tile(T)
                transpose_mod(XN, rXN, A1, B1m, s_, (lambda ti, gp: (lambda k: hTg[gp][:, k, ti * 128:(ti + 1) * 128]))(ti, gp), rhTg[gp])
            for i in range(3):
                dsp(brTi[gp][:, i, :, 0:ntok], BRv[i][:, :, T0 * 128:T0 * 128 + ntok], [RBR[i][T] for T in grp], [rbrTi[gp]])

        def compute(gi):
            grp = groups[gi]
            gp = gi % 2
            s_ = 0 if grp[0] < NLAT else 1
            ntok = 128 * len(grp)
            H, rH, BI, rBI = hTg[gp], rhTg[gp], brTi[gp], rbrTi[gp]
            for dc in range(8):
                for i in range(3):
                    p = cnts["pc"] % 3
                    cnts["pc"] += 1
                    bg, rbg, bb, rbb = bank[2 * p], rbank[2 * p], bank[2 * p + 1], rbank[2 * p + 1]
                    for k in range(8):
                        mm(bg[:, 0:ntok], wgt[:, k, i * 1024 + dc * 128:i * 1024 + (dc + 1) * 128], H[:, k, 0:ntok], k == 0, k == 7, [rw, rH], [rbg])
                    for k in range(4):
                        mm(bb[:, 0:ntok], wbr[:, i, k, dc * 128:(dc + 1) * 128], BI[:, i, k, 0:ntok], k == 0, k == 3, [rw, rBI], [rbb])
                    sgi, rsgi = sgm[cnts["pc"] % 2], rsgm[cnts["pc"] % 2]
                    actf(sgi[:, 0:ntok], bg[:, 0:ntok], AF.Sigmoid, [rbg], [rsgi])
                    if i == 0:
                        tt(acc[:, 0:ntok], sgi[:, 0:ntok], bb[:, 0:ntok], ALU.mult, [rsgi, rbb], [racc])
                    elif i == 1:
                        tt(tmp[:, 0:ntok], sgi[:, 0:ntok], bb[:, 0:ntok], ALU.mult, [rsgi, rbb], [rtmpm])
                        tt(acc[:, 0:ntok], acc[:, 0:ntok], tmp[:, 0:ntok], ALU.add, [racc, rtmpm], [racc], eng="POOL")
                    else:
                        tt(tmp[:, 0:ntok], sgi[:, 0:ntok], bb[:, 0:ntok], ALU.mult, [rsgi, rbb], [rtmpm])
                        tt(mT[:, dc, 0:ntok], acc[:, 0:ntok], tmp[:, 0:ntok], ALU.add, [racc, rtmpm], [rmT], eng="POOL")
            for ti, T in enumerate(grp):
                j = cnts["xc"] % 2
                cnts["xc"] += 1
                xoi, rxoi = xo[j], rxo[j]
                dsp(xr[j][:], srows(T), [RS[T]], [rxr[j]])
                for nb in range(2):
                    for k in range(8):
                        mm(bank[6][:, :], mT[:, k, ti * 128:(ti + 1) * 128], wo[:, k, nb * 512:(nb + 1) * 512], k == 0, k == 7, [rmT, rw], [rbank[6]])
                    tt(xoi[:, nb * 512:(nb + 1) * 512], bank[6][:, :], rowbcA[:, s_, nb * 512:(nb + 1) * 512], ALU.mult, [rbank[6], rrowA], [rxoi])
                tt(xoi[:], xoi[:], xr[j][:], ALU.add, [rxoi, rxr[j]], [rxoi], eng="POOL")
                dsp(srows(T), xoi[:], [rxoi], [RS[T]])

        prologue(0)
        for gi in range(len(groups)):
            chains = []
            P.capture_start()
            compute(gi)
            chains.append(P.capture_end())
            if gi + 1 < len(groups):
                P.capture_start()
                prologue(gi + 1)
                chains.append(P.capture_end())
            P.emit_zip(chains)

    def phase_moe(stk, l, last):
        streams = [(0, list(range(NLAT)), 512, 4, 128)]
        if not last:
            streams.append((1, [NLAT, NLAT + 1], 32, 1, 32))
        aff = sb("aff", [128, NT, NEXP], F32, stk)
        raff = Res()
        idxS = [sb("idxS%d" % i, [128, 4, NEXP], I32, stk) for i in range(2)]
        idxH = [sb("idxH%d" % i, [128, 4, NEXP], I32, stk) for i in range(2)]
        wsel = [sb("wsel%d" % i, [128, 4, NEXP], F32, stk) for i in range(2)]
        ridx = [Res(), Res()]

        with contextlib.ExitStack() as s1:
            wr = sb("wr", [128, 8, NEXP], BF16, s1)
            rwr = Res()
            dpl(wr[:], w_rt[l].rearrange("(k p) e -> p k e", p=128), [], [rwr])
            h2fs = [sb("h2fA", [128, D], F32, s1), sb("h2fB", [128, D], F32, s1)]
            rh2fs = [Res(), Res()]
            h2b = [sb("h2b%d" % i, [128, D], BF16, s1) for i in range(2)]
            rh2 = [Res(), Res()]
            h2T = [sb("h2T%d" % i, [128, 8, 128], BF16, s1) for i in range(2)]
            rh2T = [Res(), Res()]
            sm = [sb("sm%d" % i, [128, 8], F32, s1) for i in range(2)]
            ex = [sb("ex%d" % i, [128, NEXP], F32, s1) for i in range(2)]
            rsmx = [Res(), Res()]
            allT = [(s_, T) for (s_, tiles, cap, nch, rows) in streams for T in tiles]

            def st1(j):
                s_, T = allT[j]
                i = j % 2
                X, rX, XN, rXN, S, rS = norm_tile(T)
                h2f, rh2f = h2fs[i], rh2fs[i]
                stt(h2f[:], X[:], S[:, 2:3], rowbcA[:, s_, :], ALU.mult, ALU.mult, [rX, rS, rrowA], [rh2f])
                tt(h2b[i][:], h2f[:], rowbcB[:, s_, :], ALU.add, [rh2f, rrowB], [rh2[i]], eng="pool")
                dsp(H2[T * 128:(T + 1) * 128, :], h2b[i][:], [rh2[i]], [RH2[T]])
                for half in range(2):
                    with P.atomic():
                        for kk in range(4):
                            k = half * 4 + kk
                            tr(bankT[:, half * 512 + kk * 128:half * 512 + (kk + 1) * 128], h2b[i][:, k * 128:(k + 1) * 128], identb[:], [rh2[i], rcb], [rbT[half]])
                        cpa(h2T[i][:, half * 4:(half + 1) * 4, :], bankT[:, half * 512:(half + 1) * 512].rearrange("p (k t) -> p k t", t=128), [rbT[half]], [rh2T[i]])

            def st2(j):
                s_, T = allT[j]
                i = j % 2
                bk, rbk = bank[2 + i], rbank[2 + i]
                for k in range(8):
                    mm(bk[:, 0:NEXP], h2T[i][:, k, :], wr[:, k, :], k == 0, k == 7, [rh2T[i], rwr], [rbk])
                smi, exi, rsi = sm[i], ex[i], rsmx[i]
                P.dve(lambda: nc.vector.tensor_reduce(out=smi[:, 0:1], in_=bk[:, 0:NEXP], axis=AX.X, op=ALU.max), [rbk], [rsi])
                tsc(smi[:, 1:2], smi[:, 0:1], -1.0, None, ALU.mult, None, [rsi], [rsi])
                mset(smi[:, 2:3], 0.0, [rsi])
                actf(exi[:], bk[:, 0:NEXP], AF.Exp, [rbk, rsi], [rsi], bias=smi[:, 1:2], accum_out=smi[:, 2:3])
                P.dve(lambda: nc.vector.reciprocal(out=smi[:, 3:4], in_=smi[:, 2:3]), [rsi], [rsi])
                tsc(aff[:, T, :], exi[:], smi[:, 3:4], None, ALU.mult, None, [rsi], [raff])

            for j0 in range(0, len(allT), 2):
                chains = []
                for j in (j0, j0 + 1):
                    if j < len(allT):
                        P.capture_start()
                        st1(j)
                        st2(j)
                        chains.append(P.capture_end())
                P.emit_zip(chains)
            build_rowbc(rowbcA, rrowA, lambda s, c: modT[:, 40 + c, s:s + 1])
            P.flush()
        if _DBG.get("cstop") == "1":
            return

        with contextlib.ExitStack() as s2:
            ge = sb("ge", [128, NLAT, NEXP], BF16, s2)
            maskf = sb("maskf", [128, NLAT, NEXP], F32, s2)
            basef = sb("basef", [128, NLAT, NEXP], F32, s2)
            keyf = sb("keyf", [128, NLAT, NEXP], F32, s2)
            cntp = sb("cntp", [128, NLAT, NEXP], F32, s2)
            vals = sb("vals", [128, NLAT, NEXP, 4], BF16, s2)
            valf = sb("valf", [128, NLAT, NEXP], F32, s2)
            lo = sb("lo", [128, NEXP], F32, s2)
            mid = sb("mid", [128, NEXP], F32, s2)
            cpr = sb("cpr", [128, NEXP], F32, s2)
            cntr = sb("cntr", [128, NEXP], F32, s2)
            rr = Res()
            OH = [sb("OH%d" % i, [128, 512], BF16, s2) for i in range(4)]
            rOH = [Res() for _ in range(4)]
            lst = sb("lst", [128, NEXP, 4, 4], F32, s2)
            idxf = sb("idxf", [128, 4, NEXP], F32, s2)
            NIT = 28
            for (s, tiles, cap, nch, rows) in streams:
                nt = len(tiles)
                T0 = tiles[0]
                av = aff[:, T0:T0 + nt, :]
                W = nt * NEXP
                mset(lo[:], 0.0, [rr])
                mset(mid[:], 0.5, [rr])
                for it in range(NIT):
                    half = 0.5 ** (it + 1)
                    tt(ge[:, 0:nt, :], av, bc(mid[:], 1, nt), ALU.is_ge, [raff, rr], [rr])
                    mm(bank[0][:, 0:W], onesb[:], ge[:, 0:nt, :].rearrange("p t e -> p (t e)"), True, True, [rr, rcb], [rbank[0]])
                    P.dve(lambda W=W, nt=nt: nc.vector.tensor_reduce(out=cntr[:], in_=bank[0][:, 0:W].rearrange("p (t e) -> p e t", e=NEXP), axis=AX.X, op=ALU.add),
                          [rbank[0]], [rr])
                    tsc(cpr[:], cntr[:], float(cap) - 0.5, half, ALU.is_ge, ALU.mult, [rr], [rr])
                    stt(mid[:], cpr[:], half * 0.5, lo[:], ALU.add, ALU.add, [rr], [rr])
                    tt(lo[:], lo[:], cpr[:], ALU.add, [rr], [rr])
                tt(maskf[:, 0:nt, :], av, bc(lo[:], 1, nt), ALU.is_ge, [raff, rr], [rr])
                cpv(ge[:, 0:nt, :], maskf[:, 0:nt, :], [rr], [rr])
                gef = ge[:, 0:nt, :].rearrange("p t e -> p (t e)")
                mm(bank[0][:, 0:W], trib[:], gef, True, True, [rr, rcb], [rbank[0]])
                mm(bank[1][:, 0:W], onesb[:], gef, True, True, [rr, rcb], [rbank[1]])
                cpv(cntp[:, 0:nt, :].rearrange("p t e -> p (t e)"), bank[1][:, 0:W], [rbank[1]], [rr])
                mset(basef[:, 0, :], 0.0, [rr])
                for t_ in range(1, nt):
                    tt(basef[:, t_, :], basef[:, t_ - 1, :], cntp[:, t_ - 1, :], ALU.add, [rr], [rr])
                tt(keyf[:, 0:nt, :].rearrange("p t e -> p (t e)"), bank[0][:, 0:W], basef[:, 0:nt, :].rearrange("p t e -> p (t e)"), ALU.add, [rbank[0], rr], [rr])
                stt(keyf[:, 0:nt, :], keyf[:, 0:nt, :], 1.0, maskf[:, 0:nt, :], ALU.add, ALU.mult, [rr], [rr])
                tsc(keyf[:, 0:nt, :], keyf[:, 0:nt, :], -1.0, None, ALU.add, None, [rr], [rr])
                cpv(vals[:, 0:nt, :, 0], bc(ct("ttv")[:, 0:nt], 2, NEXP), [rct], [rr])
                cpv(vals[:, 0:nt, :, 1], ct("jcol").unsqueeze(2).to_broadcast([128, nt, NEXP]), [rct], [rr])
                cpv(vals[:, 0:nt, :, 2], av, [raff], [rr])
                cpv(valf[:, 0:nt, :], vals[:, 0:nt, :, 2], [rr], [rr])
                tt(vals[:, 0:nt, :, 3], av, valf[:, 0:nt, :], ALU.subtract, [raff, rr], [rr])
                capw = nch * 128
                oc = 0
                rvals = rr
                for e in range(NEXP):
                    for t_ in range(nt):
                        i = oc % 4
                        oc += 1
                        tsc(OH[i][:, 0:capw], ct("iota512")[:, 0:capw], keyf[:, t_, e:e + 1], None, ALU.is_equal, None, [rct, rvals], [rOH[i]],
                            eng=("dve" if i % 2 == 0 else "pool"))
                        for c in range(nch):
                            mm(bank[3][:, (e * 4 + c) * 4:(e * 4 + c) * 4 + 4], OH[i][:, c * 128:(c + 1) * 128], vals[:, t_, e, :],
                               e == 0 and t_ == 0 and c == 0, e == NEXP - 1 and t_ == nt - 1 and c == nch - 1, [rOH[i], rvals], [rbank[3]])
                lv = lst[:, :, 0:nch, :]
                cpv(lv, bank[3][:, 0:256].rearrange("p (e c v) -> p e c v", c=4, v=4)[:, :, 0:nch, :], [rbank[3], rvals], [rr])
                iv = idxf[:, 0:nch, :].rearrange("p c e -> p e c")
                stt(iv, lv[:, :, :, 0], 128.0, lv[:, :, :, 1], ALU.mult, ALU.add, [rr], [rr])
                cpv(idxS[s][:, 0:nch, :], idxf[:, 0:nch, :], [rr], [ridx[s]])
                tsc(idxf[:, 0:nch, :], idxf[:, 0:nch, :], float(T0 * 128), None, ALU.add, None, [rr], [rr])
                cpv(idxH[s][:, 0:nch, :], idxf[:, 0:nch, :], [rr], [ridx[s]])
                tt(wsel[s][:, 0:nch, :].rearrange("p c e -> p e c"), lv[:, :, :, 2], lv[:, :, :, 3], ALU.add, [rr], [ridx[s]])
            P.flush()
        if _DBG.get("cstop") == "2":
            return

        wg = [sb("wg%d" % i, [128, 8, D], BF16, stk) for i in range(2)]
        wu = [sb("wu%d" % i, [128, 8, D], BF16, stk) for i in range(2)]
        wd = [sb("wd%d" % i, [128, 8, D], BF16, stk) for i in range(2)]
        rwe = [Res(), Res()]
        NS = len(streams)
        xs = [sb("xs%d" % s_, [128, 4 if s_ == 0 else 1, D], BF16, stk) for s_ in range(NS)]
        rxs = [Res() for _ in range(NS)]
        xsT = [[sb("xsT%d_%d" % (s_, i), [128, 8, 512 if s_ == 0 else 32], BF16, stk) for i in range(2)] for s_ in range(NS)]
        rxsT = [[Res(), Res()] for _ in range(NS)]
        sa = [sb("sa%d" % i, [128, 512], F32, stk) for i in range(2)]
        rsa = [Res(), Res()]
        hf = [sb("hf%d" % s_, [128, 8, 512 if s_ == 0 else 32], BF16, stk) for s_ in range(NS)]
        rhf = [Res() for _ in range(NS)]
        yscs = [sb("ysc%d" % i, [128, D], F32, stk) for i in range(4)]
        ryscs = [Res() for _ in range(4)]
        ycn = {"n": 0}

        def loads(e):
            par = e % 2
            for (wt_, src) in ((wg, w_eg), (wu, w_eu), (wd, w_ed)):
                for hh in range(2):
                    dpl(wt_[par][:, hh * 4:(hh + 1) * 4, :], src[l][e][hh * 512:(hh + 1) * 512, :].rearrange("(k p) n -> p k n", p=128), [], [rwe[par]])

        def gather(e):
            for (s, tiles, cap, nch, rows) in streams:
                for c in range(nch):
                    (lambda c, s, rows, e: P.dma("pool", lambda: nc.gpsimd.indirect_dma_start(
                        out=xs[s][0:rows, c, :], out_offset=None, in_=H2,
                        in_offset=bass.IndirectOffsetOnAxis(ap=idxH[s][0:rows, c, e:e + 1], axis=0)),
                        [ridx[s]] + RH2, [rxs[s]]))(c, s, rows, e)

        def G(e):
            par = e % 2
            for (s, tiles, cap, nch, rows) in streams:
                ntok = nch * rows
                for k in range(8):
                    half = k % 2
                    with P.atomic():
                        for c in range(nch):
                            tr(bankT[:, half * 512 + c * rows: half * 512 + (c + 1) * rows], xs[s][0:rows, c, k * 128:(k + 1) * 128], identb[0:rows, 0:rows],
                               [rxs[s], rcb], [rbT[half]])
                        if k % 2 == 0:
                            cpa(xsT[s][par][:, k, 0:ntok], bankT[:, half * 512: half * 512 + ntok], [rbT[half]], [rxsT[s][par]])
                        else:
                            cpv(xsT[s][par][:, k, 0:ntok], bankT[:, half * 512: half * 512 + ntok], [rbT[half]], [rxsT[s][par]])

        sac = sb("sac", [128, 64], F32, stk)
        rsac = Res()
        yscc = sb("yscc", [128, D], F32, stk)
        ryscc = Res()

        def up(e, si):
            par = e % 2
            (s, tiles, cap, nch, rows) = streams[si]
            ntok = nch * rows
            for fc in range(8):
                if s == 0:
                    p = fc % 2
                    ba, rba, bu, rbu = bank[2 * p][:, 0:ntok], rbank[2 * p], bank[2 * p + 1][:, 0:ntok], rbank[2 * p + 1]
                    sai, rsai = sa[p][:, 0:ntok], rsa[p]
                else:
                    ba, rba, bu, rbu = bank[6][:, 0:ntok], rbank[6], bank[6][:, 64:64 + ntok], rbank[6]
                    sai, rsai = sac[:, 0:ntok], rsac
                for k in range(8):
                    mm(ba, wg[par][:, k, fc * 128:(fc + 1) * 128], xsT[s][par][:, k, 0:ntok], k == 0, k == 7, [rwe[par], rxsT[s][par]], [rba])
                for k in range(8):
                    mm(bu, wu[par][:, k, fc * 128:(fc + 1) * 128], xsT[s][par][:, k, 0:ntok], k == 0, k == 7, [rwe[par], rxsT[s][par]], [rbu])
                actf(sai, ba, AF.Silu, [rba], [rsai])
                tt(hf[s][:, fc, 0:ntok], sai, bu, ALU.mult, [rsai, rbu], [rhf[s]])

        def down(e, si):
            par = e % 2
            (s, tiles, cap, nch, rows) = streams[si]
            dst = out if s == 0 else xc
            rstream = [RS[T] for T in tiles]
            for c in range(nch):
                if s == 0:
                    ysc, rysc = yscs[ycn["n"] % 4], ryscs[ycn["n"] % 4]
                    ycn["n"] += 1
                else:
                    ysc, rysc = yscc, ryscc
                for nb in range(2):
                    if s == 0:
                        bd, rbd = bank[4 + nb], rbank[4 + nb]
                    else:
                        bd, rbd = bank[6], rbank[6]
                    for fc in range(8):
                        mm(bd[0:rows, :], hf[s][:, fc, c * rows:(c + 1) * rows], wd[par][:, fc, nb * 512:(nb + 1) * 512], fc == 0, fc == 7, [rhf[s], rwe[par]], [rbd])
                    stt(ysc[0:rows, nb * 512:(nb + 1) * 512], bd[0:rows, :], wsel[s][0:rows, c, e:e + 1], rowbcA[0:rows, s, nb * 512:(nb + 1) * 512],
                        ALU.mult, ALU.mult, [rbd, ridx[s], rrowA], [rysc])
                if _DBG.get("noscatter"):
                    continue
                (lambda c, s, rows, e, dst, rstream, ysc, rysc: P.dma("pool", lambda: nc.gpsimd.indirect_dma_start(
                    out=dst, out_offset=bass.IndirectOffsetOnAxis(ap=idxS[s][0:rows, c, e:e + 1], axis=0),
                    in_=ysc[0:rows, :], in_offset=None, compute_op=ALU.add),
                    [rysc, ridx[s]] + rstream, rstream))(c, s, rows, e, dst, rstream, ysc, rysc)

        gather(0)
        loads(0)
        G(0)
        for e in range(NEXP):
            if e + 1 < NEXP:
                gather(e + 1)
                loads(e + 1)
            chains = []
            for si in range(len(streams)):
                P.capture_start()
                up(e, si)
                down(e, si)
                chains.append(P.capture_end())
            if e + 1 < NEXP:
                P.capture_start()
                G(e + 1)
                chains.append((P.capture_end(), 0.45, 0.95))
            P.emit_zip(chains)

    for l in range(DEPTH):
        last = (l == DEPTH - 1)
        tiles_ctx = [NLAT, NLAT + 1]
        tiles_lat = list(range(NLAT))

        with contextlib.ExitStack() as ps:
            cTt = sb("cTt", [128, 8, 2], F32, ps)
            bmT = sb("bmT", [128, 48], F32, ps)
            n1T = sb("n1T", [128, 8], F32, ps)
            n2T = sb("n2T", [128, 8], F32, ps)
            wm = [sb("wm%d" % i, [128, 8, 1024], BF16, ps) for i in range(2)]
            rwm = [Res(), Res()]
            rsm = Res()
            P.dma("sp", lambda: nc.sync.dma_start(out=cTt[:], in_=cT_d.rearrange("p (k s) -> p k s", s=2)), writes=[rsm])
            P.dma("sp", lambda: nc.sync.dma_start(out=bmT[:], in_=bmT_d[l]), writes=[rsm])
            P.dma("sp", lambda: nc.sync.dma_start(out=n1T[:], in_=n1T_d[l]), writes=[rsm])
            P.dma("sp", lambda: nc.sync.dma_start(out=n2T[:], in_=n2T_d[l]), writes=[rsm])
            P.act(lambda: nc.scalar.activation(out=cact[:], in_=cTt[:], func=AF.Silu), reads=[rsm], writes=[rmod])
            for sec in range(6 if stop_after != "M1" else 0):
                i = sec % 2
                (lambda sec, i: P.dma("pool", lambda: nc.gpsimd.dma_start(
                    out=wm[i][:], in_=w_mod[l][:, sec * 1024:(sec + 1) * 1024].rearrange("(k p) n -> p k n", p=128)),
                    writes=[rwm[i]]))(sec, i)
                for j in range(8):
                    col = (sec * 8 + j) * 2
                    for k in range(8):
                        (lambda i, j, k, col: P.pe(lambda: nc.tensor.matmul(bank[2][:, col:col + 2], lhsT=wm[i][:, k, j * 128:(j + 1) * 128],
                                                                            rhs=cact[:, k, :], start=(k == 0), stop=(k == 7)),
                                                   reads=[rwm[i], rmod], writes=[rbank[2]]))(i, j, k, col)
            if stop_after in ("M1", "M2"):
                P.flush()
                break
            P.dve(lambda: nc.vector.tensor_tensor(out=modT[:], in0=bank[2][:, 0:96].rearrange("p (j s) -> p j s", s=2),
                                                  in1=bc(bmT[:], 2, 2), op=ALU.add), reads=[rbank[2], rsm], writes=[rmod])
            if stop_after == "M3":
                P.flush()
                break
            for (Ax, nT, sec) in ((A1, n1T, 1), (A2, n2T, 4)):
                (lambda Ax, nT, sec: P.dve(lambda: nc.vector.scalar_tensor_tensor(
                    out=Ax[:], in0=modT[:, sec * 8:(sec + 1) * 8, :], scalar=1.0, in1=bc(nT[:], 2, 2), op0=ALU.add, op1=ALU.mult),
                    reads=[rmod, rsm], writes=[rmod]))(Ax, nT, sec)
            P.flush()
        B1m = modT[:, 0:8, :]
        B2m = modT[:, 24:32, :]
        if stop_after == "M" and l == 0:
            break

        with contextlib.ExitStack() as pAB:
            ST = sb("ST", [128, NT, 2, 2, 128], BF16, pAB)
            rST = Res("ST")
            decbc = sb("decbc", [128, 8], F32, pAB)
            dechp = sb("dechp", [128, 4], F32, pAB)
            lgbc = sb("lgbc", [128, 8], F32, pAB)
            lghp = sb("lghp", [128, 4], F32, pAB)
            DTt = sb("DT", [128, 4, 128], F32, pAB)
            DfT = sb("DfT", [128, 2, 2, 128], F32, pAB)
            wkd = sb("wkd", [128, 2, 4], F32, pAB)
            Gam = sb("Gam", [128, 4], F32, pAB)
            e1 = sb("e1", [128, 128], F32, pAB)
            e2 = sb("e2", [128, 128], F32, pAB)
            rdec = Res("dec")
            re1 = Res()
            P.dma("sp", lambda: nc.sync.dma_start(out=decbc[:], in_=decbc_d[l]), writes=[rdec])
            P.dma("sp", lambda: nc.sync.dma_start(out=dechp[:], in_=dechp_d[l]), writes=[rdec])
            for (src, dst) in ((decbc, lgbc), (dechp, lghp)):
                (lambda src, dst: (
                    P.act(lambda: nc.scalar.activation(out=dst[:], in_=src[:], func=AF.Exp, scale=-1.0), reads=[rdec], writes=[rdec]),
                    P.dve(lambda: nc.vector.tensor_scalar(out=dst[:], in0=dst[:], scalar1=1.0, scalar2=None, op0=ALU.add), reads=[rdec], writes=[rdec]),
                    P.act(lambda: nc.scalar.activation(out=dst[:], in_=dst[:], func=AF.Ln), reads=[rdec], writes=[rdec]),
                    P.dve(lambda: nc.vector.tensor_scalar(out=dst[:], in0=dst[:], scalar1=-1.0, scalar2=None, op0=ALU.mult), reads=[rdec], writes=[rdec])))(src, dst)
            KS = 0.125
            for h in range(4):
                (lambda h: (
                    P.act(lambda: nc.scalar.activation(out=e1[:], in_=ct("PF"), func=AF.Exp, scale=lgbc[:, h:h + 1]), reads=[rdec, rct], writes=[re1]),
                    P.dve(lambda: nc.vector.tensor_tensor(out=e1[:], in0=e1[:], in1=ct("MF"), op=ALU.mult), reads=[re1, rct], writes=[re1]),
                    P.act(lambda: nc.scalar.activation(out=e2[:], in_=ct("PB"), func=AF.Exp, scale=lgbc[:, 4 + h:5 + h]), reads=[rdec, rct], writes=[re1]),
                    P.dve(lambda: nc.vector.tensor_tensor(out=e2[:], in0=e2[:], in1=ct("MB"), op=ALU.mult), reads=[re1, rct], writes=[re1]),
                    P.dve(lambda: nc.vector.tensor_tensor(out=e1[:], in0=e1[:], in1=e2[:], op=ALU.add), reads=[re1], writes=[re1]),
                    P.dve(lambda: nc.vector.tensor_scalar(out=DTt[:, h, :], in0=e1[:], scalar1=KS, scalar2=None, op0=ALU.mult), reads=[re1], writes=[rdec])))(h)
            P.act(lambda: nc.scalar.activation(out=wkd[:, 0, :], in_=lgbc[:, 0:4], func=AF.Exp, scale=ct("cjm")), reads=[rdec, rct], writes=[rdec])
            P.act(lambda: nc.scalar.activation(out=wkd[:, 1, :], in_=lgbc[:, 4:8], func=AF.Exp, scale=ct("jcol")), reads=[rdec, rct], writes=[rdec])
            P.dve(lambda: nc.vector.tensor_scalar(out=wkd[:], in0=wkd[:], scalar1=KS, scalar2=None, op0=ALU.mult), reads=[rdec], writes=[rdec])
            for dr in range(2):
                for hb in range(2):
                    (lambda dr, hb: (
                        P.act(lambda: nc.scalar.activation(out=DfT[:, dr, hb, :], in_=ct("iota1") if dr == 0 else ct("cmi"), func=AF.Exp,
                                                           scale=lghp[:, dr * 2 + hb: dr * 2 + hb + 1]), reads=[rdec, rct], writes=[rdec]),
                        P.act(lambda: nc.scalar.activation(out=Gam[:, dr * 2 + hb: dr * 2 + hb + 1], in_=lghp[:, dr * 2 + hb: dr * 2 + hb + 1],
                                                           func=AF.Exp, scale=128.0), reads=[rdec], writes=[rdec])))(dr, hb)

            with contextlib.ExitStack() as pA:
                wkv = sb("wkv", [128, 8, 768], BF16, pA)
                rwkv = Res()
                P.dma("pool", lambda: nc.gpsimd.dma_start(out=wkv[:], in_=w_in[l][:, 256:1024].rearrange("(k p) n -> p k n", p=128)), writes=[rwkv])
                UPD = sb("UPD", [128, NT, 2, 2, 128], F32, pA)
                rUPD = Res()
                hTa = [sb("hTa%d" % i, [128, 8, 128], BF16, pA) for i in range(2)]
                rhTa = [Res(), Res()]
                t1 = sb("t1a", [128, 256], F32, pA)
                t2 = sb("t2a", [128, 256], F32, pA)
                kr = sb("kra", [128, 256], F32, pA)
                rt = Res()
                kw = [sb("kwa%d" % i, [128, 2, 256], BF16, pA) for i in range(2)]
                vb = [sb("vba%d" % i, [128, 512], BF16, pA) for i in range(2)]
                rkw = [Res(), Res()]
                tilesA = tiles_ctx + tiles_lat
                t1s = [t1, sb("t1a2", [128, 256], F32, pA)]
                t2s = [t2, sb("t2a2", [128, 256], F32, pA)]
                krs = [kr, sb("kra2", [128, 256], F32, pA)]
                rts = [rt, Res()]
                abanks = [(0, 1, 2), (4, 5, 6)]

                def chainA(n_, st):
                    T = tilesA[n_]
                    s = 0 if T < NLAT else 1
                    ik, iv, iu = abanks[st]
                    bk, rbk, bv, rbv, bu, rbu = bank[ik], rbank[ik], bank[iv], rbank[iv], bank[iu], rbank[iu]
                    X, rX, XN, rXN, S, rS = norm_tile(T)
                    load_rope(T)
                    H = hTa[st]
                    transpose_mod(XN, rXN, A1, B1m, s, (lambda H: (lambda k: H[:, k, :]))(H), rhTa[st])
                    for k in range(8):
                        mm(bk[:, 0:256], H[:, k, :], wkv[:, k, 0:256], k == 0, k == 7, [rhTa[st], rwkv], [rbk])
                    for k in range(8):
                        mm(bv[:, 0:512], H[:, k, :], wkv[:, k, 256:768], k == 0, k == 7, [rhTa[st], rwkv], [rbv])
                    RT, rRT = ropet[T % 2], rrope[T % 2]
                    krv = krs[st][:].rearrange("p (h d) -> p h d", d=64)
                    rope_apply(P, nc, bk[:, 0:256], rbk, RT, rRT, 4, t1s[st], t2s[st], krv, rts[st], rts[st])
                    KW, V = kw[st], vb[st]
                    for dr in range(2):
                        tt(KW[:, dr, :].rearrange("p (h d) -> p h d", d=64), krv, bc(wkd[:, dr, :], 2, 64), ALU.mult, [rts[st], rdec], [rkw[st]])
                    cpa(V[:], bv[:], [rbv], [rkw[st]])
                    for dr in range(2):
                        for hb in range(2):
                            mm(bu[:, hb * 256:(hb + 1) * 256], KW[:, dr, hb * 128:(hb + 1) * 128], V[:, hb * 256:(hb + 1) * 256], True, True,
                               [rkw[st]], [rbu])
                        for hb in range(2):
                            cpv(UPD[0:64, T, dr, hb, :], bu[0:64, hb * 256:hb * 256 + 128], [rbu], [rUPD])
                            cpa(UPD[64:128, T, dr, hb, :], bu[64:128, hb * 256 + 128:hb * 256 + 256], [rbu], [rUPD])

                for n_ in range(0, len(tilesA), 2):
                    chains = []
                    for st in range(2):
                        if n_ + st < len(tilesA):
                            P.capture_start()
                            chainA(n_ + st, st)
                            chains.append(P.capture_end())
                    P.emit_zip(chains)
                cur = [sb("cur%d" % i, [128, 2, 128], F32, pA) for i in range(2)]
                rcur = Res()
                c0, c1 = NLAT, NLAT + 1
                for dr in range(2):
                    first, second = (c0, c1) if dr == 0 else (c1, c0)
                    (lambda dr, first, second: (
                        P.dve(lambda: nc.vector.memset(ST[:, first, dr, :, :], 0.0), writes=[rST]),
                        P.dve(lambda: nc.vector.tensor_copy(out=ST[:, second, dr, :, :], in_=UPD[:, first, dr, :, :]), reads=[rUPD], writes=[rST])))(dr, first, second)
                    order = list(range(NLAT)) if dr == 0 else list(range(NLAT - 1, -1, -1))
                    for hb in range(2):
                        (lambda dr, hb, first, second: P.dve(lambda: nc.vector.scalar_tensor_tensor(
                            out=cur[0][:, hb, :], in0=UPD[:, first, dr, hb, :], scalar=Gam[:, dr * 2 + hb: dr * 2 + hb + 1],
                            in1=UPD[:, second, dr, hb, :], op0=ALU.mult, op1=ALU.add), reads=[rUPD, rdec], writes=[rcur]))(dr, hb, first, second)
                    pp = 0
                    for n in order:
                        (lambda dr, n, pp: P.act(lambda: nc.scalar.copy(out=ST[:, n, dr, :, :], in_=cur[pp][:]), reads=[rcur], writes=[rST]))(dr, n, pp)
                        for hb in range(2):
                            (lambda dr, hb, n, pp: P.dve(lambda: nc.vector.scalar_tensor_tensor(
                                out=cur[1 - pp][:, hb, :], in0=cur[pp][:, hb, :], scalar=Gam[:, dr * 2 + hb: dr * 2 + hb + 1],
                                in1=UPD[:, n, dr, hb, :], op0=ALU.mult, op1=ALU.add), reads=[rUPD, rdec, rcur], writes=[rcur]))(dr, hb, n, pp)
                        pp = 1 - pp
                P.flush()
            if stop_after == "A" and l == 0:
                break

            with contextlib.ExitStack() as pB:
                phase_B(pB, l, last, ST, rST, DTt, DfT, rdec)
                P.flush()
        if stop_after == "B" and l == 0:
            break

        with contextlib.ExitStack() as pM:
            rowbcA = sb("rowbcA", [128, 2, D], F32, pM)
            build_rowbc(rowbcA, rrowA, lambda s, c: modT[:, 16 + c, s:s + 1])
            phase_merge(pM, l, last)
            P.flush()
        if stop_after == "Bp" and l == 0:
            break

        with contextlib.ExitStack() as pC:
            rowbcA = sb("rowbcA", [128, 2, D], F32, pC)
            rowbcB = sb("rowbcB", [128, 2, D], F32, pC)
            build_rowbc(rowbcA, rrowA, lambda s, c: A2[:, c, s:s + 1])
            build_rowbc(rowbcB, rrowB, lambda s, c: modT[:, 24 + c, s:s + 1])
            phase_moe(pC, l, last)
            P.flush()
        if stop_after == "C" and l == 0:
            break

    P.flush()
    _DBG["stats"] = dict(nops=len(P.ops), nwait=P.nwait, ecount=dict(P.ecount), dmax=max(P.dcount))


def rope_apply(P, nc, src, rsrc, RT, rRT, nh, t1, t2, dst, rtmp, rdst, pool=False):
    s3 = src.rearrange("p (h d) -> p h d", d=64)
    t13 = t1[:, 0:nh * 64].rearrange("p (h d) -> p h d", d=64)
    t23 = t2[:, 0:nh * 64].rearrange("p (h d) -> p h d", d=64)
    (P.pool if pool else P.dve)(lambda: (nc.gpsimd if pool else nc.vector).tensor_tensor(out=t13, in0=s3, in1=bc(RT[:, 0:64], 1, nh), op=ALU.mult), reads=[rsrc, rRT], writes=[rtmp])
    (P.pool if pool else P.dve)(lambda: (nc.gpsimd if pool else nc.vector).tensor_tensor(out=t23[:, :, 0:32], in0=s3[:, :, 32:64], in1=bc(RT[:, 64:96], 1, nh), op=ALU.mult),
          reads=[rsrc, rRT], writes=[rtmp])
    (P.pool if pool else P.dve)(lambda: (nc.gpsimd if pool else nc.vector).tensor_tensor(out=t23[:, :, 32:64], in0=s3[:, :, 0:32], in1=bc(RT[:, 96:128], 1, nh), op=ALU.mult),
          reads=[rsrc, rRT], writes=[rtmp])
    (P.pool if pool else P.dve)(lambda: (nc.gpsimd if pool else nc.vector).tensor_tensor(out=dst, in0=t13, in1=t23, op=ALU.add), reads=[rtmp], writes=[rdst])


def _host_inputs(inp):
    f32 = np.float32
    g = lambda k: np.ascontiguousarray(np.asarray(inp[k], dtype=f32))
    x, c, ctx, c_ctx = g("x"), g("c"), g("ctx"), g("c_ctx")
    B = x.shape[0]
    shared = {}
    shared["w_mod"] = g("w_mod")
    shared["bmT"] = np.ascontiguousarray(g("b_mod").reshape(DEPTH, 48, 128).transpose(0, 2, 1))
    shared["n1T"] = np.ascontiguousarray(g("norm1_g").reshape(DEPTH, 8, 128).transpose(0, 2, 1))
    shared["n2T"] = np.ascontiguousarray(g("norm2_g").reshape(DEPTH, 8, 128).transpose(0, 2, 1))
    shared["w_in"] = g("w_in")
    df, db = g("ret_decay_fwd"), g("ret_decay_bwd")
    decbc = np.concatenate([df, db], axis=1)
    shared["decbc"] = np.ascontiguousarray(np.broadcast_to(decbc[:, None, :], (DEPTH, 128, 8)))
    hp = np.zeros((DEPTH, 128, 4), f32)
    for dr, dd in enumerate((df, db)):
        for hb in range(2):
            hp[:, 0:64, dr * 2 + hb] = dd[:, 2 * hb][:, None]
            hp[:, 64:128, dr * 2 + hb] = dd[:, 2 * hb + 1][:, None]
    shared["dechp"] = hp
    shared["sgu_wT"] = np.ascontiguousarray(g("sgu_w").transpose(0, 1, 3, 2))
    shared["sgu_bbc"] = np.ascontiguousarray(np.broadcast_to(g("sgu_b").reshape(DEPTH, 1, 512), (DEPTH, 128, 512)))
    shared["qg"] = np.ascontiguousarray(np.broadcast_to(g("q_norm_g")[:, None, :], (DEPTH, 128, 64)))
    shared["kg"] = np.ascontiguousarray(np.broadcast_to(g("k_norm_g")[:, None, :], (DEPTH, 128, 64)))
    shared["sink"] = np.ascontiguousarray(np.broadcast_to(g("attn_sink")[:, None, :], (DEPTH, 128, 8)))
    for k in ("w_branch", "w_out", "w_router", "w_exp_gate", "w_exp_up", "w_exp_down"):
        shared[k] = g(k)
    shared["rope"] = _rope_tables()
    shared["ctab"] = _const_table()
    maps = []
    for b in range(B):
        m = dict(shared)
        m["x"] = x[b]
        m["ctxin"] = ctx[b]
        cT = np.stack([c[b].reshape(8, 128).T, c_ctx.reshape(8, 128).T], axis=-1)
        m["cT"] = np.ascontiguousarray(cT.reshape(128, 16))
        maps.append(m)
    return maps


_NC_CACHE = {}


def kernel(**inputs):
    maps = _host_inputs(inputs)
    if "nc" not in _NC_CACHE:
        _NC_CACHE["nc"] = build_program()
    nc = _NC_CACHE["nc"]
    n = len(maps)
    res = run_bass_kernel_spmd(nc, maps, core_ids=list(range(n)))
    return np.stack([np.asarray(r["out"], dtype=np.float32) for r in res.results], axis=0)
```

```python
import contextlib
import numpy as np
import concourse.bass as bass
import concourse.mybir as mybir
from concourse.bass_utils import run_bass_kernel_spmd

F32 = mybir.dt.float32
BF16 = mybir.dt.bfloat16
I32 = mybir.dt.int32
AF = mybir.ActivationFunctionType
ALU = mybir.AluOpType
AX = mybir.AxisListType

D = 1024
SEQ = 4096
CTX = 256
NLAT = 32
NT = 34
DEPTH = 2
NEXP = 16
EPS = 1e-6
INW = 6400
NG = 3328

_DBG = {}


class Res:
    __slots__ = ("name", "w", "r", "excl")

    def __init__(self, name="", excl=False):
        self.name = name
        self.w = None
        self.r = []
        self.excl = excl


class _Op:
    __slots__ = ("eng", "fn", "reads", "writes", "dma", "deps", "inc", "val", "semid")

    def __init__(self, eng, fn, reads, writes, dma):
        self.eng = eng
        self.fn = fn
        self.reads = reads
        self.writes = writes
        self.dma = dma
        self.deps = ()
        self.inc = False
        self.val = 0
        self.semid = None


class Prog:
    NDMASEM = 64
    NHW = 32

    def __init__(self, nc, es):
        self.nc = nc
        self.ops = []
        self.done = 0
        self.engs = {"pe": nc.tensor, "act": nc.scalar, "dve": nc.vector,
                     "pool": nc.gpsimd, "sp": nc.sync}
        self.sems = {e: es.enter_context(nc.semaphore("sem_" + e)) for e in self.engs}
        self.dsems = [es.enter_context(nc.semaphore("dsem%d" % i)) for i in range(self.NDMASEM)]
        self.dlast = [None] * self.NDMASEM
        self.drr = 0
        self.drr_sw = 0
        self.ecount = {e: 0 for e in self.engs}
        self.dcount = [0] * self.NDMASEM
        self.waited = {e: {} for e in self.engs}
        self.nwait = 0
        self._cap = None
        self._unit = None

    def add(self, eng, fn, reads=(), writes=(), dma=False):
        reads = tuple(reads)
        writes = tuple(writes)
        ex = tuple(r for r in reads if r.excl)
        if ex:
            reads = tuple(r for r in reads if not r.excl)
            writes = writes + ex
        op = _Op(eng, fn, reads, writes, dma)
        if self._cap is not None:
            if self._unit is not None:
                self._unit.append(op)
            else:
                self._cap.append([op])
        else:
            self.ops.append(op)

    def capture_start(self):
        self._cap = []
        self._unit = None

    def capture_end(self):
        cap = self._cap
        self._cap = None
        return cap

    @contextlib.contextmanager
    def atomic(self):
        if self._cap is None or self._unit is not None:
            yield
            return
        self._unit = []
        try:
            yield
        finally:
            self._cap.append(self._unit)
            self._unit = None

    def emit_zip(self, chains):
        items = []
        for ci, ch in enumerate(chains):
            lo, hi = 0.0, 1.0
            if isinstance(ch, tuple):
                ch, lo, hi = ch
            n = len(ch)
            for ui, unit in enumerate(ch):
                items.append((lo + (hi - lo) * (ui + 0.5) / n, ci, ui, unit))
        items.sort(key=lambda t: (t[0], t[1]))
        for _, _, _, unit in items:
            self.ops.extend(unit)

    def pe(self, fn, reads=(), writes=()):
        self.add("pe", fn, reads, writes)

    def act(self, fn, reads=(), writes=()):
        self.add("act", fn, reads, writes)

    def dve(self, fn, reads=(), writes=()):
        self.add("dve", fn, reads, writes)

    def pool(self, fn, reads=(), writes=()):
        self.add("pool", fn, reads, writes)

    def dma(self, eng, fn, reads=(), writes=()):
        self.add(eng, fn, reads, writes, dma=True)

    def flush(self):
        ops = self.ops
        base = self.done
        n = len(ops)
        last_compute = {}
        for i in range(base, n):
            op = ops[i]
            raw = set()
            war = set()
            for r in op.reads:
                if r.w is not None:
                    raw.add(r.w)
            for w in op.writes:
                if w.w is not None:
                    raw.add(w.w)
                war.update(w.r)
            if op.dma:
                if op.eng == "pool":
                    s = self.NHW + self.drr_sw
                    self.drr_sw = (self.drr_sw + 1) % (self.NDMASEM - self.NHW)
                else:
                    s = self.drr
                    self.drr = (self.drr + 1) % self.NHW
                op.semid = s
                if self.dlast[s] is not None:
                    raw.add(self.dlast[s])
                self.dlast[s] = i
            else:
                last_compute[op.eng] = i
            deps = set()
            for j in raw:
                if j < base or j == i:
                    continue
                o = ops[j]
                if (not o.dma) and (not op.dma) and o.eng == op.eng and op.eng == "pe":
                    continue
                deps.add(j)
            for j in war:
                if j < base or j == i:
                    continue
                o = ops[j]
                if (not o.dma) and o.eng == op.eng and not op.dma and op.eng == "pe":
                    continue
                deps.add(j)
            for r in op.reads:
                r.r.append(i)
            for w in op.writes:
                w.w = i
                w.r = []
            best = {}
            for j in deps:
                o = ops[j]
                key = ("d", o.semid) if o.dma else ("e", o.eng)
                if key not in best or best[key] < j:
                    best[key] = j
            op.deps = tuple(best.values())
            for j in op.deps:
                ops[j].inc = True
        for e, i in last_compute.items():
            ops[i].inc = True
        for i in range(base, n):
            op = ops[i]
            if op.dma:
                self.dcount[op.semid] += 16
                op.val = self.dcount[op.semid]
            elif op.inc:
                self.ecount[op.eng] += 1
                op.val = self.ecount[op.eng]
        for i in range(base, n):
            op = ops[i]
            eng = self.engs[op.eng]
            wt = self.waited[op.eng]
            for j in op.deps:
                o = ops[j]
                if o.dma:
                    key, sem = ("d", o.semid), self.dsems[o.semid]
                else:
                    key, sem = ("e", o.eng), self.sems[o.eng]
                if wt.get(key, 0) >= o.val:
                    continue
                eng.wait_ge(sem, o.val)
                self.nwait += 1
                wt[key] = o.val
            inst = op.fn()
            if op.dma:
                inst.then_inc(self.dsems[op.semid], 16)
            elif op.inc:
                inst.then_inc(self.sems[op.eng], 1)
            op.fn = None
        for e, eng in self.engs.items():
            wt = self.waited[e]
            for x in self.engs:
                if x == e:
                    continue
                v = self.ecount[x]
                if v > wt.get(("e", x), 0):
                    eng.wait_ge(self.sems[x], v)
                    wt[("e", x)] = v
            for s in range(self.NDMASEM):
                v = self.dcount[s]
                if v > wt.get(("d", s), 0):
                    eng.wait_ge(self.dsems[s], v)
                    wt[("d", s)] = v
        self.done = n


def bc(ap, axis, n):
    s = list(ap.shape)
    s.insert(axis, n)
    return ap.unsqueeze(axis).to_broadcast(s)


def _rope_tables():
    f32 = np.float32
    rows = SEQ // 64
    row = np.repeat(np.arange(rows), 64).astype(f32)
    col = np.tile(np.arange(64), rows).astype(f32)
    nf = 16
    inv = np.power(f32(10000.0), -np.arange(nf, dtype=f32) / f32(nf)).astype(f32)
    ang = np.concatenate([row[:, None] * inv, col[:, None] * inv], axis=-1).astype(f32)
    cos = np.cos(ang).astype(f32)
    sin = np.sin(ang).astype(f32)
    tab = np.zeros((NT * 128, 128), f32)
    tab[:SEQ, 0:32] = cos
    tab[:SEQ, 32:64] = cos
    tab[:SEQ, 64:96] = -sin
    tab[:SEQ, 96:128] = sin
    tab[SEQ:, 0:64] = 1.0
    return tab.reshape(NT, 128, 128)


CT = {}


def _const_table():
    cols = []

    def put(name, arr):
        arr = np.asarray(arr, np.float32).reshape(128, -1)
        CT[name] = (sum(c.shape[1] for c in cols), arr.shape[1])
        cols.append(arr)

    j = np.arange(128)[:, None]
    i = np.arange(128)[None, :]
    put("ident", (j == i))
    put("ones", np.ones((128, 128)))
    put("PF", np.maximum(i - j, 0))
    put("PB", np.maximum(j - i, 0))
    put("MF", (i >= j))
    put("MB", (j > i))
    put("iota1", np.broadcast_to(i + 1, (128, 128)))
    put("cmi", np.broadcast_to(128 - i, (128, 128)))
    put("cjm", 127 - j)
    put("jcol", j)
    put("tri", (j < i))
    put("iota512", np.broadcast_to(np.arange(512)[None, :], (128, 512)))
    mp = np.where(j >= i, 0.0, -30000.0)
    mn = np.where(j <= i, 0.0, -30000.0)
    put("mprev", np.tile(mp, (1, 4)))
    put("mnext", np.tile(mn, (1, 4)))
    put("ttv", np.broadcast_to(np.arange(NLAT)[None, :], (128, NLAT)))
    put("eps", np.full((128, 1), EPS))
    return np.concatenate(cols, axis=1)


def build_program(stop_after=None, dbg_out=False):
    nc = bass.Bass("TRN2", target_bir_lowering=False)
    es = contextlib.ExitStack()
    with es:
        _build(nc, es, stop_after, dbg_out)
    return nc


def _build(nc, es, stop_after, dbg_out):
    ctab_np = _const_table()
    NCT = ctab_np.shape[1]

    def din(name, shape, dt=F32):
        return nc.dram_tensor(name, list(shape), dt, kind="ExternalInput").ap()

    x_in = din("x", [SEQ, D])
    ctx_in = din("ctxin", [CTX, D])
    cT_d = din("cT", [128, 16])
    w_mod = din("w_mod", [DEPTH, D, 6 * D])
    bmT_d = din("bmT", [DEPTH, 128, 48])
    n1T_d = din("n1T", [DEPTH, 128, 8])
    n2T_d = din("n2T", [DEPTH, 128, 8])
    w_in = din("w_in", [DEPTH, D, INW])
    decbc_d = din("decbc", [DEPTH, 128, 8])
    dechp_d = din("dechp", [DEPTH, 128, 4])
    swT_d = din("sgu_wT", [DEPTH, 4, 128, 128])
    sbb_d = din("sgu_bbc", [DEPTH, 128, 512])
    qg_d = din("qg", [DEPTH, 128, 64])
    kg_d = din("kg", [DEPTH, 128, 64])
    sink_d = din("sink", [DEPTH, 128, 8])
    w_br = din("w_branch", [DEPTH, 3, 512, D])
    w_out = din("w_out", [DEPTH, D, D])
    w_rt = din("w_router", [DEPTH, D, NEXP])
    w_eg = din("w_exp_gate", [DEPTH, NEXP, D, D])
    w_eu = din("w_exp_up", [DEPTH, NEXP, D, D])
    w_ed = din("w_exp_down", [DEPTH, NEXP, D, D])
    rope_d = din("rope", [NT, 128, 128])
    ctab_d = din("ctab", [128, NCT])
    out = nc.dram_tensor("out", [SEQ, D], F32, kind="ExternalOutput").ap()
    xc = nc.dram_tensor("xc", [CTX, D], F32, kind="ExternalOutput").ap()
    BR = nc.dram_tensor("BR", [3, 512, NT * 128], BF16, kind="ExternalOutput").ap()
    H2 = nc.dram_tensor("H2", [NT * 128, D], BF16, kind="ExternalOutput").ap()

    P = Prog(nc, es)

    _uid = [0]

    def sb(name, shape, dt, stack=es):
        _uid[0] += 1
        return stack.enter_context(nc.sbuf_tensor("%s_%d" % (name, _uid[0]), list(shape), dt))

    bank = [es.enter_context(nc.psum_tensor("bank%d" % i, [128, 512], F32)) for i in range(7)]
    bankT = es.enter_context(nc.psum_tensor("bankT", [128, 1024], BF16))
    rbank = [Res("bank%d" % i, excl=True) for i in range(7)]
    _rbT = Res("bankT", excl=True)
    rbT = [_rbT, _rbT]

    RS = [Res("S%d" % t) for t in range(NT)]
    RBR = [[Res() for _ in range(NT)] for _ in range(3)]
    RH2 = [Res() for _ in range(NT)]

    def srows(T):
        return out[T * 128:(T + 1) * 128, :] if T < NLAT else xc[(T - NLAT) * 128:(T - NLAT + 1) * 128, :]

    ctab = sb("ctab", [128, NCT], F32)
    rct = Res("ctab")
    P.dma("sp", lambda: nc.sync.dma_start(out=ctab[:], in_=ctab_d), writes=[rct])

    def ct(name):
        o, n = CT[name]
        return ctab[:, o:o + n]

    identb = sb("identb", [128, 128], BF16)
    onesb = sb("onesb", [128, 128], BF16)
    trib = sb("trib", [128, 128], BF16)
    mprevb = sb("mprevb", [128, 512], BF16)
    mnextb = sb("mnextb", [128, 512], BF16)
    rcb = Res("constb")
    for dst, nm in ((identb, "ident"), (onesb, "ones"), (trib, "tri"), (mprevb, "mprev"), (mnextb, "mnext")):
        (lambda dst, nm: P.dve(lambda: nc.vector.tensor_copy(out=dst[:], in_=ct(nm)), reads=[rct], writes=[rcb]))(dst, nm)
    epsc = ct("eps")

    for q in range(8):
        (lambda q: P.dma("sp", lambda: nc.sync.dma_start(out=out[q * 512:(q + 1) * 512, :], in_=x_in[q * 512:(q + 1) * 512, :]),
                         writes=RS[q * 4:(q + 1) * 4]))(q)
    P.dma("sp", lambda: nc.sync.dma_start(out=xc, in_=ctx_in), writes=RS[NLAT:NT])

    cact = sb("cact", [128, 8, 2], BF16)
    modT = sb("modT", [128, 48, 2], F32)
    A1 = sb("A1", [128, 8, 2], F32)
    A2 = sb("A2", [128, 8, 2], F32)
    rmod = Res("mod")
    rowbcA = None
    rowbcB = None
    rrowA = Res("rowA")
    rrowB = Res("rowB")
    dgt = [sb("dgt%d" % i, [128, 128], F32) for i in range(2)]
    rdg = [Res(), Res()]

    xt = [sb("xt%d" % i, [128, D], F32) for i in range(2)]
    rxt = [Res(), Res()]
    sqj = sb("sqj", [128, D], F32)
    rsq = Res()
    ssum = [sb("ssum%d" % i, [128, 4], F32) for i in range(2)]
    rss = [Res(), Res()]
    xn = [sb("xn%d" % i, [128, D], BF16) for i in range(2)]
    rxn = [Res(), Res()]
    ropet = [sb("ropet%d" % i, [128, 128], F32) for i in range(2)]
    rrope = [Res(), Res()]
    cnt = {"tile": 0, "ev": 0}

    def load_rope(T):
        i = T % 2
        P.dma("sp", lambda: nc.sync.dma_start(out=ropet[i][:], in_=rope_d[T]), writes=[rrope[i]])
        return ropet[i], rrope[i]

    def norm_tile(T, want_x=False):
        i = cnt["tile"] % 2
        cnt["tile"] += 1
        X, rX, S, rS, XN, rXN = xt[i], rxt[i], ssum[i], rss[i], xn[i], rxn[i]
        P.dma("sp", lambda: nc.sync.dma_start(out=X[:], in_=srows(T)), reads=[RS[T]], writes=[rX])
        P.dve(lambda: nc.vector.memset(S[:], 0.0), writes=[rS])
        P.act(lambda: nc.scalar.activation(out=sqj[:], in_=X[:], func=AF.Square, accum_out=S[:, 0:1]),
              reads=[rX], writes=[rsq, rS])
        P.act(lambda: nc.scalar.activation(out=S[:, 1:2], in_=S[:, 0:1], func=AF.Sqrt, scale=1.0 / D, bias=epsc),
              reads=[rS, rct], writes=[rS])
        P.dve(lambda: nc.vector.reciprocal(out=S[:, 2:3], in_=S[:, 1:2]), reads=[rS], writes=[rS])
        P.dve(lambda: nc.vector.tensor_scalar(out=XN[:], in0=X[:], scalar1=S[:, 2:3], scalar2=None, op0=ALU.mult),
              reads=[rX, rS], writes=[rXN])
        return X, rX, XN, rXN, S, rS

    def transpose_mod(XN, rXN, A, Bm, s, dst_fn, rdst):
        for half in range(2):
          with P.atomic():
            rb = rbT[half]
            for kk in range(4):
                k = half * 4 + kk
                (lambda k, kk, half: P.pe(lambda: nc.tensor.transpose(out=bankT[:, half * 512 + kk * 128: half * 512 + (kk + 1) * 128],
                                                                       in_=XN[:, k * 128:(k + 1) * 128], identity=identb[:]),
                                          reads=[rXN, rcb], writes=[rb]))(k, kk, half)
            for kk in range(4):
                k = half * 4 + kk
                src = bankT[:, half * 512 + kk * 128: half * 512 + (kk + 1) * 128]
                if kk % 2 == 0:
                    (lambda k, src: P.act(lambda: nc.scalar.activation(out=dst_fn(k), in_=src, func=AF.Identity,
                                                                       scale=A[:, k, s:s + 1], bias=Bm[:, k, s:s + 1]),
                                          reads=[rb, rmod], writes=[rdst]))(k, src)
                else:
                    (lambda k, src: P.dve(lambda: nc.vector.tensor_scalar(out=dst_fn(k), in0=src, scalar1=A[:, k, s:s + 1],
                                                                          scalar2=Bm[:, k, s:s + 1], op0=ALU.mult, op1=ALU.add),
                                          reads=[rb, rmod], writes=[rdst]))(k, src)

    def build_rowbc(dst, rdst, col_fn):
        n = 0
        for s in range(2):
            for hb in range(2):
                b = bank[hb]
                rb = rbank[hb]
                for cc in range(4):
                    c = hb * 4 + cc
                    g = n % 2
                    n += 1
                    (lambda c, cc, g, s, b, rb: (
                        P.dve(lambda: nc.vector.tensor_scalar(out=dgt[g][:], in0=ct("ident"), scalar1=col_fn(s, c), scalar2=None, op0=ALU.mult),
                              reads=[rct, rmod], writes=[rdg[g]]),
                        P.pe(lambda: nc.tensor.matmul(b[:, cc * 128:(cc + 1) * 128], lhsT=ct("ones"), rhs=dgt[g][:], start=True, stop=True),
                             reads=[rdg[g], rct], writes=[rb])))(c, cc, g, s, b, rb)
                (lambda s, hb, b, rb: P.act(lambda: nc.scalar.copy(out=dst[:, s, hb * 512:(hb + 1) * 512], in_=b[:]),
                                            reads=[rb], writes=[rdst]))(s, hb, b, rb)

    def mm(o, lhsT, rhs, start, stop, reads, writes, tp=None):
        kw = {} if (tp is None or _DBG.get("notp")) else {"tile_position": tp}
        P.pe(lambda: nc.tensor.matmul(o, lhsT=lhsT, rhs=rhs, start=start, stop=stop, **kw), reads, writes)

    def tr(o, in_, ident, reads, writes):
        P.pe(lambda: nc.tensor.transpose(out=o, in_=in_, identity=ident), reads, writes)

    def actf(o, in_, func, reads, writes, **kw):
        P.act(lambda: nc.scalar.activation(out=o, in_=in_, func=func, **kw), reads, writes)

    def cpa(o, in_, reads, writes):
        P.act(lambda: nc.scalar.copy(out=o, in_=in_), reads, writes)

    def cpv(o, in_, reads, writes, eng="dve"):
        if eng == "POOL":
            eng = "pool"
        elif not _DBG.get("usepool"):
            eng = "dve"
        e = nc.vector if eng == "dve" else nc.gpsimd
        P.add(eng, lambda: e.tensor_copy(out=o, in_=in_), reads, writes)

    def tt(o, a, b, op, reads, writes, eng="dve"):
        if eng == "POOL":
            eng = "pool"
        elif not _DBG.get("usepool"):
            eng = "dve"
        e = nc.vector if eng == "dve" else nc.gpsimd
        P.add(eng, lambda: e.tensor_tensor(out=o, in0=a, in1=b, op=op), reads, writes)

    def tsc(o, a, s1, s2, op0, op1, reads, writes, eng="dve"):
        if eng == "POOL":
            eng = "pool"
        elif not _DBG.get("usepool"):
            eng = "dve"
        e = nc.vector if eng == "dve" else nc.gpsimd
        if op1 is None:
            P.add(eng, lambda: e.tensor_scalar(out=o, in0=a, scalar1=s1, scalar2=None, op0=op0), reads, writes)
        else:
            P.add(eng, lambda: e.tensor_scalar(out=o, in0=a, scalar1=s1, scalar2=s2, op0=op0, op1=op1), reads, writes)

    def stt(o, a, s, b, op0, op1, reads, writes, eng="dve"):
        if eng == "POOL":
            eng = "pool"
        elif not _DBG.get("usepool"):
            eng = "dve"
        e = nc.vector if eng == "dve" else nc.gpsimd
        P.add(eng, lambda: e.scalar_tensor_tensor(out=o, in0=a, scalar=s, in1=b, op0=op0, op1=op1), reads, writes)

    def mset(o, v, writes, eng="dve"):
        if eng == "POOL":
            eng = "pool"
        elif not _DBG.get("usepool"):
            eng = "dve"
        e = nc.vector if eng == "dve" else nc.gpsimd
        P.add(eng, lambda: e.memset(o, v), (), writes)

    def dsp(o, in_, reads, writes):
        P.dma("sp", lambda: nc.sync.dma_start(out=o, in_=in_), reads, writes)

    def dpl(o, in_, reads, writes):
        P.dma("pool", lambda: nc.gpsimd.dma_start(out=o, in_=in_), reads, writes)

    def rsqrt_chain(dst_sd, dst_r, src, scale, reads_res, wres):
        n = src.shape[0]
        actf(dst_sd, src, AF.Sqrt, [reads_res, rct], [wres], scale=scale, bias=epsc[0:n, :])
        P.dve(lambda: nc.vector.reciprocal(out=dst_r, in_=dst_sd), [wres], [wres])

    def kwview(w):
        return w.rearrange("(k p) n -> p k n", p=128)

    def phase_B(stk, l, last, ST, rST, DTt, DfT, rdec):
        wng = sb("wng", [128, 8, NG], BF16, stk)
        rwng = Res()
        for q in range(4):
            c0, c1 = q * 832, (q + 1) * 832
            dpl(wng[:, :, c0:c1], kwview(w_in[l][:, c0:c1]), [], [rwng])
        wsT = sb("wsT", [128, 4, 128], BF16, stk)
        sbb = sb("sbb", [128, 512], F32, stk)
        qg = sb("qg", [128, 64], F32, stk)
        kg = sb("kg", [128, 64], F32, stk)
        esink = sb("esink", [128, 8], F32, stk)
        rsm = Res()
        dpl(wsT[:], swT_d[l].rearrange("g j i -> j g i"), [], [rsm])
        dsp(sbb[:], sbb_d[l], [], [rsm])
        dsp(qg[:], qg_d[l], [], [rsm])
        dsp(kg[:], kg_d[l], [], [rsm])
        dsp(esink[:], sink_d[l], [], [rsm])
        tsc(qg[:], qg[:], 0.125, None, ALU.mult, None, [rsm], [rsm])
        actf(esink[:], esink[:], AF.Exp, [rsm], [rsm])
        KT_all = sb("KT_all", [128, NT, 128], BF16, stk)
        V_all = sb("V_all", [128, NT, 2, 80], BF16, stk)
        rKV = [Res() for _ in range(NT)]
        mset(V_all[:], 1.0, rKV)
        hT = [sb("hTb%d" % i, [128, 8, 128], BF16, stk) for i in range(2)]
        rhT = [Res(), Res()]
        zqk = [sb("zqk%d" % i, [128, 512], F32, stk) for i in range(2)]
        vb = [sb("vb%d" % i, [128, 512], BF16, stk) for i in range(2)]
        sg = [sb("sg%d" % i, [128, 512], BF16, stk) for i in range(2)]
        uT = [sb("uT%d" % i, [128, 512], BF16, stk) for i in range(2)]
        gv = [sb("gv%d" % i, [128, 512], F32, stk) for i in range(2)]
        zq = [sb("zq%d" % i, [128, 512], F32, stk) for i in range(2)]
        zk = [sb("zk%d" % i, [128, 128], F32, stk) for i in range(2)]
        rzqk, rvb, rsg, ruT, rgv, rzq, rzk = [[Res(), Res()] for _ in range(7)]
        t1 = sb("t1", [128, 512], F32, stk)
        t2 = sb("t2", [128, 512], F32, stk)
        rtmp = Res()
        t1a = sb("t1a", [128, 512], F32, stk)
        t2a = sb("t2a", [128, 512], F32, stk)
        rtmpa = Res()
        qrb = sb("qrb", [128, 256], BF16, stk)
        krb = sb("krb", [128, 256], BF16, stk)
        rqk = Res()
        qT = sb("qT", [128, 2, 128], BF16, stk)
        qfT = sb("qfT", [128, 2, 2, 128], BF16, stk)
        kT = sb("kT", [128, 2, 128], BF16, stk)
        rqT = Res()
        SD = sb("SD", [128, 512], BF16, stk)
        rSD = Res()
        bst = sb("bst", [128, 4, 6], F32, stk)
        mv = sb("mv", [128, 4, 2], F32, stk)
        sdr = sb("sdr", [128, 8], F32, stk)
        rln = Res()
        yb = sb("yb", [128, 512], BF16, stk)
        retb = sb("retb", [128, 512], BF16, stk)
        ryb = Res()
        brT = [sb("brTo%d" % i, [128, 4, 128], BF16, stk) for i in range(3)]
        rbrT = [Res(), Res(), Res()]
        vn = sb("vn", [128, 512], BF16, stk)
        stmp = sb("stmp", [128, 512], F32, stk)
        rsg2 = Res()
        bst2 = sb("bst2", [128, 6], F32, stk)
        mv2 = sb("mv2", [128, 4], F32, stk)
        sq = sb("sq", [128, 512], F32, stk)
        qn = sb("qn", [128, 512], F32, stk)
        ssq = sb("ssq", [128, 24], F32, stk)
        ratt = Res()
        sqk = sb("sqk", [128, 128], F32, stk)
        kn = sb("kn", [128, 128], F32, stk)
        ssk = sb("ssk", [128, 8], F32, stk)
        rattk = Res()
        qperm = sb("qperm", [128, 4, 2, 64], BF16, stk)
        krb2 = sb("krb2", [128, 128], BF16, stk)
        qT2 = [sb("qT2_%d" % i, [128, 4, 128], BF16, stk) for i in range(2)]
        rqT2 = [Res(), Res()]
        PT = [sb("PT%d" % i, [128, 512], BF16, stk) for i in range(2)]
        rPT = [Res(), Res()]
        den = sb("den", [128, 16], F32, stk)
        attb = sb("attb", [128, 512], BF16, stk)
        rden = Res()
        BRv = [BR[i].rearrange("(c p) t -> p c t", p=128) for i in range(3)]
        pbc = {"n": 0}

        def pbank():
            i = pbc["n"] % 2
            pbc["n"] += 1
            return bank[i], rbank[i]

        def out_branch(i, T, srcb, rsrc):
            with P.atomic():
                for c in range(4):
                    tr(bankT[:, 512 + c * 128: 512 + (c + 1) * 128], srcb[:, c * 128:(c + 1) * 128], identb[:], [rsrc, rcb], [rbT[1]])
                cpa(brT[i][:], bankT[:, 512:1024].rearrange("p (c t) -> p c t", t=128), [rbT[1]], [rbrT[i]])
            dsp(BRv[i][:, :, T * 128:(T + 1) * 128], brT[i][:], [rbrT[i]], [RBR[i][T]])

        def Pst1(T, par):
            s = 0 if T < NLAT else 1
            X, rX, XN, rXN, S, rS = norm_tile(T)
            H, rH = hT[par], rhT[par]
            transpose_mod(XN, rXN, A1, B1m, s, lambda k: H[:, k, :], rH)

        def Pst(T, full, par):
            load_rope(T)
            H, rH = hT[par], rhT[par]

            def proj(c0, c1):
                b, rb = pbank()
                for k in range(8):
                    mm(b[:, 0:c1 - c0], H[:, k, :], wng[:, k, c0:c1], k == 0, k == 7, [rH, rwng], [rb])
                return b, rb

            if full:
                b, rb = proj(0, 512)
                cpa(zqk[par][:], b[:], [rb], [rzqk[par]])
                b, rb = proj(512, 1024)
                cpv(vb[par][:], b[:], [rb], [rvb[par]])
                b, rb = proj(1024, 1536)
                actf(sg[par][:], b[:], AF.Silu, [rb], [rsg[par]])
                b, rb = pbank()
                for c in range(4):
                    for k in range(8):
                        mm(b[:, c * 128:(c + 1) * 128], wng[:, k, 1536 + c * 128:1536 + (c + 1) * 128], H[:, k, :], k == 0, k == 7, [rH, rwng], [rb])
                actf(uT[par][:], b[:], AF.Gelu_apprx_tanh, [rb], [ruT[par]])
                b, rb = proj(2048, 2560)
                actf(gv[par][:], b[:], AF.Gelu_apprx_tanh, [rb], [rgv[par]])
                b, rb = proj(2560, 3072)
                cpv(zq[par][:], b[:], [rb], [rzq[par]])
            b, rb = proj(3072, 3328)
            cpv(zk[par][:], b[:, 0:128], [rb], [rzk[par]])
            cpa(V_all[:, T, :, 0:64], b[:, 128:256].rearrange("p (k d) -> p k d", d=64), [rb], [rKV[T]])

        def chainR(T, par):
            RT, rRT = ropet[T % 2], rrope[T % 2]
            if True:
                rope_apply(P, nc, zqk[par][:, 0:256], rzqk[par], RT, rRT, 4, t1, t2, qrb[:].rearrange("p (h d) -> p h d", d=64), rtmp, rqk)
                rope_apply(P, nc, zqk[par][:, 256:512], rzqk[par], RT, rRT, 4, t1, t2, krb[:].rearrange("p (h d) -> p h d", d=64), rtmp, rqk)
                with P.atomic():
                    for hb in range(2):
                        tr(bankT[:, hb * 128:(hb + 1) * 128], qrb[:, hb * 128:(hb + 1) * 128], identb[:], [rqk, rcb], [rbT[0]])
                        tr(bankT[:, 256 + hb * 128:256 + (hb + 1) * 128], krb[:, hb * 128:(hb + 1) * 128], identb[:], [rqk, rcb], [rbT[0]])
                    qv = bankT[:, 0:256].rearrange("p (b t) -> p b t", t=128)
                    cpa(qT[:], qv, [rbT[0]], [rqT])
                    for dr in range(2):
                        tt(qfT[:, dr, :, :], qv, DfT[:, dr, :, :], ALU.mult, [rbT[0], rdec], [rqT])
                    cpa(kT[:], bankT[:, 256:512].rearrange("p (b t) -> p b t", t=128), [rbT[0]], [rqT])
                for h in range(4):
                    hb, hl = h // 2, h % 2
                    bS = 2 + hl
                    mm(bank[bS][:, hb * 128:(hb + 1) * 128], kT[64 * hl:64 * hl + 64, hb, :], qT[64 * hl:64 * hl + 64, hb, :], True, True,
                       [rqT], [rbank[bS]], tp=(64 * hl, 0))
                for h in range(4):
                    hb, hl = h // 2, h % 2
                    bS = 2 + hl
                    tt(SD[:, h * 128:(h + 1) * 128], bank[bS][:, hb * 128:(hb + 1) * 128], DTt[:, h, :], ALU.mult, [rbank[bS], rdec], [rSD])
                for h in range(4):
                    hb, hl = h // 2, h % 2
                    o = bank[2][:, h * 128:(h + 1) * 128]
                    mm(o, qfT[64 * hl:64 * hl + 64, 0, hb, :], ST[64 * hl:64 * hl + 64, T, 0, hb, :], True, False, [rqT, rST], [rbank[2]], tp=(64 * hl, 0))
                    mm(o, qfT[64 * hl:64 * hl + 64, 1, hb, :], ST[64 * hl:64 * hl + 64, T, 1, hb, :], False, False, [rqT, rST], [rbank[2]], tp=(64 * hl, 0))
                    mm(o, SD[:, h * 128:(h + 1) * 128], vb[par][:, h * 128:(h + 1) * 128], False, True, [rSD, rvb[par]], [rbank[2]])
                cpa(t1[:], bank[2][:], [rbank[2]], [rtmp])
                for h in range(4):
                    (lambda h: P.dve(lambda: nc.vector.bn_stats(out=bst[:, h, :], in_=t1[:, h * 128:(h + 1) * 128]), [rtmp], [rln]))(h)
                    (lambda h: P.dve(lambda: nc.vector.bn_aggr(out=mv[:, h, :], in_=bst[:, h, :]), [rln], [rln]))(h)
                rsqrt_chain(sdr[:, 0:4], sdr[:, 4:8], mv[:, :, 1], 1.0, rln, rln)
                for h in range(4):
                    tsc(yb[:, h * 128:(h + 1) * 128], t1[:, h * 128:(h + 1) * 128], mv[:, h, 0:1], sdr[:, 4 + h:5 + h], ALU.subtract, ALU.mult,
                        [rtmp, rln], [ryb])
                tt(retb[:], yb[:], sg[par][:], ALU.mult, [ryb, rsg[par]], [ryb], eng="POOL")
                out_branch(0, T, retb, ryb)

        def chainS(T, par):
            if True:
                P.dve(lambda: nc.vector.bn_stats(out=bst2[:], in_=gv[par][:]), [rgv[par]], [rsg2])
                P.dve(lambda: nc.vector.bn_aggr(out=mv2[:, 0:2], in_=bst2[:]), [rsg2], [rsg2])
                rsqrt_chain(mv2[:, 2:3], mv2[:, 3:4], mv2[:, 1:2], 1.0, rsg2, rsg2)
                tsc(vn[:], gv[par][:], mv2[:, 0:1], mv2[:, 3:4], ALU.subtract, ALU.mult, [rsg2, rgv[par]], [rsg2])
                for g in range(4):
                    mm(bank[4][:, g * 128:(g + 1) * 128], vn[:, g * 128:(g + 1) * 128], wsT[:, g, :], True, True, [rsg2, rsm], [rbank[4]])
                tt(stmp[:], bank[4][:], sbb[:], ALU.add, [rbank[4], rsm], [rsg2])
                tt(brT[1][:].rearrange("p c t -> p (c t)"), stmp[:], uT[par][:], ALU.mult, [rsg2, ruT[par]], [rbrT[1]], eng="POOL")
                dsp(BRv[1][:, :, T * 128:(T + 1) * 128], brT[1][:], [rbrT[1]], [RBR[1][T]])

        def chainQ(T, par):
            RT, rRT = ropet[T % 2], rrope[T % 2]
            if True:
                zq3 = zq[par][:].rearrange("p (h d) -> p h d", d=64)
                qn3 = qn[:].rearrange("p (h d) -> p h d", d=64)
                actf(sq[:], zq[par][:], AF.Square, [rzq[par]], [ratt])
                P.dve(lambda: nc.vector.tensor_reduce(out=ssq[:, 0:8], in_=sq[:].rearrange("p (h d) -> p h d", d=64), axis=AX.X, op=ALU.add), [ratt], [ratt])
                rsqrt_chain(ssq[:, 8:16], ssq[:, 16:24], ssq[:, 0:8], 1.0 / 64, ratt, ratt)
                tt(qn3, zq3, bc(ssq[:, 16:24], 2, 64), ALU.mult, [rzq[par], ratt], [ratt])
                tt(qn3, qn3, bc(qg[:], 1, 8), ALU.mult, [ratt, rsm], [ratt], eng="POOL")
                for hk in range(2):
                    rope_apply(P, nc, qn[:, hk * 256:(hk + 1) * 256], ratt, RT, rRT, 4, t1a, t2a, qperm[:, :, hk, :], rtmpa, ratt, pool=True)
                p2 = T % 2
                with P.atomic():
                    for g in range(4):
                        tr(bankT[:, g * 128:(g + 1) * 128], qperm[:, g, :, :].rearrange("p k d -> p (k d)"), identb[:], [ratt, rcb], [rbT[0]])
                    cpa(qT2[p2][:], bankT[:, 0:512].rearrange("p (g t) -> p g t", t=128), [rbT[0]], [rqT2[p2]])

        def chainK(T, par):
            RT, rRT = ropet[T % 2], rrope[T % 2]
            zk3 = zk[par][:].rearrange("p (h d) -> p h d", d=64)
            kn3 = kn[:].rearrange("p (h d) -> p h d", d=64)
            actf(sqk[:], zk[par][:], AF.Square, [rzk[par]], [rattk])
            P.dve(lambda: nc.vector.tensor_reduce(out=ssk[:, 0:2], in_=sqk[:].rearrange("p (h d) -> p h d", d=64), axis=AX.X, op=ALU.add), [rattk], [rattk])
            rsqrt_chain(ssk[:, 2:4], ssk[:, 4:6], ssk[:, 0:2], 1.0 / 64, rattk, rattk)
            tt(kn3, zk3, bc(ssk[:, 4:6], 2, 64), ALU.mult, [rzk[par], rattk], [rattk])
            tt(kn3, kn3, bc(kg[:], 1, 2), ALU.mult, [rattk, rsm], [rattk])
            rope_apply(P, nc, kn[:], rattk, RT, rRT, 2, t1a, t2a, krb2[:].rearrange("p (h d) -> p h d", d=64), rtmpa, rattk)
            with P.atomic():
                tr(bankT[:, 512:640], krb2[:], identb[:], [rattk, rcb], [rbT[1]])
                cpa(KT_all[:, T, :], bankT[:, 512:640], [rbT[1]], [rKV[T]])

        stc = {"n": 0}

        def Yst(T):
            p2 = T % 2
            if T < NLAT:
                chunks = []
                if T > 0:
                    chunks.append((T - 1, mprevb))
                chunks.append((T, None))
                chunks += [(NLAT, None), (NLAT + 1, None)]
                if T < NLAT - 1:
                    chunks.append((T + 1, mnextb))
            else:
                chunks = [(NLAT, None), (NLAT + 1, None)]
            for hk in range(2):
                ob = bank[6]
                rob = rbank[6]
                for ci, (c, m) in enumerate(chunks):
                    i = stc["n"] % 2
                    stc["n"] += 1
                    sb_, rsb = bank[5], rbank[5]
                    mm(sb_[:, :], KT_all[64 * hk:64 * hk + 64, c, :], qT2[p2][64 * hk:64 * hk + 64, :, :].rearrange("p g t -> p (g t)"),
                       True, m is None, [rKV[c], rqT2[p2]], [rsb], tp=(64 * hk, 0))
                    if m is not None:
                        mm(sb_[:, :], identb[:], m[:], False, True, [rcb], [rsb])
                    actf(PT[i][:], sb_[:, :], AF.Exp, [rsb], [rPT[i]])
                    for g in range(4):
                        mm(ob[:, g * 128:g * 128 + 65], PT[i][:, g * 128:(g + 1) * 128], V_all[:, c, hk, 0:65], ci == 0 and g == 0,
                           ci == len(chunks) - 1 and g == 3, [rPT[i], rKV[c]], [rob])
                ov = ob[:, 0:512].rearrange("p (g e) -> p g e", e=128)
                tt(den[:, hk * 4:(hk + 1) * 4], ov[:, :, 64], esink[:, hk * 4:(hk + 1) * 4], ALU.add, [rob, rsm], [rden])
                (lambda hk: P.dve(lambda: nc.vector.reciprocal(out=den[:, 8 + hk * 4:8 + (hk + 1) * 4], in_=den[:, hk * 4:(hk + 1) * 4]), [rden], [rden]))(hk)
                tt(attb[:, hk * 256:(hk + 1) * 256].rearrange("p (g d) -> p g d", d=64), ov[:, :, 0:64], bc(den[:, 8 + hk * 4:8 + (hk + 1) * 4], 2, 64),
                   ALU.mult, [rob, rden], [rden])
            out_branch(2, T, attb, rden)

        L = [NLAT, NLAT + 1] + list(range(NLAT))
        full = [(not last) if T >= NLAT else True for T in L]
        Pst1(L[0], 0)
        Pst1(L[1], 1)
        Pst(L[0], full[0], 0)
        for i, T in enumerate(L):
            par = i % 2
            chains = []
            if T < NLAT and T >= 1:
                P.capture_start()
                chainK(T, par)
                chains.append((P.capture_end(), 0.0, 0.2))
            else:
                chainK(T, par)
            if i + 1 < len(L):
                P.capture_start()
                Pst(L[i + 1], full[i + 1], (i + 1) % 2)
                chains.append(P.capture_end())
            if i + 2 < len(L):
                P.capture_start()
                Pst1(L[i + 2], i % 2)
                chains.append(P.capture_end())
            if full[i]:
                for ch in (chainR, chainS, chainQ):
                    P.capture_start()
                    ch(T, par)
                    chains.append(P.capture_end())
            if T < NLAT and T >= 1:
                P.capture_start()
                Yst(T - 1)
                chains.append(P.capture_end())
            P.emit_zip(chains)
            if T == NLAT + 1 and not last:
                Yst(NLAT)
                Yst(NLAT + 1)
            if T == NLAT - 1:
                Yst(T)

    def phase_merge(stk, l, last):
        wgt = sb("wgt", [128, 8, 3072], BF16, stk)
        wbr = sb("wbr", [128, 3, 4, D], BF16, stk)
        wo = sb("wo", [128, 8, D], BF16, stk)
        rw = Res()
        for q in range(3):
            dpl(wgt[:, :, q * 1024:(q + 1) * 1024], kwview(w_in[l][:, NG + q * 1024:NG + (q + 1) * 1024]), [], [rw])
            dpl(wbr[:, q, :, :], kwview(w_br[l][q]), [], [rw])
        dpl(wo[:], kwview(w_out[l]), [], [rw])
        hTg = [sb("hTg%d" % i, [128, 8, 512], BF16, stk) for i in range(2)]
        rhTg = [Res(), Res()]
        brTi = [sb("brTi%d" % i, [128, 3, 4, 512], BF16, stk) for i in range(2)]
        rbrTi = [Res(), Res()]
        mT = sb("mT", [128, 8, 512], BF16, stk)
        rmT = Res()
        sgm = [sb("sgm%d" % i, [128, 512], F32, stk) for i in range(2)]
        rsgm = [Res(), Res()]
        acc = sb("acc", [128, 512], F32, stk)
        tmp = sb("tmpm", [128, 512], F32, stk)
        racc = Res()
        rtmpm = Res()
        xo = [sb("xo%d" % i, [128, D], F32, stk) for i in range(2)]
        rxo = [Res(), Res()]
        xr = [sb("xr%d" % i, [128, D], F32, stk) for i in range(2)]
        rxr = [Res(), Res()]
        BRv = [BR[i].rearrange("(c p) t -> p c t", p=128) for i in range(3)]
        groups = [list(range(g * 4, g * 4 + 4)) for g in range(8)]
        if not last:
            groups = [[NLAT, NLAT + 1]] + groups
        cnts = {"pc": 0, "xc": 0}

        def prologue(gi):
            grp = groups[gi]
            gp = gi % 2
            s_ = 0 if grp[0] < NLAT else 1
            ntok = 128 * len(grp)
            T0 = grp[0]
            for ti, T in enumerate(grp):
                X, rX, XN, rXN, S, rS = norm_tile(T)
                transpose_mod(XN, rXN, A1, B1m, s_, (lambda ti, gp: (lambda k: hTg[gp][:, k, ti * 128:(ti + 1) * 128]))(ti, gp), rhTg[gp])
            for i in range(3):
                dsp(brTi[gp][:, i, :, 0:ntok], BRv[i][:, :, T0 * 128:T0 * 128 + ntok], [RBR[i][T] for T in grp], [rbrTi[gp]])

        def compute(gi):
            grp = groups[gi]
            gp = gi % 2
            s_ = 0 if grp[0] < NLAT else 1
            ntok = 128 * len(grp)
            H, rH, BI, rBI = hTg[gp], rhTg[gp], brTi[gp], rbrTi[gp]
            for dc in range(8):
                for i in range(3):
                    p = cnts["pc"] % 3
                    cnts["pc"] += 1
                    bg, rbg, bb, rbb = bank[2 * p], rbank[2 * p], bank[2 * p + 1], rbank[2 * p + 1]
                    for k in range(8):
                        mm(bg[:, 0:ntok], wgt[:, k, i * 1024 + dc * 128:i * 1024 + (dc + 1) * 128], H[:, k, 0:ntok], k == 0, k == 7, [rw, rH], [rbg])
                    for k in range(4):
                        mm(bb[:, 0:ntok], wbr[:, i, k, dc * 128:(dc + 1) * 128], BI[:, i, k, 0:ntok], k == 0, k == 3, [rw, rBI], [rbb])
                    sgi, rsgi = sgm[cnts["pc"] % 2], rsgm[cnts["pc"] % 2]
                    actf(sgi[:, 0:ntok], bg[:, 0:ntok], AF.Sigmoid, [rbg], [rsgi])
                    if i == 0:
                        tt(acc[:, 0:ntok], sgi[:, 0:ntok], bb[:, 0:ntok], ALU.mult, [rsgi, rbb], [racc])
                    elif i == 1:
                        tt(tmp[:, 0:ntok], sgi[:, 0:ntok], bb[:, 0:ntok], ALU.mult, [rsgi, rbb], [rtmpm])
                        tt(acc[:, 0:ntok], acc[:, 0:ntok], tmp[:, 0:ntok], ALU.add, [racc, rtmpm], [racc], eng="POOL")
                    else:
                        tt(tmp[:, 0:ntok], sgi[:, 0:ntok], bb[:, 0:ntok], ALU.mult, [rsgi, rbb], [rtmpm])
                        tt(mT[:, dc, 0:ntok], acc[:, 0:ntok], tmp[:, 0:ntok], ALU.add, [racc, rtmpm], [rmT], eng="POOL")
            for ti, T in enumerate(grp):
                j = cnts["xc"] % 2
                cnts["xc"] += 1
                xoi, rxoi = xo[j], rxo[j]
                dsp(xr[j][:], srows(T), [RS[T]], [rxr[j]])
                for nb in range(2):
                    for k in range(8):
                        mm(bank[6][:, :], mT[:, k, ti * 128:(ti + 1) * 128], wo[:, k, nb * 512:(nb + 1) * 512], k == 0, k == 7, [rmT, rw], [rbank[6]])
                    tt(xoi[:, nb * 512:(nb + 1) * 512], bank[6][:, :], rowbcA[:, s_, nb * 512:(nb + 1) * 512], ALU.mult, [rbank[6], rrowA], [rxoi])
                tt(xoi[:], xoi[:], xr[j][:], ALU.add, [rxoi, rxr[j]], [rxoi], eng="POOL")
                dsp(srows(T), xoi[:], [rxoi], [RS[T]])

        prologue(0)
        for gi in range(len(groups)):
            chains = []
            P.capture_start()
            compute(gi)
            chains.append(P.capture_end())
            if gi + 1 < len(groups):
                P.capture_start()
                prologue(gi + 1)
                chains.append(P.capture_end())
            P.emit_zip(chains)

    def phase_moe(stk, l, last):
        streams = [(0, list(range(NLAT)), 512, 4, 128)]
        if not last:
            streams.append((1, [NLAT, NLAT + 1], 32, 1, 32))
        aff = sb("aff", [128, NT, NEXP], F32, stk)
        raff = Res()
        idxS = [sb("idxS%d" % i, [128, 4, NEXP], I32, stk) for i in range(2)]
        idxH = [sb("idxH%d" % i, [128, 4, NEXP], I32, stk) for i in range(2)]
        wsel = [sb("wsel%d" % i, [128, 4, NEXP], F32, stk) for i in range(2)]
        ridx = [Res(), Res()]

        with contextlib.ExitStack() as s1:
            wr = sb("wr", [128, 8, NEXP], BF16, s1)
            rwr = Res()
            dpl(wr[:], w_rt[l].rearrange("(k p) e -> p k e", p=128), [], [rwr])
            h2fs = [sb("h2fA", [128, D], F32, s1), sb("h2fB", [128, D], F32, s1)]
            rh2fs = [Res(), Res()]
            h2b = [sb("h2b%d" % i, [128, D], BF16, s1) for i in range(2)]
            rh2 = [Res(), Res()]
            h2T = [sb("h2T%d" % i, [128, 8, 128], BF16, s1) for i in range(2)]
            rh2T = [Res(), Res()]
            sm = [sb("sm%d" % i, [128, 8], F32, s1) for i in range(2)]
            ex = [sb("ex%d" % i, [128, NEXP], F32, s1) for i in range(2)]
            rsmx = [Res(), Res()]
            allT = [(s_, T) for (s_, tiles, cap, nch, rows) in streams for T in tiles]

            def st1(j):
                s_, T = allT[j]
                i = j % 2
                X, rX, XN, rXN, S, rS = norm_tile(T)
                h2f, rh2f = h2fs[i], rh2fs[i]
                stt(h2f[:], X[:], S[:, 2:3], rowbcA[:, s_, :], ALU.mult, ALU.mult, [rX, rS, rrowA], [rh2f])
                tt(h2b[i][:], h2f[:], rowbcB[:, s_, :], ALU.add, [rh2f, rrowB], [rh2[i]], eng="pool")
                dsp(H2[T * 128:(T + 1) * 128, :], h2b[i][:], [rh2[i]], [RH2[T]])
                for half in range(2):
                    with P.atomic():
                        for kk in range(4):
                            k = half * 4 + kk
                            tr(bankT[:, half * 512 + kk * 128:half * 512 + (kk + 1) * 128], h2b[i][:, k * 128:(k + 1) * 128], identb[:], [rh2[i], rcb], [rbT[half]])
                        cpa(h2T[i][:, half * 4:(half + 1) * 4, :], bankT[:, half * 512:(half + 1) * 512].rearrange("p (k t) -> p k t", t=128), [rbT[half]], [rh2T[i]])

            def st2(j):
                s_, T = allT[j]
                i = j % 2
                bk, rbk = bank[2 + i], rbank[2 + i]
                for k in range(8):
                    mm(bk[:, 0:NEXP], h2T[i][:, k, :], wr[:, k, :], k == 0, k == 7, [rh2T[i], rwr], [rbk])
                smi, exi, rsi = sm[i], ex[i], rsmx[i]
                P.dve(lambda: nc.vector.tensor_reduce(out=smi[:, 0:1], in_=bk[:, 0:NEXP], axis=AX.X, op=ALU.max), [rbk], [rsi])
                tsc(smi[:, 1:2], smi[:, 0:1], -1.0, None, ALU.mult, None, [rsi], [rsi])
                mset(smi[:, 2:3], 0.0, [rsi])
                actf(exi[:], bk[:, 0:NEXP], AF.Exp, [rbk, rsi], [rsi], bias=smi[:, 1:2], accum_out=smi[:, 2:3])
                P.dve(lambda: nc.vector.reciprocal(out=smi[:, 3:4], in_=smi[:, 2:3]), [rsi], [rsi])
                tsc(aff[:, T, :], exi[:], smi[:, 3:4], None, ALU.mult, None, [rsi], [raff])

            for j0 in range(0, len(allT), 2):
                chains = []
                for j in (j0, j0 + 1):
                    if j < len(allT):
                        P.capture_start()
                        st1(j)
                        st2(j)
                        chains.append(P.capture_end())
                P.emit_zip(chains)
            build_rowbc(rowbcA, rrowA, lambda s, c: modT[:, 40 + c, s:s + 1])
            P.flush()
        if _DBG.get("cstop") == "1":
            return

        with contextlib.ExitStack() as s2:
            ge = sb("ge", [128, NLAT, NEXP], BF16, s2)
            maskf = sb("maskf", [128, NLAT, NEXP], F32, s2)
            basef = sb("basef", [128, NLAT, NEXP], F32, s2)
            keyf = sb("keyf", [128, NLAT, NEXP], F32, s2)
            cntp = sb("cntp", [128, NLAT, NEXP], F32, s2)
            vals = sb("vals", [128, NLAT, NEXP, 4], BF16, s2)
            valf = sb("valf", [128, NLAT, NEXP], F32, s2)
            lo = sb("lo", [128, NEXP], F32, s2)
            mid = sb("mid", [128, NEXP], F32, s2)
            cpr = sb("cpr", [128, NEXP], F32, s2)
            cntr = sb("cntr", [128, NEXP], F32, s2)
            rr = Res()
            OH = [sb("OH%d" % i, [128, 512], BF16, s2) for i in range(4)]
            rOH = [Res() for _ in range(4)]
            lst = sb("lst", [128, NEXP, 4, 4], F32, s2)
            idxf = sb("idxf", [128, 4, NEXP], F32, s2)
            NIT = 28
            for (s, tiles, cap, nch, rows) in streams:
                nt = len(tiles)
                T0 = tiles[0]
                av = aff[:, T0:T0 + nt, :]
                W = nt * NEXP
                mset(lo[:], 0.0, [rr])
                mset(mid[:], 0.5, [rr])
                for it in range(NIT):
                    half = 0.5 ** (it + 1)
                    tt(ge[:, 0:nt, :], av, bc(mid[:], 1, nt), ALU.is_ge, [raff, rr], [rr])
                    mm(bank[0][:, 0:W], onesb[:], ge[:, 0:nt, :].rearrange("p t e -> p (t e)"), True, True, [rr, rcb], [rbank[0]])
                    P.dve(lambda W=W, nt=nt: nc.vector.tensor_reduce(out=cntr[:], in_=bank[0][:, 0:W].rearrange("p (t e) -> p e t", e=NEXP), axis=AX.X, op=ALU.add),
                          [rbank[0]], [rr])
                    tsc(cpr[:], cntr[:], float(cap) - 0.5, half, ALU.is_ge, ALU.mult, [rr], [rr])
                    stt(mid[:], cpr[:], half * 0.5, lo[:], ALU.add, ALU.add, [rr], [rr])
                    tt(lo[:], lo[:], cpr[:], ALU.add, [rr], [rr])
                tt(maskf[:, 0:nt, :], av, bc(lo[:], 1, nt), ALU.is_ge, [raff, rr], [rr])
                cpv(ge[:, 0:nt, :], maskf[:, 0:nt, :], [rr], [rr])
                gef = ge[:, 0:nt, :].rearrange("p t e -> p (t e)")
                mm(bank[0][:, 0:W], trib[:], gef, True, True, [rr, rcb], [rbank[0]])
                mm(bank[1][:, 0:W], onesb[:], gef, True, True, [rr, rcb], [rbank[1]])
                cpv(cntp[:, 0:nt, :].rearrange("p t e -> p (t e)"), bank[1][:, 0:W], [rbank[1]], [rr])
                mset(basef[:, 0, :], 0.0, [rr])
                for t_ in range(1, nt):
                    tt(basef[:, t_, :], basef[:, t_ - 1, :], cntp[:, t_ - 1, :], ALU.add, [rr], [rr])
                tt(keyf[:, 0:nt, :].rearrange("p t e -> p (t e)"), bank[0][:, 0:W], basef[:, 0:nt, :].rearrange("p t e -> p (t e)"), ALU.add, [rbank[0], rr], [rr])
                stt(keyf[:, 0:nt, :], keyf[:, 0:nt, :], 1.0, maskf[:, 0:nt, :], ALU.add, ALU.mult, [rr], [rr])
                tsc(keyf[:, 0:nt, :], keyf[:, 0:nt, :], -1.0, None, ALU.add, None, [rr], [rr])
                cpv(vals[:, 0:nt, :, 0], bc(ct("ttv")[:, 0:nt], 2, NEXP), [rct], [rr])
                cpv(vals[:, 0:nt, :, 1], ct("jcol").unsqueeze(2).to_broadcast([128, nt, NEXP]), [rct], [rr])
                cpv(vals[:, 0:nt, :, 2], av, [raff], [rr])
                cpv(valf[:, 0:nt, :], vals[:, 0:nt, :, 2], [rr], [rr])
                tt(vals[:, 0:nt, :, 3], av, valf[:, 0:nt, :], ALU.subtract, [raff, rr], [rr])
                capw = nch * 128
                oc = 0
                rvals = rr
                for e in range(NEXP):
                    for t_ in range(nt):
                        i = oc % 4
                        oc += 1
                        tsc(OH[i][:, 0:capw], ct("iota512")[:, 0:capw], keyf[:, t_, e:e + 1], None, ALU.is_equal, None, [rct, rvals], [rOH[i]],
                            eng="dve")
                        for c in range(nch):
                            mm(bank[3][:, (e * 4 + c) * 4:(e * 4 + c) * 4 + 4], OH[i][:, c * 128:(c + 1) * 128], vals[:, t_, e, :],
                               e == 0 and t_ == 0 and c == 0, e == NEXP - 1 and t_ == nt - 1 and c == nch - 1, [rOH[i], rvals], [rbank[3]])
                lv = lst[:, :, 0:nch, :]
                cpv(lv, bank[3][:, 0:256].rearrange("p (e c v) -> p e c v", c=4, v=4)[:, :, 0:nch, :], [rbank[3], rvals], [rr])
                iv = idxf[:, 0:nch, :].rearrange("p c e -> p e c")
                stt(iv, lv[:, :, :, 0], 128.0, lv[:, :, :, 1], ALU.mult, ALU.add, [rr], [rr])
                cpv(idxS[s][:, 0:nch, :], idxf[:, 0:nch, :], [rr], [ridx[s]])
                tsc(idxf[:, 0:nch, :], idxf[:, 0:nch, :], float(T0 * 128), None, ALU.add, None, [rr], [rr])
                cpv(idxH[s][:, 0:nch, :], idxf[:, 0:nch, :], [rr], [ridx[s]])
                tt(wsel[s][:, 0:nch, :].rearrange("p c e -> p e c"), lv[:, :, :, 2], lv[:, :, :, 3], ALU.add, [rr], [ridx[s]])
            P.flush()
        if _DBG.get("cstop") == "2":
            return

        wg = [sb("wg%d" % i, [128, 8, D], BF16, stk) for i in range(2)]
        wu = [sb("wu%d" % i, [128, 8, D], BF16, stk) for i in range(2)]
        wd = [sb("wd%d" % i, [128, 8, D], BF16, stk) for i in range(2)]
        rwe = [Res(), Res()]
        NS = len(streams)
        xs = [sb("xs%d" % s_, [128, 4 if s_ == 0 else 1, D], BF16, stk) for s_ in range(NS)]
        rxs = [Res() for _ in range(NS)]
        xsT = [[sb("xsT%d_%d" % (s_, i), [128, 8, 512 if s_ == 0 else 32], BF16, stk) for i in range(2)] for s_ in range(NS)]
        rxsT = [[Res(), Res()] for _ in range(NS)]
        sa = [sb("sa%d" % i, [128, 512], F32, stk) for i in range(2)]
        rsa = [Res(), Res()]
        hf = [sb("hf%d" % s_, [128, 8, 512 if s_ == 0 else 32], BF16, stk) for s_ in range(NS)]
        rhf = [Res() for _ in range(NS)]
        yscs = [sb("ysc%d" % i, [128, D], F32, stk) for i in range(4)]
        ryscs = [Res() for _ in range(4)]
        ycn = {"n": 0}

        def loads(e):
            par = e % 2
            for (wt_, src) in ((wg, w_eg), (wu, w_eu), (wd, w_ed)):
                for hh in range(2):
                    dpl(wt_[par][:, hh * 4:(hh + 1) * 4, :], src[l][e][hh * 512:(hh + 1) * 512, :].rearrange("(k p) n -> p k n", p=128), [], [rwe[par]])

        def gather(e):
            for (s, tiles, cap, nch, rows) in streams:
                for c in range(nch):
                    (lambda c, s, rows, e: P.dma("pool", lambda: nc.gpsimd.indirect_dma_start(
                        out=xs[s][0:rows, c, :], out_offset=None, in_=H2,
                        in_offset=bass.IndirectOffsetOnAxis(ap=idxH[s][0:rows, c, e:e + 1], axis=0)),
                        [ridx[s]] + RH2, [rxs[s]]))(c, s, rows, e)

        def G(e):
            par = e % 2
            for (s, tiles, cap, nch, rows) in streams:
                ntok = nch * rows
                for k in range(8):
                    half = k % 2
                    with P.atomic():
                        for c in range(nch):
                            tr(bankT[:, half * 512 + c * rows: half * 512 + (c + 1) * rows], xs[s][0:rows, c, k * 128:(k + 1) * 128], identb[0:rows, 0:rows],
                               [rxs[s], rcb], [rbT[half]])
                        if k % 2 == 0:
                            cpa(xsT[s][par][:, k, 0:ntok], bankT[:, half * 512: half * 512 + ntok], [rbT[half]], [rxsT[s][par]])
                        else:
                            cpv(xsT[s][par][:, k, 0:ntok], bankT[:, half * 512: half * 512 + ntok], [rbT[half]], [rxsT[s][par]])

        sac = sb("sac", [128, 64], F32, stk)
        rsac = Res()
        yscc = sb("yscc", [128, D], F32, stk)
        ryscc = Res()

        def up(e, si):
            par = e % 2
            (s, tiles, cap, nch, rows) = streams[si]
            ntok = nch * rows
            for fc in range(8):
                if s == 0:
                    p = fc % 2
                    ba, rba, bu, rbu = bank[2 * p][:, 0:ntok], rbank[2 * p], bank[2 * p + 1][:, 0:ntok], rbank[2 * p + 1]
                    sai, rsai = sa[p][:, 0:ntok], rsa[p]
                else:
                    ba, rba, bu, rbu = bank[6][:, 0:ntok], rbank[6], bank[6][:, 64:64 + ntok], rbank[6]
                    sai, rsai = sac[:, 0:ntok], rsac
                for k in range(8):
                    mm(ba, wg[par][:, k, fc * 128:(fc + 1) * 128], xsT[s][par][:, k, 0:ntok], k == 0, k == 7, [rwe[par], rxsT[s][par]], [rba])
                for k in range(8):
                    mm(bu, wu[par][:, k, fc * 128:(fc + 1) * 128], xsT[s][par][:, k, 0:ntok], k == 0, k == 7, [rwe[par], rxsT[s][par]], [rbu])
                actf(sai, ba, AF.Silu, [rba], [rsai])
                tt(hf[s][:, fc, 0:ntok], sai, bu, ALU.mult, [rsai, rbu], [rhf[s]])

        def down(e, si):
            par = e % 2
            (s, tiles, cap, nch, rows) = streams[si]
            dst = out if s == 0 else xc
            rstream = [RS[T] for T in tiles]
            for c in range(nch):
                if s == 0:
                    ysc, rysc = yscs[ycn["n"] % 4], ryscs[ycn["n"] % 4]
                    ycn["n"] += 1
                else:
                    ysc, rysc = yscc, ryscc
                for nb in range(2):
                    if s == 0:
                        bd, rbd = bank[4 + nb], rbank[4 + nb]
                    else:
                        bd, rbd = bank[6], rbank[6]
                    for fc in range(8):
                        mm(bd[0:rows, :], hf[s][:, fc, c * rows:(c + 1) * rows], wd[par][:, fc, nb * 512:(nb + 1) * 512], fc == 0, fc == 7, [rhf[s], rwe[par]], [rbd])
                    stt(ysc[0:rows, nb * 512:(nb + 1) * 512], bd[0:rows, :], wsel[s][0:rows, c, e:e + 1], rowbcA[0:rows, s, nb * 512:(nb + 1) * 512],
                        ALU.mult, ALU.mult, [rbd, ridx[s], rrowA], [rysc])
                if _DBG.get("noscatter"):
                    continue
                (lambda c, s, rows, e, dst, rstream, ysc, rysc: P.dma("pool", lambda: nc.gpsimd.indirect_dma_start(
                    out=dst, out_offset=bass.IndirectOffsetOnAxis(ap=idxS[s][0:rows, c, e:e + 1], axis=0),
                    in_=ysc[0:rows, :], in_offset=None, compute_op=ALU.add),
                    [rysc, ridx[s]] + rstream, rstream))(c, s, rows, e, dst, rstream, ysc, rysc)

        gather(0)
        loads(0)
        G(0)
        for e in range(NEXP):
            if e + 1 < NEXP:
                gather(e + 1)
                loads(e + 1)
            chains = []
            for si in range(len(streams)):
                P.capture_start()
                up(e, si)
                down(e, si)
                chains.append(P.capture_end())
            if e + 1 < NEXP:
                P.capture_start()
                G(e + 1)
                chains.append((P.capture_end(), 0.6, 0.98))
            P.emit_zip(chains)

    for l in range(DEPTH):
        last = (l == DEPTH - 1)
        tiles_ctx = [NLAT, NLAT + 1]
        tiles_lat = list(range(NLAT))

        with contextlib.ExitStack() as ps:
            cTt = sb("cTt", [128, 8, 2], F32, ps)
            bmT = sb("bmT", [128, 48], F32, ps)
            n1T = sb("n1T", [128, 8], F32, ps)
            n2T = sb("n2T", [128, 8], F32, ps)
            wm = [sb("wm%d" % i, [128, 8, 1024], BF16, ps) for i in range(2)]
            rwm = [Res(), Res()]
            rsm = Res()
            P.dma("sp", lambda: nc.sync.dma_start(out=cTt[:], in_=cT_d.rearrange("p (k s) -> p k s", s=2)), writes=[rsm])
            P.dma("sp", lambda: nc.sync.dma_start(out=bmT[:], in_=bmT_d[l]), writes=[rsm])
            P.dma("sp", lambda: nc.sync.dma_start(out=n1T[:], in_=n1T_d[l]), writes=[rsm])
            P.dma("sp", lambda: nc.sync.dma_start(out=n2T[:], in_=n2T_d[l]), writes=[rsm])
            P.act(lambda: nc.scalar.activation(out=cact[:], in_=cTt[:], func=AF.Silu), reads=[rsm], writes=[rmod])
            for sec in range(6 if stop_after != "M1" else 0):
                i = sec % 2
                (lambda sec, i: P.dma("pool", lambda: nc.gpsimd.dma_start(
                    out=wm[i][:], in_=w_mod[l][:, sec * 1024:(sec + 1) * 1024].rearrange("(k p) n -> p k n", p=128)),
                    writes=[rwm[i]]))(sec, i)
                for j in range(8):
                    col = (sec * 8 + j) * 2
                    for k in range(8):
                        (lambda i, j, k, col: P.pe(lambda: nc.tensor.matmul(bank[2][:, col:col + 2], lhsT=wm[i][:, k, j * 128:(j + 1) * 128],
                                                                            rhs=cact[:, k, :], start=(k == 0), stop=(k == 7)),
                                                   reads=[rwm[i], rmod], writes=[rbank[2]]))(i, j, k, col)
            if stop_after in ("M1", "M2"):
                P.flush()
                break
            P.dve(lambda: nc.vector.tensor_tensor(out=modT[:], in0=bank[2][:, 0:96].rearrange("p (j s) -> p j s", s=2),
                                                  in1=bc(bmT[:], 2, 2), op=ALU.add), reads=[rbank[2], rsm], writes=[rmod])
            if stop_after == "M3":
                P.flush()
                break
            for (Ax, nT, sec) in ((A1, n1T, 1), (A2, n2T, 4)):
                (lambda Ax, nT, sec: P.dve(lambda: nc.vector.scalar_tensor_tensor(
                    out=Ax[:], in0=modT[:, sec * 8:(sec + 1) * 8, :], scalar=1.0, in1=bc(nT[:], 2, 2), op0=ALU.add, op1=ALU.mult),
                    reads=[rmod, rsm], writes=[rmod]))(Ax, nT, sec)
            P.flush()
        B1m = modT[:, 0:8, :]
        B2m = modT[:, 24:32, :]
        if stop_after == "M" and l == 0:
            break

        with contextlib.ExitStack() as pAB:
            ST = sb("ST", [128, NT, 2, 2, 128], BF16, pAB)
            rST = Res("ST")
            decbc = sb("decbc", [128, 8], F32, pAB)
            dechp = sb("dechp", [128, 4], F32, pAB)
            lgbc = sb("lgbc", [128, 8], F32, pAB)
            lghp = sb("lghp", [128, 4], F32, pAB)
            DTt = sb("DT", [128, 4, 128], F32, pAB)
            DfT = sb("DfT", [128, 2, 2, 128], F32, pAB)
            wkd = sb("wkd", [128, 2, 4], F32, pAB)
            Gam = sb("Gam", [128, 4], F32, pAB)
            e1 = sb("e1", [128, 128], F32, pAB)
            e2 = sb("e2", [128, 128], F32, pAB)
            rdec = Res("dec")
            re1 = Res()
            P.dma("sp", lambda: nc.sync.dma_start(out=decbc[:], in_=decbc_d[l]), writes=[rdec])
            P.dma("sp", lambda: nc.sync.dma_start(out=dechp[:], in_=dechp_d[l]), writes=[rdec])
            for (src, dst) in ((decbc, lgbc), (dechp, lghp)):
                (lambda src, dst: (
                    P.act(lambda: nc.scalar.activation(out=dst[:], in_=src[:], func=AF.Exp, scale=-1.0), reads=[rdec], writes=[rdec]),
                    P.dve(lambda: nc.vector.tensor_scalar(out=dst[:], in0=dst[:], scalar1=1.0, scalar2=None, op0=ALU.add), reads=[rdec], writes=[rdec]),
                    P.act(lambda: nc.scalar.activation(out=dst[:], in_=dst[:], func=AF.Ln), reads=[rdec], writes=[rdec]),
                    P.dve(lambda: nc.vector.tensor_scalar(out=dst[:], in0=dst[:], scalar1=-1.0, scalar2=None, op0=ALU.mult), reads=[rdec], writes=[rdec])))(src, dst)
            KS = 0.125
            for h in range(4):
                (lambda h: (
                    P.act(lambda: nc.scalar.activation(out=e1[:], in_=ct("PF"), func=AF.Exp, scale=lgbc[:, h:h + 1]), reads=[rdec, rct], writes=[re1]),
                    P.dve(lambda: nc.vector.tensor_tensor(out=e1[:], in0=e1[:], in1=ct("MF"), op=ALU.mult), reads=[re1, rct], writes=[re1]),
                    P.act(lambda: nc.scalar.activation(out=e2[:], in_=ct("PB"), func=AF.Exp, scale=lgbc[:, 4 + h:5 + h]), reads=[rdec, rct], writes=[re1]),
                    P.dve(lambda: nc.vector.tensor_tensor(out=e2[:], in0=e2[:], in1=ct("MB"), op=ALU.mult), reads=[re1, rct], writes=[re1]),
                    P.dve(lambda: nc.vector.tensor_tensor(out=e1[:], in0=e1[:], in1=e2[:], op=ALU.add), reads=[re1], writes=[re1]),
                    P.dve(lambda: nc.vector.tensor_scalar(out=DTt[:, h, :], in0=e1[:], scalar1=KS, scalar2=None, op0=ALU.mult), reads=[re1], writes=[rdec])))(h)
            P.act(lambda: nc.scalar.activation(out=wkd[:, 0, :], in_=lgbc[:, 0:4], func=AF.Exp, scale=ct("cjm")), reads=[rdec, rct], writes=[rdec])
            P.act(lambda: nc.scalar.activation(out=wkd[:, 1, :], in_=lgbc[:, 4:8], func=AF.Exp, scale=ct("jcol")), reads=[rdec, rct], writes=[rdec])
            P.dve(lambda: nc.vector.tensor_scalar(out=wkd[:], in0=wkd[:], scalar1=KS, scalar2=None, op0=ALU.mult), reads=[rdec], writes=[rdec])
            for dr in range(2):
                for hb in range(2):
                    (lambda dr, hb: (
                        P.act(lambda: nc.scalar.activation(out=DfT[:, dr, hb, :], in_=ct("iota1") if dr == 0 else ct("cmi"), func=AF.Exp,
                                                           scale=lghp[:, dr * 2 + hb: dr * 2 + hb + 1]), reads=[rdec, rct], writes=[rdec]),
                        P.act(lambda: nc.scalar.activation(out=Gam[:, dr * 2 + hb: dr * 2 + hb + 1], in_=lghp[:, dr * 2 + hb: dr * 2 + hb + 1],
                                                           func=AF.Exp, scale=128.0), reads=[rdec], writes=[rdec])))(dr, hb)

            with contextlib.ExitStack() as pA:
                wkv = sb("wkv", [128, 8, 768], BF16, pA)
                rwkv = Res()
                P.dma("pool", lambda: nc.gpsimd.dma_start(out=wkv[:], in_=w_in[l][:, 256:1024].rearrange("(k p) n -> p k n", p=128)), writes=[rwkv])
                UPD = sb("UPD", [128, NT, 2, 2, 128], F32, pA)
                rUPD = Res()
                hTa = [sb("hTa%d" % i, [128, 8, 128], BF16, pA) for i in range(2)]
                rhTa = [Res(), Res()]
                t1 = sb("t1a", [128, 256], F32, pA)
                t2 = sb("t2a", [128, 256], F32, pA)
                kr = sb("kra", [128, 256], F32, pA)
                rt = Res()
                kw = [sb("kwa%d" % i, [128, 2, 256], BF16, pA) for i in range(2)]
                vb = [sb("vba%d" % i, [128, 512], BF16, pA) for i in range(2)]
                rkw = [Res(), Res()]
                tilesA = tiles_ctx + tiles_lat
                t1s = [t1, sb("t1a2", [128, 256], F32, pA)]
                t2s = [t2, sb("t2a2", [128, 256], F32, pA)]
                krs = [kr, sb("kra2", [128, 256], F32, pA)]
                rts = [rt, Res()]
                abanks = [(0, 1, 2), (4, 5, 6)]

                def chainA(n_, st):
                    T = tilesA[n_]
                    s = 0 if T < NLAT else 1
                    ik, iv, iu = abanks[st]
                    bk, rbk, bv, rbv, bu, rbu = bank[ik], rbank[ik], bank[iv], rbank[iv], bank[iu], rbank[iu]
                    X, rX, XN, rXN, S, rS = norm_tile(T)
                    load_rope(T)
                    H = hTa[st]
                    transpose_mod(XN, rXN, A1, B1m, s, (lambda H: (lambda k: H[:, k, :]))(H), rhTa[st])
                    for k in range(8):
                        mm(bk[:, 0:256], H[:, k, :], wkv[:, k, 0:256], k == 0, k == 7, [rhTa[st], rwkv], [rbk])
                    for k in range(8):
                        mm(bv[:, 0:512], H[:, k, :], wkv[:, k, 256:768], k == 0, k == 7, [rhTa[st], rwkv], [rbv])
                    RT, rRT = ropet[T % 2], rrope[T % 2]
                    krv = krs[st][:].rearrange("p (h d) -> p h d", d=64)
                    rope_apply(P, nc, bk[:, 0:256], rbk, RT, rRT, 4, t1s[st], t2s[st], krv, rts[st], rts[st])
                    KW, V = kw[st], vb[st]
                    for dr in range(2):
                        tt(KW[:, dr, :].rearrange("p (h d) -> p h d", d=64), krv, bc(wkd[:, dr, :], 2, 64), ALU.mult, [rts[st], rdec], [rkw[st]])
                    cpa(V[:], bv[:], [rbv], [rkw[st]])
                    for dr in range(2):
                        for hb in range(2):
                            mm(bu[:, hb * 256:(hb + 1) * 256], KW[:, dr, hb * 128:(hb + 1) * 128], V[:, hb * 256:(hb + 1) * 256], True, True,
                               [rkw[st]], [rbu])
                        for hb in range(2):
                            cpv(UPD[0:64, T, dr, hb, :], bu[0:64, hb * 256:hb * 256 + 128], [rbu], [rUPD])
                            cpa(UPD[64:128, T, dr, hb, :], bu[64:128, hb * 256 + 128:hb * 256 + 256], [rbu], [rUPD])

                for n_ in range(0, len(tilesA), 2):
                    chains = []
                    for st in range(2):
                        if n_ + st < len(tilesA):
                            P.capture_start()
                            chainA(n_ + st, st)
                            chains.append(P.capture_end())
                    P.emit_zip(chains)
                cur = [sb("cur%d" % i, [128, 2, 128], F32, pA) for i in range(2)]
                rcur = Res()
                c0, c1 = NLAT, NLAT + 1
                for dr in range(2):
                    first, second = (c0, c1) if dr == 0 else (c1, c0)
                    (lambda dr, first, second: (
                        P.dve(lambda: nc.vector.memset(ST[:, first, dr, :, :], 0.0), writes=[rST]),
                        P.dve(lambda: nc.vector.tensor_copy(out=ST[:, second, dr, :, :], in_=UPD[:, first, dr, :, :]), reads=[rUPD], writes=[rST])))(dr, first, second)
                    order = list(range(NLAT)) if dr == 0 else list(range(NLAT - 1, -1, -1))
                    for hb in range(2):
                        (lambda dr, hb, first, second: P.dve(lambda: nc.vector.scalar_tensor_tensor(
                            out=cur[0][:, hb, :], in0=UPD[:, first, dr, hb, :], scalar=Gam[:, dr * 2 + hb: dr * 2 + hb + 1],
                            in1=UPD[:, second, dr, hb, :], op0=ALU.mult, op1=ALU.add), reads=[rUPD, rdec], writes=[rcur]))(dr, hb, first, second)
                    pp = 0
                    for n in order:
                        (lambda dr, n, pp: P.act(lambda: nc.scalar.copy(out=ST[:, n, dr, :, :], in_=cur[pp][:]), reads=[rcur], writes=[rST]))(dr, n, pp)
                        for hb in range(2):
                            (lambda dr, hb, n, pp: P.dve(lambda: nc.vector.scalar_tensor_tensor(
                                out=cur[1 - pp][:, hb, :], in0=cur[pp][:, hb, :], scalar=Gam[:, dr * 2 + hb: dr * 2 + hb + 1],
                                in1=UPD[:, n, dr, hb, :], op0=ALU.mult, op1=ALU.add), reads=[rUPD, rdec, rcur], writes=[rcur]))(dr, hb, n, pp)
                        pp = 1 - pp
                P.flush()
            if stop_after == "A" and l == 0:
                break

            with contextlib.ExitStack() as pB:
                phase_B(pB, l, last, ST, rST, DTt, DfT, rdec)
                P.flush()
        if stop_after == "B" and l == 0:
            break

        with contextlib.ExitStack() as pM:
            rowbcA = sb("rowbcA", [128, 2, D], F32, pM)
            build_rowbc(rowbcA, rrowA, lambda s, c: modT[:, 16 + c, s:s + 1])
            phase_merge(pM, l, last)
            P.flush()
        if stop_after == "Bp" and l == 0:
            break

        with contextlib.ExitStack() as pC:
            rowbcA = sb("rowbcA", [128, 2, D], F32, pC)
            rowbcB = sb("rowbcB", [128, 2, D], F32, pC)
            build_rowbc(rowbcA, rrowA, lambda s, c: A2[:, c, s:s + 1])
            build_rowbc(rowbcB, rrowB, lambda s, c: modT[:, 24 + c, s:s + 1])
            phase_moe(pC, l, last)
            P.flush()
        if stop_after == "C" and l == 0:
            break

    P.flush()
    _DBG["stats"] = dict(nops=len(P.ops), nwait=P.nwait, ecount=dict(P.ecount), dmax=max(P.dcount))


def rope_apply(P, nc, src, rsrc, RT, rRT, nh, t1, t2, dst, rtmp, rdst, pool=False):
    s3 = src.rearrange("p (h d) -> p h d", d=64)
    t13 = t1[:, 0:nh * 64].rearrange("p (h d) -> p h d", d=64)
    t23 = t2[:, 0:nh * 64].rearrange("p (h d) -> p h d", d=64)
    (P.pool if pool else P.dve)(lambda: (nc.gpsimd if pool else nc.vector).tensor_tensor(out=t13, in0=s3, in1=bc(RT[:, 0:64], 1, nh), op=ALU.mult), reads=[rsrc, rRT], writes=[rtmp])
    (P.pool if pool else P.dve)(lambda: (nc.gpsimd if pool else nc.vector).tensor_tensor(out=t23[:, :, 0:32], in0=s3[:, :, 32:64], in1=bc(RT[:, 64:96], 1, nh), op=ALU.mult),
          reads=[rsrc, rRT], writes=[rtmp])
    (P.pool if pool else P.dve)(lambda: (nc.gpsimd if pool else nc.vector).tensor_tensor(out=t23[:, :, 32:64], in0=s3[:, :, 0:32], in1=bc(RT[:, 96:128], 1, nh), op=ALU.mult),
          reads=[rsrc, rRT], writes=[rtmp])
    (P.pool if pool else P.dve)(lambda: (nc.gpsimd if pool else nc.vector).tensor_tensor(out=dst, in0=t13, in1=t23, op=ALU.add), reads=[rtmp], writes=[rdst])


def _host_inputs(inp):
    f32 = np.float32
    g = lambda k: np.ascontiguousarray(np.asarray(inp[k], dtype=f32))
    x, c, ctx, c_ctx = g("x"), g("c"), g("ctx"), g("c_ctx")
    B = x.shape[0]
    shared = {}
    shared["w_mod"] = g("w_mod")
    shared["bmT"] = np.ascontiguousarray(g("b_mod").reshape(DEPTH, 48, 128).transpose(0, 2, 1))
    shared["n1T"] = np.ascontiguousarray(g("norm1_g").reshape(DEPTH, 8, 128).transpose(0, 2, 1))
    shared["n2T"] = np.ascontiguousarray(g("norm2_g").reshape(DEPTH, 8, 128).transpose(0, 2, 1))
    shared["w_in"] = g("w_in")
    df, db = g("ret_decay_fwd"), g("ret_decay_bwd")
    decbc = np.concatenate([df, db], axis=1)
    shared["decbc"] = np.ascontiguousarray(np.broadcast_to(decbc[:, None, :], (DEPTH, 128, 8)))
    hp = np.zeros((DEPTH, 128, 4), f32)
    for dr, dd in enumerate((df, db)):
        for hb in range(2):
            hp[:, 0:64, dr * 2 + hb] = dd[:, 2 * hb][:, None]
            hp[:, 64:128, dr * 2 + hb] = dd[:, 2 * hb + 1][:, None]
    shared["dechp"] = hp
    shared["sgu_wT"] = np.ascontiguousarray(g("sgu_w").transpose(0, 1, 3, 2))
    shared["sgu_bbc"] = np.ascontiguousarray(np.broadcast_to(g("sgu_b").reshape(DEPTH, 1, 512), (DEPTH, 128, 512)))
    shared["qg"] = np.ascontiguousarray(np.broadcast_to(g("q_norm_g")[:, None, :], (DEPTH, 128, 64)))
    shared["kg"] = np.ascontiguousarray(np.broadcast_to(g("k_norm_g")[:, None, :], (DEPTH, 128, 64)))
    shared["sink"] = np.ascontiguousarray(np.broadcast_to(g("attn_sink")[:, None, :], (DEPTH, 128, 8)))
    for k in ("w_branch", "w_out", "w_router", "w_exp_gate", "w_exp_up", "w_exp_down"):
        shared[k] = g(k)
    shared["rope"] = _rope_tables()
    shared["ctab"] = _const_table()
    maps = []
    for b in range(B):
        m = dict(shared)
        m["x"] = x[b]
        m["ctxin"] = ctx[b]
        cT = np.stack([c[b].reshape(8, 128).T, c_ctx.reshape(8, 128).T], axis=-1)
        m["cT"] = np.ascontiguousarray(cT.reshape(128, 16))
        maps.append(m)
    return maps


_NC_CACHE = {}


def kernel(**inputs):
    maps = _host_inputs(inputs)
    if "nc" not in _NC_CACHE:
        _NC_CACHE["nc"] = build_program()
    nc = _NC_CACHE["nc"]
    n = len(maps)
    res = run_bass_kernel_spmd(nc, maps, core_ids=list(range(n)))
    return np.stack([np.asarray(r["out"], dtype=np.float32) for r in res.results], axis=0)
```

```python
import contextlib
import numpy as np
import concourse.bass as bass
import concourse.mybir as mybir
from concourse.bass_utils import run_bass_kernel_spmd

F32 = mybir.dt.float32
BF16 = mybir.dt.bfloat16
I32 = mybir.dt.int32
AF = mybir.ActivationFunctionType
ALU = mybir.AluOpType
AX = mybir.AxisListType

D = 1024
SEQ = 4096
CTX = 256
NLAT = 32
NT = 34
DEPTH = 2
NEXP = 16
EPS = 1e-6
INW = 6400
NG = 3328

_DBG = {}


class Res:
    __slots__ = ("name", "w", "r", "excl")

    def __init__(self, name="", excl=False):
        self.name = name
        self.w = None
        self.r = []
        self.excl = excl


class _Op:
    __slots__ = ("eng", "fn", "reads", "writes", "dma", "deps", "inc", "val", "semid")

    def __init__(self, eng, fn, reads, writes, dma):
        self.eng = eng
        self.fn = fn
        self.reads = reads
        self.writes = writes
        self.dma = dma
        self.deps = ()
        self.inc = False
        self.val = 0
        self.semid = None


class Prog:
    NDMASEM = 64
    NHW = 32

    def __init__(self, nc, es):
        self.nc = nc
        self.ops = []
        self.done = 0
        self.engs = {"pe": nc.tensor, "act": nc.scalar, "dve": nc.vector,
                     "pool": nc.gpsimd, "sp": nc.sync}
        self.sems = {e: es.enter_context(nc.semaphore("sem_" + e)) for e in self.engs}
        self.dsems = [es.enter_context(nc.semaphore("dsem%d" % i)) for i in range(self.NDMASEM)]
        self.dlast = [None] * self.NDMASEM
        self.drr = 0
        self.drr_sw = 0
        self.ecount = {e: 0 for e in self.engs}
        self.dcount = [0] * self.NDMASEM
        self.waited = {e: {} for e in self.engs}
        self.nwait = 0
        self._cap = None
        self._unit = None

    def add(self, eng, fn, reads=(), writes=(), dma=False):
        reads = tuple(reads)
        writes = tuple(writes)
        ex = tuple(r for r in reads if r.excl)
        if ex:
            reads = tuple(r for r in reads if not r.excl)
            writes = writes + ex
        op = _Op(eng, fn, reads, writes, dma)
        if self._cap is not None:
            if self._unit is not None:
                self._unit.append(op)
            else:
                self._cap.append([op])
        else:
            self.ops.append(op)

    def capture_start(self):
        self._cap = []
        self._unit = None

    def capture_end(self):
        cap = self._cap
        self._cap = None
        return cap

    @contextlib.contextmanager
    def atomic(self):
        if self._cap is None or self._unit is not None:
            yield
            return
        self._unit = []
        try:
            yield
        finally:
            self._cap.append(self._unit)
            self._unit = None

    def emit_zip(self, chains):
        items = []
        for ci, ch in enumerate(chains):
            lo, hi = 0.0, 1.0
            if isinstance(ch, tuple):
                ch, lo, hi = ch
            n = len(ch)
            for ui, unit in enumerate(ch):
                items.append((lo + (hi - lo) * (ui + 0.5) / n, ci, ui, unit))
        items.sort(key=lambda t: (t[0], t[1]))
        for _, _, _, unit in items:
            self.ops.extend(unit)

    def pe(self, fn, reads=(), writes=()):
        self.add("pe", fn, reads, writes)

    def act(self, fn, reads=(), writes=()):
        self.add("act", fn, reads, writes)

    def dve(self, fn, reads=(), writes=()):
        self.add("dve", fn, reads, writes)

    def pool(self, fn, reads=(), writes=()):
        self.add("pool", fn, reads, writes)

    def dma(self, eng, fn, reads=(), writes=()):
        self.add(eng, fn, reads, writes, dma=True)

    def flush(self):
        ops = self.ops
        base = self.done
        n = len(ops)
        last_compute = {}
        for i in range(base, n):
            op = ops[i]
            raw = set()
            war = set()
            for r in op.reads:
                if r.w is not None:
                    raw.add(r.w)
            for w in op.writes:
                if w.w is not None:
                    raw.add(w.w)
                war.update(w.r)
            if op.dma:
                if op.eng == "pool":
                    s = self.NHW + self.drr_sw
                    self.drr_sw = (self.drr_sw + 1) % (self.NDMASEM - self.NHW)
                else:
                    s = self.drr
                    self.drr = (self.drr + 1) % self.NHW
                op.semid = s
                if self.dlast[s] is not None:
                    raw.add(self.dlast[s])
                self.dlast[s] = i
            else:
                last_compute[op.eng] = i
            deps = set()
            for j in raw:
                if j < base or j == i:
                    continue
                o = ops[j]
                if (not o.dma) and (not op.dma) and o.eng == op.eng and op.eng == "pe":
                    continue
                deps.add(j)
            for j in war:
                if j < base or j == i:
                    continue
                o = ops[j]
                if (not o.dma) and o.eng == op.eng and not op.dma and op.eng == "pe":
                    continue
                deps.add(j)
            for r in op.reads:
                r.r.append(i)
            for w in op.writes:
                w.w = i
                w.r = []
            best = {}
            for j in deps:
                o = ops[j]
                key = ("d", o.semid) if o.dma else ("e", o.eng)
                if key not in best or best[key] < j:
                    best[key] = j
            op.deps = tuple(best.values())
            for j in op.deps:
                ops[j].inc = True
        for e, i in last_compute.items():
            ops[i].inc = True
        for i in range(base, n):
            op = ops[i]
            if op.dma:
                self.dcount[op.semid] += 16
                op.val = self.dcount[op.semid]
            elif op.inc:
                self.ecount[op.eng] += 1
                op.val = self.ecount[op.eng]
        for i in range(base, n):
            op = ops[i]
            eng = self.engs[op.eng]
            wt = self.waited[op.eng]
            for j in op.deps:
                o = ops[j]
                if o.dma:
                    key, sem = ("d", o.semid), self.dsems[o.semid]
                else:
                    key, sem = ("e", o.eng), self.sems[o.eng]
                if wt.get(key, 0) >= o.val:
                    continue
                eng.wait_ge(sem, o.val)
                self.nwait += 1
                wt[key] = o.val
            inst = op.fn()
            if op.dma:
                inst.then_inc(self.dsems[op.semid], 16)
            elif op.inc:
                inst.then_inc(self.sems[op.eng], 1)
            op.fn = None
        for e, eng in self.engs.items():
            wt = self.waited[e]
            for x in self.engs:
                if x == e:
                    continue
                v = self.ecount[x]
                if v > wt.get(("e", x), 0):
                    eng.wait_ge(self.sems[x], v)
                    wt[("e", x)] = v
            for s in range(self.NDMASEM):
                v = self.dcount[s]
                if v > wt.get(("d", s), 0):
                    eng.wait_ge(self.dsems[s], v)
                    wt[("d", s)] = v
        self.done = n


def bc(ap, axis, n):
    s = list(ap.shape)
    s.insert(axis, n)
    return ap.unsqueeze(axis).to_broadcast(s)


def _rope_tables():
    f32 = np.float32
    rows = SEQ // 64
    row = np.repeat(np.arange(rows), 64).astype(f32)
    col = np.tile(np.arange(64), rows).astype(f32)
    nf = 16
    inv = np.power(f32(10000.0), -np.arange(nf, dtype=f32) / f32(nf)).astype(f32)
    ang = np.concatenate([row[:, None] * inv, col[:, None] * inv], axis=-1).astype(f32)
    cos = np.cos(ang).astype(f32)
    sin = np.sin(ang).astype(f32)
    tab = np.zeros((NT * 128, 128), f32)
    tab[:SEQ, 0:32] = cos
    tab[:SEQ, 32:64] = cos
    tab[:SEQ, 64:96] = -sin
    tab[:SEQ, 96:128] = sin
    tab[SEQ:, 0:64] = 1.0
    return tab.reshape(NT, 128, 128)


CT = {}


def _const_table():
    cols = []

    def put(name, arr):
        arr = np.asarray(arr, np.float32).reshape(128, -1)
        CT[name] = (sum(c.shape[1] for c in cols), arr.shape[1])
        cols.append(arr)

    j = np.arange(128)[:, None]
    i = np.arange(128)[None, :]
    put("ident", (j == i))
    put("ones", np.ones((128, 128)))
    put("PF", np.maximum(i - j, 0))
    put("PB", np.maximum(j - i, 0))
    put("MF", (i >= j))
    put("MB", (j > i))
    put("iota1", np.broadcast_to(i + 1, (128, 128)))
    put("cmi", np.broadcast_to(128 - i, (128, 128)))
    put("cjm", 127 - j)
    put("jcol", j)
    put("tri", (j < i))
    put("iota512", np.broadcast_to(np.arange(512)[None, :], (128, 512)))
    mp = np.where(j >= i, 0.0, -30000.0)
    mn = np.where(j <= i, 0.0, -30000.0)
    put("mprev", np.tile(mp, (1, 4)))
    put("mnext", np.tile(mn, (1, 4)))
    put("ttv", np.broadcast_to(np.arange(NLAT)[None, :], (128, NLAT)))
    put("eps", np.full((128, 1), EPS))
    return np.concatenate(cols, axis=1)


def build_program(stop_after=None, dbg_out=False):
    nc = bass.Bass("TRN2", target_bir_lowering=False)
    es = contextlib.ExitStack()
    with es:
        _build(nc, es, stop_after, dbg_out)
    return nc


def _build(nc, es, stop_after, dbg_out):
    ctab_np = _const_table()
    NCT = ctab_np.shape[1]

    def din(name, shape, dt=F32):
        return nc.dram_tensor(name, list(shape), dt, kind="ExternalInput").ap()

    x_in = din("x", [SEQ, D])
    ctx_in = din("ctxin", [CTX, D])
    cT_d = din("cT", [128, 16])
    w_mod = din("w_mod", [DEPTH, D, 6 * D])
    bmT_d = din("bmT", [DEPTH, 128, 48])
    n1T_d = din("n1T", [DEPTH, 128, 8])
    n2T_d = din("n2T", [DEPTH, 128, 8])
    w_in = din("w_in", [DEPTH, D, INW])
    decbc_d = din("decbc", [DEPTH, 128, 8])
    dechp_d = din("dechp", [DEPTH, 128, 4])
    swT_d = din("sgu_wT", [DEPTH, 4, 128, 128])
    sbb_d = din("sgu_bbc", [DEPTH, 128, 512])
    qg_d = din("qg", [DEPTH, 128, 64])
    kg_d = din("kg", [DEPTH, 128, 64])
    sink_d = din("sink", [DEPTH, 128, 8])
    w_br = din("w_branch", [DEPTH, 3, 512, D])
    w_out = din("w_out", [DEPTH, D, D])
    w_rt = din("w_router", [DEPTH, D, NEXP])
    w_eg = din("w_exp_gate", [DEPTH, NEXP, D, D])
    w_eu = din("w_exp_up", [DEPTH, NEXP, D, D])
    w_ed = din("w_exp_down", [DEPTH, NEXP, D, D])
    rope_d = din("rope", [NT, 128, 128])
    ctab_d = din("ctab", [128, NCT])
    out = nc.dram_tensor("out", [SEQ, D], F32, kind="ExternalOutput").ap()
    xc = nc.dram_tensor("xc", [CTX, D], F32, kind="ExternalOutput").ap()
    BR = nc.dram_tensor("BR", [3, 512, NT * 128], BF16, kind="ExternalOutput").ap()
    H2 = nc.dram_tensor("H2", [NT * 128, D], BF16, kind="ExternalOutput").ap()

    P = Prog(nc, es)

    _uid = [0]

    def sb(name, shape, dt, stack=es):
        _uid[0] += 1
        return stack.enter_context(nc.sbuf_tensor("%s_%d" % (name, _uid[0]), list(shape), dt))

    bank = [es.enter_context(nc.psum_tensor("bank%d" % i, [128, 512], F32)) for i in range(7)]
    bankT = es.enter_context(nc.psum_tensor("bankT", [128, 1024], BF16))
    rbank = [Res("bank%d" % i, excl=True) for i in range(7)]
    _rbT = Res("bankT", excl=True)
    rbT = [_rbT, _rbT]

    RS = [Res("S%d" % t) for t in range(NT)]
    RBR = [[Res() for _ in range(NT)] for _ in range(3)]
    RH2 = [Res() for _ in range(NT)]

    def srows(T):
        return out[T * 128:(T + 1) * 128, :] if T < NLAT else xc[(T - NLAT) * 128:(T - NLAT + 1) * 128, :]

    ctab = sb("ctab", [128, NCT], F32)
    rct = Res("ctab")
    P.dma("sp", lambda: nc.sync.dma_start(out=ctab[:], in_=ctab_d), writes=[rct])

    def ct(name):
        o, n = CT[name]
        return ctab[:, o:o + n]

    identb = sb("identb", [128, 128], BF16)
    onesb = sb("onesb", [128, 128], BF16)
    trib = sb("trib", [128, 128], BF16)
    mprevb = sb("mprevb", [128, 512], BF16)
    mnextb = sb("mnextb", [128, 512], BF16)
    rcb = Res("constb")
    for dst, nm in ((identb, "ident"), (onesb, "ones"), (trib, "tri"), (mprevb, "mprev"), (mnextb, "mnext")):
        (lambda dst, nm: P.dve(lambda: nc.vector.tensor_copy(out=dst[:], in_=ct(nm)), reads=[rct], writes=[rcb]))(dst, nm)
    epsc = ct("eps")
    negh = sb("negh", [128, 8], F32)
    rnegh = Res("negh")
    P.dve(lambda: nc.vector.memset(negh[:], -0.5), writes=[rnegh])

    for q in range(8):
        (lambda q: P.dma("sp", lambda: nc.sync.dma_start(out=out[q * 512:(q + 1) * 512, :], in_=x_in[q * 512:(q + 1) * 512, :]),
                         writes=RS[q * 4:(q + 1) * 4]))(q)
    P.dma("sp", lambda: nc.sync.dma_start(out=xc, in_=ctx_in), writes=RS[NLAT:NT])

    cact = sb("cact", [128, 8, 2], BF16)
    modT = sb("modT", [128, 48, 2], F32)
    A1 = sb("A1", [128, 8, 2], F32)
    A2 = sb("A2", [128, 8, 2], F32)
    rmod = Res("mod")
    rowbcA = None
    rowbcB = None
    rrowA = Res("rowA")
    rrowB = Res("rowB")
    dgt = [sb("dgt%d" % i, [128, 128], F32) for i in range(2)]
    rdg = [Res(), Res()]

    xt = [sb("xt%d" % i, [128, D], F32) for i in range(2)]
    rxt = [Res(), Res()]
    sqj = sb("sqj", [128, D], F32)
    rsq = Res()
    ssum = [sb("ssum%d" % i, [128, 4], F32) for i in range(2)]
    rss = [Res(), Res()]
    xn = [sb("xn%d" % i, [128, D], BF16) for i in range(2)]
    rxn = [Res(), Res()]
    ropet = [sb("ropet%d" % i, [128, 128], F32) for i in range(2)]
    rrope = [Res(), Res()]
    cnt = {"tile": 0, "ev": 0}

    def load_rope(T):
        i = T % 2
        P.dma("sp", lambda: nc.sync.dma_start(out=ropet[i][:], in_=rope_d[T]), writes=[rrope[i]])
        return ropet[i], rrope[i]

    def norm_tile(T, want_x=False):
        i = cnt["tile"] % 2
        cnt["tile"] += 1
        X, rX, S, rS, XN, rXN = xt[i], rxt[i], ssum[i], rss[i], xn[i], rxn[i]
        P.dma("sp", lambda: nc.sync.dma_start(out=X[:], in_=srows(T)), reads=[RS[T]], writes=[rX])
        P.dve(lambda: nc.vector.memset(S[:], 0.0), writes=[rS])
        P.act(lambda: nc.scalar.activation(out=sqj[:], in_=X[:], func=AF.Square, accum_out=S[:, 0:1]),
              reads=[rX], writes=[rsq, rS])
        P.dve(lambda: nc.vector.tensor_scalar(out=S[:, 1:2], in0=S[:, 0:1], scalar1=1.0 / D, scalar2=EPS, op0=ALU.mult, op1=ALU.add),
              reads=[rS], writes=[rS])
        P.pool(lambda: nc.gpsimd.tensor_tensor(out=S[:, 2:3], in0=S[:, 1:2], in1=negh[:, 0:1], op=ALU.pow), reads=[rS, rnegh], writes=[rS])
        P.dve(lambda: nc.vector.tensor_scalar(out=XN[:], in0=X[:], scalar1=S[:, 2:3], scalar2=None, op0=ALU.mult),
              reads=[rX, rS], writes=[rXN])
        return X, rX, XN, rXN, S, rS

    def transpose_mod(XN, rXN, A, Bm, s, dst_fn, rdst):
        for half in range(2):
          with P.atomic():
            rb = rbT[half]
            for kk in range(4):
                k = half * 4 + kk
                (lambda k, kk, half: P.pe(lambda: nc.tensor.transpose(out=bankT[:, half * 512 + kk * 128: half * 512 + (kk + 1) * 128],
                                                                       in_=XN[:, k * 128:(k + 1) * 128], identity=identb[:]),
                                          reads=[rXN, rcb], writes=[rb]))(k, kk, half)
            for kk in range(4):
                k = half * 4 + kk
                src = bankT[:, half * 512 + kk * 128: half * 512 + (kk + 1) * 128]
                if kk % 2 == 0:
                    (lambda k, src: P.act(lambda: nc.scalar.activation(out=dst_fn(k), in_=src, func=AF.Identity,
                                                                       scale=A[:, k, s:s + 1], bias=Bm[:, k, s:s + 1]),
                                          reads=[rb, rmod], writes=[rdst]))(k, src)
                else:
                    (lambda k, src: P.dve(lambda: nc.vector.tensor_scalar(out=dst_fn(k), in0=src, scalar1=A[:, k, s:s + 1],
                                                                          scalar2=Bm[:, k, s:s + 1], op0=ALU.mult, op1=ALU.add),
                                          reads=[rb, rmod], writes=[rdst]))(k, src)

    def build_rowbc(dst, rdst, col_fn):
        n = 0
        for s in range(2):
            for hb in range(2):
                b = bank[hb]
                rb = rbank[hb]
                for cc in range(4):
                    c = hb * 4 + cc
                    g = n % 2
                    n += 1
                    (lambda c, cc, g, s, b, rb: (
                        P.dve(lambda: nc.vector.tensor_scalar(out=dgt[g][:], in0=ct("ident"), scalar1=col_fn(s, c), scalar2=None, op0=ALU.mult),
                              reads=[rct, rmod], writes=[rdg[g]]),
                        P.pe(lambda: nc.tensor.matmul(b[:, cc * 128:(cc + 1) * 128], lhsT=ct("ones"), rhs=dgt[g][:], start=True, stop=True),
                             reads=[rdg[g], rct], writes=[rb])))(c, cc, g, s, b, rb)
                (lambda s, hb, b, rb: P.act(lambda: nc.scalar.copy(out=dst[:, s, hb * 512:(hb + 1) * 512], in_=b[:]),
                                            reads=[rb], writes=[rdst]))(s, hb, b, rb)

    def mm(o, lhsT, rhs, start, stop, reads, writes, tp=None):
        kw = {} if (tp is None or _DBG.get("notp")) else {"tile_position": tp}
        P.pe(lambda: nc.tensor.matmul(o, lhsT=lhsT, rhs=rhs, start=start, stop=stop, **kw), reads, writes)

    def tr(o, in_, ident, reads, writes):
        P.pe(lambda: nc.tensor.transpose(out=o, in_=in_, identity=ident), reads, writes)

    def actf(o, in_, func, reads, writes, **kw):
        P.act(lambda: nc.scalar.activation(out=o, in_=in_, func=func, **kw), reads, writes)

    def cpa(o, in_, reads, writes):
        P.act(lambda: nc.scalar.copy(out=o, in_=in_), reads, writes)

    def cpv(o, in_, reads, writes, eng="dve"):
        if eng == "POOL":
            eng = "pool"
        elif not _DBG.get("usepool"):
            eng = "dve"
        e = nc.vector if eng == "dve" else nc.gpsimd
        P.add(eng, lambda: e.tensor_copy(out=o, in_=in_), reads, writes)

    def tt(o, a, b, op, reads, writes, eng="dve"):
        if eng == "POOL":
            eng = "pool"
        elif not _DBG.get("usepool"):
            eng = "dve"
        e = nc.vector if eng == "dve" else nc.gpsimd
        P.add(eng, lambda: e.tensor_tensor(out=o, in0=a, in1=b, op=op), reads, writes)

    def tsc(o, a, s1, s2, op0, op1, reads, writes, eng="dve"):
        if eng == "POOL":
            eng = "pool"
        elif not _DBG.get("usepool"):
            eng = "dve"
        e = nc.vector if eng == "dve" else nc.gpsimd
        if op1 is None:
            P.add(eng, lambda: e.tensor_scalar(out=o, in0=a, scalar1=s1, scalar2=None, op0=op0), reads, writes)
        else:
            P.add(eng, lambda: e.tensor_scalar(out=o, in0=a, scalar1=s1, scalar2=s2, op0=op0, op1=op1), reads, writes)

    def stt(o, a, s, b, op0, op1, reads, writes, eng="dve"):
        if eng == "POOL":
            eng = "pool"
        elif not _DBG.get("usepool"):
            eng = "dve"
        e = nc.vector if eng == "dve" else nc.gpsimd
        P.add(eng, lambda: e.scalar_tensor_tensor(out=o, in0=a, scalar=s, in1=b, op0=op0, op1=op1), reads, writes)

    def mset(o, v, writes, eng="dve"):
        if eng == "POOL":
            eng = "pool"
        elif not _DBG.get("usepool"):
            eng = "dve"
        e = nc.vector if eng == "dve" else nc.gpsimd
        P.add(eng, lambda: e.memset(o, v), (), writes)

    def dsp(o, in_, reads, writes):
        P.dma("sp", lambda: nc.sync.dma_start(out=o, in_=in_), reads, writes)

    def dpl(o, in_, reads, writes):
        P.dma("pool", lambda: nc.gpsimd.dma_start(out=o, in_=in_), reads, writes)

    def rsqrt_chain(dst_sd, dst_r, src, scale, reads_res, wres):
        n, w = src.shape[0], src.shape[1]
        tsc(dst_sd, src, scale, EPS, ALU.mult, ALU.add, [reads_res], [wres])
        P.pool(lambda: nc.gpsimd.tensor_tensor(out=dst_r, in0=dst_sd, in1=negh[0:n, 0:w], op=ALU.pow), [wres, rnegh], [wres])

    def kwview(w):
        return w.rearrange("(k p) n -> p k n", p=128)

    def phase_B(stk, l, last, ST, rST, DTt, DfT, rdec):
        wng = sb("wng", [128, 8, NG], BF16, stk)
        rwng = Res()
        for q in range(4):
            c0, c1 = q * 832, (q + 1) * 832
            dpl(wng[:, :, c0:c1], kwview(w_in[l][:, c0:c1]), [], [rwng])
        wsT = sb("wsT", [128, 4, 128], BF16, stk)
        sbb = sb("sbb", [128, 512], F32, stk)
        qg = sb("qg", [128, 64], F32, stk)
        kg = sb("kg", [128, 64], F32, stk)
        esink = sb("esink", [128, 8], F32, stk)
        rsm = Res()
        dpl(wsT[:], swT_d[l].rearrange("g j i -> j g i"), [], [rsm])
        dsp(sbb[:], sbb_d[l], [], [rsm])
        dsp(qg[:], qg_d[l], [], [rsm])
        dsp(kg[:], kg_d[l], [], [rsm])
        dsp(esink[:], sink_d[l], [], [rsm])
        tsc(qg[:], qg[:], 0.125, None, ALU.mult, None, [rsm], [rsm])
        actf(esink[:], esink[:], AF.Exp, [rsm], [rsm])
        KT_all = sb("KT_all", [128, NT, 128], BF16, stk)
        V_all = sb("V_all", [128, NT, 2, 80], BF16, stk)
        rKV = [Res() for _ in range(NT)]
        mset(V_all[:], 1.0, rKV)
        hT = [sb("hTb%d" % i, [128, 8, 128], BF16, stk) for i in range(2)]
        rhT = [Res(), Res()]
        zqk = [sb("zqk%d" % i, [128, 512], F32, stk) for i in range(2)]
        vb = [sb("vb%d" % i, [128, 512], BF16, stk) for i in range(2)]
        sg = [sb("sg%d" % i, [128, 512], BF16, stk) for i in range(2)]
        uT = [sb("uT%d" % i, [128, 512], BF16, stk) for i in range(2)]
        gv = [sb("gv%d" % i, [128, 512], F32, stk) for i in range(2)]
        zq = [sb("zq%d" % i, [128, 512], F32, stk) for i in range(2)]
        zk = [sb("zk%d" % i, [128, 128], F32, stk) for i in range(2)]
        rzqk, rvb, rsg, ruT, rgv, rzq, rzk = [[Res(), Res()] for _ in range(7)]
        t1 = sb("t1", [128, 512], F32, stk)
        t2 = sb("t2", [128, 512], F32, stk)
        rtmp = Res()
        t1a = sb("t1a", [128, 512], F32, stk)
        t2a = sb("t2a", [128, 512], F32, stk)
        rtmpa = Res()
        qrb = sb("qrb", [128, 256], BF16, stk)
        krb = sb("krb", [128, 256], BF16, stk)
        rqk = Res()
        qT = sb("qT", [128, 2, 128], BF16, stk)
        qfT = sb("qfT", [128, 2, 2, 128], BF16, stk)
        kT = sb("kT", [128, 2, 128], BF16, stk)
        rqT = Res()
        SD = sb("SD", [128, 512], BF16, stk)
        rSD = Res()
        bst = sb("bst", [128, 4, 6], F32, stk)
        mv = sb("mv", [128, 4, 2], F32, stk)
        sdr = sb("sdr", [128, 8], F32, stk)
        rln = Res()
        yb = sb("yb", [128, 512], BF16, stk)
        retb = sb("retb", [128, 512], BF16, stk)
        ryb = Res()
        brT = [sb("brTo%d" % i, [128, 4, 128], BF16, stk) for i in range(3)]
        rbrT = [Res(), Res(), Res()]
        vn = sb("vn", [128, 512], BF16, stk)
        stmp = sb("stmp", [128, 512], F32, stk)
        rsg2 = Res()
        bst2 = sb("bst2", [128, 6], F32, stk)
        mv2 = sb("mv2", [128, 4], F32, stk)
        sq = sb("sq", [128, 512], F32, stk)
        qn = sb("qn", [128, 512], F32, stk)
        ssq = sb("ssq", [128, 24], F32, stk)
        ratt = Res()
        sqk = sb("sqk", [128, 128], F32, stk)
        kn = sb("kn", [128, 128], F32, stk)
        ssk = sb("ssk", [128, 8], F32, stk)
        rattk = Res()
        qperm = sb("qperm", [128, 4, 2, 64], BF16, stk)
        krb2 = sb("krb2", [128, 128], BF16, stk)
        qT2 = [sb("qT2_%d" % i, [128, 4, 128], BF16, stk) for i in range(2)]
        rqT2 = [Res(), Res()]
        PT = [sb("PT%d" % i, [128, 512], BF16, stk) for i in range(2)]
        rPT = [Res(), Res()]
        den = sb("den", [128, 16], F32, stk)
        attb = sb("attb", [128, 512], BF16, stk)
        rden = Res()
        BRv = [BR[i].rearrange("(c p) t -> p c t", p=128) for i in range(3)]
        tg = sb("tg", [128, 512], F32, stk)
        rtg = Res()
        pbc = {"n": 0}

        def pbank():
            i = pbc["n"] % 2
            pbc["n"] += 1
            return bank[i], rbank[i]

        def out_branch(i, T, srcb, rsrc):
            with P.atomic():
                for c in range(4):
                    tr(bankT[:, 512 + c * 128: 512 + (c + 1) * 128], srcb[:, c * 128:(c + 1) * 128], identb[:], [rsrc, rcb], [rbT[1]])
                cpa(brT[i][:], bankT[:, 512:1024].rearrange("p (c t) -> p c t", t=128), [rbT[1]], [rbrT[i]])
            dsp(BRv[i][:, :, T * 128:(T + 1) * 128], brT[i][:], [rbrT[i]], [RBR[i][T]])

        def Pst1(T, par):
            s = 0 if T < NLAT else 1
            X, rX, XN, rXN, S, rS = norm_tile(T)
            H, rH = hT[par], rhT[par]
            transpose_mod(XN, rXN, A1, B1m, s, lambda k: H[:, k, :], rH)

        def Pst(T, full, par):
            load_rope(T)
            H, rH = hT[par], rhT[par]

            def proj(c0, c1):
                b, rb = pbank()
                for k in range(8):
                    mm(b[:, 0:c1 - c0], H[:, k, :], wng[:, k, c0:c1], k == 0, k == 7, [rH, rwng], [rb])
                return b, rb

            if full:
                b, rb = proj(0, 512)
                cpa(zqk[par][:], b[:], [rb], [rzqk[par]])
                b, rb = proj(512, 1024)
                cpv(vb[par][:], b[:], [rb], [rvb[par]])
                b, rb = proj(1024, 1536)
                actf(tg[:], b[:], AF.Tanh, [rb], [rtg], scale=0.5)
                stt(sg[par][:], tg[:], 1.0, b[:], ALU.add, ALU.mult, [rtg, rb], [rsg[par]])
                b, rb = pbank()
                for c in range(4):
                    for k in range(8):
                        mm(b[:, c * 128:(c + 1) * 128], wng[:, k, 1536 + c * 128:1536 + (c + 1) * 128], H[:, k, :], k == 0, k == 7, [rH, rwng], [rb])
                actf(uT[par][:], b[:], AF.Gelu_apprx_tanh, [rb], [ruT[par]])
                b, rb = proj(2048, 2560)
                actf(gv[par][:], b[:], AF.Gelu_apprx_tanh, [rb], [rgv[par]])
                b, rb = proj(2560, 3072)
                cpv(zq[par][:], b[:], [rb], [rzq[par]])
            b, rb = proj(3072, 3328)
            cpv(zk[par][:], b[:, 0:128], [rb], [rzk[par]])
            cpa(V_all[:, T, :, 0:64], b[:, 128:256].rearrange("p (k d) -> p k d", d=64), [rb], [rKV[T]])

        def chainR(T, par):
            RT, rRT = ropet[T % 2], rrope[T % 2]
            if True:
                rope_apply(P, nc, zqk[par][:, 0:256], rzqk[par], RT, rRT, 4, t1, t2, qrb[:].rearrange("p (h d) -> p h d", d=64), rtmp, rqk)
                rope_apply(P, nc, zqk[par][:, 256:512], rzqk[par], RT, rRT, 4, t1, t2, krb[:].rearrange("p (h d) -> p h d", d=64), rtmp, rqk)
                with P.atomic():
                    for hb in range(2):
                        tr(bankT[:, hb * 128:(hb + 1) * 128], qrb[:, hb * 128:(hb + 1) * 128], identb[:], [rqk, rcb], [rbT[0]])
                        tr(bankT[:, 256 + hb * 128:256 + (hb + 1) * 128], krb[:, hb * 128:(hb + 1) * 128], identb[:], [rqk, rcb], [rbT[0]])
                    qv = bankT[:, 0:256].rearrange("p (b t) -> p b t", t=128)
                    cpa(qT[:], qv, [rbT[0]], [rqT])
                    for dr in range(2):
                        tt(qfT[:, dr, :, :], qv, DfT[:, dr, :, :], ALU.mult, [rbT[0], rdec], [rqT])
                    cpa(kT[:], bankT[:, 256:512].rearrange("p (b t) -> p b t", t=128), [rbT[0]], [rqT])
                for h in range(4):
                    hb, hl = h // 2, h % 2
                    bS = 2 + hl
                    mm(bank[bS][:, hb * 128:(hb + 1) * 128], kT[64 * hl:64 * hl + 64, hb, :], qT[64 * hl:64 * hl + 64, hb, :], True, True,
                       [rqT], [rbank[bS]], tp=(64 * hl, 0))
                for h in range(4):
                    hb, hl = h // 2, h % 2
                    bS = 2 + hl
                    tt(SD[:, h * 128:(h + 1) * 128], bank[bS][:, hb * 128:(hb + 1) * 128], DTt[:, h, :], ALU.mult, [rbank[bS], rdec], [rSD])
                for h in range(4):
                    hb, hl = h // 2, h % 2
                    o = bank[2][:, h * 128:(h + 1) * 128]
                    mm(o, qfT[64 * hl:64 * hl + 64, 0, hb, :], ST[64 * hl:64 * hl + 64, T, 0, hb, :], True, False, [rqT, rST], [rbank[2]], tp=(64 * hl, 0))
                    mm(o, qfT[64 * hl:64 * hl + 64, 1, hb, :], ST[64 * hl:64 * hl + 64, T, 1, hb, :], False, False, [rqT, rST], [rbank[2]], tp=(64 * hl, 0))
                    mm(o, SD[:, h * 128:(h + 1) * 128], vb[par][:, h * 128:(h + 1) * 128], False, True, [rSD, rvb[par]], [rbank[2]])
                cpa(t1[:], bank[2][:], [rbank[2]], [rtmp])
                for h in range(4):
                    (lambda h: P.dve(lambda: nc.vector.bn_stats(out=bst[:, h, :], in_=t1[:, h * 128:(h + 1) * 128]), [rtmp], [rln]))(h)
                    (lambda h: P.dve(lambda: nc.vector.bn_aggr(out=mv[:, h, :], in_=bst[:, h, :]), [rln], [rln]))(h)
                rsqrt_chain(sdr[:, 0:4], sdr[:, 4:8], mv[:, :, 1], 1.0, rln, rln)
                tsc(sdr[:, 4:8], sdr[:, 4:8], 0.5, None, ALU.mult, None, [rln], [rln])
                for h in range(4):
                    tsc(yb[:, h * 128:(h + 1) * 128], t1[:, h * 128:(h + 1) * 128], mv[:, h, 0:1], sdr[:, 4 + h:5 + h], ALU.subtract, ALU.mult,
                        [rtmp, rln], [ryb])
                tt(retb[:], yb[:], sg[par][:], ALU.mult, [ryb, rsg[par]], [ryb], eng="POOL")
                out_branch(0, T, retb, ryb)

        def chainS(T, par):
            if True:
                P.dve(lambda: nc.vector.bn_stats(out=bst2[:], in_=gv[par][:]), [rgv[par]], [rsg2])
                P.dve(lambda: nc.vector.bn_aggr(out=mv2[:, 0:2], in_=bst2[:]), [rsg2], [rsg2])
                rsqrt_chain(mv2[:, 2:3], mv2[:, 3:4], mv2[:, 1:2], 1.0, rsg2, rsg2)
                tsc(vn[:], gv[par][:], mv2[:, 0:1], mv2[:, 3:4], ALU.subtract, ALU.mult, [rsg2, rgv[par]], [rsg2])
                for g in range(4):
                    mm(bank[4][:, g * 128:(g + 1) * 128], vn[:, g * 128:(g + 1) * 128], wsT[:, g, :], True, True, [rsg2, rsm], [rbank[4]])
                tt(stmp[:], bank[4][:], sbb[:], ALU.add, [rbank[4], rsm], [rsg2])
                tt(brT[1][:].rearrange("p c t -> p (c t)"), stmp[:], uT[par][:], ALU.mult, [rsg2, ruT[par]], [rbrT[1]], eng="POOL")
                dsp(BRv[1][:, :, T * 128:(T + 1) * 128], brT[1][:], [rbrT[1]], [RBR[1][T]])

        def chainQ(T, par):
            RT, rRT = ropet[T % 2], rrope[T % 2]
            if True:
                zq3 = zq[par][:].rearrange("p (h d) -> p h d", d=64)
                qn3 = qn[:].rearrange("p (h d) -> p h d", d=64)
                actf(sq[:], zq[par][:], AF.Square, [rzq[par]], [ratt])
                P.dve(lambda: nc.vector.tensor_reduce(out=ssq[:, 0:8], in_=sq[:].rearrange("p (h d) -> p h d", d=64), axis=AX.X, op=ALU.add), [ratt], [ratt])
                rsqrt_chain(ssq[:, 8:16], ssq[:, 16:24], ssq[:, 0:8], 1.0 / 64, ratt, ratt)
                tt(qn3, zq3, bc(ssq[:, 16:24], 2, 64), ALU.mult, [rzq[par], ratt], [ratt])
                tt(qn3, qn3, bc(qg[:], 1, 8), ALU.mult, [ratt, rsm], [ratt], eng="POOL")
                for hk in range(2):
                    rope_apply(P, nc, qn[:, hk * 256:(hk + 1) * 256], ratt, RT, rRT, 4, t1a, t2a, qperm[:, :, hk, :], rtmpa, ratt, pool=True)
                p2 = T % 2
                with P.atomic():
                    for g in range(4):
                        tr(bankT[:, g * 128:(g + 1) * 128], qperm[:, g, :, :].rearrange("p k d -> p (k d)"), identb[:], [ratt, rcb], [rbT[0]])
                    cpa(qT2[p2][:], bankT[:, 0:512].rearrange("p (g t) -> p g t", t=128), [rbT[0]], [rqT2[p2]])

        def chainK(T, par):
            RT, rRT = ropet[T % 2], rrope[T % 2]
            zk3 = zk[par][:].rearrange("p (h d) -> p h d", d=64)
            kn3 = kn[:].rearrange("p (h d) -> p h d", d=64)
            actf(sqk[:], zk[par][:], AF.Square, [rzk[par]], [rattk])
            P.dve(lambda: nc.vector.tensor_reduce(out=ssk[:, 0:2], in_=sqk[:].rearrange("p (h d) -> p h d", d=64), axis=AX.X, op=ALU.add), [rattk], [rattk])
            rsqrt_chain(ssk[:, 2:4], ssk[:, 4:6], ssk[:, 0:2], 1.0 / 64, rattk, rattk)
            tt(kn3, zk3, bc(ssk[:, 4:6], 2, 64), ALU.mult, [rzk[par], rattk], [rattk])
            tt(kn3, kn3, bc(kg[:], 1, 2), ALU.mult, [rattk, rsm], [rattk])
            rope_apply(P, nc, kn[:], rattk, RT, rRT, 2, t1a, t2a, krb2[:].rearrange("p (h d) -> p h d", d=64), rtmpa, rattk)
            with P.atomic():
                tr(bankT[:, 512:640], krb2[:], identb[:], [rattk, rcb], [rbT[1]])
                cpa(KT_all[:, T, :], bankT[:, 512:640], [rbT[1]], [rKV[T]])

        stc = {"n": 0}

        def Yst(T):
            p2 = T % 2
            if T < NLAT:
                chunks = []
                if T > 0:
                    chunks.append((T - 1, mprevb))
                chunks.append((T, None))
                chunks += [(NLAT, None), (NLAT + 1, None)]
                if T < NLAT - 1:
                    chunks.append((T + 1, mnextb))
            else:
                chunks = [(NLAT, None), (NLAT + 1, None)]
            for hk in range(2):
                ob = bank[6]
                rob = rbank[6]
                for ci, (c, m) in enumerate(chunks):
                    i = stc["n"] % 2
                    stc["n"] += 1
                    sb_, rsb = bank[5], rbank[5]
                    mm(sb_[:, :], KT_all[64 * hk:64 * hk + 64, c, :], qT2[p2][64 * hk:64 * hk + 64, :, :].rearrange("p g t -> p (g t)"),
                       True, m is None, [rKV[c], rqT2[p2]], [rsb], tp=(64 * hk, 0))
                    if m is not None:
                        mm(sb_[:, :], identb[:], m[:], False, True, [rcb], [rsb])
                    actf(PT[i][:], sb_[:, :], AF.Exp, [rsb], [rPT[i]])
                    for g in range(4):
                        mm(ob[:, g * 128:g * 128 + 65], PT[i][:, g * 128:(g + 1) * 128], V_all[:, c, hk, 0:65], ci == 0 and g == 0,
                           ci == len(chunks) - 1 and g == 3, [rPT[i], rKV[c]], [rob])
                ov = ob[:, 0:512].rearrange("p (g e) -> p g e", e=128)
                tt(den[:, hk * 4:(hk + 1) * 4], ov[:, :, 64], esink[:, hk * 4:(hk + 1) * 4], ALU.add, [rob, rsm], [rden])
                (lambda hk: P.dve(lambda: nc.vector.reciprocal(out=den[:, 8 + hk * 4:8 + (hk + 1) * 4], in_=den[:, hk * 4:(hk + 1) * 4]), [rden], [rden]))(hk)
                tt(attb[:, hk * 256:(hk + 1) * 256].rearrange("p (g d) -> p g d", d=64), ov[:, :, 0:64], bc(den[:, 8 + hk * 4:8 + (hk + 1) * 4], 2, 64),
                   ALU.mult, [rob, rden], [rden])
            out_branch(2, T, attb, rden)

        L = [NLAT, NLAT + 1] + list(range(NLAT))
        full = [(not last) if T >= NLAT else True for T in L]
        Pst1(L[0], 0)
        Pst1(L[1], 1)
        Pst(L[0], full[0], 0)
        for i, T in enumerate(L):
            par = i % 2
            chains = []
            if T < NLAT and T >= 1:
                P.capture_start()
                chainK(T, par)
                chains.append((P.capture_end(), 0.0, 0.2))
            else:
                chainK(T, par)
            if i + 1 < len(L):
                P.capture_start()
                Pst(L[i + 1], full[i + 1], (i + 1) % 2)
                chains.append(P.capture_end())
            if i + 2 < len(L):
                P.capture_start()
                Pst1(L[i + 2], i % 2)
                chains.append(P.capture_end())
            if full[i]:
                for ch in (chainR, chainS, chainQ):
                    P.capture_start()
                    ch(T, par)
                    chains.append(P.capture_end())
            if T < NLAT and T >= 1:
                P.capture_start()
                Yst(T - 1)
                chains.append(P.capture_end())
            P.emit_zip(chains)
            if T == NLAT + 1 and not last:
                Yst(NLAT)
                Yst(NLAT + 1)
            if T == NLAT - 1:
                Yst(T)

    def phase_merge(stk, l, last):
        wgt = sb("wgt", [128, 8, 3072], BF16, stk)
        wbr = sb("wbr", [128, 3, 4, D], BF16, stk)
        wo = sb("wo", [128, 8, D], BF16, stk)
        rw = Res()
        for q in range(3):
            dpl(wgt[:, :, q * 1024:(q + 1) * 1024], kwview(w_in[l][:, NG + q * 1024:NG + (q + 1) * 1024]), [], [rw])
            dpl(wbr[:, q, :, :], kwview(w_br[l][q]), [], [rw])
        dpl(wo[:], kwview(w_out[l]), [], [rw])
        hTg = [sb("hTg%d" % i, [128, 8, 512], BF16, stk) for i in range(2)]
        rhTg = [Res(), Res()]
        brTi = [sb("brTi%d" % i, [128, 3, 4, 512], BF16, stk) for i in range(2)]
        rbrTi = [Res(), Res()]
        mT = sb("mT", [128, 8, 512], BF16, stk)
        rmT = Res()
        sgm = [sb("sgm%d" % i, [128, 512], F32, stk) for i in range(2)]
        rsgm = [Res(), Res()]
        acc = sb("acc", [128, 512], F32, stk)
        tmp = sb("tmpm", [128, 512], F32, stk)
        racc = Res()
        rtmpm = Res()
        xo = [sb("xo%d" % i, [128, D], F32, stk) for i in range(2)]
        rxo = [Res(), Res()]
        xr = [sb("xr%d" % i, [128, D], F32, stk) for i in range(2)]
        rxr = [Res(), Res()]
        BRv = [BR[i].rearrange("(c p) t -> p c t", p=128) for i in range(3)]
        groups = [list(range(g * 4, g * 4 + 4)) for g in range(8)]
        if not last:
            groups = [[NLAT, NLAT + 1]] + groups
        cnts = {"pc": 0, "xc": 0}

        def prologue(gi):
            grp = groups[gi]
            gp = gi % 2
            s_ = 0 if grp[0] < NLAT else 1
            ntok = 128 * len(grp)
            T0 = grp[0]
            for ti, T in enumerate(grp):
                X, rX, XN, rXN, S, rS = norm_tile(T)
                transpose_mod(XN, rXN, A1, B1m, s_, (lambda ti, gp: (lambda k: hTg[gp][:, k, ti * 128:(ti + 1) * 128]))(ti, gp), rhTg[gp])
            for i in range(3):
                dsp(brTi[gp][:, i, :, 0:ntok], BRv[i][:, :, T0 * 128:T0 * 128 + ntok], [RBR[i][T] for T in grp], [rbrTi[gp]])

        def compute(gi):
            grp = groups[gi]
            gp = gi % 2
            s_ = 0 if grp[0] < NLAT else 1
            ntok = 128 * len(grp)
            H, rH, BI, rBI = hTg[gp], rhTg[gp], brTi[gp], rbrTi[gp]
            for dc in range(8):
                for i in range(3):
                    p = cnts["pc"] % 3
                    cnts["pc"] += 1
                    bg, rbg, bb, rbb = bank[2 * p], rbank[2 * p], bank[2 * p + 1], rbank[2 * p + 1]
                    for k in range(8):
                        mm(bg[:, 0:ntok], wgt[:, k, i * 1024 + dc * 128:i * 1024 + (dc + 1) * 128], H[:, k, 0:ntok], k == 0, k == 7, [rw, rH], [rbg])
                    for k in range(4):
                        mm(bb[:, 0:ntok], wbr[:, i, k, dc * 128:(dc + 1) * 128], BI[:, i, k, 0:ntok], k == 0, k == 3, [rw, rBI], [rbb])
                    sgi, rsgi = sgm[cnts["pc"] % 2], rsgm[cnts["pc"] % 2]
                    actf(sgi[:, 0:ntok], bg[:, 0:ntok], AF.Sigmoid, [rbg], [rsgi])
                    if i == 0:
                        tt(acc[:, 0:ntok], sgi[:, 0:ntok], bb[:, 0:ntok], ALU.mult, [rsgi, rbb], [racc])
                    elif i == 1:
                        tt(tmp[:, 0:ntok], sgi[:, 0:ntok], bb[:, 0:ntok], ALU.mult, [rsgi, rbb], [rtmpm])
                        tt(acc[:, 0:ntok], acc[:, 0:ntok], tmp[:, 0:ntok], ALU.add, [racc, rtmpm], [racc], eng="POOL")
                    else:
                        tt(tmp[:, 0:ntok], sgi[:, 0:ntok], bb[:, 0:ntok], ALU.mult, [rsgi, rbb], [rtmpm])
                        tt(mT[:, dc, 0:ntok], acc[:, 0:ntok], tmp[:, 0:ntok], ALU.add, [racc, rtmpm], [rmT], eng="POOL")
            for ti, T in enumerate(grp):
                j = cnts["xc"] % 2
                cnts["xc"] += 1
                xoi, rxoi = xo[j], rxo[j]
                dsp(xr[j][:], srows(T), [RS[T]], [rxr[j]])
                for nb in range(2):
                    for k in range(8):
                        mm(bank[6][:, :], mT[:, k, ti * 128:(ti + 1) * 128], wo[:, k, nb * 512:(nb + 1) * 512], k == 0, k == 7, [rmT, rw], [rbank[6]])
                    tt(xoi[:, nb * 512:(nb + 1) * 512], bank[6][:, :], rowbcA[:, s_, nb * 512:(nb + 1) * 512], ALU.mult, [rbank[6], rrowA], [rxoi])
                tt(xoi[:], xoi[:], xr[j][:], ALU.add, [rxoi, rxr[j]], [rxoi], eng="POOL")
                dsp(srows(T), xoi[:], [rxoi], [RS[T]])

        prologue(0)
        for gi in range(len(groups)):
            chains = []
            P.capture_start()
            compute(gi)
            chains.append(P.capture_end())
            if gi + 1 < len(groups):
                P.capture_start()
                prologue(gi + 1)
                chains.append(P.capture_end())
            P.emit_zip(chains)

    def phase_moe(stk, l, last):
        streams = [(0, list(range(NLAT)), 512, 4, 128)]
        if not last:
            streams.append((1, [NLAT, NLAT + 1], 32, 1, 32))
        aff = sb("aff", [128, NT, NEXP], F32, stk)
        raff = Res()
        idxS = [sb("idxS%d" % i, [128, 4, NEXP], I32, stk) for i in range(2)]
        idxH = [sb("idxH%d" % i, [128, 4, NEXP], I32, stk) for i in range(2)]
        wsel = [sb("wsel%d" % i, [128, 4, NEXP], F32, stk) for i in range(2)]
        ridx = [Res(), Res()]

        with contextlib.ExitStack() as s1:
            wr = sb("wr", [128, 8, NEXP], BF16, s1)
            rwr = Res()
            dpl(wr[:], w_rt[l].rearrange("(k p) e -> p k e", p=128), [], [rwr])
            h2fs = [sb("h2fA", [128, D], F32, s1), sb("h2fB", [128, D], F32, s1)]
            rh2fs = [Res(), Res()]
            h2b = [sb("h2b%d" % i, [128, D], BF16, s1) for i in range(2)]
            rh2 = [Res(), Res()]
            h2T = [sb("h2T%d" % i, [128, 8, 128], BF16, s1) for i in range(2)]
            rh2T = [Res(), Res()]
            sm = [sb("sm%d" % i, [128, 8], F32, s1) for i in range(2)]
            ex = [sb("ex%d" % i, [128, NEXP], F32, s1) for i in range(2)]
            rsmx = [Res(), Res()]
            allT = [(s_, T) for (s_, tiles, cap, nch, rows) in streams for T in tiles]

            def st1(j):
                s_, T = allT[j]
                i = j % 2
                X, rX, XN, rXN, S, rS = norm_tile(T)
                h2f, rh2f = h2fs[i], rh2fs[i]
                stt(h2f[:], X[:], S[:, 2:3], rowbcA[:, s_, :], ALU.mult, ALU.mult, [rX, rS, rrowA], [rh2f])
                tt(h2b[i][:], h2f[:], rowbcB[:, s_, :], ALU.add, [rh2f, rrowB], [rh2[i]], eng="pool")
                dsp(H2[T * 128:(T + 1) * 128, :], h2b[i][:], [rh2[i]], [RH2[T]])
                for half in range(2):
                    with P.atomic():
                        for kk in range(4):
                            k = half * 4 + kk
                            tr(bankT[:, half * 512 + kk * 128:half * 512 + (kk + 1) * 128], h2b[i][:, k * 128:(k + 1) * 128], identb[:], [rh2[i], rcb], [rbT[half]])
                        cpa(h2T[i][:, half * 4:(half + 1) * 4, :], bankT[:, half * 512:(half + 1) * 512].rearrange("p (k t) -> p k t", t=128), [rbT[half]], [rh2T[i]])

            def st2(j):
                s_, T = allT[j]
                i = j % 2
                bk, rbk = bank[2 + i], rbank[2 + i]
                for k in range(8):
                    mm(bk[:, 0:NEXP], h2T[i][:, k, :], wr[:, k, :], k == 0, k == 7, [rh2T[i], rwr], [rbk])
                smi, exi, rsi = sm[i], ex[i], rsmx[i]
                P.dve(lambda: nc.vector.tensor_reduce(out=smi[:, 0:1], in_=bk[:, 0:NEXP], axis=AX.X, op=ALU.max), [rbk], [rsi])
                tsc(smi[:, 1:2], smi[:, 0:1], -1.0, None, ALU.mult, None, [rsi], [rsi])
                mset(smi[:, 2:3], 0.0, [rsi])
                actf(exi[:], bk[:, 0:NEXP], AF.Exp, [rbk, rsi], [rsi], bias=smi[:, 1:2], accum_out=smi[:, 2:3])
                P.dve(lambda: nc.vector.reciprocal(out=smi[:, 3:4], in_=smi[:, 2:3]), [rsi], [rsi])
                tsc(aff[:, T, :], exi[:], smi[:, 3:4], None, ALU.mult, None, [rsi], [raff])

            for j0 in range(0, len(allT), 2):
                chains = []
                for j in (j0, j0 + 1):
                    if j < len(allT):
                        P.capture_start()
                        st1(j)
                        st2(j)
                        chains.append(P.capture_end())
                P.emit_zip(chains)
            build_rowbc(rowbcA, rrowA, lambda s, c: modT[:, 40 + c, s:s + 1])
            P.flush()
        if _DBG.get("cstop") == "1":
            return

        with contextlib.ExitStack() as s2:
            ge = sb("ge", [128, NLAT, NEXP], BF16, s2)
            maskf = sb("maskf", [128, NLAT, NEXP], F32, s2)
            basef = sb("basef", [128, NLAT, NEXP], F32, s2)
            keyf = sb("keyf", [128, NLAT, NEXP], F32, s2)
            cntp = sb("cntp", [128, NLAT, NEXP], F32, s2)
            vals = sb("vals", [128, NLAT, NEXP, 4], BF16, s2)
            valf = sb("valf", [128, NLAT, NEXP], F32, s2)
            lo = sb("lo", [128, NEXP], F32, s2)
            mid = sb("mid", [128, NEXP], F32, s2)
            cpr = sb("cpr", [128, NEXP], F32, s2)
            cntr = sb("cntr", [128, NEXP], F32, s2)
            rr = Res()
            OH = [sb("OH%d" % i, [128, 512], BF16, s2) for i in range(4)]
            rOH = [Res() for _ in range(4)]
            lst = sb("lst", [128, NEXP, 4, 4], F32, s2)
            idxf = sb("idxf", [128, 4, NEXP], F32, s2)
            NIT = 28
            for (s, tiles, cap, nch, rows) in streams:
                nt = len(tiles)
                T0 = tiles[0]
                av = aff[:, T0:T0 + nt, :]
                W = nt * NEXP
                mset(lo[:], 0.0, [rr])
                mset(mid[:], 0.5, [rr])
                for it in range(NIT):
                    half = 0.5 ** (it + 1)
                    tt(ge[:, 0:nt, :], av, bc(mid[:], 1, nt), ALU.is_ge, [raff, rr], [rr])
                    mm(bank[0][:, 0:W], onesb[:], ge[:, 0:nt, :].rearrange("p t e -> p (t e)"), True, True, [rr, rcb], [rbank[0]])
                    P.dve(lambda W=W, nt=nt: nc.vector.tensor_reduce(out=cntr[:], in_=bank[0][:, 0:W].rearrange("p (t e) -> p e t", e=NEXP), axis=AX.X, op=ALU.add),
                          [rbank[0]], [rr])
                    tsc(cpr[:], cntr[:], float(cap) - 0.5, half, ALU.is_ge, ALU.mult, [rr], [rr])
                    stt(mid[:], cpr[:], half * 0.5, lo[:], ALU.add, ALU.add, [rr], [rr])
                    tt(lo[:], lo[:], cpr[:], ALU.add, [rr], [rr])
                tt(maskf[:, 0:nt, :], av, bc(lo[:], 1, nt), ALU.is_ge, [raff, rr], [rr])
                cpv(ge[:, 0:nt, :], maskf[:, 0:nt, :], [rr], [rr])
                gef = ge[:, 0:nt, :].rearrange("p t e -> p (t e)")
                mm(bank[0][:, 0:W], trib[:], gef, True, True, [rr, rcb], [rbank[0]])
                mm(bank[1][:, 0:W], onesb[:], gef, True, True, [rr, rcb], [rbank[1]])
                cpv(cntp[:, 0:nt, :].rearrange("p t e -> p (t e)"), bank[1][:, 0:W], [rbank[1]], [rr])
                mset(basef[:, 0, :], 0.0, [rr])
                for t_ in range(1, nt):
                    tt(basef[:, t_, :], basef[:, t_ - 1, :], cntp[:, t_ - 1, :], ALU.add, [rr], [rr])
                tt(keyf[:, 0:nt, :].rearrange("p t e -> p (t e)"), bank[0][:, 0:W], basef[:, 0:nt, :].rearrange("p t e -> p (t e)"), ALU.add, [rbank[0], rr], [rr])
                stt(keyf[:, 0:nt, :], keyf[:, 0:nt, :], 1.0, maskf[:, 0:nt, :], ALU.add, ALU.mult, [rr], [rr])
                tsc(keyf[:, 0:nt, :], keyf[:, 0:nt, :], -1.0, None, ALU.add, None, [rr], [rr])
                cpv(vals[:, 0:nt, :, 0], bc(ct("ttv")[:, 0:nt], 2, NEXP), [rct], [rr])
                cpv(vals[:, 0:nt, :, 1], ct("jcol").unsqueeze(2).to_broadcast([128, nt, NEXP]), [rct], [rr])
                cpv(vals[:, 0:nt, :, 2], av, [raff], [rr])
                cpv(valf[:, 0:nt, :], vals[:, 0:nt, :, 2], [rr], [rr])
                tt(vals[:, 0:nt, :, 3], av, valf[:, 0:nt, :], ALU.subtract, [raff, rr], [rr])
                capw = nch * 128
                oc = 0
                rvals = rr
                for e in range(NEXP):
                    for t_ in range(nt):
                        i = oc % 4
                        oc += 1
                        tsc(OH[i][:, 0:capw], ct("iota512")[:, 0:capw], keyf[:, t_, e:e + 1], None, ALU.is_equal, None, [rct, rvals], [rOH[i]],
                            eng="dve")
                        for c in range(nch):
                            mm(bank[3][:, (e * 4 + c) * 4:(e * 4 + c) * 4 + 4], OH[i][:, c * 128:(c + 1) * 128], vals[:, t_, e, :],
                               e == 0 and t_ == 0 and c == 0, e == NEXP - 1 and t_ == nt - 1 and c == nch - 1, [rOH[i], rvals], [rbank[3]])
                lv = lst[:, :, 0:nch, :]
                cpv(lv, bank[3][:, 0:256].rearrange("p (e c v) -> p e c v", c=4, v=4)[:, :, 0:nch, :], [rbank[3], rvals], [rr])
                iv = idxf[:, 0:nch, :].rearrange("p c e -> p e c")
                stt(iv, lv[:, :, :, 0], 128.0, lv[:, :, :, 1], ALU.mult, ALU.add, [rr], [rr])
                cpv(idxS[s][:, 0:nch, :], idxf[:, 0:nch, :], [rr], [ridx[s]])
                tsc(idxf[:, 0:nch, :], idxf[:, 0:nch, :], float(T0 * 128), None, ALU.add, None, [rr], [rr])
                cpv(idxH[s][:, 0:nch, :], idxf[:, 0:nch, :], [rr], [ridx[s]])
                tt(wsel[s][:, 0:nch, :].rearrange("p c e -> p e c"), lv[:, :, :, 2], lv[:, :, :, 3], ALU.add, [rr], [ridx[s]])
            P.flush()
        if _DBG.get("cstop") == "2":
            return

        wg = [sb("wg%d" % i, [128, 8, D], BF16, stk) for i in range(2)]
        wu = [sb("wu%d" % i, [128, 8, D], BF16, stk) for i in range(2)]
        wd = [sb("wd%d" % i, [128, 8, D], BF16, stk) for i in range(2)]
        rwe = [Res(), Res()]
        NS = len(streams)
        xs = [sb("xs%d" % s_, [128, 4 if s_ == 0 else 1, D], BF16, stk) for s_ in range(NS)]
        rxs = [Res() for _ in range(NS)]
        xsT = [[sb("xsT%d_%d" % (s_, i), [128, 8, 512 if s_ == 0 else 32], BF16, stk) for i in range(2)] for s_ in range(NS)]
        rxsT = [[Res(), Res()] for _ in range(NS)]
        sa = [sb("sa%d" % i, [128, 512], F32, stk) for i in range(2)]
        rsa = [Res(), Res()]
        hf = [sb("hf%d" % s_, [128, 8, 512 if s_ == 0 else 32], BF16, stk) for s_ in range(NS)]
        rhf = [Res() for _ in range(NS)]
        yscs = [sb("ysc%d" % i, [128, D], F32, stk) for i in range(4)]
        ryscs = [Res() for _ in range(4)]
        ycn = {"n": 0}

        def loads(e):
            par = e % 2
            for (wt_, src) in ((wg, w_eg), (wu, w_eu), (wd, w_ed)):
                for hh in range(2):
                    dpl(wt_[par][:, hh * 4:(hh + 1) * 4, :], src[l][e][hh * 512:(hh + 1) * 512, :].rearrange("(k p) n -> p k n", p=128), [], [rwe[par]])

        def gather(e):
            for (s, tiles, cap, nch, rows) in streams:
                for c in range(nch):
                    (lambda c, s, rows, e: P.dma("pool", lambda: nc.gpsimd.indirect_dma_start(
                        out=xs[s][0:rows, c, :], out_offset=None, in_=H2,
                        in_offset=bass.IndirectOffsetOnAxis(ap=idxH[s][0:rows, c, e:e + 1], axis=0)),
                        [ridx[s]] + RH2, [rxs[s]]))(c, s, rows, e)

        def G(e):
            par = e % 2
            for (s, tiles, cap, nch, rows) in streams:
                ntok = nch * rows
                for k in range(8):
                    half = k % 2
                    with P.atomic():
                        for c in range(nch):
                            tr(bankT[:, half * 512 + c * rows: half * 512 + (c + 1) * rows], xs[s][0:rows, c, k * 128:(k + 1) * 128], identb[0:rows, 0:rows],
                               [rxs[s], rcb], [rbT[half]])
                        if k % 2 == 0:
                            cpa(xsT[s][par][:, k, 0:ntok], bankT[:, half * 512: half * 512 + ntok], [rbT[half]], [rxsT[s][par]])
                        else:
                            cpv(xsT[s][par][:, k, 0:ntok], bankT[:, half * 512: half * 512 + ntok], [rbT[half]], [rxsT[s][par]])

        sac = sb("sac", [128, 64], F32, stk)
        rsac = Res()
        yscc = sb("yscc", [128, D], F32, stk)
        ryscc = Res()

        def up(e, si):
            par = e % 2
            (s, tiles, cap, nch, rows) = streams[si]
            ntok = nch * rows
            for fc in range(8):
                if s == 0:
                    p = fc % 2
                    ba, rba, bu, rbu = bank[2 * p][:, 0:ntok], rbank[2 * p], bank[2 * p + 1][:, 0:ntok], rbank[2 * p + 1]
                    sai, rsai = sa[p][:, 0:ntok], rsa[p]
                else:
                    ba, rba, bu, rbu = bank[6][:, 0:ntok], rbank[6], bank[6][:, 64:64 + ntok], rbank[6]
                    sai, rsai = sac[:, 0:ntok], rsac
                for k in range(8):
                    mm(ba, wg[par][:, k, fc * 128:(fc + 1) * 128], xsT[s][par][:, k, 0:ntok], k == 0, k == 7, [rwe[par], rxsT[s][par]], [rba])
                for k in range(8):
                    mm(bu, wu[par][:, k, fc * 128:(fc + 1) * 128], xsT[s][par][:, k, 0:ntok], k == 0, k == 7, [rwe[par], rxsT[s][par]], [rbu])
                actf(sai, ba, AF.Silu, [rba], [rsai])
                tt(hf[s][:, fc, 0:ntok], sai, bu, ALU.mult, [rsai, rbu], [rhf[s]])

        def down(e, si):
            par = e % 2
            (s, tiles, cap, nch, rows) = streams[si]
            dst = out if s == 0 else xc
            rstream = [RS[T] for T in tiles]
            for c in range(nch):
                if s == 0:
                    ysc, rysc = yscs[ycn["n"] % 4], ryscs[ycn["n"] % 4]
                    ycn["n"] += 1
                else:
                    ysc, rysc = yscc, ryscc
                for nb in range(2):
                    if s == 0:
                        bd, rbd = bank[4 + nb], rbank[4 + nb]
                    else:
                        bd, rbd = bank[6], rbank[6]
                    for fc in range(8):
                        mm(bd[0:rows, :], hf[s][:, fc, c * rows:(c + 1) * rows], wd[par][:, fc, nb * 512:(nb + 1) * 512], fc == 0, fc == 7, [rhf[s], rwe[par]], [rbd])
                    stt(ysc[0:rows, nb * 512:(nb + 1) * 512], bd[0:rows, :], wsel[s][0:rows, c, e:e + 1], rowbcA[0:rows, s, nb * 512:(nb + 1) * 512],
                        ALU.mult, ALU.mult, [rbd, ridx[s], rrowA], [rysc])
                if _DBG.get("noscatter"):
                    continue
                (lambda c, s, rows, e, dst, rstream, ysc, rysc: P.dma("pool", lambda: nc.gpsimd.indirect_dma_start(
                    out=dst, out_offset=bass.IndirectOffsetOnAxis(ap=idxS[s][0:rows, c, e:e + 1], axis=0),
                    in_=ysc[0:rows, :], in_offset=None, compute_op=ALU.add),
                    [rysc, ridx[s]] + rstream, rstream))(c, s, rows, e, dst, rstream, ysc, rysc)

        gather(0)
        loads(0)
        G(0)
        for e in range(NEXP):
            if e + 1 < NEXP:
                gather(e + 1)
                loads(e + 1)
            chains = []
            for si in range(len(streams)):
                P.capture_start()
                up(e, si)
                down(e, si)
                chains.append(P.capture_end())
            if e + 1 < NEXP:
                P.capture_start()
                G(e + 1)
                chains.append((P.capture_end(), 0.6, 0.98))
            P.emit_zip(chains)

    for l in range(DEPTH):
        last = (l == DEPTH - 1)
        tiles_ctx = [NLAT, NLAT + 1]
        tiles_lat = list(range(NLAT))

        with contextlib.ExitStack() as ps:
            cTt = sb("cTt", [128, 8, 2], F32, ps)
            bmT = sb("bmT", [128, 48], F32, ps)
            n1T = sb("n1T", [128, 8], F32, ps)
            n2T = sb("n2T", [128, 8], F32, ps)
            wm = [sb("wm%d" % i, [128, 8, 1024], BF16, ps) for i in range(2)]
            rwm = [Res(), Res()]
            rsm = Res()
            P.dma("sp", lambda: nc.sync.dma_start(out=cTt[:], in_=cT_d.rearrange("p (k s) -> p k s", s=2)), writes=[rsm])
            P.dma("sp", lambda: nc.sync.dma_start(out=bmT[:], in_=bmT_d[l]), writes=[rsm])
            P.dma("sp", lambda: nc.sync.dma_start(out=n1T[:], in_=n1T_d[l]), writes=[rsm])
            P.dma("sp", lambda: nc.sync.dma_start(out=n2T[:], in_=n2T_d[l]), writes=[rsm])
            P.act(lambda: nc.scalar.activation(out=cact[:], in_=cTt[:], func=AF.Silu), reads=[rsm], writes=[rmod])
            for sec in range(6 if stop_after != "M1" else 0):
                i = sec % 2
                (lambda sec, i: P.dma("pool", lambda: nc.gpsimd.dma_start(
                    out=wm[i][:], in_=w_mod[l][:, sec * 1024:(sec + 1) * 1024].rearrange("(k p) n -> p k n", p=128)),
                    writes=[rwm[i]]))(sec, i)
                for j in range(8):
                    col = (sec * 8 + j) * 2
                    for k in range(8):
                        (lambda i, j, k, col: P.pe(lambda: nc.tensor.matmul(bank[2][:, col:col + 2], lhsT=wm[i][:, k, j * 128:(j + 1) * 128],
                                                                            rhs=cact[:, k, :], start=(k == 0), stop=(k == 7)),
                                                   reads=[rwm[i], rmod], writes=[rbank[2]]))(i, j, k, col)
            if stop_after in ("M1", "M2"):
                P.flush()
                break
            P.dve(lambda: nc.vector.tensor_tensor(out=modT[:], in0=bank[2][:, 0:96].rearrange("p (j s) -> p j s", s=2),
                                                  in1=bc(bmT[:], 2, 2), op=ALU.add), reads=[rbank[2], rsm], writes=[rmod])
            if stop_after == "M3":
                P.flush()
                break
            for (Ax, nT, sec) in ((A1, n1T, 1), (A2, n2T, 4)):
                (lambda Ax, nT, sec: P.dve(lambda: nc.vector.scalar_tensor_tensor(
                    out=Ax[:], in0=modT[:, sec * 8:(sec + 1) * 8, :], scalar=1.0, in1=bc(nT[:], 2, 2), op0=ALU.add, op1=ALU.mult),
                    reads=[rmod, rsm], writes=[rmod]))(Ax, nT, sec)
            P.flush()
        B1m = modT[:, 0:8, :]
        B2m = modT[:, 24:32, :]
        if stop_after == "M" and l == 0:
            break

        with contextlib.ExitStack() as pAB:
            ST = sb("ST", [128, NT, 2, 2, 128], BF16, pAB)
            rST = Res("ST")
            decbc = sb("decbc", [128, 8], F32, pAB)
            dechp = sb("dechp", [128, 4], F32, pAB)
            lgbc = sb("lgbc", [128, 8], F32, pAB)
            lghp = sb("lghp", [128, 4], F32, pAB)
            DTt = sb("DT", [128, 4, 128], F32, pAB)
            DfT = sb("DfT", [128, 2, 2, 128], F32, pAB)
            wkd = sb("wkd", [128, 2, 4], F32, pAB)
            Gam = sb("Gam", [128, 4], F32, pAB)
            e1 = sb("e1", [128, 128], F32, pAB)
            e2 = sb("e2", [128, 128], F32, pAB)
            rdec = Res("dec")
            re1 = Res()
            P.dma("sp", lambda: nc.sync.dma_start(out=decbc[:], in_=decbc_d[l]), writes=[rdec])
            P.dma("sp", lambda: nc.sync.dma_start(out=dechp[:], in_=dechp_d[l]), writes=[rdec])
            for (src, dst) in ((decbc, lgbc), (dechp, lghp)):
                (lambda src, dst: (
                    P.act(lambda: nc.scalar.activation(out=dst[:], in_=src[:], func=AF.Exp, scale=-1.0), reads=[rdec], writes=[rdec]),
                    P.dve(lambda: nc.vector.tensor_scalar(out=dst[:], in0=dst[:], scalar1=1.0, scalar2=None, op0=ALU.add), reads=[rdec], writes=[rdec]),
                    P.act(lambda: nc.scalar.activation(out=dst[:], in_=dst[:], func=AF.Ln), reads=[rdec], writes=[rdec]),
                    P.dve(lambda: nc.vector.tensor_scalar(out=dst[:], in0=dst[:], scalar1=-1.0, scalar2=None, op0=ALU.mult), reads=[rdec], writes=[rdec])))(src, dst)
            KS = 0.125
            for h in range(4):
                (lambda h: (
                    P.act(lambda: nc.scalar.activation(out=e1[:], in_=ct("PF"), func=AF.Exp, scale=lgbc[:, h:h + 1]), reads=[rdec, rct], writes=[re1]),
                    P.dve(lambda: nc.vector.tensor_tensor(out=e1[:], in0=e1[:], in1=ct("MF"), op=ALU.mult), reads=[re1, rct], writes=[re1]),
                    P.act(lambda: nc.scalar.activation(out=e2[:], in_=ct("PB"), func=AF.Exp, scale=lgbc[:, 4 + h:5 + h]), reads=[rdec, rct], writes=[re1]),
                    P.dve(lambda: nc.vector.tensor_tensor(out=e2[:], in0=e2[:], in1=ct("MB"), op=ALU.mult), reads=[re1, rct], writes=[re1]),
                    P.dve(lambda: nc.vector.tensor_tensor(out=e1[:], in0=e1[:], in1=e2[:], op=ALU.add), reads=[re1], writes=[re1]),
                    P.dve(lambda: nc.vector.tensor_scalar(out=DTt[:, h, :], in0=e1[:], scalar1=KS, scalar2=None, op0=ALU.mult), reads=[re1], writes=[rdec])))(h)
            P.act(lambda: nc.scalar.activation(out=wkd[:, 0, :], in_=lgbc[:, 0:4], func=AF.Exp, scale=ct("cjm")), reads=[rdec, rct], writes=[rdec])
            P.act(lambda: nc.scalar.activation(out=wkd[:, 1, :], in_=lgbc[:, 4:8], func=AF.Exp, scale=ct("jcol")), reads=[rdec, rct], writes=[rdec])
            P.dve(lambda: nc.vector.tensor_scalar(out=wkd[:], in0=wkd[:], scalar1=KS, scalar2=None, op0=ALU.mult), reads=[rdec], writes=[rdec])
            for dr in range(2):
                for hb in range(2):
                    (lambda dr, hb: (
                        P.act(lambda: nc.scalar.activation(out=DfT[:, dr, hb, :], in_=ct("iota1") if dr == 0 else ct("cmi"), func=AF.Exp,
                                                           scale=lghp[:, dr * 2 + hb: dr * 2 + hb + 1]), reads=[rdec, rct], writes=[rdec]),
                        P.act(lambda: nc.scalar.activation(out=Gam[:, dr * 2 + hb: dr * 2 + hb + 1], in_=lghp[:, dr * 2 + hb: dr * 2 + hb + 1],
                                                           func=AF.Exp, scale=128.0), reads=[rdec], writes=[rdec])))(dr, hb)

            with contextlib.ExitStack() as pA:
                wkv = sb("wkv", [128, 8, 768], BF16, pA)
                rwkv = Res()
                P.dma("pool", lambda: nc.gpsimd.dma_start(out=wkv[:], in_=w_in[l][:, 256:1024].rearrange("(k p) n -> p k n", p=128)), writes=[rwkv])
                UPD = sb("UPD", [128, NT, 2, 2, 128], F32, pA)
                rUPD = Res()
                hTa = [sb("hTa%d" % i, [128, 8, 128], BF16, pA) for i in range(2)]
                rhTa = [Res(), Res()]
                t1 = sb("t1a", [128, 256], F32, pA)
                t2 = sb("t2a", [128, 256], F32, pA)
                kr = sb("kra", [128, 256], F32, pA)
                rt = Res()
                kw = [sb("kwa%d" % i, [128, 2, 256], BF16, pA) for i in range(2)]
                vb = [sb("vba%d" % i, [128, 512], BF16, pA) for i in range(2)]
                rkw = [Res(), Res()]
                tilesA = tiles_ctx + tiles_lat
                t1s = [t1, sb("t1a2", [128, 256], F32, pA)]
                t2s = [t2, sb("t2a2", [128, 256], F32, pA)]
                krs = [kr, sb("kra2", [128, 256], F32, pA)]
                rts = [rt, Res()]
                abanks = [(0, 1, 2), (4, 5, 6)]

                def chainA(n_, st):
                    T = tilesA[n_]
                    s = 0 if T < NLAT else 1
                    ik, iv, iu = abanks[st]
                    bk, rbk, bv, rbv, bu, rbu = bank[ik], rbank[ik], bank[iv], rbank[iv], bank[iu], rbank[iu]
                    X, rX, XN, rXN, S, rS = norm_tile(T)
                    load_rope(T)
                    H = hTa[st]
                    transpose_mod(XN, rXN, A1, B1m, s, (lambda H: (lambda k: H[:, k, :]))(H), rhTa[st])
                    for k in range(8):
                        mm(bk[:, 0:256], H[:, k, :], wkv[:, k, 0:256], k == 0, k == 7, [rhTa[st], rwkv], [rbk])
                    for k in range(8):
                        mm(bv[:, 0:512], H[:, k, :], wkv[:, k, 256:768], k == 0, k == 7, [rhTa[st], rwkv], [rbv])
                    RT, rRT = ropet[T % 2], rrope[T % 2]
                    krv = krs[st][:].rearrange("p (h d) -> p h d", d=64)
                    rope_apply(P, nc, bk[:, 0:256], rbk, RT, rRT, 4, t1s[st], t2s[st], krv, rts[st], rts[st])
                    KW, V = kw[st], vb[st]
                    for dr in range(2):
                        tt(KW[:, dr, :].rearrange("p (h d) -> p h d", d=64), krv, bc(wkd[:, dr, :], 2, 64), ALU.mult, [rts[st], rdec], [rkw[st]])
                    cpa(V[:], bv[:], [rbv], [rkw[st]])
                    for dr in range(2):
                        for hb in range(2):
                            mm(bu[:, hb * 256:(hb + 1) * 256], KW[:, dr, hb * 128:(hb + 1) * 128], V[:, hb * 256:(hb + 1) * 256], True, True,
                               [rkw[st]], [rbu])
                        for hb in range(2):
                            cpv(UPD[0:64, T, dr, hb, :], bu[0:64, hb * 256:hb * 256 + 128], [rbu], [rUPD])
                            cpa(UPD[64:128, T, dr, hb, :], bu[64:128, hb * 256 + 128:hb * 256 + 256], [rbu], [rUPD])

                for n_ in range(0, len(tilesA), 2):
                    chains = []
                    for st in range(2):
                        if n_ + st < len(tilesA):
                            P.capture_start()
                            chainA(n_ + st, st)
                            chains.append(P.capture_end())
                    P.emit_zip(chains)
                cur = [sb("cur%d" % i, [128, 2, 128], F32, pA) for i in range(2)]
                rcur = Res()
                c0, c1 = NLAT, NLAT + 1
                for dr in range(2):
                    first, second = (c0, c1) if dr == 0 else (c1, c0)
                    (lambda dr, first, second: (
                        P.dve(lambda: nc.vector.memset(ST[:, first, dr, :, :], 0.0), writes=[rST]),
                        P.dve(lambda: nc.vector.tensor_copy(out=ST[:, second, dr, :, :], in_=UPD[:, first, dr, :, :]), reads=[rUPD], writes=[rST])))(dr, first, second)
                    order = list(range(NLAT)) if dr == 0 else list(range(NLAT - 1, -1, -1))
                    for hb in range(2):
                        (lambda dr, hb, first, second: P.dve(lambda: nc.vector.scalar_tensor_tensor(
                            out=cur[0][:, hb, :], in0=UPD[:, first, dr, hb, :], scalar=Gam[:, dr * 2 + hb: dr * 2 + hb + 1],
                            in1=UPD[:, second, dr, hb, :], op0=ALU.mult, op1=ALU.add), reads=[rUPD, rdec], writes=[rcur]))(dr, hb, first, second)
                    pp = 0
                    for n in order:
                        (lambda dr, n, pp: P.act(lambda: nc.scalar.copy(out=ST[:, n, dr, :, :], in_=cur[pp][:]), reads=[rcur], writes=[rST]))(dr, n, pp)
                        for hb in range(2):
                            (lambda dr, hb, n, pp: P.dve(lambda: nc.vector.scalar_tensor_tensor(
                                out=cur[1 - pp][:, hb, :], in0=cur[pp][:, hb, :], scalar=Gam[:, dr * 2 + hb: dr * 2 + hb + 1],
                                in1=UPD[:, n, dr, hb, :], op0=ALU.mult, op1=ALU.add), reads=[rUPD, rdec, rcur], writes=[rcur]))(dr, hb, n, pp)
                        pp = 1 - pp
                P.flush()
            if stop_after == "A" and l == 0:
                break

            with contextlib.ExitStack() as pB:
                phase_B(pB, l, last, ST, rST, DTt, DfT, rdec)
                P.flush()
        if stop_after == "B" and l == 0:
            break

        with contextlib.ExitStack() as pM:
            rowbcA = sb("rowbcA", [128, 2, D], F32, pM)
            build_rowbc(rowbcA, rrowA, lambda s, c: modT[:, 16 + c, s:s + 1])
            phase_merge(pM, l, last)
            P.flush()
        if stop_after == "Bp" and l == 0:
            break

        with contextlib.ExitStack() as pC:
            rowbcA = sb("rowbcA", [128, 2, D], F32, pC)
            rowbcB = sb("rowbcB", [128, 2, D], F32, pC)
            build_rowbc(rowbcA, rrowA, lambda s, c: A2[:, c, s:s + 1])
            build_rowbc(rowbcB, rrowB, lambda s, c: modT[:, 24 + c, s:s + 1])
            phase_moe(pC, l, last)
            P.flush()
        if stop_after == "C" and l == 0:
            break

    P.flush()
    _DBG["stats"] = dict(nops=len(P.ops), nwait=P.nwait, ecount=dict(P.ecount), dmax=max(P.dcount))


def rope_apply(P, nc, src, rsrc, RT, rRT, nh, t1, t2, dst, rtmp, rdst, pool=False):
    s3 = src.rearrange("p (h d) -> p h d", d=64)
    t13 = t1[:, 0:nh * 64].rearrange("p (h d) -> p h d", d=64)
    t23 = t2[:, 0:nh * 64].rearrange("p (h d) -> p h d", d=64)
    (P.pool if pool else P.dve)(lambda: (nc.gpsimd if pool else nc.vector).tensor_tensor(out=t13, in0=s3, in1=bc(RT[:, 0:64], 1, nh), op=ALU.mult), reads=[rsrc, rRT], writes=[rtmp])
    (P.pool if pool else P.dve)(lambda: (nc.gpsimd if pool else nc.vector).tensor_tensor(out=t23[:, :, 0:32], in0=s3[:, :, 32:64], in1=bc(RT[:, 64:96], 1, nh), op=ALU.mult),
          reads=[rsrc, rRT], writes=[rtmp])
    (P.pool if pool else P.dve)(lambda: (nc.gpsimd if pool else nc.vector).tensor_tensor(out=t23[:, :, 32:64], in0=s3[:, :, 0:32], in1=bc(RT[:, 96:128], 1, nh), op=ALU.mult),
          reads=[rsrc, rRT], writes=[rtmp])
    (P.pool if pool else P.dve)(lambda: (nc.gpsimd if pool else nc.vector).tensor_tensor(out=dst, in0=t13, in1=t23, op=ALU.add), reads=[rtmp], writes=[rdst])


def _host_inputs(inp):
    f32 = np.float32
    g = lambda k: np.ascontiguousarray(np.asarray(inp[k], dtype=f32))
    x, c, ctx, c_ctx = g("x"), g("c"), g("ctx"), g("c_ctx")
    B = x.shape[0]
    shared = {}
    shared["w_mod"] = g("w_mod")
    shared["bmT"] = np.ascontiguousarray(g("b_mod").reshape(DEPTH, 48, 128).transpose(0, 2, 1))
    shared["n1T"] = np.ascontiguousarray(g("norm1_g").reshape(DEPTH, 8, 128).transpose(0, 2, 1))
    shared["n2T"] = np.ascontiguousarray(g("norm2_g").reshape(DEPTH, 8, 128).transpose(0, 2, 1))
    shared["w_in"] = g("w_in")
    df, db = g("ret_decay_fwd"), g("ret_decay_bwd")
    decbc = np.concatenate([df, db], axis=1)
    shared["decbc"] = np.ascontiguousarray(np.broadcast_to(decbc[:, None, :], (DEPTH, 128, 8)))
    hp = np.zeros((DEPTH, 128, 4), f32)
    for dr, dd in enumerate((df, db)):
        for hb in range(2):
            hp[:, 0:64, dr * 2 + hb] = dd[:, 2 * hb][:, None]
            hp[:, 64:128, dr * 2 + hb] = dd[:, 2 * hb + 1][:, None]
    shared["dechp"] = hp
    shared["sgu_wT"] = np.ascontiguousarray(g("sgu_w").transpose(0, 1, 3, 2))
    shared["sgu_bbc"] = np.ascontiguousarray(np.broadcast_to(g("sgu_b").reshape(DEPTH, 1, 512), (DEPTH, 128, 512)))
    shared["qg"] = np.ascontiguousarray(np.broadcast_to(g("q_norm_g")[:, None, :], (DEPTH, 128, 64)))
    shared["kg"] = np.ascontiguousarray(np.broadcast_to(g("k_norm_g")[:, None, :], (DEPTH, 128, 64)))
    shared["sink"] = np.ascontiguousarray(np.broadcast_to(g("attn_sink")[:, None, :], (DEPTH, 128, 8)))
    for k in ("w_branch", "w_out", "w_router", "w_exp_gate", "w_exp_up", "w_exp_down"):
        shared[k] = g(k)
    shared["rope"] = _rope_tables()
    shared["ctab"] = _const_table()
    maps = []
    for b in range(B):
        m = dict(shared)
        m["x"] = x[b]
        m["ctxin"] = ctx[b]
        cT = np.stack([c[b].reshape(8, 128).T, c_ctx.reshape(8, 128).T], axis=-1)
        m["cT"] = np.ascontiguousarray(cT.reshape(128, 16))
        maps.append(m)
    return maps


_NC_CACHE = {}


def kernel(**inputs):
    maps = _host_inputs(inputs)
    if "nc" not in _NC_CACHE:
        _NC_CACHE["nc"] = build_program()
    nc = _NC_CACHE["nc"]
    n = len(maps)
    res = run_bass_kernel_spmd(nc, maps, core_ids=list(range(n)))
    return np.stack([np.asarray(r["out"], dtype=np.float32) for r in res.results], axis=0)
```

```python
import contextlib
import numpy as np
import concourse.bass as bass
import concourse.mybir as mybir
from concourse.bass_utils import run_bass_kernel_spmd

F32 = mybir.dt.float32
BF16 = mybir.dt.bfloat16
I32 = mybir.dt.int32
AF = mybir.ActivationFunctionType
ALU = mybir.AluOpType
AX = mybir.AxisListType

D = 1024
SEQ = 4096
CTX = 256
NLAT = 32
NT = 34
DEPTH = 2
NEXP = 16
EPS = 1e-6
INW = 6400
NG = 3328

_DBG = {}


class Res:
    __slots__ = ("name", "w", "r", "excl")

    def __init__(self, name="", excl=False):
        self.name = name
        self.w = None
        self.r = []
        self.excl = excl


class _Op:
    __slots__ = ("eng", "fn", "reads", "writes", "dma", "deps", "inc", "val", "semid")

    def __init__(self, eng, fn, reads, writes, dma):
        self.eng = eng
        self.fn = fn
        self.reads = reads
        self.writes = writes
        self.dma = dma
        self.deps = ()
        self.inc = False
        self.val = 0
        self.semid = None


class Prog:
    NDMASEM = 64
    NHW = 32

    def __init__(self, nc, es):
        self.nc = nc
        self.ops = []
        self.done = 0
        self.engs = {"pe": nc.tensor, "act": nc.scalar, "dve": nc.vector,
                     "pool": nc.gpsimd, "sp": nc.sync}
        self.sems = {e: es.enter_context(nc.semaphore("sem_" + e)) for e in self.engs}
        self.dsems = [es.enter_context(nc.semaphore("dsem%d" % i)) for i in range(self.NDMASEM)]
        self.dlast = [None] * self.NDMASEM
        self.drr = 0
        self.drr_sw = 0
        self.ecount = {e: 0 for e in self.engs}
        self.dcount = [0] * self.NDMASEM
        self.waited = {e: {} for e in self.engs}
        self.nwait = 0
        self._cap = None
        self._unit = None

    def add(self, eng, fn, reads=(), writes=(), dma=False):
        reads = tuple(reads)
        writes = tuple(writes)
        ex = tuple(r for r in reads if r.excl)
        if ex:
            reads = tuple(r for r in reads if not r.excl)
            writes = writes + ex
        op = _Op(eng, fn, reads, writes, dma)
        if self._cap is not None:
            if self._unit is not None:
                self._unit.append(op)
            else:
                self._cap.append([op])
        else:
            self.ops.append(op)

    def capture_start(self):
        self._cap = []
        self._unit = None

    def capture_end(self):
        cap = self._cap
        self._cap = None
        return cap

    @contextlib.contextmanager
    def atomic(self):
        if self._cap is None or self._unit is not None:
            yield
            return
        self._unit = []
        try:
            yield
        finally:
            self._cap.append(self._unit)
            self._unit = None

    def emit_zip(self, chains):
        items = []
        for ci, ch in enumerate(chains):
            lo, hi = 0.0, 1.0
            if isinstance(ch, tuple):
                ch, lo, hi = ch
            n = len(ch)
            for ui, unit in enumerate(ch):
                items.append((lo + (hi - lo) * (ui + 0.5) / n, ci, ui, unit))
        items.sort(key=lambda t: (t[0], t[1]))
        for _, _, _, unit in items:
            self.ops.extend(unit)

    def pe(self, fn, reads=(), writes=()):
        self.add("pe", fn, reads, writes)

    def act(self, fn, reads=(), writes=()):
        self.add("act", fn, reads, writes)

    def dve(self, fn, reads=(), writes=()):
        self.add("dve", fn, reads, writes)

    def pool(self, fn, reads=(), writes=()):
        self.add("pool", fn, reads, writes)

    def dma(self, eng, fn, reads=(), writes=()):
        self.add(eng, fn, reads, writes, dma=True)

    def flush(self):
        ops = self.ops
        base = self.done
        n = len(ops)
        last_compute = {}
        for i in range(base, n):
            op = ops[i]
            raw = set()
            war = set()
            for r in op.reads:
                if r.w is not None:
                    raw.add(r.w)
            for w in op.writes:
                if w.w is not None:
                    raw.add(w.w)
                war.update(w.r)
            if op.dma:
                if op.eng == "pool":
                    s = self.NHW + self.drr_sw
                    self.drr_sw = (self.drr_sw + 1) % (self.NDMASEM - self.NHW)
                else:
                    s = self.drr
                    self.drr = (self.drr + 1) % self.NHW
                op.semid = s
                if self.dlast[s] is not None:
                    raw.add(self.dlast[s])
                self.dlast[s] = i
            else:
                last_compute[op.eng] = i
            deps = set()
            for j in raw:
                if j < base or j == i:
                    continue
                o = ops[j]
                if (not o.dma) and (not op.dma) and o.eng == op.eng and op.eng == "pe":
                    continue
                deps.add(j)
            for j in war:
                if j < base or j == i:
                    continue
                o = ops[j]
                if (not o.dma) and o.eng == op.eng and not op.dma and op.eng == "pe":
                    continue
                deps.add(j)
            for r in op.reads:
                r.r.append(i)
            for w in op.writes:
                w.w = i
                w.r = []
            best = {}
            for j in deps:
                o = ops[j]
                key = ("d", o.semid) if o.dma else ("e", o.eng)
                if key not in best or best[key] < j:
                    best[key] = j
            op.deps = tuple(best.values())
            for j in op.deps:
                ops[j].inc = True
        for e, i in last_compute.items():
            ops[i].inc = True
        for i in range(base, n):
            op = ops[i]
            if op.dma:
                self.dcount[op.semid] += 16
                op.val = self.dcount[op.semid]
            elif op.inc:
                self.ecount[op.eng] += 1
                op.val = self.ecount[op.eng]
        for i in range(base, n):
            op = ops[i]
            eng = self.engs[op.eng]
            wt = self.waited[op.eng]
            for j in op.deps:
                o = ops[j]
                if o.dma:
                    key, sem = ("d", o.semid), self.dsems[o.semid]
                else:
                    key, sem = ("e", o.eng), self.sems[o.eng]
                if wt.get(key, 0) >= o.val:
                    continue
                eng.wait_ge(sem, o.val)
                self.nwait += 1
                wt[key] = o.val
            inst = op.fn()
            if op.dma:
                inst.then_inc(self.dsems[op.semid], 16)
            elif op.inc:
                inst.then_inc(self.sems[op.eng], 1)
            op.fn = None
        for e, eng in self.engs.items():
            wt = self.waited[e]
            for x in self.engs:
                if x == e:
                    continue
                v = self.ecount[x]
                if v > wt.get(("e", x), 0):
                    eng.wait_ge(self.sems[x], v)
                    wt[("e", x)] = v
            for s in range(self.NDMASEM):
                v = self.dcount[s]
                if v > wt.get(("d", s), 0):
                    eng.wait_ge(self.dsems[s], v)
                    wt[("d", s)] = v
        self.done = n


def bc(ap, axis, n):
    s = list(ap.shape)
    s.insert(axis, n)
    return ap.unsqueeze(axis).to_broadcast(s)


def _rope_tables():
    f32 = np.float32
    rows = SEQ // 64
    row = np.repeat(np.arange(rows), 64).astype(f32)
    col = np.tile(np.arange(64), rows).astype(f32)
    nf = 16
    inv = np.power(f32(10000.0), -np.arange(nf, dtype=f32) / f32(nf)).astype(f32)
    ang = np.concatenate([row[:, None] * inv, col[:, None] * inv], axis=-1).astype(f32)
    cos = np.cos(ang).astype(f32)
    sin = np.sin(ang).astype(f32)
    tab = np.zeros((NT * 128, 128), f32)
    tab[:SEQ, 0:32] = cos
    tab[:SEQ, 32:64] = cos
    tab[:SEQ, 64:96] = -sin
    tab[:SEQ, 96:128] = sin
    tab[SEQ:, 0:64] = 1.0
    return tab.reshape(NT, 128, 128)


CT = {}


def _const_table():
    cols = []

    def put(name, arr):
        arr = np.asarray(arr, np.float32).reshape(128, -1)
        CT[name] = (sum(c.shape[1] for c in cols), arr.shape[1])
        cols.append(arr)

    j = np.arange(128)[:, None]
    i = np.arange(128)[None, :]
    put("ident", (j == i))
    put("ones", np.ones((128, 128)))
    put("PF", np.maximum(i - j, 0))
    put("PB", np.maximum(j - i, 0))
    put("MF", (i >= j))
    put("MB", (j > i))
    put("iota1", np.broadcast_to(i + 1, (128, 128)))
    put("cmi", np.broadcast_to(128 - i, (128, 128)))
    put("cjm", 127 - j)
    put("jcol", j)
    put("tri", (j < i))
    put("iota512", np.broadcast_to(np.arange(512)[None, :], (128, 512)))
    mp = np.where(j >= i, 0.0, -30000.0)
    mn = np.where(j <= i, 0.0, -30000.0)
    put("mprev", np.tile(mp, (1, 4)))
    put("mnext", np.tile(mn, (1, 4)))
    put("ttv", np.broadcast_to(np.arange(NLAT)[None, :], (128, NLAT)))
    put("eps", np.full((128, 1), EPS))
    return np.concatenate(cols, axis=1)


def build_program(stop_after=None, dbg_out=False):
    nc = bass.Bass("TRN2", target_bir_lowering=False)
    es = contextlib.ExitStack()
    with es:
        _build(nc, es, stop_after, dbg_out)
    return nc


def _build(nc, es, stop_after, dbg_out):
    ctab_np = _const_table()
    NCT = ctab_np.shape[1]

    def din(name, shape, dt=F32):
        return nc.dram_tensor(name, list(shape), dt, kind="ExternalInput").ap()

    x_in = din("x", [SEQ, D])
    ctx_in = din("ctxin", [CTX, D])
    cT_d = din("cT", [128, 16])
    w_mod = din("w_mod", [DEPTH, D, 6 * D])
    bmT_d = din("bmT", [DEPTH, 128, 48])
    n1T_d = din("n1T", [DEPTH, 128, 8])
    n2T_d = din("n2T", [DEPTH, 128, 8])
    w_in = din("w_in", [DEPTH, D, INW])
    decbc_d = din("decbc", [DEPTH, 128, 8])
    dechp_d = din("dechp", [DEPTH, 128, 4])
    swT_d = din("sgu_wT", [DEPTH, 4, 128, 128])
    sbb_d = din("sgu_bbc", [DEPTH, 128, 512])
    qg_d = din("qg", [DEPTH, 128, 64])
    kg_d = din("kg", [DEPTH, 128, 64])
    sink_d = din("sink", [DEPTH, 128, 8])
    w_br = din("w_branch", [DEPTH, 3, 512, D])
    w_out = din("w_out", [DEPTH, D, D])
    w_rt = din("w_router", [DEPTH, D, NEXP])
    w_eg = din("w_exp_gate", [DEPTH, NEXP, D, D])
    w_eu = din("w_exp_up", [DEPTH, NEXP, D, D])
    w_ed = din("w_exp_down", [DEPTH, NEXP, D, D])
    rope_d = din("rope", [NT, 128, 128])
    ctab_d = din("ctab", [128, NCT])
    out = nc.dram_tensor("out", [SEQ, D], F32, kind="ExternalOutput").ap()
    xc = nc.dram_tensor("xc", [CTX, D], F32, kind="ExternalOutput").ap()
    BR = nc.dram_tensor("BR", [3, 512, NT * 128], BF16, kind="ExternalOutput").ap()
    H2 = nc.dram_tensor("H2", [NT * 128, D], BF16, kind="ExternalOutput").ap()

    P = Prog(nc, es)

    _uid = [0]

    def sb(name, shape, dt, stack=es):
        _uid[0] += 1
        return stack.enter_context(nc.sbuf_tensor("%s_%d" % (name, _uid[0]), list(shape), dt))

    bank = [es.enter_context(nc.psum_tensor("bank%d" % i, [128, 512], F32)) for i in range(7)]
    bankT = es.enter_context(nc.psum_tensor("bankT", [128, 1024], BF16))
    rbank = [Res("bank%d" % i, excl=True) for i in range(7)]
    _rbT = Res("bankT", excl=True)
    rbT = [_rbT, _rbT]

    RS = [Res("S%d" % t) for t in range(NT)]
    RBR = [[Res() for _ in range(NT)] for _ in range(3)]
    RH2 = [Res() for _ in range(NT)]

    def srows(T):
        return out[T * 128:(T + 1) * 128, :] if T < NLAT else xc[(T - NLAT) * 128:(T - NLAT + 1) * 128, :]

    ctab = sb("ctab", [128, NCT], F32)
    rct = Res("ctab")
    P.dma("sp", lambda: nc.sync.dma_start(out=ctab[:], in_=ctab_d), writes=[rct])

    def ct(name):
        o, n = CT[name]
        return ctab[:, o:o + n]

    identb = sb("identb", [128, 128], BF16)
    onesb = sb("onesb", [128, 128], BF16)
    trib = sb("trib", [128, 128], BF16)
    mprevb = sb("mprevb", [128, 512], BF16)
    mnextb = sb("mnextb", [128, 512], BF16)
    rcb = Res("constb")
    for dst, nm in ((identb, "ident"), (onesb, "ones"), (trib, "tri"), (mprevb, "mprev"), (mnextb, "mnext")):
        (lambda dst, nm: P.dve(lambda: nc.vector.tensor_copy(out=dst[:], in_=ct(nm)), reads=[rct], writes=[rcb]))(dst, nm)
    epsc = ct("eps")
    negh = sb("negh", [128, 8], F32)
    rnegh = Res("negh")
    P.dve(lambda: nc.vector.memset(negh[:], -0.5), writes=[rnegh])

    for q in range(8):
        (lambda q: P.dma("sp", lambda: nc.sync.dma_start(out=out[q * 512:(q + 1) * 512, :], in_=x_in[q * 512:(q + 1) * 512, :]),
                         writes=RS[q * 4:(q + 1) * 4]))(q)
    P.dma("sp", lambda: nc.sync.dma_start(out=xc, in_=ctx_in), writes=RS[NLAT:NT])

    cact = sb("cact", [128, 8, 2], BF16)
    modT = sb("modT", [128, 48, 2], F32)
    A1 = sb("A1", [128, 8, 2], F32)
    A2 = sb("A2", [128, 8, 2], F32)
    rmod = Res("mod")
    rowbcA = None
    rowbcB = None
    rrowA = Res("rowA")
    rrowB = Res("rowB")
    dgt = [sb("dgt%d" % i, [128, 128], F32) for i in range(2)]
    rdg = [Res(), Res()]

    xt = [sb("xt%d" % i, [128, D], F32) for i in range(2)]
    rxt = [Res(), Res()]
    sqj = sb("sqj", [128, D], F32)
    rsq = Res()
    ssum = [sb("ssum%d" % i, [128, 4], F32) for i in range(2)]
    rss = [Res(), Res()]
    xn = [sb("xn%d" % i, [128, D], BF16) for i in range(2)]
    rxn = [Res(), Res()]
    ropet = [sb("ropet%d" % i, [128, 128], F32) for i in range(2)]
    rrope = [Res(), Res()]
    cnt = {"tile": 0, "ev": 0}

    def load_rope(T):
        i = T % 2
        P.dma("sp", lambda: nc.sync.dma_start(out=ropet[i][:], in_=rope_d[T]), writes=[rrope[i]])
        return ropet[i], rrope[i]

    def norm_tile(T, want_x=False):
        i = cnt["tile"] % 2
        cnt["tile"] += 1
        X, rX, S, rS, XN, rXN = xt[i], rxt[i], ssum[i], rss[i], xn[i], rxn[i]
        P.dma("sp", lambda: nc.sync.dma_start(out=X[:], in_=srows(T)), reads=[RS[T]], writes=[rX])
        P.dve(lambda: nc.vector.memset(S[:], 0.0), writes=[rS])
        P.act(lambda: nc.scalar.activation(out=sqj[:], in_=X[:], func=AF.Square, accum_out=S[:, 0:1]),
              reads=[rX], writes=[rsq, rS])
        P.dve(lambda: nc.vector.tensor_scalar(out=S[:, 1:2], in0=S[:, 0:1], scalar1=1.0 / D, scalar2=EPS, op0=ALU.mult, op1=ALU.add),
              reads=[rS], writes=[rS])
        P.pool(lambda: nc.gpsimd.tensor_tensor(out=S[:, 2:3], in0=S[:, 1:2], in1=negh[:, 0:1], op=ALU.pow), reads=[rS, rnegh], writes=[rS])
        P.dve(lambda: nc.vector.tensor_scalar(out=XN[:], in0=X[:], scalar1=S[:, 2:3], scalar2=None, op0=ALU.mult),
              reads=[rX, rS], writes=[rXN])
        return X, rX, XN, rXN, S, rS

    def transpose_mod(XN, rXN, A, Bm, s, dst_fn, rdst):
        for half in range(2):
          with P.atomic():
            rb = rbT[half]
            for kk in range(4):
                k = half * 4 + kk
                (lambda k, kk, half: P.pe(lambda: nc.tensor.transpose(out=bankT[:, half * 512 + kk * 128: half * 512 + (kk + 1) * 128],
                                                                       in_=XN[:, k * 128:(k + 1) * 128], identity=identb[:]),
                                          reads=[rXN, rcb], writes=[rb]))(k, kk, half)
            for kk in range(4):
                k = half * 4 + kk
                src = bankT[:, half * 512 + kk * 128: half * 512 + (kk + 1) * 128]
                if kk % 2 == 0:
                    (lambda k, src: P.act(lambda: nc.scalar.activation(out=dst_fn(k), in_=src, func=AF.Identity,
                                                                       scale=A[:, k, s:s + 1], bias=Bm[:, k, s:s + 1]),
                                          reads=[rb, rmod], writes=[rdst]))(k, src)
                else:
                    (lambda k, src: P.dve(lambda: nc.vector.tensor_scalar(out=dst_fn(k), in0=src, scalar1=A[:, k, s:s + 1],
                                                                          scalar2=Bm[:, k, s:s + 1], op0=ALU.mult, op1=ALU.add),
                                          reads=[rb, rmod], writes=[rdst]))(k, src)

    def build_rowbc(dst, rdst, col_fn):
        n = 0
        for s in range(2):
            for hb in range(2):
                b = bank[hb]
                rb = rbank[hb]
                for cc in range(4):
                    c = hb * 4 + cc
                    g = n % 2
                    n += 1
                    (lambda c, cc, g, s, b, rb: (
                        P.dve(lambda: nc.vector.tensor_scalar(out=dgt[g][:], in0=ct("ident"), scalar1=col_fn(s, c), scalar2=None, op0=ALU.mult),
                              reads=[rct, rmod], writes=[rdg[g]]),
                        P.pe(lambda: nc.tensor.matmul(b[:, cc * 128:(cc + 1) * 128], lhsT=ct("ones"), rhs=dgt[g][:], start=True, stop=True),
                             reads=[rdg[g], rct], writes=[rb])))(c, cc, g, s, b, rb)
                (lambda s, hb, b, rb: P.act(lambda: nc.scalar.copy(out=dst[:, s, hb * 512:(hb + 1) * 512], in_=b[:]),
                                            reads=[rb], writes=[rdst]))(s, hb, b, rb)

    def mm(o, lhsT, rhs, start, stop, reads, writes, tp=None):
        kw = {} if (tp is None or _DBG.get("notp")) else {"tile_position": tp}
        P.pe(lambda: nc.tensor.matmul(o, lhsT=lhsT, rhs=rhs, start=start, stop=stop, **kw), reads, writes)

    def tr(o, in_, ident, reads, writes):
        P.pe(lambda: nc.tensor.transpose(out=o, in_=in_, identity=ident), reads, writes)

    def actf(o, in_, func, reads, writes, **kw):
        P.act(lambda: nc.scalar.activation(out=o, in_=in_, func=func, **kw), reads, writes)

    def cpa(o, in_, reads, writes):
        P.act(lambda: nc.scalar.copy(out=o, in_=in_), reads, writes)

    def cpv(o, in_, reads, writes, eng="dve"):
        if eng == "POOL":
            eng = "pool"
        elif not _DBG.get("usepool"):
            eng = "dve"
        e = nc.vector if eng == "dve" else nc.gpsimd
        P.add(eng, lambda: e.tensor_copy(out=o, in_=in_), reads, writes)

    def tt(o, a, b, op, reads, writes, eng="dve"):
        if eng == "POOL":
            eng = "pool"
        elif not _DBG.get("usepool"):
            eng = "dve"
        e = nc.vector if eng == "dve" else nc.gpsimd
        P.add(eng, lambda: e.tensor_tensor(out=o, in0=a, in1=b, op=op), reads, writes)

    def tsc(o, a, s1, s2, op0, op1, reads, writes, eng="dve"):
        if eng == "POOL":
            eng = "pool"
        elif not _DBG.get("usepool"):
            eng = "dve"
        e = nc.vector if eng == "dve" else nc.gpsimd
        if op1 is None:
            P.add(eng, lambda: e.tensor_scalar(out=o, in0=a, scalar1=s1, scalar2=None, op0=op0), reads, writes)
        else:
            P.add(eng, lambda: e.tensor_scalar(out=o, in0=a, scalar1=s1, scalar2=s2, op0=op0, op1=op1), reads, writes)

    def stt(o, a, s, b, op0, op1, reads, writes, eng="dve"):
        if eng == "POOL":
            eng = "pool"
        elif not _DBG.get("usepool"):
            eng = "dve"
        e = nc.vector if eng == "dve" else nc.gpsimd
        P.add(eng, lambda: e.scalar_tensor_tensor(out=o, in0=a, scalar=s, in1=b, op0=op0, op1=op1), reads, writes)

    def mset(o, v, writes, eng="dve"):
        if eng == "POOL":
            eng = "pool"
        elif not _DBG.get("usepool"):
            eng = "dve"
        e = nc.vector if eng == "dve" else nc.gpsimd
        P.add(eng, lambda: e.memset(o, v), (), writes)

    def dsp(o, in_, reads, writes):
        P.dma("sp", lambda: nc.sync.dma_start(out=o, in_=in_), reads, writes)

    def dpl(o, in_, reads, writes):
        P.dma("pool", lambda: nc.gpsimd.dma_start(out=o, in_=in_), reads, writes)

    def rsqrt_chain(dst_sd, dst_r, src, scale, reads_res, wres):
        n, w = src.shape[0], src.shape[1]
        tsc(dst_sd, src, scale, EPS, ALU.mult, ALU.add, [reads_res], [wres])
        P.pool(lambda: nc.gpsimd.tensor_tensor(out=dst_r, in0=dst_sd, in1=negh[0:n, 0:w], op=ALU.pow), [wres, rnegh], [wres])

    def kwview(w):
        return w.rearrange("(k p) n -> p k n", p=128)

    def phase_B(stk, l, last, ST, rST, DTt, DfT, rdec):
        wng = sb("wng", [128, 8, NG], BF16, stk)
        rwng = Res()
        for q in range(4):
            c0, c1 = q * 832, (q + 1) * 832
            dpl(wng[:, :, c0:c1], kwview(w_in[l][:, c0:c1]), [], [rwng])
        wsT = sb("wsT", [128, 4, 128], BF16, stk)
        sbb = sb("sbb", [128, 512], F32, stk)
        qg = sb("qg", [128, 64], F32, stk)
        kg = sb("kg", [128, 64], F32, stk)
        esink = sb("esink", [128, 8], F32, stk)
        rsm = Res()
        dpl(wsT[:], swT_d[l].rearrange("g j i -> j g i"), [], [rsm])
        dsp(sbb[:], sbb_d[l], [], [rsm])
        dsp(qg[:], qg_d[l], [], [rsm])
        dsp(kg[:], kg_d[l], [], [rsm])
        dsp(esink[:], sink_d[l], [], [rsm])
        tsc(qg[:], qg[:], 0.125, None, ALU.mult, None, [rsm], [rsm])
        actf(esink[:], esink[:], AF.Exp, [rsm], [rsm])
        KT_all = sb("KT_all", [128, NT, 128], BF16, stk)
        V_all = sb("V_all", [128, NT, 2, 80], BF16, stk)
        rKV = [Res() for _ in range(NT)]
        mset(V_all[:], 1.0, rKV)
        hT = [sb("hTb%d" % i, [128, 8, 128], BF16, stk) for i in range(2)]
        rhT = [Res(), Res()]
        zqk = [sb("zqk%d" % i, [128, 512], F32, stk) for i in range(2)]
        vb = [sb("vb%d" % i, [128, 512], BF16, stk) for i in range(2)]
        sg = [sb("sg%d" % i, [128, 512], BF16, stk) for i in range(2)]
        uT = [sb("uT%d" % i, [128, 512], BF16, stk) for i in range(2)]
        gv = [sb("gv%d" % i, [128, 512], F32, stk) for i in range(2)]
        zq = [sb("zq%d" % i, [128, 512], F32, stk) for i in range(2)]
        zk = [sb("zk%d" % i, [128, 128], F32, stk) for i in range(2)]
        rzqk, rvb, rsg, ruT, rgv, rzq, rzk = [[Res(), Res()] for _ in range(7)]
        t1 = sb("t1", [128, 512], F32, stk)
        t2 = sb("t2", [128, 512], F32, stk)
        rtmp = Res()
        t1a = sb("t1a", [128, 512], F32, stk)
        t2a = sb("t2a", [128, 512], F32, stk)
        rtmpa = Res()
        qrb = sb("qrb", [128, 256], BF16, stk)
        krb = sb("krb", [128, 256], BF16, stk)
        rqk = Res()
        qT = sb("qT", [128, 2, 128], BF16, stk)
        qfT = sb("qfT", [128, 2, 2, 128], BF16, stk)
        kT = sb("kT", [128, 2, 128], BF16, stk)
        rqT = Res()
        SD = sb("SD", [128, 512], BF16, stk)
        rSD = Res()
        bst = sb("bst", [128, 4, 6], F32, stk)
        mv = sb("mv", [128, 4, 2], F32, stk)
        sdr = sb("sdr", [128, 8], F32, stk)
        rln = Res()
        yb = sb("yb", [128, 512], BF16, stk)
        retb = sb("retb", [128, 512], BF16, stk)
        ryb = Res()
        brT = [sb("brTo%d" % i, [128, 4, 128], BF16, stk) for i in range(3)]
        rbrT = [Res(), Res(), Res()]
        vn = sb("vn", [128, 512], BF16, stk)
        stmp = sb("stmp", [128, 512], F32, stk)
        rsg2 = Res()
        bst2 = sb("bst2", [128, 6], F32, stk)
        mv2 = sb("mv2", [128, 4], F32, stk)
        sq = sb("sq", [128, 512], F32, stk)
        qn = sb("qn", [128, 512], F32, stk)
        ssq = sb("ssq", [128, 24], F32, stk)
        ratt = Res()
        sqk = sb("sqk", [128, 128], F32, stk)
        kn = sb("kn", [128, 128], F32, stk)
        ssk = sb("ssk", [128, 8], F32, stk)
        rattk = Res()
        qperm = sb("qperm", [128, 4, 2, 64], BF16, stk)
        krb2 = sb("krb2", [128, 128], BF16, stk)
        qT2 = [sb("qT2_%d" % i, [128, 4, 128], BF16, stk) for i in range(2)]
        rqT2 = [Res(), Res()]
        PT = [sb("PT%d" % i, [128, 512], BF16, stk) for i in range(2)]
        rPT = [Res(), Res()]
        den = sb("den", [128, 16], F32, stk)
        attb = sb("attb", [128, 512], BF16, stk)
        rden = Res()
        BRv = [BR[i].rearrange("(c p) t -> p c t", p=128) for i in range(3)]
        tg = sb("tg", [128, 512], F32, stk)
        rtg = Res()
        pbc = {"n": 0}

        def pbank():
            i = pbc["n"] % 2
            pbc["n"] += 1
            return bank[i], rbank[i]

        def out_branch(i, T, srcb, rsrc):
            with P.atomic():
                for c in range(4):
                    tr(bankT[:, 512 + c * 128: 512 + (c + 1) * 128], srcb[:, c * 128:(c + 1) * 128], identb[:], [rsrc, rcb], [rbT[1]])
                cpa(brT[i][:], bankT[:, 512:1024].rearrange("p (c t) -> p c t", t=128), [rbT[1]], [rbrT[i]])
            dsp(BRv[i][:, :, T * 128:(T + 1) * 128], brT[i][:], [rbrT[i]], [RBR[i][T]])

        def Pst1(T, par):
            s = 0 if T < NLAT else 1
            X, rX, XN, rXN, S, rS = norm_tile(T)
            H, rH = hT[par], rhT[par]
            transpose_mod(XN, rXN, A1, B1m, s, lambda k: H[:, k, :], rH)

        def Pst(T, full, par):
            load_rope(T)
            H, rH = hT[par], rhT[par]

            def proj(c0, c1):
                b, rb = pbank()
                for k in range(8):
                    mm(b[:, 0:c1 - c0], H[:, k, :], wng[:, k, c0:c1], k == 0, k == 7, [rH, rwng], [rb])
                return b, rb

            if full:
                b, rb = proj(0, 512)
                cpa(zqk[par][:], b[:], [rb], [rzqk[par]])
                b, rb = proj(512, 1024)
                cpv(vb[par][:], b[:], [rb], [rvb[par]])
                b, rb = proj(1024, 1536)
                actf(tg[:], b[:], AF.Tanh, [rb], [rtg], scale=0.5)
                stt(sg[par][:], tg[:], 1.0, b[:], ALU.add, ALU.mult, [rtg, rb], [rsg[par]])
                b, rb = pbank()
                for c in range(4):
                    for k in range(8):
                        mm(b[:, c * 128:(c + 1) * 128], wng[:, k, 1536 + c * 128:1536 + (c + 1) * 128], H[:, k, :], k == 0, k == 7, [rH, rwng], [rb])
                actf(uT[par][:], b[:], AF.Gelu_apprx_tanh, [rb], [ruT[par]])
                b, rb = proj(2048, 2560)
                actf(gv[par][:], b[:], AF.Gelu_apprx_tanh, [rb], [rgv[par]])
                b, rb = proj(2560, 3072)
                cpv(zq[par][:], b[:], [rb], [rzq[par]])
            b, rb = proj(3072, 3328)
            cpv(zk[par][:], b[:, 0:128], [rb], [rzk[par]])
            cpa(V_all[:, T, :, 0:64], b[:, 128:256].rearrange("p (k d) -> p k d", d=64), [rb], [rKV[T]])

        def chainR(T, par):
            RT, rRT = ropet[T % 2], rrope[T % 2]
            if True:
                rope_apply(P, nc, zqk[par][:, 0:256], rzqk[par], RT, rRT, 4, t1, t2, qrb[:].rearrange("p (h d) -> p h d", d=64), rtmp, rqk)
                rope_apply(P, nc, zqk[par][:, 256:512], rzqk[par], RT, rRT, 4, t1, t2, krb[:].rearrange("p (h d) -> p h d", d=64), rtmp, rqk)
                with P.atomic():
                    for hb in range(2):
                        tr(bankT[:, hb * 128:(hb + 1) * 128], qrb[:, hb * 128:(hb + 1) * 128], identb[:], [rqk, rcb], [rbT[0]])
                        tr(bankT[:, 256 + hb * 128:256 + (hb + 1) * 128], krb[:, hb * 128:(hb + 1) * 128], identb[:], [rqk, rcb], [rbT[0]])
                    qv = bankT[:, 0:256].rearrange("p (b t) -> p b t", t=128)
                    cpa(qT[:], qv, [rbT[0]], [rqT])
                    for dr in range(2):
                        tt(qfT[:, dr, :, :], qv, DfT[:, dr, :, :], ALU.mult, [rbT[0], rdec], [rqT])
                    cpa(kT[:], bankT[:, 256:512].rearrange("p (b t) -> p b t", t=128), [rbT[0]], [rqT])
                for h in range(4):
                    hb, hl = h // 2, h % 2
                    bS = 2 + hl
                    mm(bank[bS][:, hb * 128:(hb + 1) * 128], kT[64 * hl:64 * hl + 64, hb, :], qT[64 * hl:64 * hl + 64, hb, :], True, True,
                       [rqT], [rbank[bS]], tp=(64 * hl, 0))
                for h in range(4):
                    hb, hl = h // 2, h % 2
                    bS = 2 + hl
                    tt(SD[:, h * 128:(h + 1) * 128], bank[bS][:, hb * 128:(hb + 1) * 128], DTt[:, h, :], ALU.mult, [rbank[bS], rdec], [rSD])
                for h in range(4):
                    hb, hl = h // 2, h % 2
                    o = bank[2][:, h * 128:(h + 1) * 128]
                    mm(o, qfT[64 * hl:64 * hl + 64, 0, hb, :], ST[64 * hl:64 * hl + 64, T, 0, hb, :], True, False, [rqT, rST], [rbank[2]], tp=(64 * hl, 0))
                    mm(o, qfT[64 * hl:64 * hl + 64, 1, hb, :], ST[64 * hl:64 * hl + 64, T, 1, hb, :], False, False, [rqT, rST], [rbank[2]], tp=(64 * hl, 0))
                    mm(o, SD[:, h * 128:(h + 1) * 128], vb[par][:, h * 128:(h + 1) * 128], False, True, [rSD, rvb[par]], [rbank[2]])
                cpa(t1[:], bank[2][:], [rbank[2]], [rtmp])
                for h in range(4):
                    (lambda h: P.dve(lambda: nc.vector.bn_stats(out=bst[:, h, :], in_=t1[:, h * 128:(h + 1) * 128]), [rtmp], [rln]))(h)
                    (lambda h: P.dve(lambda: nc.vector.bn_aggr(out=mv[:, h, :], in_=bst[:, h, :]), [rln], [rln]))(h)
                rsqrt_chain(sdr[:, 0:4], sdr[:, 4:8], mv[:, :, 1], 1.0, rln, rln)
                tsc(sdr[:, 4:8], sdr[:, 4:8], 0.5, None, ALU.mult, None, [rln], [rln])
                for h in range(4):
                    tsc(yb[:, h * 128:(h + 1) * 128], t1[:, h * 128:(h + 1) * 128], mv[:, h, 0:1], sdr[:, 4 + h:5 + h], ALU.subtract, ALU.mult,
                        [rtmp, rln], [ryb])
                tt(retb[:], yb[:], sg[par][:], ALU.mult, [ryb, rsg[par]], [ryb], eng="POOL")
                out_branch(0, T, retb, ryb)

        def chainS(T, par):
            if True:
                P.dve(lambda: nc.vector.bn_stats(out=bst2[:], in_=gv[par][:]), [rgv[par]], [rsg2])
                P.dve(lambda: nc.vector.bn_aggr(out=mv2[:, 0:2], in_=bst2[:]), [rsg2], [rsg2])
                rsqrt_chain(mv2[:, 2:3], mv2[:, 3:4], mv2[:, 1:2], 1.0, rsg2, rsg2)
                tsc(vn[:], gv[par][:], mv2[:, 0:1], mv2[:, 3:4], ALU.subtract, ALU.mult, [rsg2, rgv[par]], [rsg2])
                for g in range(4):
                    mm(bank[4][:, g * 128:(g + 1) * 128], vn[:, g * 128:(g + 1) * 128], wsT[:, g, :], True, True, [rsg2, rsm], [rbank[4]])
                tt(stmp[:], bank[4][:], sbb[:], ALU.add, [rbank[4], rsm], [rsg2])
                tt(brT[1][:].rearrange("p c t -> p (c t)"), stmp[:], uT[par][:], ALU.mult, [rsg2, ruT[par]], [rbrT[1]], eng="POOL")
                dsp(BRv[1][:, :, T * 128:(T + 1) * 128], brT[1][:], [rbrT[1]], [RBR[1][T]])

        def chainQ(T, par):
            RT, rRT = ropet[T % 2], rrope[T % 2]
            if True:
                zq3 = zq[par][:].rearrange("p (h d) -> p h d", d=64)
                qn3 = qn[:].rearrange("p (h d) -> p h d", d=64)
                actf(sq[:], zq[par][:], AF.Square, [rzq[par]], [ratt])
                P.dve(lambda: nc.vector.tensor_reduce(out=ssq[:, 0:8], in_=sq[:].rearrange("p (h d) -> p h d", d=64), axis=AX.X, op=ALU.add), [ratt], [ratt])
                rsqrt_chain(ssq[:, 8:16], ssq[:, 16:24], ssq[:, 0:8], 1.0 / 64, ratt, ratt)
                tt(qn3, zq3, bc(ssq[:, 16:24], 2, 64), ALU.mult, [rzq[par], ratt], [ratt])
                tt(qn3, qn3, bc(qg[:], 1, 8), ALU.mult, [ratt, rsm], [ratt], eng="POOL")
                for hk in range(2):
                    rope_apply(P, nc, qn[:, hk * 256:(hk + 1) * 256], ratt, RT, rRT, 4, t1a, t2a, qperm[:, :, hk, :], rtmpa, ratt, pool=True)
                p2 = T % 2
                with P.atomic():
                    for g in range(4):
                        tr(bankT[:, g * 128:(g + 1) * 128], qperm[:, g, :, :].rearrange("p k d -> p (k d)"), identb[:], [ratt, rcb], [rbT[0]])
                    cpa(qT2[p2][:], bankT[:, 0:512].rearrange("p (g t) -> p g t", t=128), [rbT[0]], [rqT2[p2]])

        def chainK(T, par):
            RT, rRT = ropet[T % 2], rrope[T % 2]
            zk3 = zk[par][:].rearrange("p (h d) -> p h d", d=64)
            kn3 = kn[:].rearrange("p (h d) -> p h d", d=64)
            actf(sqk[:], zk[par][:], AF.Square, [rzk[par]], [rattk])
            P.dve(lambda: nc.vector.tensor_reduce(out=ssk[:, 0:2], in_=sqk[:].rearrange("p (h d) -> p h d", d=64), axis=AX.X, op=ALU.add), [rattk], [rattk])
            rsqrt_chain(ssk[:, 2:4], ssk[:, 4:6], ssk[:, 0:2], 1.0 / 64, rattk, rattk)
            tt(kn3, zk3, bc(ssk[:, 4:6], 2, 64), ALU.mult, [rzk[par], rattk], [rattk])
            tt(kn3, kn3, bc(kg[:], 1, 2), ALU.mult, [rattk, rsm], [rattk])
            rope_apply(P, nc, kn[:], rattk, RT, rRT, 2, t1a, t2a, krb2[:].rearrange("p (h d) -> p h d", d=64), rtmpa, rattk)
            with P.atomic():
                tr(bankT[:, 512:640], krb2[:], identb[:], [rattk, rcb], [rbT[1]])
                cpa(KT_all[:, T, :], bankT[:, 512:640], [rbT[1]], [rKV[T]])

        stc = {"n": 0}

        def Yst(T):
            p2 = T % 2
            if T < NLAT:
                chunks = []
                if T > 0:
                    chunks.append((T - 1, mprevb))
                chunks.append((T, None))
                chunks += [(NLAT, None), (NLAT + 1, None)]
                if T < NLAT - 1:
                    chunks.append((T + 1, mnextb))
            else:
                chunks = [(NLAT, None), (NLAT + 1, None)]
            for hk in range(2):
                ob = bank[6]
                rob = rbank[6]
                for ci, (c, m) in enumerate(chunks):
                    i = stc["n"] % 2
                    stc["n"] += 1
                    sb_, rsb = bank[5], rbank[5]
                    mm(sb_[:, :], KT_all[64 * hk:64 * hk + 64, c, :], qT2[p2][64 * hk:64 * hk + 64, :, :].rearrange("p g t -> p (g t)"),
                       True, m is None, [rKV[c], rqT2[p2]], [rsb], tp=(64 * hk, 0))
                    if m is not None:
                        mm(sb_[:, :], identb[:], m[:], False, True, [rcb], [rsb])
                    actf(PT[i][:], sb_[:, :], AF.Exp, [rsb], [rPT[i]])
                    for g in range(4):
                        mm(ob[:, g * 128:g * 128 + 65], PT[i][:, g * 128:(g + 1) * 128], V_all[:, c, hk, 0:65], ci == 0 and g == 0,
                           ci == len(chunks) - 1 and g == 3, [rPT[i], rKV[c]], [rob])
                ov = ob[:, 0:512].rearrange("p (g e) -> p g e", e=128)
                tt(den[:, hk * 4:(hk + 1) * 4], ov[:, :, 64], esink[:, hk * 4:(hk + 1) * 4], ALU.add, [rob, rsm], [rden])
                (lambda hk: P.dve(lambda: nc.vector.reciprocal(out=den[:, 8 + hk * 4:8 + (hk + 1) * 4], in_=den[:, hk * 4:(hk + 1) * 4]), [rden], [rden]))(hk)
                tt(attb[:, hk * 256:(hk + 1) * 256].rearrange("p (g d) -> p g d", d=64), ov[:, :, 0:64], bc(den[:, 8 + hk * 4:8 + (hk + 1) * 4], 2, 64),
                   ALU.mult, [rob, rden], [rden])
            out_branch(2, T, attb, rden)

        L = [NLAT, NLAT + 1] + list(range(NLAT))
        full = [(not last) if T >= NLAT else True for T in L]
        Pst1(L[0], 0)
        Pst1(L[1], 1)
        Pst(L[0], full[0], 0)
        for i, T in enumerate(L):
            par = i % 2
            chains = []
            if T < NLAT and T >= 1:
                P.capture_start()
                chainK(T, par)
                chains.append((P.capture_end(), 0.0, 0.2))
            else:
                chainK(T, par)
            if i + 1 < len(L):
                P.capture_start()
                Pst(L[i + 1], full[i + 1], (i + 1) % 2)
                chains.append(P.capture_end())
            if i + 2 < len(L):
                P.capture_start()
                Pst1(L[i + 2], i % 2)
                chains.append(P.capture_end())
            if full[i]:
                for ch in (chainR, chainS, chainQ):
                    P.capture_start()
                    ch(T, par)
                    chains.append(P.capture_end())
            if T < NLAT and T >= 1:
                P.capture_start()
                Yst(T - 1)
                chains.append(P.capture_end())
            P.emit_zip(chains)
            if T == NLAT + 1 and not last:
                Yst(NLAT)
                Yst(NLAT + 1)
            if T == NLAT - 1:
                Yst(T)

    def phase_merge(stk, l, last):
        wgt = sb("wgt", [128, 8, 3072], BF16, stk)
        wbr = sb("wbr", [128, 3, 4, D], BF16, stk)
        wo = sb("wo", [128, 8, D], BF16, stk)
        rw = Res()
        for q in range(3):
            dpl(wgt[:, :, q * 1024:(q + 1) * 1024], kwview(w_in[l][:, NG + q * 1024:NG + (q + 1) * 1024]), [], [rw])
            dpl(wbr[:, q, :, :], kwview(w_br[l][q]), [], [rw])
        dpl(wo[:], kwview(w_out[l]), [], [rw])
        hTg = [sb("hTg%d" % i, [128, 8, 512], BF16, stk) for i in range(2)]
        rhTg = [Res(), Res()]
        brTi = [sb("brTi%d" % i, [128, 3, 4, 512], BF16, stk) for i in range(2)]
        rbrTi = [Res(), Res()]
        mT = sb("mT", [128, 8, 512], BF16, stk)
        rmT = Res()
        sgm = [sb("sgm%d" % i, [128, 512], F32, stk) for i in range(2)]
        rsgm = [Res(), Res()]
        acc = sb("acc", [128, 512], F32, stk)
        tmp = sb("tmpm", [128, 512], F32, stk)
        racc = Res()
        rtmpm = Res()
        xo = [sb("xo%d" % i, [128, D], F32, stk) for i in range(2)]
        rxo = [Res(), Res()]
        xr = [sb("xr%d" % i, [128, D], F32, stk) for i in range(2)]
        rxr = [Res(), Res()]
        BRv = [BR[i].rearrange("(c p) t -> p c t", p=128) for i in range(3)]
        groups = [list(range(g * 4, g * 4 + 4)) for g in range(8)]
        if not last:
            groups = [[NLAT, NLAT + 1]] + groups
        cnts = {"pc": 0, "xc": 0}

        def prologue(gi):
            grp = groups[gi]
            gp = gi % 2
            s_ = 0 if grp[0] < NLAT else 1
            ntok = 128 * len(grp)
            T0 = grp[0]
            for ti, T in enumerate(grp):
                X, rX, XN, rXN, S, rS = norm_tile(T)
                transpose_mod(XN, rXN, A1, B1m, s_, (lambda ti, gp: (lambda k: hTg[gp][:, k, ti * 128:(ti + 1) * 128]))(ti, gp), rhTg[gp])
            for i in range(3):
                dsp(brTi[gp][:, i, :, 0:ntok], BRv[i][:, :, T0 * 128:T0 * 128 + ntok], [RBR[i][T] for T in grp], [rbrTi[gp]])

        def compute(gi):
            grp = groups[gi]
            gp = gi % 2
            s_ = 0 if grp[0] < NLAT else 1
            ntok = 128 * len(grp)
            H, rH, BI, rBI = hTg[gp], rhTg[gp], brTi[gp], rbrTi[gp]
            for dc in range(8):
                for i in range(3):
                    p = cnts["pc"] % 3
                    cnts["pc"] += 1
                    bg, rbg, bb, rbb = bank[2 * p], rbank[2 * p], bank[2 * p + 1], rbank[2 * p + 1]
                    for k in range(8):
                        mm(bg[:, 0:ntok], wgt[:, k, i * 1024 + dc * 128:i * 1024 + (dc + 1) * 128], H[:, k, 0:ntok], k == 0, k == 7, [rw, rH], [rbg])
                    for k in range(4):
                        mm(bb[:, 0:ntok], wbr[:, i, k, dc * 128:(dc + 1) * 128], BI[:, i, k, 0:ntok], k == 0, k == 3, [rw, rBI], [rbb])
                    sgi, rsgi = sgm[cnts["pc"] % 2], rsgm[cnts["pc"] % 2]
                    actf(sgi[:, 0:ntok], bg[:, 0:ntok], AF.Sigmoid, [rbg], [rsgi])
                    if i == 0:
                        tt(acc[:, 0:ntok], sgi[:, 0:ntok], bb[:, 0:ntok], ALU.mult, [rsgi, rbb], [racc])
                    elif i == 1:
                        tt(tmp[:, 0:ntok], sgi[:, 0:ntok], bb[:, 0:ntok], ALU.mult, [rsgi, rbb], [rtmpm])
                        tt(acc[:, 0:ntok], acc[:, 0:ntok], tmp[:, 0:ntok], ALU.add, [racc, rtmpm], [racc], eng="POOL")
                    else:
                        tt(tmp[:, 0:ntok], sgi[:, 0:ntok], bb[:, 0:ntok], ALU.mult, [rsgi, rbb], [rtmpm])
                        tt(mT[:, dc, 0:ntok], acc[:, 0:ntok], tmp[:, 0:ntok], ALU.add, [racc, rtmpm], [rmT], eng="POOL")
            for ti, T in enumerate(grp):
                j = cnts["xc"] % 2
                cnts["xc"] += 1
                xoi, rxoi = xo[j], rxo[j]
                dsp(xr[j][:], srows(T), [RS[T]], [rxr[j]])
                for nb in range(2):
                    for k in range(8):
                        mm(bank[6][:, :], mT[:, k, ti * 128:(ti + 1) * 128], wo[:, k, nb * 512:(nb + 1) * 512], k == 0, k == 7, [rmT, rw], [rbank[6]])
                    tt(xoi[:, nb * 512:(nb + 1) * 512], bank[6][:, :], rowbcA[:, s_, nb * 512:(nb + 1) * 512], ALU.mult, [rbank[6], rrowA], [rxoi])
                tt(xoi[:], xoi[:], xr[j][:], ALU.add, [rxoi, rxr[j]], [rxoi], eng="POOL")
                dsp(srows(T), xoi[:], [rxoi], [RS[T]])

        prologue(0)
        for gi in range(len(groups)):
            chains = []
            P.capture_start()
            compute(gi)
            chains.append(P.capture_end())
            if gi + 1 < len(groups):
                P.capture_start()
                prologue(gi + 1)
                chains.append(P.capture_end())
            P.emit_zip(chains)

    def phase_moe(stk, l, last):
        streams = [(0, list(range(NLAT)), 512, 4, 128)]
        if not last:
            streams.append((1, [NLAT, NLAT + 1], 32, 1, 32))
        aff = sb("aff", [128, NT, NEXP], F32, stk)
        raff = Res()
        idxS = [sb("idxS%d" % i, [128, 4, NEXP], I32, stk) for i in range(2)]
        idxH = [sb("idxH%d" % i, [128, 4, NEXP], I32, stk) for i in range(2)]
        wsel = [sb("wsel%d" % i, [128, 4, NEXP], F32, stk) for i in range(2)]
        ridx = [Res(), Res()]

        with contextlib.ExitStack() as s1:
            wr = sb("wr", [128, 8, NEXP], BF16, s1)
            rwr = Res()
            dpl(wr[:], w_rt[l].rearrange("(k p) e -> p k e", p=128), [], [rwr])
            h2fs = [sb("h2fA", [128, D], F32, s1), sb("h2fB", [128, D], F32, s1)]
            rh2fs = [Res(), Res()]
            h2b = [sb("h2b%d" % i, [128, D], BF16, s1) for i in range(2)]
            rh2 = [Res(), Res()]
            h2T = [sb("h2T%d" % i, [128, 8, 128], BF16, s1) for i in range(2)]
            rh2T = [Res(), Res()]
            sm = [sb("sm%d" % i, [128, 8], F32, s1) for i in range(2)]
            ex = [sb("ex%d" % i, [128, NEXP], F32, s1) for i in range(2)]
            rsmx = [Res(), Res()]
            allT = [(s_, T) for (s_, tiles, cap, nch, rows) in streams for T in tiles]

            def st1(j):
                s_, T = allT[j]
                i = j % 2
                X, rX, XN, rXN, S, rS = norm_tile(T)
                h2f, rh2f = h2fs[i], rh2fs[i]
                stt(h2f[:], X[:], S[:, 2:3], rowbcA[:, s_, :], ALU.mult, ALU.mult, [rX, rS, rrowA], [rh2f])
                tt(h2b[i][:], h2f[:], rowbcB[:, s_, :], ALU.add, [rh2f, rrowB], [rh2[i]], eng="pool")
                dsp(H2[T * 128:(T + 1) * 128, :], h2b[i][:], [rh2[i]], [RH2[T]])
                for half in range(2):
                    with P.atomic():
                        for kk in range(4):
                            k = half * 4 + kk
                            tr(bankT[:, half * 512 + kk * 128:half * 512 + (kk + 1) * 128], h2b[i][:, k * 128:(k + 1) * 128], identb[:], [rh2[i], rcb], [rbT[half]])
                        cpa(h2T[i][:, half * 4:(half + 1) * 4, :], bankT[:, half * 512:(half + 1) * 512].rearrange("p (k t) -> p k t", t=128), [rbT[half]], [rh2T[i]])

            def st2(j):
                s_, T = allT[j]
                i = j % 2
                bk, rbk = bank[2 + i], rbank[2 + i]
                for k in range(8):
                    mm(bk[:, 0:NEXP], h2T[i][:, k, :], wr[:, k, :], k == 0, k == 7, [rh2T[i], rwr], [rbk])
                smi, exi, rsi = sm[i], ex[i], rsmx[i]
                P.dve(lambda: nc.vector.tensor_reduce(out=smi[:, 0:1], in_=bk[:, 0:NEXP], axis=AX.X, op=ALU.max), [rbk], [rsi])
                tsc(smi[:, 1:2], smi[:, 0:1], -1.0, None, ALU.mult, None, [rsi], [rsi])
                mset(smi[:, 2:3], 0.0, [rsi])
                actf(exi[:], bk[:, 0:NEXP], AF.Exp, [rbk, rsi], [rsi], bias=smi[:, 1:2], accum_out=smi[:, 2:3])
                P.dve(lambda: nc.vector.reciprocal(out=smi[:, 3:4], in_=smi[:, 2:3]), [rsi], [rsi])
                tsc(aff[:, T, :], exi[:], smi[:, 3:4], None, ALU.mult, None, [rsi], [raff])

            for j0 in range(0, len(allT), 2):
                chains = []
                for j in (j0, j0 + 1):
                    if j < len(allT):
                        P.capture_start()
                        st1(j)
                        st2(j)
                        chains.append(P.capture_end())
                P.emit_zip(chains)
            build_rowbc(rowbcA, rrowA, lambda s, c: modT[:, 40 + c, s:s + 1])
            P.flush()
        if _DBG.get("cstop") == "1":
            return

        with contextlib.ExitStack() as s2:
            ge = sb("ge", [128, NLAT, NEXP], BF16, s2)
            maskf = sb("maskf", [128, NLAT, NEXP], F32, s2)
            basef = sb("basef", [128, NLAT, NEXP], F32, s2)
            keyf = sb("keyf", [128, NLAT, NEXP], F32, s2)
            cntp = sb("cntp", [128, NLAT, NEXP], F32, s2)
            vals = sb("vals", [128, NLAT, NEXP, 4], BF16, s2)
            valf = sb("valf", [128, NLAT, NEXP], F32, s2)
            lo = sb("lo", [128, NEXP], F32, s2)
            mid = sb("mid", [128, NEXP], F32, s2)
            cpr = sb("cpr", [128, NEXP], F32, s2)
            cntr = sb("cntr", [128, NEXP], F32, s2)
            rr = Res()
            OH = [sb("OH%d" % i, [128, 512], BF16, s2) for i in range(4)]
            rOH = [Res() for _ in range(4)]
            lst = sb("lst", [128, NEXP, 4, 4], F32, s2)
            idxf = sb("idxf", [128, 4, NEXP], F32, s2)
            NIT = 26
            for (s, tiles, cap, nch, rows) in streams:
                nt = len(tiles)
                T0 = tiles[0]
                av = aff[:, T0:T0 + nt, :]
                W = nt * NEXP
                mset(lo[:], 0.0, [rr])
                mset(mid[:], 0.5, [rr])
                for it in range(NIT):
                    half = 0.5 ** (it + 1)
                    tt(ge[:, 0:nt, :], av, bc(mid[:], 1, nt), ALU.is_ge, [raff, rr], [rr])
                    mm(bank[0][:, 0:W], onesb[:], ge[:, 0:nt, :].rearrange("p t e -> p (t e)"), True, True, [rr, rcb], [rbank[0]])
                    P.dve(lambda W=W, nt=nt: nc.vector.tensor_reduce(out=cntr[:], in_=bank[0][:, 0:W].rearrange("p (t e) -> p e t", e=NEXP), axis=AX.X, op=ALU.add),
                          [rbank[0]], [rr])
                    tsc(cpr[:], cntr[:], float(cap) - 0.5, half, ALU.is_ge, ALU.mult, [rr], [rr])
                    stt(mid[:], cpr[:], half * 0.5, lo[:], ALU.add, ALU.add, [rr], [rr])
                    tt(lo[:], lo[:], cpr[:], ALU.add, [rr], [rr])
                tt(maskf[:, 0:nt, :], av, bc(lo[:], 1, nt), ALU.is_ge, [raff, rr], [rr])
                cpv(ge[:, 0:nt, :], maskf[:, 0:nt, :], [rr], [rr])
                gef = ge[:, 0:nt, :].rearrange("p t e -> p (t e)")
                mm(bank[0][:, 0:W], trib[:], gef, True, True, [rr, rcb], [rbank[0]])
                mm(bank[1][:, 0:W], onesb[:], gef, True, True, [rr, rcb], [rbank[1]])
                cpv(cntp[:, 0:nt, :].rearrange("p t e -> p (t e)"), bank[1][:, 0:W], [rbank[1]], [rr])
                mset(basef[:, 0, :], 0.0, [rr])
                for t_ in range(1, nt):
                    tt(basef[:, t_, :], basef[:, t_ - 1, :], cntp[:, t_ - 1, :], ALU.add, [rr], [rr])
                tt(keyf[:, 0:nt, :].rearrange("p t e -> p (t e)"), bank[0][:, 0:W], basef[:, 0:nt, :].rearrange("p t e -> p (t e)"), ALU.add, [rbank[0], rr], [rr])
                stt(keyf[:, 0:nt, :], keyf[:, 0:nt, :], 1.0, maskf[:, 0:nt, :], ALU.add, ALU.mult, [rr], [rr])
                tsc(keyf[:, 0:nt, :], keyf[:, 0:nt, :], -1.0, None, ALU.add, None, [rr], [rr])
                cpv(vals[:, 0:nt, :, 0], bc(ct("ttv")[:, 0:nt], 2, NEXP), [rct], [rr])
                cpv(vals[:, 0:nt, :, 1], ct("jcol").unsqueeze(2).to_broadcast([128, nt, NEXP]), [rct], [rr])
                cpv(vals[:, 0:nt, :, 2], av, [raff], [rr])
                cpv(valf[:, 0:nt, :], vals[:, 0:nt, :, 2], [rr], [rr])
                tt(vals[:, 0:nt, :, 3], av, valf[:, 0:nt, :], ALU.subtract, [raff, rr], [rr])
                capw = nch * 128
                oc = 0
                rvals = rr
                for e in range(NEXP):
                    for t_ in range(nt):
                        i = oc % 4
                        oc += 1
                        tsc(OH[i][:, 0:capw], ct("iota512")[:, 0:capw], keyf[:, t_, e:e + 1], None, ALU.is_equal, None, [rct, rvals], [rOH[i]],
                            eng="dve")
                        for c in range(nch):
                            mm(bank[3][:, (e * 4 + c) * 4:(e * 4 + c) * 4 + 4], OH[i][:, c * 128:(c + 1) * 128], vals[:, t_, e, :],
                               e == 0 and t_ == 0 and c == 0, e == NEXP - 1 and t_ == nt - 1 and c == nch - 1, [rOH[i], rvals], [rbank[3]])
                lv = lst[:, :, 0:nch, :]
                cpv(lv, bank[3][:, 0:256].rearrange("p (e c v) -> p e c v", c=4, v=4)[:, :, 0:nch, :], [rbank[3], rvals], [rr])
                iv = idxf[:, 0:nch, :].rearrange("p c e -> p e c")
                stt(iv, lv[:, :, :, 0], 128.0, lv[:, :, :, 1], ALU.mult, ALU.add, [rr], [rr])
                cpv(idxS[s][:, 0:nch, :], idxf[:, 0:nch, :], [rr], [ridx[s]])
                tsc(idxf[:, 0:nch, :], idxf[:, 0:nch, :], float(T0 * 128), None, ALU.add, None, [rr], [rr])
                cpv(idxH[s][:, 0:nch, :], idxf[:, 0:nch, :], [rr], [ridx[s]])
                tt(wsel[s][:, 0:nch, :].rearrange("p c e -> p e c"), lv[:, :, :, 2], lv[:, :, :, 3], ALU.add, [rr], [ridx[s]])
            P.flush()
        if _DBG.get("cstop") == "2":
            return

        wg = [sb("wg%d" % i, [128, 8, D], BF16, stk) for i in range(2)]
        wu = [sb("wu%d" % i, [128, 8, D], BF16, stk) for i in range(2)]
        wd = [sb("wd%d" % i, [128, 8, D], BF16, stk) for i in range(2)]
        rwe = [Res(), Res()]
        NS = len(streams)
        xs = [sb("xs%d" % s_, [128, 4 if s_ == 0 else 1, D], BF16, stk) for s_ in range(NS)]
        rxs = [Res() for _ in range(NS)]
        xsT = [[sb("xsT%d_%d" % (s_, i), [128, 8, 512 if s_ == 0 else 32], BF16, stk) for i in range(2)] for s_ in range(NS)]
        rxsT = [[Res(), Res()] for _ in range(NS)]
        sa = [sb("sa%d" % i, [128, 512], F32, stk) for i in range(2)]
        rsa = [Res(), Res()]
        hf = [sb("hf%d" % s_, [128, 8, 512 if s_ == 0 else 32], BF16, stk) for s_ in range(NS)]
        rhf = [Res() for _ in range(NS)]
        yscs = [sb("ysc%d" % i, [128, D], F32, stk) for i in range(4)]
        ryscs = [Res() for _ in range(4)]
        ycn = {"n": 0}

        def loads(e):
            par = e % 2
            for (wt_, src) in ((wg, w_eg), (wu, w_eu), (wd, w_ed)):
                for hh in range(2):
                    dpl(wt_[par][:, hh * 4:(hh + 1) * 4, :], src[l][e][hh * 512:(hh + 1) * 512, :].rearrange("(k p) n -> p k n", p=128), [], [rwe[par]])

        def gather(e):
            for (s, tiles, cap, nch, rows) in streams:
                for c in range(nch):
                    (lambda c, s, rows, e: P.dma("pool", lambda: nc.gpsimd.indirect_dma_start(
                        out=xs[s][0:rows, c, :], out_offset=None, in_=H2,
                        in_offset=bass.IndirectOffsetOnAxis(ap=idxH[s][0:rows, c, e:e + 1], axis=0)),
                        [ridx[s]] + RH2, [rxs[s]]))(c, s, rows, e)

        def G(e):
            par = e % 2
            for (s, tiles, cap, nch, rows) in streams:
                ntok = nch * rows
                for k in range(8):
                    half = k % 2
                    with P.atomic():
                        for c in range(nch):
                            tr(bankT[:, half * 512 + c * rows: half * 512 + (c + 1) * rows], xs[s][0:rows, c, k * 128:(k + 1) * 128], identb[0:rows, 0:rows],
                               [rxs[s], rcb], [rbT[half]])
                        if k % 2 == 0:
                            cpa(xsT[s][par][:, k, 0:ntok], bankT[:, half * 512: half * 512 + ntok], [rbT[half]], [rxsT[s][par]])
                        else:
                            cpv(xsT[s][par][:, k, 0:ntok], bankT[:, half * 512: half * 512 + ntok], [rbT[half]], [rxsT[s][par]])

        sac = sb("sac", [128, 64], F32, stk)
        rsac = Res()
        yscc = sb("yscc", [128, D], F32, stk)
        ryscc = Res()

        def up(e, si):
            par = e % 2
            (s, tiles, cap, nch, rows) = streams[si]
            ntok = nch * rows
            for fc in range(8):
                if s == 0:
                    p = fc % 2
                    ba, rba, bu, rbu = bank[2 * p][:, 0:ntok], rbank[2 * p], bank[2 * p + 1][:, 0:ntok], rbank[2 * p + 1]
                    sai, rsai = sa[p][:, 0:ntok], rsa[p]
                else:
                    ba, rba, bu, rbu = bank[6][:, 0:ntok], rbank[6], bank[6][:, 64:64 + ntok], rbank[6]
                    sai, rsai = sac[:, 0:ntok], rsac
                for k in range(8):
                    mm(ba, wg[par][:, k, fc * 128:(fc + 1) * 128], xsT[s][par][:, k, 0:ntok], k == 0, k == 7, [rwe[par], rxsT[s][par]], [rba])
                for k in range(8):
                    mm(bu, wu[par][:, k, fc * 128:(fc + 1) * 128], xsT[s][par][:, k, 0:ntok], k == 0, k == 7, [rwe[par], rxsT[s][par]], [rbu])
                actf(sai, ba, AF.Silu, [rba], [rsai])
                tt(hf[s][:, fc, 0:ntok], sai, bu, ALU.mult, [rsai, rbu], [rhf[s]])

        def down(e, si):
            par = e % 2
            (s, tiles, cap, nch, rows) = streams[si]
            dst = out if s == 0 else xc
            rstream = [RS[T] for T in tiles]
            for c in range(nch):
                if s == 0:
                    ysc, rysc = yscs[ycn["n"] % 4], ryscs[ycn["n"] % 4]
                    ycn["n"] += 1
                else:
                    ysc, rysc = yscc, ryscc
                for nb in range(2):
                    if s == 0:
                        bd, rbd = bank[4 + nb], rbank[4 + nb]
                    else:
                        bd, rbd = bank[6], rbank[6]
                    for fc in range(8):
                        mm(bd[0:rows, :], hf[s][:, fc, c * rows:(c + 1) * rows], wd[par][:, fc, nb * 512:(nb + 1) * 512], fc == 0, fc == 7, [rhf[s], rwe[par]], [rbd])
                    stt(ysc[0:rows, nb * 512:(nb + 1) * 512], bd[0:rows, :], wsel[s][0:rows, c, e:e + 1], rowbcA[0:rows, s, nb * 512:(nb + 1) * 512],
                        ALU.mult, ALU.mult, [rbd, ridx[s], rrowA], [rysc])
                if _DBG.get("noscatter"):
                    continue
                (lambda c, s, rows, e, dst, rstream, ysc, rysc: P.dma("pool", lambda: nc.gpsimd.indirect_dma_start(
                    out=dst, out_offset=bass.IndirectOffsetOnAxis(ap=idxS[s][0:rows, c, e:e + 1], axis=0),
                    in_=ysc[0:rows, :], in_offset=None, compute_op=ALU.add),
                    [rysc, ridx[s]] + rstream, rstream))(c, s, rows, e, dst, rstream, ysc, rysc)

        gather(0)
        loads(0)
        G(0)
        for e in range(NEXP):
            if e + 1 < NEXP:
                gather(e + 1)
                loads(e + 1)
            chains = []
            for si in range(len(streams)):
                P.capture_start()
                up(e, si)
                down(e, si)
                chains.append(P.capture_end())
            if e + 1 < NEXP:
                P.capture_start()
                G(e + 1)
                chains.append((P.capture_end(), 0.6, 0.98))
            P.emit_zip(chains)

    for l in range(DEPTH):
        last = (l == DEPTH - 1)
        tiles_ctx = [NLAT, NLAT + 1]
        tiles_lat = list(range(NLAT))

        with contextlib.ExitStack() as ps:
            cTt = sb("cTt", [128, 8, 2], F32, ps)
            bmT = sb("bmT", [128, 48], F32, ps)
            n1T = sb("n1T", [128, 8], F32, ps)
            n2T = sb("n2T", [128, 8], F32, ps)
            wm = [sb("wm%d" % i, [128, 8, 1024], BF16, ps) for i in range(2)]
            rwm = [Res(), Res()]
            rsm = Res()
            P.dma("sp", lambda: nc.sync.dma_start(out=cTt[:], in_=cT_d.rearrange("p (k s) -> p k s", s=2)), writes=[rsm])
            P.dma("sp", lambda: nc.sync.dma_start(out=bmT[:], in_=bmT_d[l]), writes=[rsm])
            P.dma("sp", lambda: nc.sync.dma_start(out=n1T[:], in_=n1T_d[l]), writes=[rsm])
            P.dma("sp", lambda: nc.sync.dma_start(out=n2T[:], in_=n2T_d[l]), writes=[rsm])
            P.act(lambda: nc.scalar.activation(out=cact[:], in_=cTt[:], func=AF.Silu), reads=[rsm], writes=[rmod])
            for sec in range(6 if stop_after != "M1" else 0):
                i = sec % 2
                (lambda sec, i: P.dma("pool", lambda: nc.gpsimd.dma_start(
                    out=wm[i][:], in_=w_mod[l][:, sec * 1024:(sec + 1) * 1024].rearrange("(k p) n -> p k n", p=128)),
                    writes=[rwm[i]]))(sec, i)
                for j in range(8):
                    col = (sec * 8 + j) * 2
                    for k in range(8):
                        (lambda i, j, k, col: P.pe(lambda: nc.tensor.matmul(bank[2][:, col:col + 2], lhsT=wm[i][:, k, j * 128:(j + 1) * 128],
                                                                            rhs=cact[:, k, :], start=(k == 0), stop=(k == 7)),
                                                   reads=[rwm[i], rmod], writes=[rbank[2]]))(i, j, k, col)
            if stop_after in ("M1", "M2"):
                P.flush()
                break
            P.dve(lambda: nc.vector.tensor_tensor(out=modT[:], in0=bank[2][:, 0:96].rearrange("p (j s) -> p j s", s=2),
                                                  in1=bc(bmT[:], 2, 2), op=ALU.add), reads=[rbank[2], rsm], writes=[rmod])
            if stop_after == "M3":
                P.flush()
                break
            for (Ax, nT, sec) in ((A1, n1T, 1), (A2, n2T, 4)):
                (lambda Ax, nT, sec: P.dve(lambda: nc.vector.scalar_tensor_tensor(
                    out=Ax[:], in0=modT[:, sec * 8:(sec + 1) * 8, :], scalar=1.0, in1=bc(nT[:], 2, 2), op0=ALU.add, op1=ALU.mult),
                    reads=[rmod, rsm], writes=[rmod]))(Ax, nT, sec)
            P.flush()
        B1m = modT[:, 0:8, :]
        B2m = modT[:, 24:32, :]
        if stop_after == "M" and l == 0:
            break

        with contextlib.ExitStack() as pAB:
            ST = sb("ST", [128, NT, 2, 2, 128], BF16, pAB)
            rST = Res("ST")
            decbc = sb("decbc", [128, 8], F32, pAB)
            dechp = sb("dechp", [128, 4], F32, pAB)
            lgbc = sb("lgbc", [128, 8], F32, pAB)
            lghp = sb("lghp", [128, 4], F32, pAB)
            DTt = sb("DT", [128, 4, 128], F32, pAB)
            DfT = sb("DfT", [128, 2, 2, 128], F32, pAB)
            wkd = sb("wkd", [128, 2, 4], F32, pAB)
            Gam = sb("Gam", [128, 4], F32, pAB)
            e1 = sb("e1", [128, 128], F32, pAB)
            e2 = sb("e2", [128, 128], F32, pAB)
            rdec = Res("dec")
            re1 = Res()
            P.dma("sp", lambda: nc.sync.dma_start(out=decbc[:], in_=decbc_d[l]), writes=[rdec])
            P.dma("sp", lambda: nc.sync.dma_start(out=dechp[:], in_=dechp_d[l]), writes=[rdec])
            for (src, dst) in ((decbc, lgbc), (dechp, lghp)):
                (lambda src, dst: (
                    P.act(lambda: nc.scalar.activation(out=dst[:], in_=src[:], func=AF.Exp, scale=-1.0), reads=[rdec], writes=[rdec]),
                    P.dve(lambda: nc.vector.tensor_scalar(out=dst[:], in0=dst[:], scalar1=1.0, scalar2=None, op0=ALU.add), reads=[rdec], writes=[rdec]),
                    P.act(lambda: nc.scalar.activation(out=dst[:], in_=dst[:], func=AF.Ln), reads=[rdec], writes=[rdec]),
                    P.dve(lambda: nc.vector.tensor_scalar(out=dst[:], in0=dst[:], scalar1=-1.0, scalar2=None, op0=ALU.mult), reads=[rdec], writes=[rdec])))(src, dst)
            KS = 0.125
            for h in range(4):
                (lambda h: (
                    P.act(lambda: nc.scalar.activation(out=e1[:], in_=ct("PF"), func=AF.Exp, scale=lgbc[:, h:h + 1]), reads=[rdec, rct], writes=[re1]),
                    P.dve(lambda: nc.vector.tensor_tensor(out=e1[:], in0=e1[:], in1=ct("MF"), op=ALU.mult), reads=[re1, rct], writes=[re1]),
                    P.act(lambda: nc.scalar.activation(out=e2[:], in_=ct("PB"), func=AF.Exp, scale=lgbc[:, 4 + h:5 + h]), reads=[rdec, rct], writes=[re1]),
                    P.dve(lambda: nc.vector.tensor_tensor(out=e2[:], in0=e2[:], in1=ct("MB"), op=ALU.mult), reads=[re1, rct], writes=[re1]),
                    P.dve(lambda: nc.vector.tensor_tensor(out=e1[:], in0=e1[:], in1=e2[:], op=ALU.add), reads=[re1], writes=[re1]),
                    P.dve(lambda: nc.vector.tensor_scalar(out=DTt[:, h, :], in0=e1[:], scalar1=KS, scalar2=None, op0=ALU.mult), reads=[re1], writes=[rdec])))(h)
            P.act(lambda: nc.scalar.activation(out=wkd[:, 0, :], in_=lgbc[:, 0:4], func=AF.Exp, scale=ct("cjm")), reads=[rdec, rct], writes=[rdec])
            P.act(lambda: nc.scalar.activation(out=wkd[:, 1, :], in_=lgbc[:, 4:8], func=AF.Exp, scale=ct("jcol")), reads=[rdec, rct], writes=[rdec])
            P.dve(lambda: nc.vector.tensor_scalar(out=wkd[:], in0=wkd[:], scalar1=KS, scalar2=None, op0=ALU.mult), reads=[rdec], writes=[rdec])
            for dr in range(2):
                for hb in range(2):
                    (lambda dr, hb: (
                        P.act(lambda: nc.scalar.activation(out=DfT[:, dr, hb, :], in_=ct("iota1") if dr == 0 else ct("cmi"), func=AF.Exp,
                                                           scale=lghp[:, dr * 2 + hb: dr * 2 + hb + 1]), reads=[rdec, rct], writes=[rdec]),
                        P.act(lambda: nc.scalar.activation(out=Gam[:, dr * 2 + hb: dr * 2 + hb + 1], in_=lghp[:, dr * 2 + hb: dr * 2 + hb + 1],
                                                           func=AF.Exp, scale=128.0), reads=[rdec], writes=[rdec])))(dr, hb)

            with contextlib.ExitStack() as pA:
                wkv = sb("wkv", [128, 8, 768], BF16, pA)
                rwkv = Res()
                P.dma("pool", lambda: nc.gpsimd.dma_start(out=wkv[:], in_=w_in[l][:, 256:1024].rearrange("(k p) n -> p k n", p=128)), writes=[rwkv])
                UPD = sb("UPD", [128, NT, 2, 2, 128], F32, pA)
                rUPD = Res()
                hTa = [sb("hTa%d" % i, [128, 8, 128], BF16, pA) for i in range(2)]
                rhTa = [Res(), Res()]
                t1 = sb("t1a", [128, 256], F32, pA)
                t2 = sb("t2a", [128, 256], F32, pA)
                kr = sb("kra", [128, 256], F32, pA)
                rt = Res()
                kw = [sb("kwa%d" % i, [128, 2, 256], BF16, pA) for i in range(2)]
                vb = [sb("vba%d" % i, [128, 512], BF16, pA) for i in range(2)]
                rkw = [Res(), Res()]
                tilesA = tiles_ctx + tiles_lat
                t1s = [t1, sb("t1a2", [128, 256], F32, pA)]
                t2s = [t2, sb("t2a2", [128, 256], F32, pA)]
                krs = [kr, sb("kra2", [128, 256], F32, pA)]
                rts = [rt, Res()]
                abanks = [(0, 1, 2), (4, 5, 6)]

                hTa4 = [hTa[0], hTa[1], sb("hTa2", [128, 8, 128], BF16, pA), sb("hTa3", [128, 8, 128], BF16, pA)]
                rhTa4 = [rhTa[0], rhTa[1], Res(), Res()]

                def chainA1(n_):
                    T = tilesA[n_]
                    s = 0 if T < NLAT else 1
                    X, rX, XN, rXN, S, rS = norm_tile(T)
                    H = hTa4[n_ % 4]
                    transpose_mod(XN, rXN, A1, B1m, s, (lambda H: (lambda k: H[:, k, :]))(H), rhTa4[n_ % 4])

                def chainA(n_, st):
                    T = tilesA[n_]
                    ik, iv, iu = abanks[st]
                    bk, rbk, bv, rbv, bu, rbu = bank[ik], rbank[ik], bank[iv], rbank[iv], bank[iu], rbank[iu]
                    load_rope(T)
                    H, rH = hTa4[n_ % 4], rhTa4[n_ % 4]
                    for k in range(8):
                        mm(bk[:, 0:256], H[:, k, :], wkv[:, k, 0:256], k == 0, k == 7, [rH, rwkv], [rbk])
                    for k in range(8):
                        mm(bv[:, 0:512], H[:, k, :], wkv[:, k, 256:768], k == 0, k == 7, [rH, rwkv], [rbv])
                    RT, rRT = ropet[T % 2], rrope[T % 2]
                    krv = krs[st][:].rearrange("p (h d) -> p h d", d=64)
                    rope_apply(P, nc, bk[:, 0:256], rbk, RT, rRT, 4, t1s[st], t2s[st], krv, rts[st], rts[st])
                    KW, V = kw[st], vb[st]
                    for dr in range(2):
                        tt(KW[:, dr, :].rearrange("p (h d) -> p h d", d=64), krv, bc(wkd[:, dr, :], 2, 64), ALU.mult, [rts[st], rdec], [rkw[st]])
                    cpa(V[:], bv[:], [rbv], [rkw[st]])
                    for dr in range(2):
                        for hb in range(2):
                            mm(bu[:, hb * 256:(hb + 1) * 256], KW[:, dr, hb * 128:(hb + 1) * 128], V[:, hb * 256:(hb + 1) * 256], True, True,
                               [rkw[st]], [rbu])
                        for hb in range(2):
                            cpv(UPD[0:64, T, dr, hb, :], bu[0:64, hb * 256:hb * 256 + 128], [rbu], [rUPD])
                            cpa(UPD[64:128, T, dr, hb, :], bu[64:128, hb * 256 + 128:hb * 256 + 256], [rbu], [rUPD])

                chainA1(0)
                chainA1(1)
                for n_ in range(0, len(tilesA), 2):
                    chains = []
                    for st in range(2):
                        if n_ + st < len(tilesA):
                            P.capture_start()
                            chainA(n_ + st, st)
                            chains.append(P.capture_end())
                    for st in range(2):
                        if n_ + 2 + st < len(tilesA):
                            P.capture_start()
                            chainA1(n_ + 2 + st)
                            chains.append(P.capture_end())
                    P.emit_zip(chains)
                cur = [sb("cur%d" % i, [128, 2, 128], F32, pA) for i in range(2)]
                rcur = Res()
                c0, c1 = NLAT, NLAT + 1
                for dr in range(2):
                    first, second = (c0, c1) if dr == 0 else (c1, c0)
                    (lambda dr, first, second: (
                        P.dve(lambda: nc.vector.memset(ST[:, first, dr, :, :], 0.0), writes=[rST]),
                        P.dve(lambda: nc.vector.tensor_copy(out=ST[:, second, dr, :, :], in_=UPD[:, first, dr, :, :]), reads=[rUPD], writes=[rST])))(dr, first, second)
                    order = list(range(NLAT)) if dr == 0 else list(range(NLAT - 1, -1, -1))
                    for hb in range(2):
                        (lambda dr, hb, first, second: P.dve(lambda: nc.vector.scalar_tensor_tensor(
                            out=cur[0][:, hb, :], in0=UPD[:, first, dr, hb, :], scalar=Gam[:, dr * 2 + hb: dr * 2 + hb + 1],
                            in1=UPD[:, second, dr, hb, :], op0=ALU.mult, op1=ALU.add), reads=[rUPD, rdec], writes=[rcur]))(dr, hb, first, second)
                    pp = 0
                    for n in order:
                        (lambda dr, n, pp: P.act(lambda: nc.scalar.copy(out=ST[:, n, dr, :, :], in_=cur[pp][:]), reads=[rcur], writes=[rST]))(dr, n, pp)
                        for hb in range(2):
                            (lambda dr, hb, n, pp: P.dve(lambda: nc.vector.scalar_tensor_tensor(
                                out=cur[1 - pp][:, hb, :], in0=cur[pp][:, hb, :], scalar=Gam[:, dr * 2 + hb: dr * 2 + hb + 1],
                                in1=UPD[:, n, dr, hb, :], op0=ALU.mult, op1=ALU.add), reads=[rUPD, rdec, rcur], writes=[rcur]))(dr, hb, n, pp)
                        pp = 1 - pp
                P.flush()
            if stop_after == "A" and l == 0:
                break

            with contextlib.ExitStack() as pB:
                phase_B(pB, l, last, ST, rST, DTt, DfT, rdec)
                P.flush()
        if stop_after == "B" and l == 0:
            break

        with contextlib.ExitStack() as pM:
            rowbcA = sb("rowbcA", [128, 2, D], F32, pM)
            build_rowbc(rowbcA, rrowA, lambda s, c: modT[:, 16 + c, s:s + 1])
            phase_merge(pM, l, last)
            P.flush()
        if stop_after == "Bp" and l == 0:
            break

        with contextlib.ExitStack() as pC:
            rowbcA = sb("rowbcA", [128, 2, D], F32, pC)
            rowbcB = sb("rowbcB", [128, 2, D], F32, pC)
            build_rowbc(rowbcA, rrowA, lambda s, c: A2[:, c, s:s + 1])
            build_rowbc(rowbcB, rrowB, lambda s, c: modT[:, 24 + c, s:s + 1])
            phase_moe(pC, l, last)
            P.flush()
        if stop_after == "C" and l == 0:
            break

    P.flush()
    _DBG["stats"] = dict(nops=len(P.ops), nwait=P.nwait, ecount=dict(P.ecount), dmax=max(P.dcount))


def rope_apply(P, nc, src, rsrc, RT, rRT, nh, t1, t2, dst, rtmp, rdst, pool=False):
    s3 = src.rearrange("p (h d) -> p h d", d=64)
    t13 = t1[:, 0:nh * 64].rearrange("p (h d) -> p h d", d=64)
    t23 = t2[:, 0:nh * 64].rearrange("p (h d) -> p h d", d=64)
    (P.pool if pool else P.dve)(lambda: (nc.gpsimd if pool else nc.vector).tensor_tensor(out=t13, in0=s3, in1=bc(RT[:, 0:64], 1, nh), op=ALU.mult), reads=[rsrc, rRT], writes=[rtmp])
    (P.pool if pool else P.dve)(lambda: (nc.gpsimd if pool else nc.vector).tensor_tensor(out=t23[:, :, 0:32], in0=s3[:, :, 32:64], in1=bc(RT[:, 64:96], 1, nh), op=ALU.mult),
          reads=[rsrc, rRT], writes=[rtmp])
    (P.pool if pool else P.dve)(lambda: (nc.gpsimd if pool else nc.vector).tensor_tensor(out=t23[:, :, 32:64], in0=s3[:, :, 0:32], in1=bc(RT[:, 96:128], 1, nh), op=ALU.mult),
          reads=[rsrc, rRT], writes=[rtmp])
    (P.pool if pool else P.dve)(lambda: (nc.gpsimd if pool else nc.vector).tensor_tensor(out=dst, in0=t13, in1=t23, op=ALU.add), reads=[rtmp], writes=[rdst])


def _host_inputs(inp):
    f32 = np.float32
    g = lambda k: np.ascontiguousarray(np.asarray(inp[k], dtype=f32))
    x, c, ctx, c_ctx = g("x"), g("c"), g("ctx"), g("c_ctx")
    B = x.shape[0]
    shared = {}
    shared["w_mod"] = g("w_mod")
    shared["bmT"] = np.ascontiguousarray(g("b_mod").reshape(DEPTH, 48, 128).transpose(0, 2, 1))
    shared["n1T"] = np.ascontiguousarray(g("norm1_g").reshape(DEPTH, 8, 128).transpose(0, 2, 1))
    shared["n2T"] = np.ascontiguousarray(g("norm2_g").reshape(DEPTH, 8, 128).transpose(0, 2, 1))
    shared["w_in"] = g("w_in")
    df, db = g("ret_decay_fwd"), g("ret_decay_bwd")
    decbc = np.concatenate([df, db], axis=1)
    shared["decbc"] = np.ascontiguousarray(np.broadcast_to(decbc[:, None, :], (DEPTH, 128, 8)))
    hp = np.zeros((DEPTH, 128, 4), f32)
    for dr, dd in enumerate((df, db)):
        for hb in range(2):
            hp[:, 0:64, dr * 2 + hb] = dd[:, 2 * hb][:, None]
            hp[:, 64:128, dr * 2 + hb] = dd[:, 2 * hb + 1][:, None]
    shared["dechp"] = hp
    shared["sgu_wT"] = np.ascontiguousarray(g("sgu_w").transpose(0, 1, 3, 2))
    shared["sgu_bbc"] = np.ascontiguousarray(np.broadcast_to(g("sgu_b").reshape(DEPTH, 1, 512), (DEPTH, 128, 512)))
    shared["qg"] = np.ascontiguousarray(np.broadcast_to(g("q_norm_g")[:, None, :], (DEPTH, 128, 64)))
    shared["kg"] = np.ascontiguousarray(np.broadcast_to(g("k_norm_g")[:, None, :], (DEPTH, 128, 64)))
    shared["sink"] = np.ascontiguousarray(np.broadcast_to(g("attn_sink")[:, None, :], (DEPTH, 128, 8)))
    for k in ("w_branch", "w_out", "w_router", "w_exp_gate", "w_exp_up", "w_exp_down"):
        shared[k] = g(k)
    shared["rope"] = _rope_tables()
    shared["ctab"] = _const_table()
    maps = []
    for b in range(B):
        m = dict(shared)
        m["x"] = x[b]
        m["ctxin"] = ctx[b]
        cT = np.stack([c[b].reshape(8, 128).T, c_ctx.reshape(8, 128).T], axis=-1)
        m["cT"] = np.ascontiguousarray(cT.reshape(128, 16))
        maps.append(m)
    return maps


_NC_CACHE = {}


def kernel(**inputs):
    maps = _host_inputs(inputs)
    if "nc" not in _NC_CACHE:
        _NC_CACHE["nc"] = build_program()
    nc = _NC_CACHE["nc"]
    n = len(maps)
    res = run_bass_kernel_spmd(nc, maps, core_ids=list(range(n)))
    return np.stack([np.asarray(r["out"], dtype=np.float32) for r in res.results], axis=0)
```

```python
import contextlib
import numpy as np
import concourse.bass as bass
import concourse.mybir as mybir
from concourse.bass_utils import run_bass_kernel_spmd

F32 = mybir.dt.float32
BF16 = mybir.dt.bfloat16
I32 = mybir.dt.int32
AF = mybir.ActivationFunctionType
ALU = mybir.AluOpType
AX = mybir.AxisListType

D = 1024
SEQ = 4096
CTX = 256
NLAT = 32
NT = 34
DEPTH = 2
NEXP = 16
EPS = 1e-6
INW = 6400
NG = 3328

_DBG = {}


class Res:
    __slots__ = ("name", "w", "r", "excl")

    def __init__(self, name="", excl=False):
        self.name = name
        self.w = None
        self.r = []
        self.excl = excl


class _Op:
    __slots__ = ("eng", "fn", "reads", "writes", "dma", "deps", "inc", "val", "semid")

    def __init__(self, eng, fn, reads, writes, dma):
        self.eng = eng
        self.fn = fn
        self.reads = reads
        self.writes = writes
        self.dma = dma
        self.deps = ()
        self.inc = False
        self.val = 0
        self.semid = None


class Prog:
    NDMASEM = 64
    NHW = 32

    def __init__(self, nc, es):
        self.nc = nc
        self.ops = []
        self.done = 0
        self.engs = {"pe": nc.tensor, "act": nc.scalar, "dve": nc.vector,
                     "pool": nc.gpsimd, "sp": nc.sync}
        self.sems = {e: es.enter_context(nc.semaphore("sem_" + e)) for e in self.engs}
        self.dsems = [es.enter_context(nc.semaphore("dsem%d" % i)) for i in range(self.NDMASEM)]
        self.dlast = [None] * self.NDMASEM
        self.drr = 0
        self.drr_sw = 0
        self.ecount = {e: 0 for e in self.engs}
        self.dcount = [0] * self.NDMASEM
        self.waited = {e: {} for e in self.engs}
        self.nwait = 0
        self._cap = None
        self._unit = None

    def add(self, eng, fn, reads=(), writes=(), dma=False):
        reads = tuple(reads)
        writes = tuple(writes)
        ex = tuple(r for r in reads if r.excl)
        if ex:
            reads = tuple(r for r in reads if not r.excl)
            writes = writes + ex
        op = _Op(eng, fn, reads, writes, dma)
        if self._cap is not None:
            if self._unit is not None:
                self._unit.append(op)
            else:
                self._cap.append([op])
        else:
            self.ops.append(op)

    def capture_start(self):
        self._cap = []
        self._unit = None

    def capture_end(self):
        cap = self._cap
        self._cap = None
        return cap

    @contextlib.contextmanager
    def atomic(self):
        if self._cap is None or self._unit is not None:
            yield
            return
        self._unit = []
        try:
            yield
        finally:
            self._cap.append(self._unit)
            self._unit = None

    def emit_zip(self, chains):
        items = []
        for ci, ch in enumerate(chains):
            lo, hi = 0.0, 1.0
            if isinstance(ch, tuple):
                ch, lo, hi = ch
            n = len(ch)
            for ui, unit in enumerate(ch):
                items.append((lo + (hi - lo) * (ui + 0.5) / n, ci, ui, unit))
        items.sort(key=lambda t: (t[0], t[1]))
        for _, _, _, unit in items:
            self.ops.extend(unit)

    def pe(self, fn, reads=(), writes=()):
        self.add("pe", fn, reads, writes)

    def act(self, fn, reads=(), writes=()):
        self.add("act", fn, reads, writes)

    def dve(self, fn, reads=(), writes=()):
        self.add("dve", fn, reads, writes)

    def pool(self, fn, reads=(), writes=()):
        self.add("pool", fn, reads, writes)

    def dma(self, eng, fn, reads=(), writes=()):
        self.add(eng, fn, reads, writes, dma=True)

    def flush(self):
        ops = self.ops
        base = self.done
        n = len(ops)
        last_compute = {}
        for i in range(base, n):
            op = ops[i]
            raw = set()
            war = set()
            for r in op.reads:
                if r.w is not None:
                    raw.add(r.w)
            for w in op.writes:
                if w.w is not None:
                    raw.add(w.w)
                war.update(w.r)
            if op.dma:
                if op.eng == "pool":
                    s = self.NHW + self.drr_sw
                    self.drr_sw = (self.drr_sw + 1) % (self.NDMASEM - self.NHW)
                else:
                    s = self.drr
                    self.drr = (self.drr + 1) % self.NHW
                op.semid = s
                if self.dlast[s] is not None:
                    raw.add(self.dlast[s])
                self.dlast[s] = i
            else:
                last_compute[op.eng] = i
            deps = set()
            for j in raw:
                if j < base or j == i:
                    continue
                o = ops[j]
                if (not o.dma) and (not op.dma) and o.eng == op.eng and op.eng == "pe":
                    continue
                deps.add(j)
            for j in war:
                if j < base or j == i:
                    continue
                o = ops[j]
                if (not o.dma) and o.eng == op.eng and not op.dma and op.eng == "pe":
                    continue
                deps.add(j)
            for r in op.reads:
                r.r.append(i)
            for w in op.writes:
                w.w = i
                w.r = []
            best = {}
            for j in deps:
                o = ops[j]
                key = ("d", o.semid) if o.dma else ("e", o.eng)
                if key not in best or best[key] < j:
                    best[key] = j
            op.deps = tuple(best.values())
            for j in op.deps:
                ops[j].inc = True
        for e, i in last_compute.items():
            ops[i].inc = True
        for i in range(base, n):
            op = ops[i]
            if op.dma:
                self.dcount[op.semid] += 16
                op.val = self.dcount[op.semid]
            elif op.inc:
                self.ecount[op.eng] += 1
                op.val = self.ecount[op.eng]
        for i in range(base, n):
            op = ops[i]
            eng = self.engs[op.eng]
            wt = self.waited[op.eng]
            for j in op.deps:
                o = ops[j]
                if o.dma:
                    key, sem = ("d", o.semid), self.dsems[o.semid]
                else:
                    key, sem = ("e", o.eng), self.sems[o.eng]
                if wt.get(key, 0) >= o.val:
                    continue
                eng.wait_ge(sem, o.val)
                self.nwait += 1
                wt[key] = o.val
            inst = op.fn()
            if op.dma:
                inst.then_inc(self.dsems[op.semid], 16)
            elif op.inc:
                inst.then_inc(self.sems[op.eng], 1)
            op.fn = None
        for e, eng in self.engs.items():
            wt = self.waited[e]
            for x in self.engs:
                if x == e:
                    continue
                v = self.ecount[x]
                if v > wt.get(("e", x), 0):
                    eng.wait_ge(self.sems[x], v)
                    wt[("e", x)] = v
            for s in range(self.NDMASEM):
                v = self.dcount[s]
                if v > wt.get(("d", s), 0):
                    eng.wait_ge(self.dsems[s], v)
                    wt[("d", s)] = v
        self.done = n


def bc(ap, axis, n):
    s = list(ap.shape)
    s.insert(axis, n)
    return ap.unsqueeze(axis).to_broadcast(s)


def _rope_tables():
    f32 = np.float32
    rows = SEQ // 64
    row = np.repeat(np.arange(rows), 64).astype(f32)
    col = np.tile(np.arange(64), rows).astype(f32)
    nf = 16
    inv = np.power(f32(10000.0), -np.arange(nf, dtype=f32) / f32(nf)).astype(f32)
    ang = np.concatenate([row[:, None] * inv, col[:, None] * inv], axis=-1).astype(f32)
    cos = np.cos(ang).astype(f32)
    sin = np.sin(ang).astype(f32)
    tab = np.zeros((NT * 128, 128), f32)
    tab[:SEQ, 0:32] = cos
    tab[:SEQ, 32:64] = cos
    tab[:SEQ, 64:96] = -sin
    tab[:SEQ, 96:128] = sin
    tab[SEQ:, 0:64] = 1.0
    return tab.reshape(NT, 128, 128)


CT = {}


def _const_table():
    cols = []

    def put(name, arr):
        arr = np.asarray(arr, np.float32).reshape(128, -1)
        CT[name] = (sum(c.shape[1] for c in cols), arr.shape[1])
        cols.append(arr)

    j = np.arange(128)[:, None]
    i = np.arange(128)[None, :]
    put("ident", (j == i))
    put("ones", np.ones((128, 128)))
    put("PF", np.maximum(i - j, 0))
    put("PB", np.maximum(j - i, 0))
    put("MF", (i >= j))
    put("MB", (j > i))
    put("iota1", np.broadcast_to(i + 1, (128, 128)))
    put("cmi", np.broadcast_to(128 - i, (128, 128)))
    put("cjm", 127 - j)
    put("jcol", j)
    put("tri", (j < i))
    put("iota512", np.broadcast_to(np.arange(512)[None, :], (128, 512)))
    mp = np.where(j >= i, 0.0, -30000.0)
    mn = np.where(j <= i, 0.0, -30000.0)
    put("mprev", np.tile(mp, (1, 4)))
    put("mnext", np.tile(mn, (1, 4)))
    put("ttv", np.broadcast_to(np.arange(NLAT)[None, :], (128, NLAT)))
    put("eps", np.full((128, 1), EPS))
    return np.concatenate(cols, axis=1)


def build_program(stop_after=None, dbg_out=False):
    nc = bass.Bass("TRN2", target_bir_lowering=False)
    es = contextlib.ExitStack()
    with es:
        _build(nc, es, stop_after, dbg_out)
    return nc


def _build(nc, es, stop_after, dbg_out):
    ctab_np = _const_table()
    NCT = ctab_np.shape[1]

    def din(name, shape, dt=F32):
        return nc.dram_tensor(name, list(shape), dt, kind="ExternalInput").ap()

    x_in = din("x", [SEQ, D])
    ctx_in = din("ctxin", [CTX, D])
    cT_d = din("cT", [128, 16])
    w_mod = din("w_mod", [DEPTH, D, 6 * D])
    bmT_d = din("bmT", [DEPTH, 128, 48])
    n1T_d = din("n1T", [DEPTH, 128, 8])
    n2T_d = din("n2T", [DEPTH, 128, 8])
    w_in = din("w_in", [DEPTH, D, INW])
    decbc_d = din("decbc", [DEPTH, 128, 8])
    dechp_d = din("dechp", [DEPTH, 128, 4])
    swT_d = din("sgu_wT", [DEPTH, 4, 128, 128])
    sbb_d = din("sgu_bbc", [DEPTH, 128, 512])
    qg_d = din("qg", [DEPTH, 128, 64])
    kg_d = din("kg", [DEPTH, 128, 64])
    sink_d = din("sink", [DEPTH, 128, 8])
    w_br = din("w_branch", [DEPTH, 3, 512, D])
    w_out = din("w_out", [DEPTH, D, D])
    w_rt = din("w_router", [DEPTH, D, NEXP])
    w_eg = din("w_exp_gate", [DEPTH, NEXP, D, D])
    w_eu = din("w_exp_up", [DEPTH, NEXP, D, D])
    w_ed = din("w_exp_down", [DEPTH, NEXP, D, D])
    rope_d = din("rope", [NT, 128, 128])
    ctab_d = din("ctab", [128, NCT])
    out = nc.dram_tensor("out", [SEQ, D], F32, kind="ExternalOutput").ap()
    xc = nc.dram_tensor("xc", [CTX, D], F32, kind="ExternalOutput").ap()
    BR = nc.dram_tensor("BR", [3, 512, NT * 128], BF16, kind="ExternalOutput").ap()
    H2 = nc.dram_tensor("H2", [NT * 128, D], BF16, kind="ExternalOutput").ap()

    P = Prog(nc, es)

    _uid = [0]

    def sb(name, shape, dt, stack=es):
        _uid[0] += 1
        return stack.enter_context(nc.sbuf_tensor("%s_%d" % (name, _uid[0]), list(shape), dt))

    bank = [es.enter_context(nc.psum_tensor("bank%d" % i, [128, 512], F32)) for i in range(7)]
    bankT = es.enter_context(nc.psum_tensor("bankT", [128, 1024], BF16))
    rbank = [Res("bank%d" % i, excl=True) for i in range(7)]
    _rbT = Res("bankT", excl=True)
    rbT = [_rbT, _rbT]

    RS = [Res("S%d" % t) for t in range(NT)]
    RBR = [[Res() for _ in range(NT)] for _ in range(3)]
    RH2 = [Res() for _ in range(NT)]

    def srows(T):
        return out[T * 128:(T + 1) * 128, :] if T < NLAT else xc[(T - NLAT) * 128:(T - NLAT + 1) * 128, :]

    ctab = sb("ctab", [128, NCT], F32)
    rct = Res("ctab")
    P.dma("sp", lambda: nc.sync.dma_start(out=ctab[:], in_=ctab_d), writes=[rct])

    def ct(name):
        o, n = CT[name]
        return ctab[:, o:o + n]

    identb = sb("identb", [128, 128], BF16)
    onesb = sb("onesb", [128, 128], BF16)
    trib = sb("trib", [128, 128], BF16)
    mprevb = sb("mprevb", [128, 512], BF16)
    mnextb = sb("mnextb", [128, 512], BF16)
    rcb = Res("constb")
    for dst, nm in ((identb, "ident"), (onesb, "ones"), (trib, "tri"), (mprevb, "mprev"), (mnextb, "mnext")):
        (lambda dst, nm: P.dve(lambda: nc.vector.tensor_copy(out=dst[:], in_=ct(nm)), reads=[rct], writes=[rcb]))(dst, nm)
    epsc = ct("eps")
    negh = sb("negh", [128, 8], F32)
    rnegh = Res("negh")
    P.dve(lambda: nc.vector.memset(negh[:], -0.5), writes=[rnegh])

    for q in range(8):
        (lambda q: P.dma("sp", lambda: nc.sync.dma_start(out=out[q * 512:(q + 1) * 512, :], in_=x_in[q * 512:(q + 1) * 512, :]),
                         writes=RS[q * 4:(q + 1) * 4]))(q)
    P.dma("sp", lambda: nc.sync.dma_start(out=xc, in_=ctx_in), writes=RS[NLAT:NT])

    cact = sb("cact", [128, 8, 2], BF16)
    modT = sb("modT", [128, 48, 2], F32)
    A1 = sb("A1", [128, 8, 2], F32)
    A2 = sb("A2", [128, 8, 2], F32)
    rmod = Res("mod")
    rowbcA = None
    rowbcB = None
    rrowA = Res("rowA")
    rrowB = Res("rowB")
    dgt = [sb("dgt%d" % i, [128, 128], F32) for i in range(2)]
    rdg = [Res(), Res()]

    xt = [sb("xt%d" % i, [128, D], F32) for i in range(2)]
    rxt = [Res(), Res()]
    sqj = sb("sqj", [128, D], F32)
    rsq = Res()
    ssum = [sb("ssum%d" % i, [128, 4], F32) for i in range(2)]
    rss = [Res(), Res()]
    xn = [sb("xn%d" % i, [128, D], BF16) for i in range(2)]
    rxn = [Res(), Res()]
    ropet = [sb("ropet%d" % i, [128, 128], F32) for i in range(2)]
    rrope = [Res(), Res()]
    cnt = {"tile": 0, "ev": 0}

    def load_rope(T):
        i = T % 2
        P.dma("sp", lambda: nc.sync.dma_start(out=ropet[i][:], in_=rope_d[T]), writes=[rrope[i]])
        return ropet[i], rrope[i]

    def norm_tile(T, want_x=False):
        i = cnt["tile"] % 2
        cnt["tile"] += 1
        X, rX, S, rS, XN, rXN = xt[i], rxt[i], ssum[i], rss[i], xn[i], rxn[i]
        P.dma("sp", lambda: nc.sync.dma_start(out=X[:], in_=srows(T)), reads=[RS[T]], writes=[rX])
        P.dve(lambda: nc.vector.memset(S[:], 0.0), writes=[rS])
        P.act(lambda: nc.scalar.activation(out=sqj[:], in_=X[:], func=AF.Square, accum_out=S[:, 0:1]),
              reads=[rX], writes=[rsq, rS])
        P.dve(lambda: nc.vector.tensor_scalar(out=S[:, 1:2], in0=S[:, 0:1], scalar1=1.0 / D, scalar2=EPS, op0=ALU.mult, op1=ALU.add),
              reads=[rS], writes=[rS])
        P.pool(lambda: nc.gpsimd.tensor_tensor(out=S[:, 2:3], in0=S[:, 1:2], in1=negh[:, 0:1], op=ALU.pow), reads=[rS, rnegh], writes=[rS])
        P.dve(lambda: nc.vector.tensor_scalar(out=XN[:], in0=X[:], scalar1=S[:, 2:3], scalar2=None, op0=ALU.mult),
              reads=[rX, rS], writes=[rXN])
        return X, rX, XN, rXN, S, rS

    def transpose_mod(XN, rXN, A, Bm, s, dst_fn, rdst):
        for half in range(2):
          with P.atomic():
            rb = rbT[half]
            for kk in range(4):
                k = half * 4 + kk
                (lambda k, kk, half: P.pe(lambda: nc.tensor.transpose(out=bankT[:, half * 512 + kk * 128: half * 512 + (kk + 1) * 128],
                                                                       in_=XN[:, k * 128:(k + 1) * 128], identity=identb[:]),
                                          reads=[rXN, rcb], writes=[rb]))(k, kk, half)
            for kk in range(4):
                k = half * 4 + kk
                src = bankT[:, half * 512 + kk * 128: half * 512 + (kk + 1) * 128]
                if kk % 2 == 0:
                    (lambda k, src: P.act(lambda: nc.scalar.activation(out=dst_fn(k), in_=src, func=AF.Identity,
                                                                       scale=A[:, k, s:s + 1], bias=Bm[:, k, s:s + 1]),
                                          reads=[rb, rmod], writes=[rdst]))(k, src)
                else:
                    (lambda k, src: P.dve(lambda: nc.vector.tensor_scalar(out=dst_fn(k), in0=src, scalar1=A[:, k, s:s + 1],
                                                                          scalar2=Bm[:, k, s:s + 1], op0=ALU.mult, op1=ALU.add),
                                          reads=[rb, rmod], writes=[rdst]))(k, src)

    def build_rowbc(dst, rdst, col_fn):
        n = 0
        for s in range(2):
            for hb in range(2):
                b = bank[hb]
                rb = rbank[hb]
                for cc in range(4):
                    c = hb * 4 + cc
                    g = n % 2
                    n += 1
                    (lambda c, cc, g, s, b, rb: (
                        P.dve(lambda: nc.vector.tensor_scalar(out=dgt[g][:], in0=ct("ident"), scalar1=col_fn(s, c), scalar2=None, op0=ALU.mult),
                              reads=[rct, rmod], writes=[rdg[g]]),
                        P.pe(lambda: nc.tensor.matmul(b[:, cc * 128:(cc + 1) * 128], lhsT=ct("ones"), rhs=dgt[g][:], start=True, stop=True),
                             reads=[rdg[g], rct], writes=[rb])))(c, cc, g, s, b, rb)
                (lambda s, hb, b, rb: P.act(lambda: nc.scalar.copy(out=dst[:, s, hb * 512:(hb + 1) * 512], in_=b[:]),
                                            reads=[rb], writes=[rdst]))(s, hb, b, rb)

    def mm(o, lhsT, rhs, start, stop, reads, writes, tp=None):
        kw = {} if (tp is None or _DBG.get("notp")) else {"tile_position": tp}
        P.pe(lambda: nc.tensor.matmul(o, lhsT=lhsT, rhs=rhs, start=start, stop=stop, **kw), reads, writes)

    def tr(o, in_, ident, reads, writes):
        P.pe(lambda: nc.tensor.transpose(out=o, in_=in_, identity=ident), reads, writes)

    def actf(o, in_, func, reads, writes, **kw):
        P.act(lambda: nc.scalar.activation(out=o, in_=in_, func=func, **kw), reads, writes)

    def cpa(o, in_, reads, writes):
        P.act(lambda: nc.scalar.copy(out=o, in_=in_), reads, writes)

    def cpv(o, in_, reads, writes, eng="dve"):
        if eng == "POOL":
            eng = "pool"
        elif not _DBG.get("usepool"):
            eng = "dve"
        e = nc.vector if eng == "dve" else nc.gpsimd
        P.add(eng, lambda: e.tensor_copy(out=o, in_=in_), reads, writes)

    def tt(o, a, b, op, reads, writes, eng="dve"):
        if eng == "POOL":
            eng = "pool"
        elif not _DBG.get("usepool"):
            eng = "dve"
        e = nc.vector if eng == "dve" else nc.gpsimd
        P.add(eng, lambda: e.tensor_tensor(out=o, in0=a, in1=b, op=op), reads, writes)

    def tsc(o, a, s1, s2, op0, op1, reads, writes, eng="dve"):
        if eng == "POOL":
            eng = "pool"
        elif not _DBG.get("usepool"):
            eng = "dve"
        e = nc.vector if eng == "dve" else nc.gpsimd
        if op1 is None:
            P.add(eng, lambda: e.tensor_scalar(out=o, in0=a, scalar1=s1, scalar2=None, op0=op0), reads, writes)
        else:
            P.add(eng, lambda: e.tensor_scalar(out=o, in0=a, scalar1=s1, scalar2=s2, op0=op0, op1=op1), reads, writes)

    def stt(o, a, s, b, op0, op1, reads, writes, eng="dve"):
        if eng == "POOL":
            eng = "pool"
        elif not _DBG.get("usepool"):
            eng = "dve"
        e = nc.vector if eng == "dve" else nc.gpsimd
        P.add(eng, lambda: e.scalar_tensor_tensor(out=o, in0=a, scalar=s, in1=b, op0=op0, op1=op1), reads, writes)

    def mset(o, v, writes, eng="dve"):
        if eng == "POOL":
            eng = "pool"
        elif not _DBG.get("usepool"):
            eng = "dve"
        e = nc.vector if eng == "dve" else nc.gpsimd
        P.add(eng, lambda: e.memset(o, v), (), writes)

    def dsp(o, in_, reads, writes):
        P.dma("sp", lambda: nc.sync.dma_start(out=o, in_=in_), reads, writes)

    def dpl(o, in_, reads, writes):
        P.dma("pool", lambda: nc.gpsimd.dma_start(out=o, in_=in_), reads, writes)

    def rsqrt_chain(dst_sd, dst_r, src, scale, reads_res, wres):
        n, w = src.shape[0], src.shape[1]
        tsc(dst_sd, src, scale, EPS, ALU.mult, ALU.add, [reads_res], [wres])
        P.pool(lambda: nc.gpsimd.tensor_tensor(out=dst_r, in0=dst_sd, in1=negh[0:n, 0:w], op=ALU.pow), [wres, rnegh], [wres])

    def kwview(w):
        return w.rearrange("(k p) n -> p k n", p=128)

    def phase_B(stk, l, last, ST, rST, DTt, DfT, rdec):
        wng = sb("wng", [128, 8, NG], BF16, stk)
        rwng = Res()
        for q in range(4):
            c0, c1 = q * 832, (q + 1) * 832
            dpl(wng[:, :, c0:c1], kwview(w_in[l][:, c0:c1]), [], [rwng])
        wsT = sb("wsT", [128, 4, 128], BF16, stk)
        sbb = sb("sbb", [128, 512], F32, stk)
        qg = sb("qg", [128, 64], F32, stk)
        kg = sb("kg", [128, 64], F32, stk)
        esink = sb("esink", [128, 8], F32, stk)
        rsm = Res()
        dpl(wsT[:], swT_d[l].rearrange("g j i -> j g i"), [], [rsm])
        dsp(sbb[:], sbb_d[l], [], [rsm])
        dsp(qg[:], qg_d[l], [], [rsm])
        dsp(kg[:], kg_d[l], [], [rsm])
        dsp(esink[:], sink_d[l], [], [rsm])
        tsc(qg[:], qg[:], 0.125, None, ALU.mult, None, [rsm], [rsm])
        actf(esink[:], esink[:], AF.Exp, [rsm], [rsm])
        KT_all = sb("KT_all", [128, NT, 128], BF16, stk)
        V_all = sb("V_all", [128, NT, 2, 80], BF16, stk)
        rKV = [Res() for _ in range(NT)]
        mset(V_all[:], 1.0, rKV)
        hT = [sb("hTb%d" % i, [128, 8, 128], BF16, stk) for i in range(2)]
        rhT = [Res(), Res()]
        zqk = [sb("zqk%d" % i, [128, 512], F32, stk) for i in range(2)]
        vb = [sb("vb%d" % i, [128, 512], BF16, stk) for i in range(2)]
        sg = [sb("sg%d" % i, [128, 512], BF16, stk) for i in range(2)]
        uT = [sb("uT%d" % i, [128, 512], BF16, stk) for i in range(2)]
        gv = [sb("gv%d" % i, [128, 512], F32, stk) for i in range(2)]
        zq = [sb("zq%d" % i, [128, 512], F32, stk) for i in range(2)]
        zk = [sb("zk%d" % i, [128, 128], F32, stk) for i in range(2)]
        rzqk, rvb, rsg, ruT, rgv, rzq, rzk = [[Res(), Res()] for _ in range(7)]
        t1 = sb("t1", [128, 512], F32, stk)
        t2 = sb("t2", [128, 512], F32, stk)
        rtmp = Res()
        t1a = sb("t1a", [128, 512], F32, stk)
        t2a = sb("t2a", [128, 512], F32, stk)
        rtmpa = Res()
        qrb = sb("qrb", [128, 256], BF16, stk)
        krb = sb("krb", [128, 256], BF16, stk)
        rqk = Res()
        qT = sb("qT", [128, 2, 128], BF16, stk)
        qfT = sb("qfT", [128, 2, 2, 128], BF16, stk)
        kT = sb("kT", [128, 2, 128], BF16, stk)
        rqT = Res()
        SD = sb("SD", [128, 512], BF16, stk)
        rSD = Res()
        bst = sb("bst", [128, 4, 6], F32, stk)
        mv = sb("mv", [128, 4, 2], F32, stk)
        sdr = sb("sdr", [128, 8], F32, stk)
        rln = Res()
        yb = sb("yb", [128, 512], BF16, stk)
        retb = sb("retb", [128, 512], BF16, stk)
        ryb = Res()
        brT = [sb("brTo%d" % i, [128, 4, 128], BF16, stk) for i in range(3)]
        rbrT = [Res(), Res(), Res()]
        vn = sb("vn", [128, 512], BF16, stk)
        stmp = sb("stmp", [128, 512], F32, stk)
        rsg2 = Res()
        bst2 = sb("bst2", [128, 6], F32, stk)
        mv2 = sb("mv2", [128, 4], F32, stk)
        sq = sb("sq", [128, 512], F32, stk)
        qn = sb("qn", [128, 512], F32, stk)
        ssq = sb("ssq", [128, 24], F32, stk)
        ratt = Res()
        sqk = sb("sqk", [128, 128], F32, stk)
        kn = sb("kn", [128, 128], F32, stk)
        ssk = sb("ssk", [128, 8], F32, stk)
        rattk = Res()
        qperm = sb("qperm", [128, 4, 2, 64], BF16, stk)
        krb2 = sb("krb2", [128, 128], BF16, stk)
        qT2 = [sb("qT2_%d" % i, [128, 4, 128], BF16, stk) for i in range(2)]
        rqT2 = [Res(), Res()]
        PT = [sb("PT%d" % i, [128, 512], BF16, stk) for i in range(2)]
        rPT = [Res(), Res()]
        den = sb("den", [128, 16], F32, stk)
        attb = sb("attb", [128, 512], BF16, stk)
        rden = Res()
        BRv = [BR[i].rearrange("(c p) t -> p c t", p=128) for i in range(3)]
        tg = sb("tg", [128, 512], F32, stk)
        rtg = Res()
        pbc = {"n": 0}

        def pbank():
            i = pbc["n"] % 2
            pbc["n"] += 1
            return bank[i], rbank[i]

        def out_branch(i, T, srcb, rsrc):
            with P.atomic():
                for c in range(4):
                    tr(bankT[:, 512 + c * 128: 512 + (c + 1) * 128], srcb[:, c * 128:(c + 1) * 128], identb[:], [rsrc, rcb], [rbT[1]])
                cpa(brT[i][:], bankT[:, 512:1024].rearrange("p (c t) -> p c t", t=128), [rbT[1]], [rbrT[i]])
            dsp(BRv[i][:, :, T * 128:(T + 1) * 128], brT[i][:], [rbrT[i]], [RBR[i][T]])

        def Pst1(T, par):
            s = 0 if T < NLAT else 1
            X, rX, XN, rXN, S, rS = norm_tile(T)
            H, rH = hT[par], rhT[par]
            transpose_mod(XN, rXN, A1, B1m, s, lambda k: H[:, k, :], rH)

        def Pst(T, full, par):
            load_rope(T)
            H, rH = hT[par], rhT[par]

            def proj(c0, c1):
                b, rb = pbank()
                for k in range(8):
                    mm(b[:, 0:c1 - c0], H[:, k, :], wng[:, k, c0:c1], k == 0, k == 7, [rH, rwng], [rb])
                return b, rb

            if full:
                b, rb = proj(0, 512)
                cpa(zqk[par][:], b[:], [rb], [rzqk[par]])
                b, rb = proj(512, 1024)
                cpv(vb[par][:], b[:], [rb], [rvb[par]])
                b, rb = proj(1024, 1536)
                actf(tg[:], b[:], AF.Tanh, [rb], [rtg], scale=0.5)
                stt(sg[par][:], tg[:], 1.0, b[:], ALU.add, ALU.mult, [rtg, rb], [rsg[par]])
                b, rb = pbank()
                for c in range(4):
                    for k in range(8):
                        mm(b[:, c * 128:(c + 1) * 128], wng[:, k, 1536 + c * 128:1536 + (c + 1) * 128], H[:, k, :], k == 0, k == 7, [rH, rwng], [rb])
                actf(uT[par][:], b[:], AF.Gelu_apprx_tanh, [rb], [ruT[par]])
                b, rb = proj(2048, 2560)
                actf(gv[par][:], b[:], AF.Gelu_apprx_tanh, [rb], [rgv[par]])
                b, rb = proj(2560, 3072)
                cpv(zq[par][:], b[:], [rb], [rzq[par]])
            b, rb = proj(3072, 3328)
            cpv(zk[par][:], b[:, 0:128], [rb], [rzk[par]])
            cpa(V_all[:, T, :, 0:64], b[:, 128:256].rearrange("p (k d) -> p k d", d=64), [rb], [rKV[T]])

        def chainR(T, par):
            RT, rRT = ropet[T % 2], rrope[T % 2]
            if True:
                rope_apply(P, nc, zqk[par][:, 0:256], rzqk[par], RT, rRT, 4, t1, t2, qrb[:].rearrange("p (h d) -> p h d", d=64), rtmp, rqk)
                rope_apply(P, nc, zqk[par][:, 256:512], rzqk[par], RT, rRT, 4, t1, t2, krb[:].rearrange("p (h d) -> p h d", d=64), rtmp, rqk)
                with P.atomic():
                    for hb in range(2):
                        tr(bankT[:, hb * 128:(hb + 1) * 128], qrb[:, hb * 128:(hb + 1) * 128], identb[:], [rqk, rcb], [rbT[0]])
                        tr(bankT[:, 256 + hb * 128:256 + (hb + 1) * 128], krb[:, hb * 128:(hb + 1) * 128], identb[:], [rqk, rcb], [rbT[0]])
                    qv = bankT[:, 0:256].rearrange("p (b t) -> p b t", t=128)
                    cpa(qT[:], qv, [rbT[0]], [rqT])
                    for dr in range(2):
                        tt(qfT[:, dr, :, :], qv, DfT[:, dr, :, :], ALU.mult, [rbT[0], rdec], [rqT])
                    cpa(kT[:], bankT[:, 256:512].rearrange("p (b t) -> p b t", t=128), [rbT[0]], [rqT])
                for h in range(4):
                    hb, hl = h // 2, h % 2
                    bS = 2 + hl
                    mm(bank[bS][:, hb * 128:(hb + 1) * 128], kT[64 * hl:64 * hl + 64, hb, :], qT[64 * hl:64 * hl + 64, hb, :], True, True,
                       [rqT], [rbank[bS]], tp=(64 * hl, 0))
                for h in range(4):
                    hb, hl = h // 2, h % 2
                    bS = 2 + hl
                    tt(SD[:, h * 128:(h + 1) * 128], bank[bS][:, hb * 128:(hb + 1) * 128], DTt[:, h, :], ALU.mult, [rbank[bS], rdec], [rSD])
                for h in range(4):
                    hb, hl = h // 2, h % 2
                    o = bank[2][:, h * 128:(h + 1) * 128]
                    mm(o, qfT[64 * hl:64 * hl + 64, 0, hb, :], ST[64 * hl:64 * hl + 64, T, 0, hb, :], True, False, [rqT, rST], [rbank[2]], tp=(64 * hl, 0))
                    mm(o, qfT[64 * hl:64 * hl + 64, 1, hb, :], ST[64 * hl:64 * hl + 64, T, 1, hb, :], False, False, [rqT, rST], [rbank[2]], tp=(64 * hl, 0))
                    mm(o, SD[:, h * 128:(h + 1) * 128], vb[par][:, h * 128:(h + 1) * 128], False, True, [rSD, rvb[par]], [rbank[2]])
                cpa(t1[:], bank[2][:], [rbank[2]], [rtmp])
                for h in range(4):
                    (lambda h: P.dve(lambda: nc.vector.bn_stats(out=bst[:, h, :], in_=t1[:, h * 128:(h + 1) * 128]), [rtmp], [rln]))(h)
                    (lambda h: P.dve(lambda: nc.vector.bn_aggr(out=mv[:, h, :], in_=bst[:, h, :]), [rln], [rln]))(h)
                rsqrt_chain(sdr[:, 0:4], sdr[:, 4:8], mv[:, :, 1], 1.0, rln, rln)
                tsc(sdr[:, 4:8], sdr[:, 4:8], 0.5, None, ALU.mult, None, [rln], [rln])
                for h in range(4):
                    tsc(yb[:, h * 128:(h + 1) * 128], t1[:, h * 128:(h + 1) * 128], mv[:, h, 0:1], sdr[:, 4 + h:5 + h], ALU.subtract, ALU.mult,
                        [rtmp, rln], [ryb])
                tt(retb[:], yb[:], sg[par][:], ALU.mult, [ryb, rsg[par]], [ryb], eng="POOL")
                out_branch(0, T, retb, ryb)

        def chainS(T, par):
            if True:
                P.dve(lambda: nc.vector.bn_stats(out=bst2[:], in_=gv[par][:]), [rgv[par]], [rsg2])
                P.dve(lambda: nc.vector.bn_aggr(out=mv2[:, 0:2], in_=bst2[:]), [rsg2], [rsg2])
                rsqrt_chain(mv2[:, 2:3], mv2[:, 3:4], mv2[:, 1:2], 1.0, rsg2, rsg2)
                tsc(vn[:], gv[par][:], mv2[:, 0:1], mv2[:, 3:4], ALU.subtract, ALU.mult, [rsg2, rgv[par]], [rsg2])
                for g in range(4):
                    mm(bank[4][:, g * 128:(g + 1) * 128], vn[:, g * 128:(g + 1) * 128], wsT[:, g, :], True, True, [rsg2, rsm], [rbank[4]])
                tt(stmp[:], bank[4][:], sbb[:], ALU.add, [rbank[4], rsm], [rsg2])
                tt(brT[1][:].rearrange("p c t -> p (c t)"), stmp[:], uT[par][:], ALU.mult, [rsg2, ruT[par]], [rbrT[1]], eng="POOL")
                dsp(BRv[1][:, :, T * 128:(T + 1) * 128], brT[1][:], [rbrT[1]], [RBR[1][T]])

        def chainQ(T, par):
            RT, rRT = ropet[T % 2], rrope[T % 2]
            if True:
                zq3 = zq[par][:].rearrange("p (h d) -> p h d", d=64)
                qn3 = qn[:].rearrange("p (h d) -> p h d", d=64)
                actf(sq[:], zq[par][:], AF.Square, [rzq[par]], [ratt])
                P.dve(lambda: nc.vector.tensor_reduce(out=ssq[:, 0:8], in_=sq[:].rearrange("p (h d) -> p h d", d=64), axis=AX.X, op=ALU.add), [ratt], [ratt])
                rsqrt_chain(ssq[:, 8:16], ssq[:, 16:24], ssq[:, 0:8], 1.0 / 64, ratt, ratt)
                tt(qn3, zq3, bc(ssq[:, 16:24], 2, 64), ALU.mult, [rzq[par], ratt], [ratt])
                tt(qn3, qn3, bc(qg[:], 1, 8), ALU.mult, [ratt, rsm], [ratt], eng="POOL")
                for hk in range(2):
                    rope_apply(P, nc, qn[:, hk * 256:(hk + 1) * 256], ratt, RT, rRT, 4, t1a, t2a, qperm[:, :, hk, :], rtmpa, ratt, pool=True)
                p2 = T % 2
                with P.atomic():
                    for g in range(4):
                        tr(bankT[:, g * 128:(g + 1) * 128], qperm[:, g, :, :].rearrange("p k d -> p (k d)"), identb[:], [ratt, rcb], [rbT[0]])
                    cpa(qT2[p2][:], bankT[:, 0:512].rearrange("p (g t) -> p g t", t=128), [rbT[0]], [rqT2[p2]])

        def chainK(T, par):
            RT, rRT = ropet[T % 2], rrope[T % 2]
            zk3 = zk[par][:].rearrange("p (h d) -> p h d", d=64)
            kn3 = kn[:].rearrange("p (h d) -> p h d", d=64)
            actf(sqk[:], zk[par][:], AF.Square, [rzk[par]], [rattk])
            P.dve(lambda: nc.vector.tensor_reduce(out=ssk[:, 0:2], in_=sqk[:].rearrange("p (h d) -> p h d", d=64), axis=AX.X, op=ALU.add), [rattk], [rattk])
            rsqrt_chain(ssk[:, 2:4], ssk[:, 4:6], ssk[:, 0:2], 1.0 / 64, rattk, rattk)
            tt(kn3, zk3, bc(ssk[:, 4:6], 2, 64), ALU.mult, [rzk[par], rattk], [rattk])
            tt(kn3, kn3, bc(kg[:], 1, 2), ALU.mult, [rattk, rsm], [rattk])
            rope_apply(P, nc, kn[:], rattk, RT, rRT, 2, t1a, t2a, krb2[:].rearrange("p (h d) -> p h d", d=64), rtmpa, rattk)
            with P.atomic():
                tr(bankT[:, 512:640], krb2[:], identb[:], [rattk, rcb], [rbT[1]])
                cpa(KT_all[:, T, :], bankT[:, 512:640], [rbT[1]], [rKV[T]])

        stc = {"n": 0}

        def Yst(T):
            p2 = T % 2
            if T < NLAT:
                chunks = []
                if T > 0:
                    chunks.append((T - 1, mprevb))
                chunks.append((T, None))
                chunks += [(NLAT, None), (NLAT + 1, None)]
                if T < NLAT - 1:
                    chunks.append((T + 1, mnextb))
            else:
                chunks = [(NLAT, None), (NLAT + 1, None)]
            for hk in range(2):
                ob = bank[6]
                rob = rbank[6]
                for ci, (c, m) in enumerate(chunks):
                    i = stc["n"] % 2
                    stc["n"] += 1
                    sb_, rsb = bank[5], rbank[5]
                    mm(sb_[:, :], KT_all[64 * hk:64 * hk + 64, c, :], qT2[p2][64 * hk:64 * hk + 64, :, :].rearrange("p g t -> p (g t)"),
                       True, m is None, [rKV[c], rqT2[p2]], [rsb], tp=(64 * hk, 0))
                    if m is not None:
                        mm(sb_[:, :], identb[:], m[:], False, True, [rcb], [rsb])
                    actf(PT[i][:], sb_[:, :], AF.Exp, [rsb], [rPT[i]])
                    for g in range(4):
                        mm(ob[:, g * 128:g * 128 + 65], PT[i][:, g * 128:(g + 1) * 128], V_all[:, c, hk, 0:65], ci == 0 and g == 0,
                           ci == len(chunks) - 1 and g == 3, [rPT[i], rKV[c]], [rob])
                ov = ob[:, 0:512].rearrange("p (g e) -> p g e", e=128)
                tt(den[:, hk * 4:(hk + 1) * 4], ov[:, :, 64], esink[:, hk * 4:(hk + 1) * 4], ALU.add, [rob, rsm], [rden])
                (lambda hk: P.dve(lambda: nc.vector.reciprocal(out=den[:, 8 + hk * 4:8 + (hk + 1) * 4], in_=den[:, hk * 4:(hk + 1) * 4]), [rden], [rden]))(hk)
                tt(attb[:, hk * 256:(hk + 1) * 256].rearrange("p (g d) -> p g d", d=64), ov[:, :, 0:64], bc(den[:, 8 + hk * 4:8 + (hk + 1) * 4], 2, 64),
                   ALU.mult, [rob, rden], [rden])
            out_branch(2, T, attb, rden)

        L = [NLAT, NLAT + 1] + list(range(NLAT))
        full = [(not last) if T >= NLAT else True for T in L]
        Pst1(L[0], 0)
        Pst1(L[1], 1)
        Pst(L[0], full[0], 0)
        for i, T in enumerate(L):
            par = i % 2
            chains = []
            if T < NLAT and T >= 1:
                P.capture_start()
                chainK(T, par)
                chains.append((P.capture_end(), 0.0, 0.2))
            else:
                chainK(T, par)
            if i + 1 < len(L):
                P.capture_start()
                Pst(L[i + 1], full[i + 1], (i + 1) % 2)
                chains.append(P.capture_end())
            if i + 2 < len(L):
                P.capture_start()
                Pst1(L[i + 2], i % 2)
                chains.append(P.capture_end())
            if full[i]:
                for ch in (chainR, chainS, chainQ):
                    P.capture_start()
                    ch(T, par)
                    chains.append(P.capture_end())
            if T < NLAT and T >= 1:
                P.capture_start()
                Yst(T - 1)
                chains.append(P.capture_end())
            P.emit_zip(chains)
            if T == NLAT + 1 and not last:
                Yst(NLAT)
                Yst(NLAT + 1)
            if T == NLAT - 1:
                Yst(T)

    def phase_merge(stk, l, last):
        wgt = sb("wgt", [128, 8, 3072], BF16, stk)
        wbr = sb("wbr", [128, 3, 4, D], BF16, stk)
        wo = sb("wo", [128, 8, D], BF16, stk)
        rw = Res()
        for q in range(3):
            dpl(wgt[:, :, q * 1024:(q + 1) * 1024], kwview(w_in[l][:, NG + q * 1024:NG + (q + 1) * 1024]), [], [rw])
            dpl(wbr[:, q, :, :], kwview(w_br[l][q]), [], [rw])
        dpl(wo[:], kwview(w_out[l]), [], [rw])
        hTg = [sb("hTg%d" % i, [128, 8, 512], BF16, stk) for i in range(2)]
        rhTg = [Res(), Res()]
        brTi = [sb("brTi%d" % i, [128, 3, 4, 512], BF16, stk) for i in range(2)]
        rbrTi = [Res(), Res()]
        mT = sb("mT", [128, 8, 512], BF16, stk)
        rmT = Res()
        sgm = [sb("sgm%d" % i, [128, 512], F32, stk) for i in range(2)]
        rsgm = [Res(), Res()]
        acc = sb("acc", [128, 512], F32, stk)
        tmp = sb("tmpm", [128, 512], F32, stk)
        racc = Res()
        rtmpm = Res()
        xo = [sb("xo%d" % i, [128, D], F32, stk) for i in range(2)]
        rxo = [Res(), Res()]
        xr = [sb("xr%d" % i, [128, D], F32, stk) for i in range(2)]
        rxr = [Res(), Res()]
        BRv = [BR[i].rearrange("(c p) t -> p c t", p=128) for i in range(3)]
        groups = [list(range(g * 4, g * 4 + 4)) for g in range(8)]
        if not last:
            groups = [[NLAT, NLAT + 1]] + groups
        cnts = {"pc": 0, "xc": 0}

        def prologue(gi):
            grp = groups[gi]
            gp = gi % 2
            s_ = 0 if grp[0] < NLAT else 1
            ntok = 128 * len(grp)
            T0 = grp[0]
            for ti, T in enumerate(grp):
                X, rX, XN, rXN, S, rS = norm_tile(T)
                transpose_mod(XN, rXN, A1, B1m, s_, (lambda ti, gp: (lambda k: hTg[gp][:, k, ti * 128:(ti + 1) * 128]))(ti, gp), rhTg[gp])
            for i in range(3):
                dsp(brTi[gp][:, i, :, 0:ntok], BRv[i][:, :, T0 * 128:T0 * 128 + ntok], [RBR[i][T] for T in grp], [rbrTi[gp]])

        def compute(gi):
            grp = groups[gi]
            gp = gi % 2
            s_ = 0 if grp[0] < NLAT else 1
            ntok = 128 * len(grp)
            H, rH, BI, rBI = hTg[gp], rhTg[gp], brTi[gp], rbrTi[gp]
            for dc in range(8):
                for i in range(3):
                    p = cnts["pc"] % 3
                    cnts["pc"] += 1
                    bg, rbg, bb, rbb = bank[2 * p], rbank[2 * p], bank[2 * p + 1], rbank[2 * p + 1]
                    for k in range(8):
                        mm(bg[:, 0:ntok], wgt[:, k, i * 1024 + dc * 128:i * 1024 + (dc + 1) * 128], H[:, k, 0:ntok], k == 0, k == 7, [rw, rH], [rbg])
                    for k in range(4):
                        mm(bb[:, 0:ntok], wbr[:, i, k, dc * 128:(dc + 1) * 128], BI[:, i, k, 0:ntok], k == 0, k == 3, [rw, rBI], [rbb])
                    sgi, rsgi = sgm[cnts["pc"] % 2], rsgm[cnts["pc"] % 2]
                    actf(sgi[:, 0:ntok], bg[:, 0:ntok], AF.Sigmoid, [rbg], [rsgi])
                    if i == 0:
                        tt(acc[:, 0:ntok], sgi[:, 0:ntok], bb[:, 0:ntok], ALU.mult, [rsgi, rbb], [racc])
                    elif i == 1:
                        tt(tmp[:, 0:ntok], sgi[:, 0:ntok], bb[:, 0:ntok], ALU.mult, [rsgi, rbb], [rtmpm])
                        tt(acc[:, 0:ntok], acc[:, 0:ntok], tmp[:, 0:ntok], ALU.add, [racc, rtmpm], [racc], eng="POOL")
                    else:
                        tt(tmp[:, 0:ntok], sgi[:, 0:ntok], bb[:, 0:ntok], ALU.mult, [rsgi, rbb], [rtmpm])
                        tt(mT[:, dc, 0:ntok], acc[:, 0:ntok], tmp[:, 0:ntok], ALU.add, [racc, rtmpm], [rmT], eng="POOL")
            for ti, T in enumerate(grp):
                j = cnts["xc"] % 2
                cnts["xc"] += 1
                xoi, rxoi = xo[j], rxo[j]
                dsp(xr[j][:], srows(T), [RS[T]], [rxr[j]])
                for nb in range(2):
                    for k in range(8):
                        mm(bank[6][:, :], mT[:, k, ti * 128:(ti + 1) * 128], wo[:, k, nb * 512:(nb + 1) * 512], k == 0, k == 7, [rmT, rw], [rbank[6]])
                    tt(xoi[:, nb * 512:(nb + 1) * 512], bank[6][:, :], rowbcA[:, s_, nb * 512:(nb + 1) * 512], ALU.mult, [rbank[6], rrowA], [rxoi])
                tt(xoi[:], xoi[:], xr[j][:], ALU.add, [rxoi, rxr[j]], [rxoi], eng="POOL")
                dsp(srows(T), xoi[:], [rxoi], [RS[T]])

        prologue(0)
        for gi in range(len(groups)):
            chains = []
            P.capture_start()
            compute(gi)
            chains.append(P.capture_end())
            if gi + 1 < len(groups):
                P.capture_start()
                prologue(gi + 1)
                chains.append(P.capture_end())
            P.emit_zip(chains)

    def phase_moe(stk, l, last):
        streams = [(0, list(range(NLAT)), 512, 4, 128)]
        if not last:
            streams.append((1, [NLAT, NLAT + 1], 32, 1, 32))
        aff = sb("aff", [128, NT, NEXP], F32, stk)
        raff = Res()
        idxS = [sb("idxS%d" % i, [128, 4, NEXP], I32, stk) for i in range(2)]
        idxH = [sb("idxH%d" % i, [128, 4, NEXP], I32, stk) for i in range(2)]
        wsel = [sb("wsel%d" % i, [128, 4, NEXP], F32, stk) for i in range(2)]
        ridx = [Res(), Res()]

        with contextlib.ExitStack() as s1:
            wr = sb("wr", [128, 8, NEXP], BF16, s1)
            rwr = Res()
            dpl(wr[:], w_rt[l].rearrange("(k p) e -> p k e", p=128), [], [rwr])
            h2fs = [sb("h2fA", [128, D], F32, s1), sb("h2fB", [128, D], F32, s1)]
            rh2fs = [Res(), Res()]
            h2b = [sb("h2b%d" % i, [128, D], BF16, s1) for i in range(2)]
            rh2 = [Res(), Res()]
            h2T = [sb("h2T%d" % i, [128, 8, 128], BF16, s1) for i in range(2)]
            rh2T = [Res(), Res()]
            sm = [sb("sm%d" % i, [128, 8], F32, s1) for i in range(2)]
            ex = [sb("ex%d" % i, [128, NEXP], F32, s1) for i in range(2)]
            rsmx = [Res(), Res()]
            allT = [(s_, T) for (s_, tiles, cap, nch, rows) in streams for T in tiles]

            def st1(j):
                s_, T = allT[j]
                i = j % 2
                X, rX, XN, rXN, S, rS = norm_tile(T)
                h2f, rh2f = h2fs[i], rh2fs[i]
                stt(h2f[:], X[:], S[:, 2:3], rowbcA[:, s_, :], ALU.mult, ALU.mult, [rX, rS, rrowA], [rh2f])
                tt(h2b[i][:], h2f[:], rowbcB[:, s_, :], ALU.add, [rh2f, rrowB], [rh2[i]], eng="pool")
                dsp(H2[T * 128:(T + 1) * 128, :], h2b[i][:], [rh2[i]], [RH2[T]])
                for half in range(2):
                    with P.atomic():
                        for kk in range(4):
                            k = half * 4 + kk
                            tr(bankT[:, half * 512 + kk * 128:half * 512 + (kk + 1) * 128], h2b[i][:, k * 128:(k + 1) * 128], identb[:], [rh2[i], rcb], [rbT[half]])
                        cpa(h2T[i][:, half * 4:(half + 1) * 4, :], bankT[:, half * 512:(half + 1) * 512].rearrange("p (k t) -> p k t", t=128), [rbT[half]], [rh2T[i]])

            def st2(j):
                s_, T = allT[j]
                i = j % 2
                bk, rbk = bank[2 + i], rbank[2 + i]
                for k in range(8):
                    mm(bk[:, 0:NEXP], h2T[i][:, k, :], wr[:, k, :], k == 0, k == 7, [rh2T[i], rwr], [rbk])
                smi, exi, rsi = sm[i], ex[i], rsmx[i]
                P.dve(lambda: nc.vector.tensor_reduce(out=smi[:, 0:1], in_=bk[:, 0:NEXP], axis=AX.X, op=ALU.max), [rbk], [rsi])
                tsc(smi[:, 1:2], smi[:, 0:1], -1.0, None, ALU.mult, None, [rsi], [rsi])
                mset(smi[:, 2:3], 0.0, [rsi])
                actf(exi[:], bk[:, 0:NEXP], AF.Exp, [rbk, rsi], [rsi], bias=smi[:, 1:2], accum_out=smi[:, 2:3])
                P.dve(lambda: nc.vector.reciprocal(out=smi[:, 3:4], in_=smi[:, 2:3]), [rsi], [rsi])
                tsc(aff[:, T, :], exi[:], smi[:, 3:4], None, ALU.mult, None, [rsi], [raff])

            for j0 in range(0, len(allT), 2):
                chains = []
                for j in (j0, j0 + 1):
                    if j < len(allT):
                        P.capture_start()
                        st1(j)
                        st2(j)
                        chains.append(P.capture_end())
                P.emit_zip(chains)
            build_rowbc(rowbcA, rrowA, lambda s, c: modT[:, 40 + c, s:s + 1])
            P.flush()
        if _DBG.get("cstop") == "1":
            return

        with contextlib.ExitStack() as s2:
            ge = sb("ge", [128, NLAT, NEXP], BF16, s2)
            maskf = sb("maskf", [128, NLAT, NEXP], F32, s2)
            basef = sb("basef", [128, NLAT, NEXP], F32, s2)
            keyf = sb("keyf", [128, NLAT, NEXP], F32, s2)
            cntp = sb("cntp", [128, NLAT, NEXP], F32, s2)
            vals = sb("vals", [128, NLAT, NEXP, 4], BF16, s2)
            valf = sb("valf", [128, NLAT, NEXP], F32, s2)
            lo = sb("lo", [128, NEXP], F32, s2)
            mid = sb("mid", [128, NEXP], F32, s2)
            cpr = sb("cpr", [128, NEXP], F32, s2)
            cntr = sb("cntr", [128, NEXP], F32, s2)
            rr = Res()
            OH = [sb("OH%d" % i, [128, 512], BF16, s2) for i in range(4)]
            rOH = [Res() for _ in range(4)]
            lst = sb("lst", [128, NEXP, 4, 4], F32, s2)
            idxf = sb("idxf", [128, 4, NEXP], F32, s2)
            NIT = 26
            for (s, tiles, cap, nch, rows) in streams:
                nt = len(tiles)
                T0 = tiles[0]
                av = aff[:, T0:T0 + nt, :]
                W = nt * NEXP
                mset(lo[:], 0.0, [rr])
                mset(mid[:], 0.5, [rr])
                for it in range(NIT):
                    half = 0.5 ** (it + 1)
                    tt(ge[:, 0:nt, :], av, bc(mid[:], 1, nt), ALU.is_ge, [raff, rr], [rr])
                    mm(bank[0][:, 0:W], onesb[:], ge[:, 0:nt, :].rearrange("p t e -> p (t e)"), True, True, [rr, rcb], [rbank[0]])
                    P.dve(lambda W=W, nt=nt: nc.vector.tensor_reduce(out=cntr[:], in_=bank[0][:, 0:W].rearrange("p (t e) -> p e t", e=NEXP), axis=AX.X, op=ALU.add),
                          [rbank[0]], [rr])
                    tsc(cpr[:], cntr[:], float(cap) - 0.5, half, ALU.is_ge, ALU.mult, [rr], [rr])
                    stt(mid[:], cpr[:], half * 0.5, lo[:], ALU.add, ALU.add, [rr], [rr])
                    tt(lo[:], lo[:], cpr[:], ALU.add, [rr], [rr])
                tt(maskf[:, 0:nt, :], av, bc(lo[:], 1, nt), ALU.is_ge, [raff, rr], [rr])
                cpv(ge[:, 0:nt, :], maskf[:, 0:nt, :], [rr], [rr])
                gef = ge[:, 0:nt, :].rearrange("p t e -> p (t e)")
                mm(bank[0][:, 0:W], trib[:], gef, True, True, [rr, rcb], [rbank[0]])
                mm(bank[1][:, 0:W], onesb[:], gef, True, True, [rr, rcb], [rbank[1]])
                cpv(cntp[:, 0:nt, :].rearrange("p t e -> p (t e)"), bank[1][:, 0:W], [rbank[1]], [rr])
                mset(basef[:, 0, :], 0.0, [rr])
                for t_ in range(1, nt):
                    tt(basef[:, t_, :], basef[:, t_ - 1, :], cntp[:, t_ - 1, :], ALU.add, [rr], [rr])
                tt(keyf[:, 0:nt, :].rearrange("p t e -> p (t e)"), bank[0][:, 0:W], basef[:, 0:nt, :].rearrange("p t e -> p (t e)"), ALU.add, [rbank[0], rr], [rr])
                stt(keyf[:, 0:nt, :], keyf[:, 0:nt, :], 1.0, maskf[:, 0:nt, :], ALU.add, ALU.mult, [rr], [rr])
                tsc(keyf[:, 0:nt, :], keyf[:, 0:nt, :], -1.0, None, ALU.add, None, [rr], [rr])
                cpv(vals[:, 0:nt, :, 0], bc(ct("ttv")[:, 0:nt], 2, NEXP), [rct], [rr])
                cpv(vals[:, 0:nt, :, 1], ct("jcol").unsqueeze(2).to_broadcast([128, nt, NEXP]), [rct], [rr])
                cpv(vals[:, 0:nt, :, 2], av, [raff], [rr])
                cpv(valf[:, 0:nt, :], vals[:, 0:nt, :, 2], [rr], [rr])
                tt(vals[:, 0:nt, :, 3], av, valf[:, 0:nt, :], ALU.subtract, [raff, rr], [rr])
                capw = nch * 128
                oc = 0
                rvals = rr
                for e in range(NEXP):
                    for t_ in range(nt):
                        i = oc % 4
                        oc += 1
                        tsc(OH[i][:, 0:capw], ct("iota512")[:, 0:capw], keyf[:, t_, e:e + 1], None, ALU.is_equal, None, [rct, rvals], [rOH[i]],
                            eng="dve")
                        for c in range(nch):
                            mm(bank[3][:, (e * 4 + c) * 4:(e * 4 + c) * 4 + 4], OH[i][:, c * 128:(c + 1) * 128], vals[:, t_, e, :],
                               e == 0 and t_ == 0 and c == 0, e == NEXP - 1 and t_ == nt - 1 and c == nch - 1, [rOH[i], rvals], [rbank[3]])
                lv = lst[:, :, 0:nch, :]
                cpv(lv, bank[3][:, 0:256].rearrange("p (e c v) -> p e c v", c=4, v=4)[:, :, 0:nch, :], [rbank[3], rvals], [rr])
                iv = idxf[:, 0:nch, :].rearrange("p c e -> p e c")
                stt(iv, lv[:, :, :, 0], 128.0, lv[:, :, :, 1], ALU.mult, ALU.add, [rr], [rr])
                cpv(idxS[s][:, 0:nch, :], idxf[:, 0:nch, :], [rr], [ridx[s]])
                tsc(idxf[:, 0:nch, :], idxf[:, 0:nch, :], float(T0 * 128), None, ALU.add, None, [rr], [rr])
                cpv(idxH[s][:, 0:nch, :], idxf[:, 0:nch, :], [rr], [ridx[s]])
                tt(wsel[s][:, 0:nch, :].rearrange("p c e -> p e c"), lv[:, :, :, 2], lv[:, :, :, 3], ALU.add, [rr], [ridx[s]])
            P.flush()
        if _DBG.get("cstop") == "2":
            return

        wg = [sb("wg%d" % i, [128, 8, D], BF16, stk) for i in range(2)]
        wu = [sb("wu%d" % i, [128, 8, D], BF16, stk) for i in range(2)]
        wd = [sb("wd%d" % i, [128, 8, D], BF16, stk) for i in range(2)]
        rwe = [Res(), Res()]
        NS = len(streams)
        xs = [sb("xs%d" % s_, [128, 4 if s_ == 0 else 1, D], BF16, stk) for s_ in range(NS)]
        rxs = [Res() for _ in range(NS)]
        xsT = [[sb("xsT%d_%d" % (s_, i), [128, 8, 512 if s_ == 0 else 32], BF16, stk) for i in range(2)] for s_ in range(NS)]
        rxsT = [[Res(), Res()] for _ in range(NS)]
        sa = [sb("sa%d" % i, [128, 512], F32, stk) for i in range(2)]
        rsa = [Res(), Res()]
        hf = [sb("hf%d" % s_, [128, 8, 512 if s_ == 0 else 32], BF16, stk) for s_ in range(NS)]
        rhf = [Res() for _ in range(NS)]
        yscs = [sb("ysc%d" % i, [128, D], F32, stk) for i in range(4)]
        ryscs = [Res() for _ in range(4)]
        ycn = {"n": 0}

        def loads(e):
            par = e % 2
            for (wt_, src) in ((wg, w_eg), (wu, w_eu), (wd, w_ed)):
                dpl(wt_[par][:], src[l][e].rearrange("(k p) n -> p k n", p=128), [], [rwe[par]])

        def gather(e):
            for (s, tiles, cap, nch, rows) in streams:
                for c in range(nch):
                    (lambda c, s, rows, e: P.dma("pool", lambda: nc.gpsimd.indirect_dma_start(
                        out=xs[s][0:rows, c, :], out_offset=None, in_=H2,
                        in_offset=bass.IndirectOffsetOnAxis(ap=idxH[s][0:rows, c, e:e + 1], axis=0)),
                        [ridx[s]] + RH2, [rxs[s]]))(c, s, rows, e)

        def G(e):
            par = e % 2
            for (s, tiles, cap, nch, rows) in streams:
                ntok = nch * rows
                for k in range(8):
                    half = k % 2
                    with P.atomic():
                        for c in range(nch):
                            tr(bankT[:, half * 512 + c * rows: half * 512 + (c + 1) * rows], xs[s][0:rows, c, k * 128:(k + 1) * 128], identb[0:rows, 0:rows],
                               [rxs[s], rcb], [rbT[half]])
                        if k % 2 == 0:
                            cpa(xsT[s][par][:, k, 0:ntok], bankT[:, half * 512: half * 512 + ntok], [rbT[half]], [rxsT[s][par]])
                        else:
                            cpv(xsT[s][par][:, k, 0:ntok], bankT[:, half * 512: half * 512 + ntok], [rbT[half]], [rxsT[s][par]])

        sac = sb("sac", [128, 64], F32, stk)
        rsac = Res()
        yscc = sb("yscc", [128, D], F32, stk)
        ryscc = Res()

        def up(e, si):
            par = e % 2
            (s, tiles, cap, nch, rows) = streams[si]
            ntok = nch * rows
            for fc in range(8):
                if s == 0:
                    p = fc % 2
                    ba, rba, bu, rbu = bank[2 * p][:, 0:ntok], rbank[2 * p], bank[2 * p + 1][:, 0:ntok], rbank[2 * p + 1]
                    sai, rsai = sa[p][:, 0:ntok], rsa[p]
                else:
                    ba, rba, bu, rbu = bank[6][:, 0:ntok], rbank[6], bank[6][:, 64:64 + ntok], rbank[6]
                    sai, rsai = sac[:, 0:ntok], rsac
                for k in range(8):
                    mm(ba, wg[par][:, k, fc * 128:(fc + 1) * 128], xsT[s][par][:, k, 0:ntok], k == 0, k == 7, [rwe[par], rxsT[s][par]], [rba])
                for k in range(8):
                    mm(bu, wu[par][:, k, fc * 128:(fc + 1) * 128], xsT[s][par][:, k, 0:ntok], k == 0, k == 7, [rwe[par], rxsT[s][par]], [rbu])
                actf(sai, ba, AF.Silu, [rba], [rsai])
                tt(hf[s][:, fc, 0:ntok], sai, bu, ALU.mult, [rsai, rbu], [rhf[s]])

        def down(e, si):
            par = e % 2
            (s, tiles, cap, nch, rows) = streams[si]
            dst = out if s == 0 else xc
            rstream = [RS[T] for T in tiles]
            for c in range(nch):
                if s == 0:
                    ysc, rysc = yscs[ycn["n"] % 4], ryscs[ycn["n"] % 4]
                    ycn["n"] += 1
                else:
                    ysc, rysc = yscc, ryscc
                for nb in range(2):
                    if s == 0:
                        bd, rbd = bank[4 + nb], rbank[4 + nb]
                    else:
                        bd, rbd = bank[6], rbank[6]
                    for fc in range(8):
                        mm(bd[0:rows, :], hf[s][:, fc, c * rows:(c + 1) * rows], wd[par][:, fc, nb * 512:(nb + 1) * 512], fc == 0, fc == 7, [rhf[s], rwe[par]], [rbd])
                    stt(ysc[0:rows, nb * 512:(nb + 1) * 512], bd[0:rows, :], wsel[s][0:rows, c, e:e + 1], rowbcA[0:rows, s, nb * 512:(nb + 1) * 512],
                        ALU.mult, ALU.mult, [rbd, ridx[s], rrowA], [rysc])
                if _DBG.get("noscatter"):
                    continue
                (lambda c, s, rows, e, dst, rstream, ysc, rysc: P.dma("pool", lambda: nc.gpsimd.indirect_dma_start(
                    out=dst, out_offset=bass.IndirectOffsetOnAxis(ap=idxS[s][0:rows, c, e:e + 1], axis=0),
                    in_=ysc[0:rows, :], in_offset=None, compute_op=ALU.add),
                    [rysc, ridx[s]] + rstream, rstream))(c, s, rows, e, dst, rstream, ysc, rysc)

        gather(0)
        loads(0)
        G(0)
        for e in range(NEXP):
            if e + 1 < NEXP:
                gather(e + 1)
                loads(e + 1)
            chains = []
            for si in range(len(streams)):
                P.capture_start()
                up(e, si)
                down(e, si)
                chains.append(P.capture_end())
            if e + 1 < NEXP:
                P.capture_start()
                G(e + 1)
                chains.append((P.capture_end(), 0.6, 0.98))
            P.emit_zip(chains)

    for l in range(DEPTH):
        last = (l == DEPTH - 1)
        tiles_ctx = [NLAT, NLAT + 1]
        tiles_lat = list(range(NLAT))

        with contextlib.ExitStack() as ps:
            cTt = sb("cTt", [128, 8, 2], F32, ps)
            bmT = sb("bmT", [128, 48], F32, ps)
            n1T = sb("n1T", [128, 8], F32, ps)
            n2T = sb("n2T", [128, 8], F32, ps)
            wm = [sb("wm%d" % i, [128, 8, 1024], BF16, ps) for i in range(2)]
            rwm = [Res(), Res()]
            rsm = Res()
            P.dma("sp", lambda: nc.sync.dma_start(out=cTt[:], in_=cT_d.rearrange("p (k s) -> p k s", s=2)), writes=[rsm])
            P.dma("sp", lambda: nc.sync.dma_start(out=bmT[:], in_=bmT_d[l]), writes=[rsm])
            P.dma("sp", lambda: nc.sync.dma_start(out=n1T[:], in_=n1T_d[l]), writes=[rsm])
            P.dma("sp", lambda: nc.sync.dma_start(out=n2T[:], in_=n2T_d[l]), writes=[rsm])
            P.act(lambda: nc.scalar.activation(out=cact[:], in_=cTt[:], func=AF.Silu), reads=[rsm], writes=[rmod])
            for sec in range(6 if stop_after != "M1" else 0):
                i = sec % 2
                (lambda sec, i: P.dma("pool", lambda: nc.gpsimd.dma_start(
                    out=wm[i][:], in_=w_mod[l][:, sec * 1024:(sec + 1) * 1024].rearrange("(k p) n -> p k n", p=128)),
                    writes=[rwm[i]]))(sec, i)
                for j in range(8):
                    col = (sec * 8 + j) * 2
                    for k in range(8):
                        (lambda i, j, k, col: P.pe(lambda: nc.tensor.matmul(bank[2][:, col:col + 2], lhsT=wm[i][:, k, j * 128:(j + 1) * 128],
                                                                            rhs=cact[:, k, :], start=(k == 0), stop=(k == 7)),
                                                   reads=[rwm[i], rmod], writes=[rbank[2]]))(i, j, k, col)
            if stop_after in ("M1", "M2"):
                P.flush()
                break
            P.dve(lambda: nc.vector.tensor_tensor(out=modT[:], in0=bank[2][:, 0:96].rearrange("p (j s) -> p j s", s=2),
                                                  in1=bc(bmT[:], 2, 2), op=ALU.add), reads=[rbank[2], rsm], writes=[rmod])
            if stop_after == "M3":
                P.flush()
                break
            for (Ax, nT, sec) in ((A1, n1T, 1), (A2, n2T, 4)):
                (lambda Ax, nT, sec: P.dve(lambda: nc.vector.scalar_tensor_tensor(
                    out=Ax[:], in0=modT[:, sec * 8:(sec + 1) * 8, :], scalar=1.0, in1=bc(nT[:], 2, 2), op0=ALU.add, op1=ALU.mult),
                    reads=[rmod, rsm], writes=[rmod]))(Ax, nT, sec)
            P.flush()
        B1m = modT[:, 0:8, :]
        B2m = modT[:, 24:32, :]
        if stop_after == "M" and l == 0:
            break

        with contextlib.ExitStack() as pAB:
            ST = sb("ST", [128, NT, 2, 2, 128], BF16, pAB)
            rST = Res("ST")
            decbc = sb("decbc", [128, 8], F32, pAB)
            dechp = sb("dechp", [128, 4], F32, pAB)
            lgbc = sb("lgbc", [128, 8], F32, pAB)
            lghp = sb("lghp", [128, 4], F32, pAB)
            DTt = sb("DT", [128, 4, 128], F32, pAB)
            DfT = sb("DfT", [128, 2, 2, 128], F32, pAB)
            wkd = sb("wkd", [128, 2, 4], F32, pAB)
            Gam = sb("Gam", [128, 4], F32, pAB)
            e1 = sb("e1", [128, 128], F32, pAB)
            e2 = sb("e2", [128, 128], F32, pAB)
            rdec = Res("dec")
            re1 = Res()
            P.dma("sp", lambda: nc.sync.dma_start(out=decbc[:], in_=decbc_d[l]), writes=[rdec])
            P.dma("sp", lambda: nc.sync.dma_start(out=dechp[:], in_=dechp_d[l]), writes=[rdec])
            for (src, dst) in ((decbc, lgbc), (dechp, lghp)):
                (lambda src, dst: (
                    P.act(lambda: nc.scalar.activation(out=dst[:], in_=src[:], func=AF.Exp, scale=-1.0), reads=[rdec], writes=[rdec]),
                    P.dve(lambda: nc.vector.tensor_scalar(out=dst[:], in0=dst[:], scalar1=1.0, scalar2=None, op0=ALU.add), reads=[rdec], writes=[rdec]),
                    P.act(lambda: nc.scalar.activation(out=dst[:], in_=dst[:], func=AF.Ln), reads=[rdec], writes=[rdec]),
                    P.dve(lambda: nc.vector.tensor_scalar(out=dst[:], in0=dst[:], scalar1=-1.0, scalar2=None, op0=ALU.mult), reads=[rdec], writes=[rdec])))(src, dst)
            KS = 0.125
            for h in range(4):
                (lambda h: (
                    P.act(lambda: nc.scalar.activation(out=e1[:], in_=ct("PF"), func=AF.Exp, scale=lgbc[:, h:h + 1]), reads=[rdec, rct], writes=[re1]),
                    P.dve(lambda: nc.vector.tensor_tensor(out=e1[:], in0=e1[:], in1=ct("MF"), op=ALU.mult), reads=[re1, rct], writes=[re1]),
                    P.act(lambda: nc.scalar.activation(out=e2[:], in_=ct("PB"), func=AF.Exp, scale=lgbc[:, 4 + h:5 + h]), reads=[rdec, rct], writes=[re1]),
                    P.dve(lambda: nc.vector.tensor_tensor(out=e2[:], in0=e2[:], in1=ct("MB"), op=ALU.mult), reads=[re1, rct], writes=[re1]),
                    P.dve(lambda: nc.vector.tensor_tensor(out=e1[:], in0=e1[:], in1=e2[:], op=ALU.add), reads=[re1], writes=[re1]),
                    P.dve(lambda: nc.vector.tensor_scalar(out=DTt[:, h, :], in0=e1[:], scalar1=KS, scalar2=None, op0=ALU.mult), reads=[re1], writes=[rdec])))(h)
            P.act(lambda: nc.scalar.activation(out=wkd[:, 0, :], in_=lgbc[:, 0:4], func=AF.Exp, scale=ct("cjm")), reads=[rdec, rct], writes=[rdec])
            P.act(lambda: nc.scalar.activation(out=wkd[:, 1, :], in_=lgbc[:, 4:8], func=AF.Exp, scale=ct("jcol")), reads=[rdec, rct], writes=[rdec])
            P.dve(lambda: nc.vector.tensor_scalar(out=wkd[:], in0=wkd[:], scalar1=KS, scalar2=None, op0=ALU.mult), reads=[rdec], writes=[rdec])
            for dr in range(2):
                for hb in range(2):
                    (lambda dr, hb: (
                        P.act(lambda: nc.scalar.activation(out=DfT[:, dr, hb, :], in_=ct("iota1") if dr == 0 else ct("cmi"), func=AF.Exp,
                                                           scale=lghp[:, dr * 2 + hb: dr * 2 + hb + 1]), reads=[rdec, rct], writes=[rdec]),
                        P.act(lambda: nc.scalar.activation(out=Gam[:, dr * 2 + hb: dr * 2 + hb + 1], in_=lghp[:, dr * 2 + hb: dr * 2 + hb + 1],
                                                           func=AF.Exp, scale=128.0), reads=[rdec], writes=[rdec])))(dr, hb)

            with contextlib.ExitStack() as pA:
                wkv = sb("wkv", [128, 8, 768], BF16, pA)
                rwkv = Res()
                P.dma("pool", lambda: nc.gpsimd.dma_start(out=wkv[:], in_=w_in[l][:, 256:1024].rearrange("(k p) n -> p k n", p=128)), writes=[rwkv])
                UPD = sb("UPD", [128, NT, 2, 2, 128], F32, pA)
                rUPD = Res()
                hTa = [sb("hTa%d" % i, [128, 8, 128], BF16, pA) for i in range(2)]
                rhTa = [Res(), Res()]
                t1 = sb("t1a", [128, 256], F32, pA)
                t2 = sb("t2a", [128, 256], F32, pA)
                kr = sb("kra", [128, 256], F32, pA)
                rt = Res()
                kw = [sb("kwa%d" % i, [128, 2, 256], BF16, pA) for i in range(2)]
                vb = [sb("vba%d" % i, [128, 512], BF16, pA) for i in range(2)]
                rkw = [Res(), Res()]
                tilesA = tiles_ctx + tiles_lat
                t1s = [t1, sb("t1a2", [128, 256], F32, pA)]
                t2s = [t2, sb("t2a2", [128, 256], F32, pA)]
                krs = [kr, sb("kra2", [128, 256], F32, pA)]
                rts = [rt, Res()]
                abanks = [(0, 1, 2), (4, 5, 6)]

                hTa4 = [hTa[0], hTa[1], sb("hTa2", [128, 8, 128], BF16, pA), sb("hTa3", [128, 8, 128], BF16, pA)]
                rhTa4 = [rhTa[0], rhTa[1], Res(), Res()]

                def chainA1(n_):
                    T = tilesA[n_]
                    s = 0 if T < NLAT else 1
                    X, rX, XN, rXN, S, rS = norm_tile(T)
                    H = hTa4[n_ % 4]
                    transpose_mod(XN, rXN, A1, B1m, s, (lambda H: (lambda k: H[:, k, :]))(H), rhTa4[n_ % 4])

                def chainA(n_, st):
                    T = tilesA[n_]
                    ik, iv, iu = abanks[st]
                    bk, rbk, bv, rbv, bu, rbu = bank[ik], rbank[ik], bank[iv], rbank[iv], bank[iu], rbank[iu]
                    load_rope(T)
                    H, rH = hTa4[n_ % 4], rhTa4[n_ % 4]
                    for k in range(8):
                        mm(bk[:, 0:256], H[:, k, :], wkv[:, k, 0:256], k == 0, k == 7, [rH, rwkv], [rbk])
                    for k in range(8):
                        mm(bv[:, 0:512], H[:, k, :], wkv[:, k, 256:768], k == 0, k == 7, [rH, rwkv], [rbv])
                    RT, rRT = ropet[T % 2], rrope[T % 2]
                    krv = krs[st][:].rearrange("p (h d) -> p h d", d=64)
                    rope_apply(P, nc, bk[:, 0:256], rbk, RT, rRT, 4, t1s[st], t2s[st], krv, rts[st], rts[st])
                    KW, V = kw[st], vb[st]
                    for dr in range(2):
                        tt(KW[:, dr, :].rearrange("p (h d) -> p h d", d=64), krv, bc(wkd[:, dr, :], 2, 64), ALU.mult, [rts[st], rdec], [rkw[st]])
                    cpa(V[:], bv[:], [rbv], [rkw[st]])
                    for dr in range(2):
                        for hb in range(2):
                            mm(bu[:, hb * 256:(hb + 1) * 256], KW[:, dr, hb * 128:(hb + 1) * 128], V[:, hb * 256:(hb + 1) * 256], True, True,
                               [rkw[st]], [rbu])
                        for hb in range(2):
                            cpv(UPD[0:64, T, dr, hb, :], bu[0:64, hb * 256:hb * 256 + 128], [rbu], [rUPD])
                            cpa(UPD[64:128, T, dr, hb, :], bu[64:128, hb * 256 + 128:hb * 256 + 256], [rbu], [rUPD])

                chainA1(0)
                chainA1(1)
                for n_ in range(0, len(tilesA), 2):
                    chains = []
                    for st in range(2):
                        if n_ + st < len(tilesA):
                            P.capture_start()
                            chainA(n_ + st, st)
                            chains.append(P.capture_end())
                    for st in range(2):
                        if n_ + 2 + st < len(tilesA):
                            P.capture_start()
                            chainA1(n_ + 2 + st)
                            chains.append(P.capture_end())
                    P.emit_zip(chains)
                cur = [sb("cur%d" % i, [128, 2, 128], F32, pA) for i in range(2)]
                rcur = Res()
                c0, c1 = NLAT, NLAT + 1
                for dr in range(2):
                    first, second = (c0, c1) if dr == 0 else (c1, c0)
                    (lambda dr, first, second: (
                        P.dve(lambda: nc.vector.memset(ST[:, first, dr, :, :], 0.0), writes=[rST]),
                        P.dve(lambda: nc.vector.tensor_copy(out=ST[:, second, dr, :, :], in_=UPD[:, first, dr, :, :]), reads=[rUPD], writes=[rST])))(dr, first, second)
                    order = list(range(NLAT)) if dr == 0 else list(range(NLAT - 1, -1, -1))
                    for hb in range(2):
                        (lambda dr, hb, first, second: P.dve(lambda: nc.vector.scalar_tensor_tensor(
                            out=cur[0][:, hb, :], in0=UPD[:, first, dr, hb, :], scalar=Gam[:, dr * 2 + hb: dr * 2 + hb + 1],
                            in1=UPD[:, second, dr, hb, :], op0=ALU.mult, op1=ALU.add), reads=[rUPD, rdec], writes=[rcur]))(dr, hb, first, second)
                    pp = 0
                    for n in order:
                        (lambda dr, n, pp: P.act(lambda: nc.scalar.copy(out=ST[:, n, dr, :, :], in_=cur[pp][:]), reads=[rcur], writes=[rST]))(dr, n, pp)
                        for hb in range(2):
                            (lambda dr, hb, n, pp: P.dve(lambda: nc.vector.scalar_tensor_tensor(
                                out=cur[1 - pp][:, hb, :], in0=cur[pp][:, hb, :], scalar=Gam[:, dr * 2 + hb: dr * 2 + hb + 1],
                                in1=UPD[:, n, dr, hb, :], op0=ALU.mult, op1=ALU.add), reads=[rUPD, rdec, rcur], writes=[rcur]))(dr, hb, n, pp)
                        pp = 1 - pp
                P.flush()
            if stop_after == "A" and l == 0:
                break

            with contextlib.ExitStack() as pB:
                phase_B(pB, l, last, ST, rST, DTt, DfT, rdec)
                P.flush()
        if stop_after == "B" and l == 0:
            break

        with contextlib.ExitStack() as pM:
            rowbcA = sb("rowbcA", [128, 2, D], F32, pM)
            build_rowbc(rowbcA, rrowA, lambda s, c: modT[:, 16 + c, s:s + 1])
            phase_merge(pM, l, last)
            P.flush()
        if stop_after == "Bp" and l == 0:
            break

        with contextlib.ExitStack() as pC:
            rowbcA = sb("rowbcA", [128, 2, D], F32, pC)
            rowbcB = sb("rowbcB", [128, 2, D], F32, pC)
            build_rowbc(rowbcA, rrowA, lambda s, c: A2[:, c, s:s + 1])
            build_rowbc(rowbcB, rrowB, lambda s, c: modT[:, 24 + c, s:s + 1])
            phase_moe(pC, l, last)
            P.flush()
        if stop_after == "C" and l == 0:
            break

    P.flush()
    _DBG["stats"] = dict(nops=len(P.ops), nwait=P.nwait, ecount=dict(P.ecount), dmax=max(P.dcount))


def rope_apply(P, nc, src, rsrc, RT, rRT, nh, t1, t2, dst, rtmp, rdst, pool=False):
    s3 = src.rearrange("p (h d) -> p h d", d=64)
    t13 = t1[:, 0:nh * 64].rearrange("p (h d) -> p h d", d=64)
    t23 = t2[:, 0:nh * 64].rearrange("p (h d) -> p h d", d=64)
    (P.pool if pool else P.dve)(lambda: (nc.gpsimd if pool else nc.vector).tensor_tensor(out=t13, in0=s3, in1=bc(RT[:, 0:64], 1, nh), op=ALU.mult), reads=[rsrc, rRT], writes=[rtmp])
    (P.pool if pool else P.dve)(lambda: (nc.gpsimd if pool else nc.vector).tensor_tensor(out=t23[:, :, 0:32], in0=s3[:, :, 32:64], in1=bc(RT[:, 64:96], 1, nh), op=ALU.mult),
          reads=[rsrc, rRT], writes=[rtmp])
    (P.pool if pool else P.dve)(lambda: (nc.gpsimd if pool else nc.vector).tensor_tensor(out=t23[:, :, 32:64], in0=s3[:, :, 0:32], in1=bc(RT[:, 96:128], 1, nh), op=ALU.mult),
          reads=[rsrc, rRT], writes=[rtmp])
    (P.pool if pool else P.dve)(lambda: (nc.gpsimd if pool else nc.vector).tensor_tensor(out=dst, in0=t13, in1=t23, op=ALU.add), reads=[rtmp], writes=[rdst])


def _host_inputs(inp):
    f32 = np.float32
    g = lambda k: np.ascontiguousarray(np.asarray(inp[k], dtype=f32))
    x, c, ctx, c_ctx = g("x"), g("c"), g("ctx"), g("c_ctx")
    B = x.shape[0]
    shared = {}
    shared["w_mod"] = g("w_mod")
    shared["bmT"] = np.ascontiguousarray(g("b_mod").reshape(DEPTH, 48, 128).transpose(0, 2, 1))
    shared["n1T"] = np.ascontiguousarray(g("norm1_g").reshape(DEPTH, 8, 128).transpose(0, 2, 1))
    shared["n2T"] = np.ascontiguousarray(g("norm2_g").reshape(DEPTH, 8, 128).transpose(0, 2, 1))
    shared["w_in"] = g("w_in")
    df, db = g("ret_decay_fwd"), g("ret_decay_bwd")
    decbc = np.concatenate([df, db], axis=1)
    shared["decbc"] = np.ascontiguousarray(np.broadcast_to(decbc[:, None, :], (DEPTH, 128, 8)))
    hp = np.zeros((DEPTH, 128, 4), f32)
    for dr, dd in enumerate((df, db)):
        for hb in range(2):
            hp[:, 0:64, dr * 2 + hb] = dd[:, 2 * hb][:, None]
            hp[:, 64:128, dr * 2 + hb] = dd[:, 2 * hb + 1][:, None]
    shared["dechp"] = hp
    shared["sgu_wT"] = np.ascontiguousarray(g("sgu_w").transpose(0, 1, 3, 2))
    shared["sgu_bbc"] = np.ascontiguousarray(np.broadcast_to(g("sgu_b").reshape(DEPTH, 1, 512), (DEPTH, 128, 512)))
    shared["qg"] = np.ascontiguousarray(np.broadcast_to(g("q_norm_g")[:, None, :], (DEPTH, 128, 64)))
    shared["kg"] = np.ascontiguousarray(np.broadcast_to(g("k_norm_g")[:, None, :], (DEPTH, 128, 64)))
    shared["sink"] = np.ascontiguousarray(np.broadcast_to(g("attn_sink")[:, None, :], (DEPTH, 128, 8)))
    for k in ("w_branch", "w_out", "w_router", "w_exp_gate", "w_exp_up", "w_exp_down"):
        shared[k] = g(k)
    shared["rope"] = _rope_tables()
    shared["ctab"] = _const_table()
    maps = []
    for b in range(B):
        m = dict(shared)
        m["x"] = x[b]
        m["ctxin"] = ctx[b]
        cT = np.stack([c[b].reshape(8, 128).T, c_ctx.reshape(8, 128).T], axis=-1)
        m["cT"] = np.ascontiguousarray(cT.reshape(128, 16))
        maps.append(m)
    return maps


_NC_CACHE = {}


def kernel(**inputs):
    maps = _host_inputs(inputs)
    if "nc" not in _NC_CACHE:
        _NC_CACHE["nc"] = build_program()
    nc = _NC_CACHE["nc"]
    n = len(maps)
    res = run_bass_kernel_spmd(nc, maps, core_ids=list(range(n)))
    return np.stack([np.asarray(r["out"], dtype=np.float32) for r in res.results], axis=0)
```

```python
import contextlib
import numpy as np
import concourse.bass as bass
import concourse.mybir as mybir
from concourse.bass_utils import run_bass_kernel_spmd

F32 = mybir.dt.float32
BF16 = mybir.dt.bfloat16
I32 = mybir.dt.int32
AF = mybir.ActivationFunctionType
ALU = mybir.AluOpType
AX = mybir.AxisListType

D = 1024
SEQ = 4096
CTX = 256
NLAT = 32
NT = 34
DEPTH = 2
NEXP = 16
EPS = 1e-6
INW = 6400
NG = 3328

_DBG = {}


class Res:
    __slots__ = ("name", "w", "r", "excl")

    def __init__(self, name="", excl=False):
        self.name = name
        self.w = None
        self.r = []
        self.excl = excl


class _Op:
    __slots__ = ("eng", "fn", "reads", "writes", "dma", "deps", "inc", "val", "semid")

    def __init__(self, eng, fn, reads, writes, dma):
        self.eng = eng
        self.fn = fn
        self.reads = reads
        self.writes = writes
        self.dma = dma
        self.deps = ()
        self.inc = False
        self.val = 0
        self.semid = None


class Prog:
    NDMASEM = 64
    NHW = 32

    def __init__(self, nc, es):
        self.nc = nc
        self.ops = []
        self.done = 0
        self.engs = {"pe": nc.tensor, "act": nc.scalar, "dve": nc.vector,
                     "pool": nc.gpsimd, "sp": nc.sync}
        self.sems = {e: es.enter_context(nc.semaphore("sem_" + e)) for e in self.engs}
        self.dsems = [es.enter_context(nc.semaphore("dsem%d" % i)) for i in range(self.NDMASEM)]
        self.dlast = [None] * self.NDMASEM
        self.drr = 0
        self.drr_sw = 0
        self.ecount = {e: 0 for e in self.engs}
        self.dcount = [0] * self.NDMASEM
        self.waited = {e: {} for e in self.engs}
        self.nwait = 0
        self._cap = None
        self._unit = None

    def add(self, eng, fn, reads=(), writes=(), dma=False):
        reads = tuple(reads)
        writes = tuple(writes)
        ex = tuple(r for r in reads if r.excl)
        if ex:
            reads = tuple(r for r in reads if not r.excl)
            writes = writes + ex
        op = _Op(eng, fn, reads, writes, dma)
        if self._cap is not None:
            if self._unit is not None:
                self._unit.append(op)
            else:
                self._cap.append([op])
        else:
            self.ops.append(op)

    def capture_start(self):
        self._cap = []
        self._unit = None

    def capture_end(self):
        cap = self._cap
        self._cap = None
        return cap

    @contextlib.contextmanager
    def atomic(self):
        if self._cap is None or self._unit is not None:
            yield
            return
        self._unit = []
        try:
            yield
        finally:
            self._cap.append(self._unit)
            self._unit = None

    def emit_zip(self, chains):
        items = []
        for ci, ch in enumerate(chains):
            lo, hi = 0.0, 1.0
            if isinstance(ch, tuple):
                ch, lo, hi = ch
            n = len(ch)
            for ui, unit in enumerate(ch):
                items.append((lo + (hi - lo) * (ui + 0.5) / n, ci, ui, unit))
        items.sort(key=lambda t: (t[0], t[1]))
        for _, _, _, unit in items:
            self.ops.extend(unit)

    def pe(self, fn, reads=(), writes=()):
        self.add("pe", fn, reads, writes)

    def act(self, fn, reads=(), writes=()):
        self.add("act", fn, reads, writes)

    def dve(self, fn, reads=(), writes=()):
        self.add("dve", fn, reads, writes)

    def pool(self, fn, reads=(), writes=()):
        self.add("pool", fn, reads, writes)

    def dma(self, eng, fn, reads=(), writes=()):
        self.add(eng, fn, reads, writes, dma=True)

    def flush(self):
        ops = self.ops
        base = self.done
        n = len(ops)
        last_compute = {}
        for i in range(base, n):
            op = ops[i]
            raw = set()
            war = set()
            for r in op.reads:
                if r.w is not None:
                    raw.add(r.w)
            for w in op.writes:
                if w.w is not None:
                    raw.add(w.w)
                war.update(w.r)
            if op.dma:
                if op.eng == "pool":
                    s = self.NHW + self.drr_sw
                    self.drr_sw = (self.drr_sw + 1) % (self.NDMASEM - self.NHW)
                else:
                    s = self.drr
                    self.drr = (self.drr + 1) % self.NHW
                op.semid = s
                if self.dlast[s] is not None:
                    raw.add(self.dlast[s])
                self.dlast[s] = i
            else:
                last_compute[op.eng] = i
            deps = set()
            for j in raw:
                if j < base or j == i:
                    continue
                o = ops[j]
                if (not o.dma) and (not op.dma) and o.eng == op.eng and op.eng == "pe":
                    continue
                deps.add(j)
            for j in war:
                if j < base or j == i:
                    continue
                o = ops[j]
                if (not o.dma) and o.eng == op.eng and not op.dma and op.eng == "pe":
                    continue
                deps.add(j)
            for r in op.reads:
                r.r.append(i)
            for w in op.writes:
                w.w = i
                w.r = []
            best = {}
            for j in deps:
                o = ops[j]
                key = ("d", o.semid) if o.dma else ("e", o.eng)
                if key not in best or best[key] < j:
                    best[key] = j
            op.deps = tuple(best.values())
            for j in op.deps:
                ops[j].inc = True
        for e, i in last_compute.items():
            ops[i].inc = True
        for i in range(base, n):
            op = ops[i]
            if op.dma:
                self.dcount[op.semid] += 16
                op.val = self.dcount[op.semid]
            elif op.inc:
                self.ecount[op.eng] += 1
                op.val = self.ecount[op.eng]
        for i in range(base, n):
            op = ops[i]
            eng = self.engs[op.eng]
            wt = self.waited[op.eng]
            for j in op.deps:
                o = ops[j]
                if o.dma:
                    key, sem = ("d", o.semid), self.dsems[o.semid]
                else:
                    key, sem = ("e", o.eng), self.sems[o.eng]
                if wt.get(key, 0) >= o.val:
                    continue
                eng.wait_ge(sem, o.val)
                self.nwait += 1
                wt[key] = o.val
            inst = op.fn()
            if op.dma:
                inst.then_inc(self.dsems[op.semid], 16)
            elif op.inc:
                inst.then_inc(self.sems[op.eng], 1)
            op.fn = None
        for e, eng in self.engs.items():
            wt = self.waited[e]
            for x in self.engs:
                if x == e:
                    continue
                v = self.ecount[x]
                if v > wt.get(("e", x), 0):
                    eng.wait_ge(self.sems[x], v)
                    wt[("e", x)] = v
            for s in range(self.NDMASEM):
                v = self.dcount[s]
                if v > wt.get(("d", s), 0):
                    eng.wait_ge(self.dsems[s], v)
                    wt[("d", s)] = v
        self.done = n


def bc(ap, axis, n):
    s = list(ap.shape)
    s.insert(axis, n)
    return ap.unsqueeze(axis).to_broadcast(s)


def _rope_tables():
    f32 = np.float32
    rows = SEQ // 64
    row = np.repeat(np.arange(rows), 64).astype(f32)
    col = np.tile(np.arange(64), rows).astype(f32)
    nf = 16
    inv = np.power(f32(10000.0), -np.arange(nf, dtype=f32) / f32(nf)).astype(f32)
    ang = np.concatenate([row[:, None] * inv, col[:, None] * inv], axis=-1).astype(f32)
    cos = np.cos(ang).astype(f32)
    sin = np.sin(ang).astype(f32)
    tab = np.zeros((NT * 128, 128), f32)
    tab[:SEQ, 0:32] = cos
    tab[:SEQ, 32:64] = cos
    tab[:SEQ, 64:96] = -sin
    tab[:SEQ, 96:128] = sin
    tab[SEQ:, 0:64] = 1.0
    return tab.reshape(NT, 128, 128)


CT = {}


def _const_table():
    cols = []

    def put(name, arr):
        arr = np.asarray(arr, np.float32).reshape(128, -1)
        CT[name] = (sum(c.shape[1] for c in cols), arr.shape[1])
        cols.append(arr)

    j = np.arange(128)[:, None]
    i = np.arange(128)[None, :]
    put("ident", (j == i))
    put("ones", np.ones((128, 128)))
    put("PF", np.maximum(i - j, 0))
    put("PB", np.maximum(j - i, 0))
    put("MF", (i >= j))
    put("MB", (j > i))
    put("iota1", np.broadcast_to(i + 1, (128, 128)))
    put("cmi", np.broadcast_to(128 - i, (128, 128)))
    put("cjm", 127 - j)
    put("jcol", j)
    put("tri", (j < i))
    put("iota512", np.broadcast_to(np.arange(512)[None, :], (128, 512)))
    mp = np.where(j >= i, 0.0, -30000.0)
    mn = np.where(j <= i, 0.0, -30000.0)
    put("mprev", np.tile(mp, (1, 4)))
    put("mnext", np.tile(mn, (1, 4)))
    put("ttv", np.broadcast_to(np.arange(NLAT)[None, :], (128, NLAT)))
    put("eps", np.full((128, 1), EPS))
    return np.concatenate(cols, axis=1)


def build_program(stop_after=None, dbg_out=False):
    nc = bass.Bass("TRN2", target_bir_lowering=False)
    es = contextlib.ExitStack()
    with es:
        _build(nc, es, stop_after, dbg_out)
    return nc


def _build(nc, es, stop_after, dbg_out):
    ctab_np = _const_table()
    NCT = ctab_np.shape[1]

    def din(name, shape, dt=F32):
        return nc.dram_tensor(name, list(shape), dt, kind="ExternalInput").ap()

    x_in = din("x", [SEQ, D])
    ctx_in = din("ctxin", [CTX, D])
    cT_d = din("cT", [128, 16])
    w_mod = din("w_mod", [DEPTH, D, 6 * D])
    bmT_d = din("bmT", [DEPTH, 128, 48])
    n1T_d = din("n1T", [DEPTH, 128, 8])
    n2T_d = din("n2T", [DEPTH, 128, 8])
    w_in = din("w_in", [DEPTH, D, INW])
    decbc_d = din("decbc", [DEPTH, 128, 8])
    dechp_d = din("dechp", [DEPTH, 128, 4])
    swT_d = din("sgu_wT", [DEPTH, 4, 128, 128])
    sbb_d = din("sgu_bbc", [DEPTH, 128, 512])
    qg_d = din("qg", [DEPTH, 128, 64])
    kg_d = din("kg", [DEPTH, 128, 64])
    sink_d = din("sink", [DEPTH, 128, 8])
    w_br = din("w_branch", [DEPTH, 3, 512, D])
    w_out = din("w_out", [DEPTH, D, D])
    w_rt = din("w_router", [DEPTH, D, NEXP])
    w_eg = din("w_exp_gate", [DEPTH, NEXP, D, D])
    w_eu = din("w_exp_up", [DEPTH, NEXP, D, D])
    w_ed = din("w_exp_down", [DEPTH, NEXP, D, D])
    rope_d = din("rope", [NT, 128, 128])
    ctab_d = din("ctab", [128, NCT])
    out = nc.dram_tensor("out", [SEQ, D], F32, kind="ExternalOutput").ap()
    xc = nc.dram_tensor("xc", [CTX, D], F32, kind="ExternalOutput").ap()
    BR = nc.dram_tensor("BR", [3, 512, NT * 128], BF16, kind="ExternalOutput").ap()
    H2 = nc.dram_tensor("H2", [NT * 128, D], BF16, kind="ExternalOutput").ap()

    P = Prog(nc, es)

    _uid = [0]

    def sb(name, shape, dt, stack=es):
        _uid[0] += 1
        return stack.enter_context(nc.sbuf_tensor("%s_%d" % (name, _uid[0]), list(shape), dt))

    bank = [es.enter_context(nc.psum_tensor("bank%d" % i, [128, 512], F32)) for i in range(7)]
    bankT = es.enter_context(nc.psum_tensor("bankT", [128, 1024], BF16))
    rbank = [Res("bank%d" % i, excl=True) for i in range(7)]
    _rbT = Res("bankT", excl=True)
    rbT = [_rbT, _rbT]

    RS = [Res("S%d" % t) for t in range(NT)]
    RBR = [[Res() for _ in range(NT)] for _ in range(3)]
    RH2 = [Res() for _ in range(NT)]

    def srows(T):
        return out[T * 128:(T + 1) * 128, :] if T < NLAT else xc[(T - NLAT) * 128:(T - NLAT + 1) * 128, :]

    ctab = sb("ctab", [128, NCT], F32)
    rct = Res("ctab")
    P.dma("sp", lambda: nc.sync.dma_start(out=ctab[:], in_=ctab_d), writes=[rct])

    def ct(name):
        o, n = CT[name]
        return ctab[:, o:o + n]

    identb = sb("identb", [128, 128], BF16)
    onesb = sb("onesb", [128, 128], BF16)
    trib = sb("trib", [128, 128], BF16)
    mprevb = sb("mprevb", [128, 512], BF16)
    mnextb = sb("mnextb", [128, 512], BF16)
    rcb = Res("constb")
    for dst, nm in ((identb, "ident"), (onesb, "ones"), (trib, "tri"), (mprevb, "mprev"), (mnextb, "mnext")):
        (lambda dst, nm: P.dve(lambda: nc.vector.tensor_copy(out=dst[:], in_=ct(nm)), reads=[rct], writes=[rcb]))(dst, nm)
    epsc = ct("eps")
    negh = sb("negh", [128, 8], F32)
    rnegh = Res("negh")
    P.dve(lambda: nc.vector.memset(negh[:], -0.5), writes=[rnegh])

    for q in range(8):
        (lambda q: P.dma("sp", lambda: nc.sync.dma_start(out=out[q * 512:(q + 1) * 512, :], in_=x_in[q * 512:(q + 1) * 512, :]),
                         writes=RS[q * 4:(q + 1) * 4]))(q)
    P.dma("sp", lambda: nc.sync.dma_start(out=xc, in_=ctx_in), writes=RS[NLAT:NT])

    cact = sb("cact", [128, 8, 2], BF16)
    modT = sb("modT", [128, 48, 2], F32)
    A1 = sb("A1", [128, 8, 2], F32)
    A2 = sb("A2", [128, 8, 2], F32)
    rmod = Res("mod")
    rowbcA = None
    rowbcB = None
    rrowA = Res("rowA")
    rrowB = Res("rowB")
    dgt = [sb("dgt%d" % i, [128, 128], F32) for i in range(2)]
    rdg = [Res(), Res()]

    xt = [sb("xt%d" % i, [128, D], F32) for i in range(2)]
    rxt = [Res(), Res()]
    sqj = sb("sqj", [128, D], F32)
    rsq = Res()
    ssum = [sb("ssum%d" % i, [128, 4], F32) for i in range(2)]
    rss = [Res(), Res()]
    xn = [sb("xn%d" % i, [128, D], BF16) for i in range(2)]
    rxn = [Res(), Res()]
    ropet = [sb("ropet%d" % i, [128, 128], F32) for i in range(2)]
    rrope = [Res(), Res()]
    cnt = {"tile": 0, "ev": 0}

    def load_rope(T):
        i = T % 2
        P.dma("sp", lambda: nc.sync.dma_start(out=ropet[i][:], in_=rope_d[T]), writes=[rrope[i]])
        return ropet[i], rrope[i]

    def norm_tile(T, want_x=False):
        i = cnt["tile"] % 2
        cnt["tile"] += 1
        X, rX, S, rS, XN, rXN = xt[i], rxt[i], ssum[i], rss[i], xn[i], rxn[i]
        P.dma("sp", lambda: nc.sync.dma_start(out=X[:], in_=srows(T)), reads=[RS[T]], writes=[rX])
        P.dve(lambda: nc.vector.memset(S[:], 0.0), writes=[rS])
        P.act(lambda: nc.scalar.activation(out=sqj[:], in_=X[:], func=AF.Square, accum_out=S[:, 0:1]),
              reads=[rX], writes=[rsq, rS])
        P.dve(lambda: nc.vector.tensor_scalar(out=S[:, 1:2], in0=S[:, 0:1], scalar1=1.0 / D, scalar2=EPS, op0=ALU.mult, op1=ALU.add),
              reads=[rS], writes=[rS])
        P.pool(lambda: nc.gpsimd.tensor_tensor(out=S[:, 2:3], in0=S[:, 1:2], in1=negh[:, 0:1], op=ALU.pow), reads=[rS, rnegh], writes=[rS])
        P.dve(lambda: nc.vector.tensor_scalar(out=XN[:], in0=X[:], scalar1=S[:, 2:3], scalar2=None, op0=ALU.mult),
              reads=[rX, rS], writes=[rXN])
        return X, rX, XN, rXN, S, rS

    def transpose_mod(XN, rXN, A, Bm, s, dst_fn, rdst):
        for half in range(2):
          with P.atomic():
            rb = rbT[half]
            for kk in range(4):
                k = half * 4 + kk
                (lambda k, kk, half: P.pe(lambda: nc.tensor.transpose(out=bankT[:, half * 512 + kk * 128: half * 512 + (kk + 1) * 128],
                                                                       in_=XN[:, k * 128:(k + 1) * 128], identity=identb[:]),
                                          reads=[rXN, rcb], writes=[rb]))(k, kk, half)
            for kk in range(4):
                k = half * 4 + kk
                src = bankT[:, half * 512 + kk * 128: half * 512 + (kk + 1) * 128]
                if kk % 2 == 0:
                    (lambda k, src: P.act(lambda: nc.scalar.activation(out=dst_fn(k), in_=src, func=AF.Identity,
                                                                       scale=A[:, k, s:s + 1], bias=Bm[:, k, s:s + 1]),
                                          reads=[rb, rmod], writes=[rdst]))(k, src)
                else:
                    (lambda k, src: P.dve(lambda: nc.vector.tensor_scalar(out=dst_fn(k), in0=src, scalar1=A[:, k, s:s + 1],
                                                                          scalar2=Bm[:, k, s:s + 1], op0=ALU.mult, op1=ALU.add),
                                          reads=[rb, rmod], writes=[rdst]))(k, src)

    def build_rowbc(dst, rdst, col_fn):
        n = 0
        for s in range(2):
            for hb in range(2):
                b = bank[hb]
                rb = rbank[hb]
                for cc in range(4):
                    c = hb * 4 + cc
                    g = n % 2
                    n += 1
                    (lambda c, cc, g, s, b, rb: (
                        P.dve(lambda: nc.vector.tensor_scalar(out=dgt[g][:], in0=ct("ident"), scalar1=col_fn(s, c), scalar2=None, op0=ALU.mult),
                              reads=[rct, rmod], writes=[rdg[g]]),
                        P.pe(lambda: nc.tensor.matmul(b[:, cc * 128:(cc + 1) * 128], lhsT=ct("ones"), rhs=dgt[g][:], start=True, stop=True),
                             reads=[rdg[g], rct], writes=[rb])))(c, cc, g, s, b, rb)
                (lambda s, hb, b, rb: P.act(lambda: nc.scalar.copy(out=dst[:, s, hb * 512:(hb + 1) * 512], in_=b[:]),
                                            reads=[rb], writes=[rdst]))(s, hb, b, rb)

    def mm(o, lhsT, rhs, start, stop, reads, writes, tp=None):
        kw = {} if (tp is None or _DBG.get("notp")) else {"tile_position": tp}
        P.pe(lambda: nc.tensor.matmul(o, lhsT=lhsT, rhs=rhs, start=start, stop=stop, **kw), reads, writes)

    def tr(o, in_, ident, reads, writes):
        P.pe(lambda: nc.tensor.transpose(out=o, in_=in_, identity=ident), reads, writes)

    def actf(o, in_, func, reads, writes, **kw):
        P.act(lambda: nc.scalar.activation(out=o, in_=in_, func=func, **kw), reads, writes)

    def cpa(o, in_, reads, writes):
        P.act(lambda: nc.scalar.copy(out=o, in_=in_), reads, writes)

    def cpv(o, in_, reads, writes, eng="dve"):
        if eng == "POOL":
            eng = "pool"
        elif not _DBG.get("usepool"):
            eng = "dve"
        e = nc.vector if eng == "dve" else nc.gpsimd
        P.add(eng, lambda: e.tensor_copy(out=o, in_=in_), reads, writes)

    def tt(o, a, b, op, reads, writes, eng="dve"):
        if eng == "POOL":
            eng = "pool"
        elif not _DBG.get("usepool"):
            eng = "dve"
        e = nc.vector if eng == "dve" else nc.gpsimd
        P.add(eng, lambda: e.tensor_tensor(out=o, in0=a, in1=b, op=op), reads, writes)

    def tsc(o, a, s1, s2, op0, op1, reads, writes, eng="dve"):
        if eng == "POOL":
            eng = "pool"
        elif not _DBG.get("usepool"):
            eng = "dve"
        e = nc.vector if eng == "dve" else nc.gpsimd
        if op1 is None:
            P.add(eng, lambda: e.tensor_scalar(out=o, in0=a, scalar1=s1, scalar2=None, op0=op0), reads, writes)
        else:
            P.add(eng, lambda: e.tensor_scalar(out=o, in0=a, scalar1=s1, scalar2=s2, op0=op0, op1=op1), reads, writes)

    def stt(o, a, s, b, op0, op1, reads, writes, eng="dve"):
        if eng == "POOL":
            eng = "pool"
        elif not _DBG.get("usepool"):
            eng = "dve"
        e = nc.vector if eng == "dve" else nc.gpsimd
        P.add(eng, lambda: e.scalar_tensor_tensor(out=o, in0=a, scalar=s, in1=b, op0=op0, op1=op1), reads, writes)

    def mset(o, v, writes, eng="dve"):
        if eng == "POOL":
            eng = "pool"
        elif not _DBG.get("usepool"):
            eng = "dve"
        e = nc.vector if eng == "dve" else nc.gpsimd
        P.add(eng, lambda: e.memset(o, v), (), writes)

    def dsp(o, in_, reads, writes):
        P.dma("sp", lambda: nc.sync.dma_start(out=o, in_=in_), reads, writes)

    def dpl(o, in_, reads, writes):
        P.dma("pool", lambda: nc.gpsimd.dma_start(out=o, in_=in_), reads, writes)

    def rsqrt_chain(dst_sd, dst_r, src, scale, reads_res, wres):
        n, w = src.shape[0], src.shape[1]
        tsc(dst_sd, src, scale, EPS, ALU.mult, ALU.add, [reads_res], [wres])
        P.pool(lambda: nc.gpsimd.tensor_tensor(out=dst_r, in0=dst_sd, in1=negh[0:n, 0:w], op=ALU.pow), [wres, rnegh], [wres])

    def kwview(w):
        return w.rearrange("(k p) n -> p k n", p=128)

    def phase_B(stk, l, last, ST, rST, DTt, DfT, rdec):
        wng = sb("wng", [128, 8, NG], BF16, stk)
        rwng = Res()
        for q in range(4):
            c0, c1 = q * 832, (q + 1) * 832
            dpl(wng[:, :, c0:c1], kwview(w_in[l][:, c0:c1]), [], [rwng])
        wsT = sb("wsT", [128, 4, 128], BF16, stk)
        sbb = sb("sbb", [128, 512], F32, stk)
        qg = sb("qg", [128, 64], F32, stk)
        kg = sb("kg", [128, 64], F32, stk)
        esink = sb("esink", [128, 8], F32, stk)
        rsm = Res()
        dpl(wsT[:], swT_d[l].rearrange("g j i -> j g i"), [], [rsm])
        dsp(sbb[:], sbb_d[l], [], [rsm])
        dsp(qg[:], qg_d[l], [], [rsm])
        dsp(kg[:], kg_d[l], [], [rsm])
        dsp(esink[:], sink_d[l], [], [rsm])
        tsc(qg[:], qg[:], 0.125, None, ALU.mult, None, [rsm], [rsm])
        actf(esink[:], esink[:], AF.Exp, [rsm], [rsm])
        KT_all = sb("KT_all", [128, NT, 128], BF16, stk)
        V_all = sb("V_all", [128, NT, 2, 80], BF16, stk)
        rKV = [Res() for _ in range(NT)]
        mset(V_all[:], 1.0, rKV)
        hT = [sb("hTb%d" % i, [128, 8, 128], BF16, stk) for i in range(2)]
        rhT = [Res(), Res()]
        zqk = [sb("zqk%d" % i, [128, 512], F32, stk) for i in range(2)]
        vb = [sb("vb%d" % i, [128, 512], BF16, stk) for i in range(2)]
        sg = [sb("sg%d" % i, [128, 512], BF16, stk) for i in range(2)]
        uT = [sb("uT%d" % i, [128, 512], BF16, stk) for i in range(2)]
        gv = [sb("gv%d" % i, [128, 512], F32, stk) for i in range(2)]
        zq = [sb("zq%d" % i, [128, 512], F32, stk) for i in range(2)]
        zk = [sb("zk%d" % i, [128, 128], F32, stk) for i in range(2)]
        rzqk, rvb, rsg, ruT, rgv, rzq, rzk = [[Res(), Res()] for _ in range(7)]
        t1 = sb("t1", [128, 512], F32, stk)
        t2 = sb("t2", [128, 512], F32, stk)
        rtmp = Res()
        t1a = sb("t1a", [128, 512], F32, stk)
        t2a = sb("t2a", [128, 512], F32, stk)
        rtmpa = Res()
        qrb = sb("qrb", [128, 256], BF16, stk)
        krb = sb("krb", [128, 256], BF16, stk)
        rqk = Res()
        qT = sb("qT", [128, 2, 128], BF16, stk)
        qfT = sb("qfT", [128, 2, 2, 128], BF16, stk)
        kT = sb("kT", [128, 2, 128], BF16, stk)
        rqT = Res()
        SD = sb("SD", [128, 512], BF16, stk)
        rSD = Res()
        bst = sb("bst", [128, 4, 6], F32, stk)
        mv = sb("mv", [128, 4, 2], F32, stk)
        sdr = sb("sdr", [128, 8], F32, stk)
        rln = Res()
        yb = sb("yb", [128, 512], BF16, stk)
        retb = sb("retb", [128, 512], BF16, stk)
        ryb = Res()
        brT = [sb("brTo%d" % i, [128, 4, 128], BF16, stk) for i in range(3)]
        rbrT = [Res(), Res(), Res()]
        vn = sb("vn", [128, 512], BF16, stk)
        stmp = sb("stmp", [128, 512], F32, stk)
        rsg2 = Res()
        bst2 = sb("bst2", [128, 6], F32, stk)
        mv2 = sb("mv2", [128, 4], F32, stk)
        sq = sb("sq", [128, 512], F32, stk)
        qn = sb("qn", [128, 512], F32, stk)
        ssq = sb("ssq", [128, 24], F32, stk)
        ratt = Res()
        sqk = sb("sqk", [128, 128], F32, stk)
        kn = sb("kn", [128, 128], F32, stk)
        ssk = sb("ssk", [128, 8], F32, stk)
        rattk = Res()
        qperm = sb("qperm", [128, 4, 2, 64], BF16, stk)
        krb2 = sb("krb2", [128, 128], BF16, stk)
        qT2 = [sb("qT2_%d" % i, [128, 4, 128], BF16, stk) for i in range(2)]
        rqT2 = [Res(), Res()]
        PT = [sb("PT%d" % i, [128, 512], BF16, stk) for i in range(2)]
        rPT = [Res(), Res()]
        den = sb("den", [128, 16], F32, stk)
        attb = sb("attb", [128, 512], BF16, stk)
        rden = Res()
        BRv = [BR[i].rearrange("(c p) t -> p c t", p=128) for i in range(3)]
        tg = sb("tg", [128, 512], F32, stk)
        rtg = Res()
        pbc = {"n": 0}

        def pbank():
            i = pbc["n"] % 2
            pbc["n"] += 1
            return bank[i], rbank[i]

        def out_branch(i, T, srcb, rsrc):
            with P.atomic():
                for c in range(4):
                    tr(bankT[:, 512 + c * 128: 512 + (c + 1) * 128], srcb[:, c * 128:(c + 1) * 128], identb[:], [rsrc, rcb], [rbT[1]])
                cpa(brT[i][:], bankT[:, 512:1024].rearrange("p (c t) -> p c t", t=128), [rbT[1]], [rbrT[i]])
            dsp(BRv[i][:, :, T * 128:(T + 1) * 128], brT[i][:], [rbrT[i]], [RBR[i][T]])

        def Pst1(T, par):
            s = 0 if T < NLAT else 1
            X, rX, XN, rXN, S, rS = norm_tile(T)
            H, rH = hT[par], rhT[par]
            transpose_mod(XN, rXN, A1, B1m, s, lambda k: H[:, k, :], rH)

        def Pst(T, full, par):
            load_rope(T)
            H, rH = hT[par], rhT[par]

            def proj(c0, c1):
                b, rb = pbank()
                for k in range(8):
                    mm(b[:, 0:c1 - c0], H[:, k, :], wng[:, k, c0:c1], k == 0, k == 7, [rH, rwng], [rb])
                return b, rb

            if full:
                b, rb = proj(0, 512)
                cpa(zqk[par][:], b[:], [rb], [rzqk[par]])
                b, rb = proj(512, 1024)
                cpv(vb[par][:], b[:], [rb], [rvb[par]])
                b, rb = proj(1024, 1536)
                actf(tg[:], b[:], AF.Tanh, [rb], [rtg], scale=0.5)
                stt(sg[par][:], tg[:], 1.0, b[:], ALU.add, ALU.mult, [rtg, rb], [rsg[par]])
                b, rb = pbank()
                for c in range(4):
                    for k in range(8):
                        mm(b[:, c * 128:(c + 1) * 128], wng[:, k, 1536 + c * 128:1536 + (c + 1) * 128], H[:, k, :], k == 0, k == 7, [rH, rwng], [rb])
                actf(uT[par][:], b[:], AF.Gelu_apprx_tanh, [rb], [ruT[par]])
                b, rb = proj(2048, 2560)
                actf(gv[par][:], b[:], AF.Gelu_apprx_tanh, [rb], [rgv[par]])
                b, rb = proj(2560, 3072)
                cpv(zq[par][:], b[:], [rb], [rzq[par]])
            b, rb = proj(3072, 3328)
            cpv(zk[par][:], b[:, 0:128], [rb], [rzk[par]])
            cpa(V_all[:, T, :, 0:64], b[:, 128:256].rearrange("p (k d) -> p k d", d=64), [rb], [rKV[T]])

        def chainR(T, par):
            RT, rRT = ropet[T % 2], rrope[T % 2]
            if True:
                rope_apply(P, nc, zqk[par][:, 0:256], rzqk[par], RT, rRT, 4, t1, t2, qrb[:].rearrange("p (h d) -> p h d", d=64), rtmp, rqk)
                rope_apply(P, nc, zqk[par][:, 256:512], rzqk[par], RT, rRT, 4, t1, t2, krb[:].rearrange("p (h d) -> p h d", d=64), rtmp, rqk)
                with P.atomic():
                    for hb in range(2):
                        tr(bankT[:, hb * 128:(hb + 1) * 128], qrb[:, hb * 128:(hb + 1) * 128], identb[:], [rqk, rcb], [rbT[0]])
                        tr(bankT[:, 256 + hb * 128:256 + (hb + 1) * 128], krb[:, hb * 128:(hb + 1) * 128], identb[:], [rqk, rcb], [rbT[0]])
                    qv = bankT[:, 0:256].rearrange("p (b t) -> p b t", t=128)
                    cpa(qT[:], qv, [rbT[0]], [rqT])
                    for dr in range(2):
                        tt(qfT[:, dr, :, :], qv, DfT[:, dr, :, :], ALU.mult, [rbT[0], rdec], [rqT])
                    cpa(kT[:], bankT[:, 256:512].rearrange("p (b t) -> p b t", t=128), [rbT[0]], [rqT])
                for h in range(4):
                    hb, hl = h // 2, h % 2
                    bS = 2 + hl
                    mm(bank[bS][:, hb * 128:(hb + 1) * 128], kT[64 * hl:64 * hl + 64, hb, :], qT[64 * hl:64 * hl + 64, hb, :], True, True,
                       [rqT], [rbank[bS]], tp=(64 * hl, 0))
                for h in range(4):
                    hb, hl = h // 2, h % 2
                    bS = 2 + hl
                    tt(SD[:, h * 128:(h + 1) * 128], bank[bS][:, hb * 128:(hb + 1) * 128], DTt[:, h, :], ALU.mult, [rbank[bS], rdec], [rSD])
                for h in range(4):
                    hb, hl = h // 2, h % 2
                    o = bank[2][:, h * 128:(h + 1) * 128]
                    mm(o, qfT[64 * hl:64 * hl + 64, 0, hb, :], ST[64 * hl:64 * hl + 64, T, 0, hb, :], True, False, [rqT, rST], [rbank[2]], tp=(64 * hl, 0))
                    mm(o, qfT[64 * hl:64 * hl + 64, 1, hb, :], ST[64 * hl:64 * hl + 64, T, 1, hb, :], False, False, [rqT, rST], [rbank[2]], tp=(64 * hl, 0))
                    mm(o, SD[:, h * 128:(h + 1) * 128], vb[par][:, h * 128:(h + 1) * 128], False, True, [rSD, rvb[par]], [rbank[2]])
                cpa(t1[:], bank[2][:], [rbank[2]], [rtmp])
                for h in range(4):
                    (lambda h: P.dve(lambda: nc.vector.bn_stats(out=bst[:, h, :], in_=t1[:, h * 128:(h + 1) * 128]), [rtmp], [rln]))(h)
                    (lambda h: P.dve(lambda: nc.vector.bn_aggr(out=mv[:, h, :], in_=bst[:, h, :]), [rln], [rln]))(h)
                rsqrt_chain(sdr[:, 0:4], sdr[:, 4:8], mv[:, :, 1], 1.0, rln, rln)
                tsc(sdr[:, 4:8], sdr[:, 4:8], 0.5, None, ALU.mult, None, [rln], [rln])
                for h in range(4):
                    tsc(yb[:, h * 128:(h + 1) * 128], t1[:, h * 128:(h + 1) * 128], mv[:, h, 0:1], sdr[:, 4 + h:5 + h], ALU.subtract, ALU.mult,
                        [rtmp, rln], [ryb])
                tt(retb[:], yb[:], sg[par][:], ALU.mult, [ryb, rsg[par]], [ryb], eng="POOL")
                out_branch(0, T, retb, ryb)

        def chainS(T, par):
            if True:
                P.dve(lambda: nc.vector.bn_stats(out=bst2[:], in_=gv[par][:]), [rgv[par]], [rsg2])
                P.dve(lambda: nc.vector.bn_aggr(out=mv2[:, 0:2], in_=bst2[:]), [rsg2], [rsg2])
                rsqrt_chain(mv2[:, 2:3], mv2[:, 3:4], mv2[:, 1:2], 1.0, rsg2, rsg2)
                tsc(vn[:], gv[par][:], mv2[:, 0:1], mv2[:, 3:4], ALU.subtract, ALU.mult, [rsg2, rgv[par]], [rsg2])
                for g in range(4):
                    mm(bank[4][:, g * 128:(g + 1) * 128], vn[:, g * 128:(g + 1) * 128], wsT[:, g, :], True, True, [rsg2, rsm], [rbank[4]])
                tt(stmp[:], bank[4][:], sbb[:], ALU.add, [rbank[4], rsm], [rsg2])
                tt(brT[1][:].rearrange("p c t -> p (c t)"), stmp[:], uT[par][:], ALU.mult, [rsg2, ruT[par]], [rbrT[1]], eng="POOL")
                dsp(BRv[1][:, :, T * 128:(T + 1) * 128], brT[1][:], [rbrT[1]], [RBR[1][T]])

        def chainQ(T, par):
            RT, rRT = ropet[T % 2], rrope[T % 2]
            if True:
                zq3 = zq[par][:].rearrange("p (h d) -> p h d", d=64)
                qn3 = qn[:].rearrange("p (h d) -> p h d", d=64)
                actf(sq[:], zq[par][:], AF.Square, [rzq[par]], [ratt])
                P.dve(lambda: nc.vector.tensor_reduce(out=ssq[:, 0:8], in_=sq[:].rearrange("p (h d) -> p h d", d=64), axis=AX.X, op=ALU.add), [ratt], [ratt])
                rsqrt_chain(ssq[:, 8:16], ssq[:, 16:24], ssq[:, 0:8], 1.0 / 64, ratt, ratt)
                tt(qn3, zq3, bc(ssq[:, 16:24], 2, 64), ALU.mult, [rzq[par], ratt], [ratt])
                tt(qn3, qn3, bc(qg[:], 1, 8), ALU.mult, [ratt, rsm], [ratt], eng="POOL")
                for hk in range(2):
                    rope_apply(P, nc, qn[:, hk * 256:(hk + 1) * 256], ratt, RT, rRT, 4, t1a, t2a, qperm[:, :, hk, :], rtmpa, ratt, pool=True)
                p2 = T % 2
                with P.atomic():
                    for g in range(4):
                        tr(bankT[:, g * 128:(g + 1) * 128], qperm[:, g, :, :].rearrange("p k d -> p (k d)"), identb[:], [ratt, rcb], [rbT[0]])
                    cpa(qT2[p2][:], bankT[:, 0:512].rearrange("p (g t) -> p g t", t=128), [rbT[0]], [rqT2[p2]])

        def chainK(T, par):
            RT, rRT = ropet[T % 2], rrope[T % 2]
            zk3 = zk[par][:].rearrange("p (h d) -> p h d", d=64)
            kn3 = kn[:].rearrange("p (h d) -> p h d", d=64)
            actf(sqk[:], zk[par][:], AF.Square, [rzk[par]], [rattk])
            P.dve(lambda: nc.vector.tensor_reduce(out=ssk[:, 0:2], in_=sqk[:].rearrange("p (h d) -> p h d", d=64), axis=AX.X, op=ALU.add), [rattk], [rattk])
            rsqrt_chain(ssk[:, 2:4], ssk[:, 4:6], ssk[:, 0:2], 1.0 / 64, rattk, rattk)
            tt(kn3, zk3, bc(ssk[:, 4:6], 2, 64), ALU.mult, [rzk[par], rattk], [rattk])
            tt(kn3, kn3, bc(kg[:], 1, 2), ALU.mult, [rattk, rsm], [rattk])
            rope_apply(P, nc, kn[:], rattk, RT, rRT, 2, t1a, t2a, krb2[:].rearrange("p (h d) -> p h d", d=64), rtmpa, rattk)
            with P.atomic():
                tr(bankT[:, 512:640], krb2[:], identb[:], [rattk, rcb], [rbT[1]])
                cpa(KT_all[:, T, :], bankT[:, 512:640], [rbT[1]], [rKV[T]])

        stc = {"n": 0}

        def Yst(T):
            p2 = T % 2
            if T < NLAT:
                chunks = []
                if T > 0:
                    chunks.append((T - 1, mprevb))
                chunks.append((T, None))
                chunks += [(NLAT, None), (NLAT + 1, None)]
                if T < NLAT - 1:
                    chunks.append((T + 1, mnextb))
            else:
                chunks = [(NLAT, None), (NLAT + 1, None)]
            for hk in range(2):
                ob = bank[6]
                rob = rbank[6]
                for ci, (c, m) in enumerate(chunks):
                    i = stc["n"] % 2
                    stc["n"] += 1
                    sb_, rsb = bank[5], rbank[5]
                    mm(sb_[:, :], KT_all[64 * hk:64 * hk + 64, c, :], qT2[p2][64 * hk:64 * hk + 64, :, :].rearrange("p g t -> p (g t)"),
                       True, m is None, [rKV[c], rqT2[p2]], [rsb], tp=(64 * hk, 0))
                    if m is not None:
                        mm(sb_[:, :], identb[:], m[:], False, True, [rcb], [rsb])
                    actf(PT[i][:], sb_[:, :], AF.Exp, [rsb], [rPT[i]])
                    for g in range(4):
                        mm(ob[:, g * 128:g * 128 + 65], PT[i][:, g * 128:(g + 1) * 128], V_all[:, c, hk, 0:65], ci == 0 and g == 0,
                           ci == len(chunks) - 1 and g == 3, [rPT[i], rKV[c]], [rob])
                ov = ob[:, 0:512].rearrange("p (g e) -> p g e", e=128)
                tt(den[:, hk * 4:(hk + 1) * 4], ov[:, :, 64], esink[:, hk * 4:(hk + 1) * 4], ALU.add, [rob, rsm], [rden])
                (lambda hk: P.dve(lambda: nc.vector.reciprocal(out=den[:, 8 + hk * 4:8 + (hk + 1) * 4], in_=den[:, hk * 4:(hk + 1) * 4]), [rden], [rden]))(hk)
                tt(attb[:, hk * 256:(hk + 1) * 256].rearrange("p (g d) -> p g d", d=64), ov[:, :, 0:64], bc(den[:, 8 + hk * 4:8 + (hk + 1) * 4], 2, 64),
                   ALU.mult, [rob, rden], [rden])
            out_branch(2, T, attb, rden)

        L = [NLAT, NLAT + 1] + list(range(NLAT))
        full = [(not last) if T >= NLAT else True for T in L]
        Pst1(L[0], 0)
        Pst1(L[1], 1)
        Pst(L[0], full[0], 0)
        for i, T in enumerate(L):
            par = i % 2
            chains = []
            if T < NLAT and T >= 1:
                P.capture_start()
                chainK(T, par)
                chains.append((P.capture_end(), 0.0, 0.2))
            else:
                chainK(T, par)
            if i + 1 < len(L):
                P.capture_start()
                Pst(L[i + 1], full[i + 1], (i + 1) % 2)
                chains.append(P.capture_end())
            if i + 2 < len(L):
                P.capture_start()
                Pst1(L[i + 2], i % 2)
                chains.append(P.capture_end())
            if full[i]:
                for ch in (chainR, chainS, chainQ):
                    P.capture_start()
                    ch(T, par)
                    chains.append(P.capture_end())
            if T < NLAT and T >= 1:
                P.capture_start()
                Yst(T - 1)
                chains.append(P.capture_end())
            P.emit_zip(chains)
            if T == NLAT + 1 and not last:
                Yst(NLAT)
                Yst(NLAT + 1)
            if T == NLAT - 1:
                Yst(T)

    def phase_merge(stk, l, last):
        wgt = sb("wgt", [128, 8, 3072], BF16, stk)
        wbr = sb("wbr", [128, 3, 4, D], BF16, stk)
        wo = sb("wo", [128, 8, D], BF16, stk)
        rw = Res()
        for q in range(3):
            dpl(wgt[:, :, q * 1024:(q + 1) * 1024], kwview(w_in[l][:, NG + q * 1024:NG + (q + 1) * 1024]), [], [rw])
            dpl(wbr[:, q, :, :], kwview(w_br[l][q]), [], [rw])
        dpl(wo[:], kwview(w_out[l]), [], [rw])
        hTg = [sb("hTg%d" % i, [128, 8, 512], BF16, stk) for i in range(2)]
        rhTg = [Res(), Res()]
        brTi = [sb("brTi%d" % i, [128, 3, 4, 512], BF16, stk) for i in range(2)]
        rbrTi = [Res(), Res()]
        mT = sb("mT", [128, 8, 512], BF16, stk)
        rmT = Res()
        sgm = [sb("sgm%d" % i, [128, 512], F32, stk) for i in range(2)]
        rsgm = [Res(), Res()]
        acc = sb("acc", [128, 512], F32, stk)
        tmp = sb("tmpm", [128, 512], F32, stk)
        racc = Res()
        rtmpm = Res()
        xo = [sb("xo%d" % i, [128, D], F32, stk) for i in range(2)]
        rxo = [Res(), Res()]
        xr = [sb("xr%d" % i, [128, D], F32, stk) for i in range(2)]
        rxr = [Res(), Res()]
        BRv = [BR[i].rearrange("(c p) t -> p c t", p=128) for i in range(3)]
        groups = [list(range(g * 4, g * 4 + 4)) for g in range(8)]
        if not last:
            groups = [[NLAT, NLAT + 1]] + groups
        cnts = {"pc": 0, "xc": 0}

        def prologue(gi):
            grp = groups[gi]
            gp = gi % 2
            s_ = 0 if grp[0] < NLAT else 1
            ntok = 128 * len(grp)
            T0 = grp[0]
            for ti, T in enumerate(grp):
                X, rX, XN, rXN, S, rS = norm_tile(T)
                transpose_mod(XN, rXN, A1, B1m, s_, (lambda ti, gp: (lambda k: hTg[gp][:, k, ti * 128:(ti + 1) * 128]))(ti, gp), rhTg[gp])
            for i in range(3):
                dsp(brTi[gp][:, i, :, 0:ntok], BRv[i][:, :, T0 * 128:T0 * 128 + ntok], [RBR[i][T] for T in grp], [rbrTi[gp]])

        def compute(gi):
            grp = groups[gi]
            gp = gi % 2
            s_ = 0 if grp[0] < NLAT else 1
            ntok = 128 * len(grp)
            H, rH, BI, rBI = hTg[gp], rhTg[gp], brTi[gp], rbrTi[gp]
            for dc in range(8):
                for i in range(3):
                    p = cnts["pc"] % 3
                    cnts["pc"] += 1
                    bg, rbg, bb, rbb = bank[2 * p], rbank[2 * p], bank[2 * p + 1], rbank[2 * p + 1]
                    for k in range(8):
                        mm(bg[:, 0:ntok], wgt[:, k, i * 1024 + dc * 128:i * 1024 + (dc + 1) * 128], H[:, k, 0:ntok], k == 0, k == 7, [rw, rH], [rbg])
                    for k in range(4):
                        mm(bb[:, 0:ntok], wbr[:, i, k, dc * 128:(dc + 1) * 128], BI[:, i, k, 0:ntok], k == 0, k == 3, [rw, rBI], [rbb])
                    sgi, rsgi = sgm[cnts["pc"] % 2], rsgm[cnts["pc"] % 2]
                    actf(sgi[:, 0:ntok], bg[:, 0:ntok], AF.Sigmoid, [rbg], [rsgi])
                    if i == 0:
                        tt(acc[:, 0:ntok], sgi[:, 0:ntok], bb[:, 0:ntok], ALU.mult, [rsgi, rbb], [racc])
                    elif i == 1:
                        tt(tmp[:, 0:ntok], sgi[:, 0:ntok], bb[:, 0:ntok], ALU.mult, [rsgi, rbb], [rtmpm])
                        tt(acc[:, 0:ntok], acc[:, 0:ntok], tmp[:, 0:ntok], ALU.add, [racc, rtmpm], [racc], eng="POOL")
                    else:
                        tt(tmp[:, 0:ntok], sgi[:, 0:ntok], bb[:, 0:ntok], ALU.mult, [rsgi, rbb], [rtmpm])
                        tt(mT[:, dc, 0:ntok], acc[:, 0:ntok], tmp[:, 0:ntok], ALU.add, [racc, rtmpm], [rmT], eng="POOL")
            for ti, T in enumerate(grp):
                j = cnts["xc"] % 2
                cnts["xc"] += 1
                xoi, rxoi = xo[j], rxo[j]
                dsp(xr[j][:], srows(T), [RS[T]], [rxr[j]])
                for nb in range(2):
                    for k in range(8):
                        mm(bank[6][:, :], mT[:, k, ti * 128:(ti + 1) * 128], wo[:, k, nb * 512:(nb + 1) * 512], k == 0, k == 7, [rmT, rw], [rbank[6]])
                    tt(xoi[:, nb * 512:(nb + 1) * 512], bank[6][:, :], rowbcA[:, s_, nb * 512:(nb + 1) * 512], ALU.mult, [rbank[6], rrowA], [rxoi])
                tt(xoi[:], xoi[:], xr[j][:], ALU.add, [rxoi, rxr[j]], [rxoi], eng="POOL")
                dsp(srows(T), xoi[:], [rxoi], [RS[T]])

        prologue(0)
        for gi in range(len(groups)):
            chains = []
            P.capture_start()
            compute(gi)
            chains.append(P.capture_end())
            if gi + 1 < len(groups):
                P.capture_start()
                prologue(gi + 1)
                chains.append(P.capture_end())
            P.emit_zip(chains)

    def phase_moe(stk, l, last):
        streams = [(0, list(range(NLAT)), 512, 4, 128)]
        if not last:
            streams.append((1, [NLAT, NLAT + 1], 32, 1, 32))
        aff = sb("aff", [128, NT, NEXP], F32, stk)
        raff = Res()
        idxS = [sb("idxS%d" % i, [128, 4, NEXP], I32, stk) for i in range(2)]
        idxH = [sb("idxH%d" % i, [128, 4, NEXP], I32, stk) for i in range(2)]
        wsel = [sb("wsel%d" % i, [128, 4, NEXP], F32, stk) for i in range(2)]
        ridx = [Res(), Res()]

        wg = [sb("wg%d" % i, [128, 8, D], BF16, stk) for i in range(2)]
        wu = [sb("wu%d" % i, [128, 8, D], BF16, stk) for i in range(2)]
        wd = [sb("wd%d" % i, [128, 8, D], BF16, stk) for i in range(2)]
        rwe = [Res(), Res()]

        def loads(e):
            par = e % 2
            for (wt_, src) in ((wg, w_eg), (wu, w_eu), (wd, w_ed)):
                dpl(wt_[par][:], src[l][e].rearrange("(k p) n -> p k n", p=128), [], [rwe[par]])

        loads(0)

        with contextlib.ExitStack() as s1:
            wr = sb("wr", [128, 8, NEXP], BF16, s1)
            rwr = Res()
            dpl(wr[:], w_rt[l].rearrange("(k p) e -> p k e", p=128), [], [rwr])
            h2fs = [sb("h2fA", [128, D], F32, s1), sb("h2fB", [128, D], F32, s1)]
            rh2fs = [Res(), Res()]
            h2b = [sb("h2b%d" % i, [128, D], BF16, s1) for i in range(2)]
            rh2 = [Res(), Res()]
            h2T = [sb("h2T%d" % i, [128, 8, 128], BF16, s1) for i in range(2)]
            rh2T = [Res(), Res()]
            sm = [sb("sm%d" % i, [128, 8], F32, s1) for i in range(2)]
            ex = [sb("ex%d" % i, [128, NEXP], F32, s1) for i in range(2)]
            rsmx = [Res(), Res()]
            allT = [(s_, T) for (s_, tiles, cap, nch, rows) in streams for T in tiles]

            def st1(j):
                s_, T = allT[j]
                i = j % 2
                X, rX, XN, rXN, S, rS = norm_tile(T)
                h2f, rh2f = h2fs[i], rh2fs[i]
                stt(h2f[:], X[:], S[:, 2:3], rowbcA[:, s_, :], ALU.mult, ALU.mult, [rX, rS, rrowA], [rh2f])
                tt(h2b[i][:], h2f[:], rowbcB[:, s_, :], ALU.add, [rh2f, rrowB], [rh2[i]], eng="pool")
                dsp(H2[T * 128:(T + 1) * 128, :], h2b[i][:], [rh2[i]], [RH2[T]])
                for half in range(2):
                    with P.atomic():
                        for kk in range(4):
                            k = half * 4 + kk
                            tr(bankT[:, half * 512 + kk * 128:half * 512 + (kk + 1) * 128], h2b[i][:, k * 128:(k + 1) * 128], identb[:], [rh2[i], rcb], [rbT[half]])
                        cpa(h2T[i][:, half * 4:(half + 1) * 4, :], bankT[:, half * 512:(half + 1) * 512].rearrange("p (k t) -> p k t", t=128), [rbT[half]], [rh2T[i]])

            def st2(j):
                s_, T = allT[j]
                i = j % 2
                bk, rbk = bank[2 + i], rbank[2 + i]
                for k in range(8):
                    mm(bk[:, 0:NEXP], h2T[i][:, k, :], wr[:, k, :], k == 0, k == 7, [rh2T[i], rwr], [rbk])
                smi, exi, rsi = sm[i], ex[i], rsmx[i]
                P.dve(lambda: nc.vector.tensor_reduce(out=smi[:, 0:1], in_=bk[:, 0:NEXP], axis=AX.X, op=ALU.max), [rbk], [rsi])
                tsc(smi[:, 1:2], smi[:, 0:1], -1.0, None, ALU.mult, None, [rsi], [rsi])
                mset(smi[:, 2:3], 0.0, [rsi])
                actf(exi[:], bk[:, 0:NEXP], AF.Exp, [rbk, rsi], [rsi], bias=smi[:, 1:2], accum_out=smi[:, 2:3])
                P.dve(lambda: nc.vector.reciprocal(out=smi[:, 3:4], in_=smi[:, 2:3]), [rsi], [rsi])
                tsc(aff[:, T, :], exi[:], smi[:, 3:4], None, ALU.mult, None, [rsi], [raff])

            for j0 in range(0, len(allT), 2):
                chains = []
                for j in (j0, j0 + 1):
                    if j < len(allT):
                        P.capture_start()
                        st1(j)
                        st2(j)
                        chains.append(P.capture_end())
                P.emit_zip(chains)
            build_rowbc(rowbcA, rrowA, lambda s, c: modT[:, 40 + c, s:s + 1])
            P.flush()
        if _DBG.get("cstop") == "1":
            return

        with contextlib.ExitStack() as s2:
            ge = sb("ge", [128, NLAT, NEXP], BF16, s2)
            maskf = sb("maskf", [128, NLAT, NEXP], F32, s2)
            basef = sb("basef", [128, NLAT, NEXP], F32, s2)
            keyf = sb("keyf", [128, NLAT, NEXP], F32, s2)
            cntp = sb("cntp", [128, NLAT, NEXP], F32, s2)
            vals = sb("vals", [128, NLAT, NEXP, 4], BF16, s2)
            valf = sb("valf", [128, NLAT, NEXP], F32, s2)
            lo = sb("lo", [128, NEXP], F32, s2)
            mid = sb("mid", [128, NEXP], F32, s2)
            cpr = sb("cpr", [128, NEXP], F32, s2)
            cntr = sb("cntr", [128, NEXP], F32, s2)
            rr = Res()
            OH = [sb("OH%d" % i, [128, 512], BF16, s2) for i in range(4)]
            rOH = [Res() for _ in range(4)]
            lst = sb("lst", [128, NEXP, 4, 4], F32, s2)
            idxf = sb("idxf", [128, 4, NEXP], F32, s2)
            NIT = 26
            for (s, tiles, cap, nch, rows) in streams:
                nt = len(tiles)
                T0 = tiles[0]
                av = aff[:, T0:T0 + nt, :]
                W = nt * NEXP
                mset(lo[:], 0.0, [rr])
                mset(mid[:], 0.5, [rr])
                for it in range(NIT):
                    half = 0.5 ** (it + 1)
                    tt(ge[:, 0:nt, :], av, bc(mid[:], 1, nt), ALU.is_ge, [raff, rr], [rr])
                    mm(bank[0][:, 0:W], onesb[:], ge[:, 0:nt, :].rearrange("p t e -> p (t e)"), True, True, [rr, rcb], [rbank[0]])
                    P.dve(lambda W=W, nt=nt: nc.vector.tensor_reduce(out=cntr[:], in_=bank[0][:, 0:W].rearrange("p (t e) -> p e t", e=NEXP), axis=AX.X, op=ALU.add),
                          [rbank[0]], [rr])
                    tsc(cpr[:], cntr[:], float(cap) - 0.5, half, ALU.is_ge, ALU.mult, [rr], [rr])
                    stt(mid[:], cpr[:], half * 0.5, lo[:], ALU.add, ALU.add, [rr], [rr])
                    tt(lo[:], lo[:], cpr[:], ALU.add, [rr], [rr])
                tt(maskf[:, 0:nt, :], av, bc(lo[:], 1, nt), ALU.is_ge, [raff, rr], [rr])
                cpv(ge[:, 0:nt, :], maskf[:, 0:nt, :], [rr], [rr])
                gef = ge[:, 0:nt, :].rearrange("p t e -> p (t e)")
                mm(bank[0][:, 0:W], trib[:], gef, True, True, [rr, rcb], [rbank[0]])
                mm(bank[1][:, 0:W], onesb[:], gef, True, True, [rr, rcb], [rbank[1]])
                cpv(cntp[:, 0:nt, :].rearrange("p t e -> p (t e)"), bank[1][:, 0:W], [rbank[1]], [rr])
                mset(basef[:, 0, :], 0.0, [rr])
                for t_ in range(1, nt):
                    tt(basef[:, t_, :], basef[:, t_ - 1, :], cntp[:, t_ - 1, :], ALU.add, [rr], [rr])
                tt(keyf[:, 0:nt, :].rearrange("p t e -> p (t e)"), bank[0][:, 0:W], basef[:, 0:nt, :].rearrange("p t e -> p (t e)"), ALU.add, [rbank[0], rr], [rr])
                stt(keyf[:, 0:nt, :], keyf[:, 0:nt, :], 1.0, maskf[:, 0:nt, :], ALU.add, ALU.mult, [rr], [rr])
                tsc(keyf[:, 0:nt, :], keyf[:, 0:nt, :], -1.0, None, ALU.add, None, [rr], [rr])
                cpv(vals[:, 0:nt, :, 0], bc(ct("ttv")[:, 0:nt], 2, NEXP), [rct], [rr])
                cpv(vals[:, 0:nt, :, 1], ct("jcol").unsqueeze(2).to_broadcast([128, nt, NEXP]), [rct], [rr])
                cpv(vals[:, 0:nt, :, 2], av, [raff], [rr])
                cpv(valf[:, 0:nt, :], vals[:, 0:nt, :, 2], [rr], [rr])
                tt(vals[:, 0:nt, :, 3], av, valf[:, 0:nt, :], ALU.subtract, [raff, rr], [rr])
                capw = nch * 128
                oc = 0
                rvals = rr
                for e in range(NEXP):
                    for t_ in range(nt):
                        i = oc % 4
                        oc += 1
                        tsc(OH[i][:, 0:capw], ct("iota512")[:, 0:capw], keyf[:, t_, e:e + 1], None, ALU.is_equal, None, [rct, rvals], [rOH[i]],
                            eng="dve")
                        for c in range(nch):
                            mm(bank[3][:, (e * 4 + c) * 4:(e * 4 + c) * 4 + 4], OH[i][:, c * 128:(c + 1) * 128], vals[:, t_, e, :],
                               e == 0 and t_ == 0 and c == 0, e == NEXP - 1 and t_ == nt - 1 and c == nch - 1, [rOH[i], rvals], [rbank[3]])
                lv = lst[:, :, 0:nch, :]
                cpv(lv, bank[3][:, 0:256].rearrange("p (e c v) -> p e c v", c=4, v=4)[:, :, 0:nch, :], [rbank[3], rvals], [rr])
                iv = idxf[:, 0:nch, :].rearrange("p c e -> p e c")
                stt(iv, lv[:, :, :, 0], 128.0, lv[:, :, :, 1], ALU.mult, ALU.add, [rr], [rr])
                cpv(idxS[s][:, 0:nch, :], idxf[:, 0:nch, :], [rr], [ridx[s]])
                tsc(idxf[:, 0:nch, :], idxf[:, 0:nch, :], float(T0 * 128), None, ALU.add, None, [rr], [rr])
                cpv(idxH[s][:, 0:nch, :], idxf[:, 0:nch, :], [rr], [ridx[s]])
                tt(wsel[s][:, 0:nch, :].rearrange("p c e -> p e c"), lv[:, :, :, 2], lv[:, :, :, 3], ALU.add, [rr], [ridx[s]])
            P.flush()
        if _DBG.get("cstop") == "2":
            return

        NS = len(streams)
        xs = [sb("xs%d" % s_, [128, 4 if s_ == 0 else 1, D], BF16, stk) for s_ in range(NS)]
        rxs = [Res() for _ in range(NS)]
        xsT = [[sb("xsT%d_%d" % (s_, i), [128, 8, 512 if s_ == 0 else 32], BF16, stk) for i in range(2)] for s_ in range(NS)]
        rxsT = [[Res(), Res()] for _ in range(NS)]
        sa = [sb("sa%d" % i, [128, 512], F32, stk) for i in range(2)]
        rsa = [Res(), Res()]
        hf = [sb("hf%d" % s_, [128, 8, 512 if s_ == 0 else 32], BF16, stk) for s_ in range(NS)]
        rhf = [Res() for _ in range(NS)]
        yscs = [sb("ysc%d" % i, [128, D], F32, stk) for i in range(4)]
        ryscs = [Res() for _ in range(4)]
        ycn = {"n": 0}

        def gather(e):
            for (s, tiles, cap, nch, rows) in streams:
                for c in range(nch):
                    (lambda c, s, rows, e: P.dma("pool", lambda: nc.gpsimd.indirect_dma_start(
                        out=xs[s][0:rows, c, :], out_offset=None, in_=H2,
                        in_offset=bass.IndirectOffsetOnAxis(ap=idxH[s][0:rows, c, e:e + 1], axis=0)),
                        [ridx[s]] + RH2, [rxs[s]]))(c, s, rows, e)

        def G(e):
            par = e % 2
            for (s, tiles, cap, nch, rows) in streams:
                ntok = nch * rows
                for k in range(8):
                    half = k % 2
                    with P.atomic():
                        for c in range(nch):
                            tr(bankT[:, half * 512 + c * rows: half * 512 + (c + 1) * rows], xs[s][0:rows, c, k * 128:(k + 1) * 128], identb[0:rows, 0:rows],
                               [rxs[s], rcb], [rbT[half]])
                        if k % 2 == 0:
                            cpa(xsT[s][par][:, k, 0:ntok], bankT[:, half * 512: half * 512 + ntok], [rbT[half]], [rxsT[s][par]])
                        else:
                            cpv(xsT[s][par][:, k, 0:ntok], bankT[:, half * 512: half * 512 + ntok], [rbT[half]], [rxsT[s][par]])

        sac = sb("sac", [128, 64], F32, stk)
        rsac = Res()
        yscc = sb("yscc", [128, D], F32, stk)
        ryscc = Res()

        def up(e, si):
            par = e % 2
            (s, tiles, cap, nch, rows) = streams[si]
            ntok = nch * rows
            for fc in range(8):
                if s == 0:
                    p = fc % 2
                    ba, rba, bu, rbu = bank[2 * p][:, 0:ntok], rbank[2 * p], bank[2 * p + 1][:, 0:ntok], rbank[2 * p + 1]
                    sai, rsai = sa[p][:, 0:ntok], rsa[p]
                else:
                    ba, rba, bu, rbu = bank[6][:, 0:ntok], rbank[6], bank[6][:, 64:64 + ntok], rbank[6]
                    sai, rsai = sac[:, 0:ntok], rsac
                for k in range(8):
                    mm(ba, wg[par][:, k, fc * 128:(fc + 1) * 128], xsT[s][par][:, k, 0:ntok], k == 0, k == 7, [rwe[par], rxsT[s][par]], [rba])
                for k in range(8):
                    mm(bu, wu[par][:, k, fc * 128:(fc + 1) * 128], xsT[s][par][:, k, 0:ntok], k == 0, k == 7, [rwe[par], rxsT[s][par]], [rbu])
                actf(sai, ba, AF.Silu, [rba], [rsai])
                tt(hf[s][:, fc, 0:ntok], sai, bu, ALU.mult, [rsai, rbu], [rhf[s]])

        def down(e, si):
            par = e % 2
            (s, tiles, cap, nch, rows) = streams[si]
            dst = out if s == 0 else xc
            rstream = [RS[T] for T in tiles]
            for c in range(nch):
                if s == 0:
                    ysc, rysc = yscs[ycn["n"] % 4], ryscs[ycn["n"] % 4]
                    ycn["n"] += 1
                else:
                    ysc, rysc = yscc, ryscc
                for nb in range(2):
                    if s == 0:
                        bd, rbd = bank[4 + nb], rbank[4 + nb]
                    else:
                        bd, rbd = bank[6], rbank[6]
                    for fc in range(8):
                        mm(bd[0:rows, :], hf[s][:, fc, c * rows:(c + 1) * rows], wd[par][:, fc, nb * 512:(nb + 1) * 512], fc == 0, fc == 7, [rhf[s], rwe[par]], [rbd])
                    stt(ysc[0:rows, nb * 512:(nb + 1) * 512], bd[0:rows, :], wsel[s][0:rows, c, e:e + 1], rowbcA[0:rows, s, nb * 512:(nb + 1) * 512],
                        ALU.mult, ALU.mult, [rbd, ridx[s], rrowA], [rysc])
                if _DBG.get("noscatter"):
                    continue
                (lambda c, s, rows, e, dst, rstream, ysc, rysc: P.dma("pool", lambda: nc.gpsimd.indirect_dma_start(
                    out=dst, out_offset=bass.IndirectOffsetOnAxis(ap=idxS[s][0:rows, c, e:e + 1], axis=0),
                    in_=ysc[0:rows, :], in_offset=None, compute_op=ALU.add),
                    [rysc, ridx[s]] + rstream, rstream))(c, s, rows, e, dst, rstream, ysc, rysc)

        gather(0)
        G(0)
        for e in range(NEXP):
            if e + 1 < NEXP:
                gather(e + 1)
                loads(e + 1)
            chains = []
            for si in range(len(streams)):
                P.capture_start()
                up(e, si)
                down(e, si)
                chains.append(P.capture_end())
            if e + 1 < NEXP:
                P.capture_start()
                G(e + 1)
                chains.append((P.capture_end(), 0.6, 0.98))
            P.emit_zip(chains)

    for l in range(DEPTH):
        last = (l == DEPTH - 1)
        tiles_ctx = [NLAT, NLAT + 1]
        tiles_lat = list(range(NLAT))

        with contextlib.ExitStack() as ps:
            cTt = sb("cTt", [128, 8, 2], F32, ps)
            bmT = sb("bmT", [128, 48], F32, ps)
            n1T = sb("n1T", [128, 8], F32, ps)
            n2T = sb("n2T", [128, 8], F32, ps)
            wm = [sb("wm%d" % i, [128, 8, 1024], BF16, ps) for i in range(2)]
            rwm = [Res(), Res()]
            rsm = Res()
            P.dma("sp", lambda: nc.sync.dma_start(out=cTt[:], in_=cT_d.rearrange("p (k s) -> p k s", s=2)), writes=[rsm])
            P.dma("sp", lambda: nc.sync.dma_start(out=bmT[:], in_=bmT_d[l]), writes=[rsm])
            P.dma("sp", lambda: nc.sync.dma_start(out=n1T[:], in_=n1T_d[l]), writes=[rsm])
            P.dma("sp", lambda: nc.sync.dma_start(out=n2T[:], in_=n2T_d[l]), writes=[rsm])
            P.act(lambda: nc.scalar.activation(out=cact[:], in_=cTt[:], func=AF.Silu), reads=[rsm], writes=[rmod])
            for sec in range(6 if stop_after != "M1" else 0):
                i = sec % 2
                (lambda sec, i: P.dma("pool", lambda: nc.gpsimd.dma_start(
                    out=wm[i][:], in_=w_mod[l][:, sec * 1024:(sec + 1) * 1024].rearrange("(k p) n -> p k n", p=128)),
                    writes=[rwm[i]]))(sec, i)
                for j in range(8):
                    col = (sec * 8 + j) * 2
                    for k in range(8):
                        (lambda i, j, k, col: P.pe(lambda: nc.tensor.matmul(bank[2][:, col:col + 2], lhsT=wm[i][:, k, j * 128:(j + 1) * 128],
                                                                            rhs=cact[:, k, :], start=(k == 0), stop=(k == 7)),
                                                   reads=[rwm[i], rmod], writes=[rbank[2]]))(i, j, k, col)
            if stop_after in ("M1", "M2"):
                P.flush()
                break
            P.dve(lambda: nc.vector.tensor_tensor(out=modT[:], in0=bank[2][:, 0:96].rearrange("p (j s) -> p j s", s=2),
                                                  in1=bc(bmT[:], 2, 2), op=ALU.add), reads=[rbank[2], rsm], writes=[rmod])
            if stop_after == "M3":
                P.flush()
                break
            for (Ax, nT, sec) in ((A1, n1T, 1), (A2, n2T, 4)):
                (lambda Ax, nT, sec: P.dve(lambda: nc.vector.scalar_tensor_tensor(
                    out=Ax[:], in0=modT[:, sec * 8:(sec + 1) * 8, :], scalar=1.0, in1=bc(nT[:], 2, 2), op0=ALU.add, op1=ALU.mult),
                    reads=[rmod, rsm], writes=[rmod]))(Ax, nT, sec)
            P.flush()
        B1m = modT[:, 0:8, :]
        B2m = modT[:, 24:32, :]
        if stop_after == "M" and l == 0:
            break

        with contextlib.ExitStack() as pAB:
            ST = sb("ST", [128, NT, 2, 2, 128], BF16, pAB)
            rST = Res("ST")
            decbc = sb("decbc", [128, 8], F32, pAB)
            dechp = sb("dechp", [128, 4], F32, pAB)
            lgbc = sb("lgbc", [128, 8], F32, pAB)
            lghp = sb("lghp", [128, 4], F32, pAB)
            DTt = sb("DT", [128, 4, 128], F32, pAB)
            DfT = sb("DfT", [128, 2, 2, 128], F32, pAB)
            wkd = sb("wkd", [128, 2, 4], F32, pAB)
            Gam = sb("Gam", [128, 4], F32, pAB)
            e1 = sb("e1", [128, 128], F32, pAB)
            e2 = sb("e2", [128, 128], F32, pAB)
            rdec = Res("dec")
            re1 = Res()
            P.dma("sp", lambda: nc.sync.dma_start(out=decbc[:], in_=decbc_d[l]), writes=[rdec])
            P.dma("sp", lambda: nc.sync.dma_start(out=dechp[:], in_=dechp_d[l]), writes=[rdec])
            for (src, dst) in ((decbc, lgbc), (dechp, lghp)):
                (lambda src, dst: (
                    P.act(lambda: nc.scalar.activation(out=dst[:], in_=src[:], func=AF.Exp, scale=-1.0), reads=[rdec], writes=[rdec]),
                    P.dve(lambda: nc.vector.tensor_scalar(out=dst[:], in0=dst[:], scalar1=1.0, scalar2=None, op0=ALU.add), reads=[rdec], writes=[rdec]),
                    P.act(lambda: nc.scalar.activation(out=dst[:], in_=dst[:], func=AF.Ln), reads=[rdec], writes=[rdec]),
                    P.dve(lambda: nc.vector.tensor_scalar(out=dst[:], in0=dst[:], scalar1=-1.0, scalar2=None, op0=ALU.mult), reads=[rdec], writes=[rdec])))(src, dst)
            KS = 0.125
            for h in range(4):
                (lambda h: (
                    P.act(lambda: nc.scalar.activation(out=e1[:], in_=ct("PF"), func=AF.Exp, scale=lgbc[:, h:h + 1]), reads=[rdec, rct], writes=[re1]),
                    P.dve(lambda: nc.vector.tensor_tensor(out=e1[:], in0=e1[:], in1=ct("MF"), op=ALU.mult), reads=[re1, rct], writes=[re1]),
                    P.act(lambda: nc.scalar.activation(out=e2[:], in_=ct("PB"), func=AF.Exp, scale=lgbc[:, 4 + h:5 + h]), reads=[rdec, rct], writes=[re1]),
                    P.dve(lambda: nc.vector.tensor_tensor(out=e2[:], in0=e2[:], in1=ct("MB"), op=ALU.mult), reads=[re1, rct], writes=[re1]),
                    P.dve(lambda: nc.vector.tensor_tensor(out=e1[:], in0=e1[:], in1=e2[:], op=ALU.add), reads=[re1], writes=[re1]),
                    P.dve(lambda: nc.vector.tensor_scalar(out=DTt[:, h, :], in0=e1[:], scalar1=KS, scalar2=None, op0=ALU.mult), reads=[re1], writes=[rdec])))(h)
            P.act(lambda: nc.scalar.activation(out=wkd[:, 0, :], in_=lgbc[:, 0:4], func=AF.Exp, scale=ct("cjm")), reads=[rdec, rct], writes=[rdec])
            P.act(lambda: nc.scalar.activation(out=wkd[:, 1, :], in_=lgbc[:, 4:8], func=AF.Exp, scale=ct("jcol")), reads=[rdec, rct], writes=[rdec])
            P.dve(lambda: nc.vector.tensor_scalar(out=wkd[:], in0=wkd[:], scalar1=KS, scalar2=None, op0=ALU.mult), reads=[rdec], writes=[rdec])
            for dr in range(2):
                for hb in range(2):
                    (lambda dr, hb: (
                        P.act(lambda: nc.scalar.activation(out=DfT[:, dr, hb, :], in_=ct("iota1") if dr == 0 else ct("cmi"), func=AF.Exp,
                                                           scale=lghp[:, dr * 2 + hb: dr * 2 + hb + 1]), reads=[rdec, rct], writes=[rdec]),
                        P.act(lambda: nc.scalar.activation(out=Gam[:, dr * 2 + hb: dr * 2 + hb + 1], in_=lghp[:, dr * 2 + hb: dr * 2 + hb + 1],
                                                           func=AF.Exp, scale=128.0), reads=[rdec], writes=[rdec])))(dr, hb)

            with contextlib.ExitStack() as pA:
                wkv = sb("wkv", [128, 8, 768], BF16, pA)
                rwkv = Res()
                P.dma("pool", lambda: nc.gpsimd.dma_start(out=wkv[:], in_=w_in[l][:, 256:1024].rearrange("(k p) n -> p k n", p=128)), writes=[rwkv])
                UPD = sb("UPD", [128, NT, 2, 2, 128], F32, pA)
                rUPD = Res()
                hTa = [sb("hTa%d" % i, [128, 8, 128], BF16, pA) for i in range(2)]
                rhTa = [Res(), Res()]
                t1 = sb("t1a", [128, 256], F32, pA)
                t2 = sb("t2a", [128, 256], F32, pA)
                kr = sb("kra", [128, 256], F32, pA)
                rt = Res()
                kw = [sb("kwa%d" % i, [128, 2, 256], BF16, pA) for i in range(2)]
                vb = [sb("vba%d" % i, [128, 512], BF16, pA) for i in range(2)]
                rkw = [Res(), Res()]
                tilesA = tiles_ctx + tiles_lat
                t1s = [t1, sb("t1a2", [128, 256], F32, pA)]
                t2s = [t2, sb("t2a2", [128, 256], F32, pA)]
                krs = [kr, sb("kra2", [128, 256], F32, pA)]
                rts = [rt, Res()]
                abanks = [(0, 1, 2), (4, 5, 6)]

                hTa4 = [hTa[0], hTa[1], sb("hTa2", [128, 8, 128], BF16, pA), sb("hTa3", [128, 8, 128], BF16, pA)]
                rhTa4 = [rhTa[0], rhTa[1], Res(), Res()]

                def chainA1(n_):
                    T = tilesA[n_]
                    s = 0 if T < NLAT else 1
                    X, rX, XN, rXN, S, rS = norm_tile(T)
                    H = hTa4[n_ % 4]
                    transpose_mod(XN, rXN, A1, B1m, s, (lambda H: (lambda k: H[:, k, :]))(H), rhTa4[n_ % 4])

                def chainA(n_, st):
                    T = tilesA[n_]
                    ik, iv, iu = abanks[st]
                    bk, rbk, bv, rbv, bu, rbu = bank[ik], rbank[ik], bank[iv], rbank[iv], bank[iu], rbank[iu]
                    load_rope(T)
                    H, rH = hTa4[n_ % 4], rhTa4[n_ % 4]
                    for k in range(8):
                        mm(bk[:, 0:256], H[:, k, :], wkv[:, k, 0:256], k == 0, k == 7, [rH, rwkv], [rbk])
                    for k in range(8):
                        mm(bv[:, 0:512], H[:, k, :], wkv[:, k, 256:768], k == 0, k == 7, [rH, rwkv], [rbv])
                    RT, rRT = ropet[T % 2], rrope[T % 2]
                    krv = krs[st][:].rearrange("p (h d) -> p h d", d=64)
                    rope_apply(P, nc, bk[:, 0:256], rbk, RT, rRT, 4, t1s[st], t2s[st], krv, rts[st], rts[st])
                    KW, V = kw[st], vb[st]
                    for dr in range(2):
                        tt(KW[:, dr, :].rearrange("p (h d) -> p h d", d=64), krv, bc(wkd[:, dr, :], 2, 64), ALU.mult, [rts[st], rdec], [rkw[st]])
                    cpa(V[:], bv[:], [rbv], [rkw[st]])
                    for dr in range(2):
                        for hb in range(2):
                            mm(bu[:, hb * 256:(hb + 1) * 256], KW[:, dr, hb * 128:(hb + 1) * 128], V[:, hb * 256:(hb + 1) * 256], True, True,
                               [rkw[st]], [rbu])
                        for hb in range(2):
                            cpv(UPD[0:64, T, dr, hb, :], bu[0:64, hb * 256:hb * 256 + 128], [rbu], [rUPD])
                            cpa(UPD[64:128, T, dr, hb, :], bu[64:128, hb * 256 + 128:hb * 256 + 256], [rbu], [rUPD])

                chainA1(0)
                chainA1(1)
                for n_ in range(0, len(tilesA), 2):
                    chains = []
                    for st in range(2):
                        if n_ + st < len(tilesA):
                            P.capture_start()
                            chainA(n_ + st, st)
                            chains.append(P.capture_end())
                    for st in range(2):
                        if n_ + 2 + st < len(tilesA):
                            P.capture_start()
                            chainA1(n_ + 2 + st)
                            chains.append(P.capture_end())
                    P.emit_zip(chains)
                cur = [sb("cur%d" % i, [128, 2, 128], F32, pA) for i in range(2)]
                rcur = Res()
                c0, c1 = NLAT, NLAT + 1
                for dr in range(2):
                    first, second = (c0, c1) if dr == 0 else (c1, c0)
                    (lambda dr, first, second: (
                        P.dve(lambda: nc.vector.memset(ST[:, first, dr, :, :], 0.0), writes=[rST]),
                        P.dve(lambda: nc.vector.tensor_copy(out=ST[:, second, dr, :, :], in_=UPD[:, first, dr, :, :]), reads=[rUPD], writes=[rST])))(dr, first, second)
                    order = list(range(NLAT)) if dr == 0 else list(range(NLAT - 1, -1, -1))
                    for hb in range(2):
                        (lambda dr, hb, first, second: P.dve(lambda: nc.vector.scalar_tensor_tensor(
                            out=cur[0][:, hb, :], in0=UPD[:, first, dr, hb, :], scalar=Gam[:, dr * 2 + hb: dr * 2 + hb + 1],
                            in1=UPD[:, second, dr, hb, :], op0=ALU.mult, op1=ALU.add), reads=[rUPD, rdec], writes=[rcur]))(dr, hb, first, second)
                    pp = 0
                    for n in order:
                        (lambda dr, n, pp: P.act(lambda: nc.scalar.copy(out=ST[:, n, dr, :, :], in_=cur[pp][:]), reads=[rcur], writes=[rST]))(dr, n, pp)
                        for hb in range(2):
                            (lambda dr, hb, n, pp: P.dve(lambda: nc.vector.scalar_tensor_tensor(
                                out=cur[1 - pp][:, hb, :], in0=cur[pp][:, hb, :], scalar=Gam[:, dr * 2 + hb: dr * 2 + hb + 1],
                                in1=UPD[:, n, dr, hb, :], op0=ALU.mult, op1=ALU.add), reads=[rUPD, rdec, rcur], writes=[rcur]))(dr, hb, n, pp)
                        pp = 1 - pp
                P.flush()
            if stop_after == "A" and l == 0:
                break

            with contextlib.ExitStack() as pB:
                phase_B(pB, l, last, ST, rST, DTt, DfT, rdec)
                P.flush()
        if stop_after == "B" and l == 0:
            break

        with contextlib.ExitStack() as pM:
            rowbcA = sb("rowbcA", [128, 2, D], F32, pM)
            build_rowbc(rowbcA, rrowA, lambda s, c: modT[:, 16 + c, s:s + 1])
            phase_merge(pM, l, last)
            P.flush()
        if stop_after == "Bp" and l == 0:
            break

        with contextlib.ExitStack() as pC:
            rowbcA = sb("rowbcA", [128, 2, D], F32, pC)
            rowbcB = sb("rowbcB", [128, 2, D], F32, pC)
            build_rowbc(rowbcA, rrowA, lambda s, c: A2[:, c, s:s + 1])
            build_rowbc(rowbcB, rrowB, lambda s, c: modT[:, 24 + c, s:s + 1])
            phase_moe(pC, l, last)
            P.flush()
        if stop_after == "C" and l == 0:
            break

    P.flush()
    _DBG["stats"] = dict(nops=len(P.ops), nwait=P.nwait, ecount=dict(P.ecount), dmax=max(P.dcount))


def rope_apply(P, nc, src, rsrc, RT, rRT, nh, t1, t2, dst, rtmp, rdst, pool=False):
    s3 = src.rearrange("p (h d) -> p h d", d=64)
    t13 = t1[:, 0:nh * 64].rearrange("p (h d) -> p h d", d=64)
    t23 = t2[:, 0:nh * 64].rearrange("p (h d) -> p h d", d=64)
    (P.pool if pool else P.dve)(lambda: (nc.gpsimd if pool else nc.vector).tensor_tensor(out=t13, in0=s3, in1=bc(RT[:, 0:64], 1, nh), op=ALU.mult), reads=[rsrc, rRT], writes=[rtmp])
    (P.pool if pool else P.dve)(lambda: (nc.gpsimd if pool else nc.vector).tensor_tensor(out=t23[:, :, 0:32], in0=s3[:, :, 32:64], in1=bc(RT[:, 64:96], 1, nh), op=ALU.mult),
          reads=[rsrc, rRT], writes=[rtmp])
    (P.pool if pool else P.dve)(lambda: (nc.gpsimd if pool else nc.vector).tensor_tensor(out=t23[:, :, 32:64], in0=s3[:, :, 0:32], in1=bc(RT[:, 96:128], 1, nh), op=ALU.mult),
          reads=[rsrc, rRT], writes=[rtmp])
    (P.pool if pool else P.dve)(lambda: (nc.gpsimd if pool else nc.vector).tensor_tensor(out=dst, in0=t13, in1=t23, op=ALU.add), reads=[rtmp], writes=[rdst])


def _host_inputs(inp):
    f32 = np.float32
    g = lambda k: np.ascontiguousarray(np.asarray(inp[k], dtype=f32))
    x, c, ctx, c_ctx = g("x"), g("c"), g("ctx"), g("c_ctx")
    B = x.shape[0]
    shared = {}
    shared["w_mod"] = g("w_mod")
    shared["bmT"] = np.ascontiguousarray(g("b_mod").reshape(DEPTH, 48, 128).transpose(0, 2, 1))
    shared["n1T"] = np.ascontiguousarray(g("norm1_g").reshape(DEPTH, 8, 128).transpose(0, 2, 1))
    shared["n2T"] = np.ascontiguousarray(g("norm2_g").reshape(DEPTH, 8, 128).transpose(0, 2, 1))
    shared["w_in"] = g("w_in")
    df, db = g("ret_decay_fwd"), g("ret_decay_bwd")
    decbc = np.concatenate([df, db], axis=1)
    shared["decbc"] = np.ascontiguousarray(np.broadcast_to(decbc[:, None, :], (DEPTH, 128, 8)))
    hp = np.zeros((DEPTH, 128, 4), f32)
    for dr, dd in enumerate((df, db)):
        for hb in range(2):
            hp[:, 0:64, dr * 2 + hb] = dd[:, 2 * hb][:, None]
            hp[:, 64:128, dr * 2 + hb] = dd[:, 2 * hb + 1][:, None]
    shared["dechp"] = hp
    shared["sgu_wT"] = np.ascontiguousarray(g("sgu_w").transpose(0, 1, 3, 2))
    shared["sgu_bbc"] = np.ascontiguousarray(np.broadcast_to(g("sgu_b").reshape(DEPTH, 1, 512), (DEPTH, 128, 512)))
    shared["qg"] = np.ascontiguousarray(np.broadcast_to(g("q_norm_g")[:, None, :], (DEPTH, 128, 64)))
    shared["kg"] = np.ascontiguousarray(np.broadcast_to(g("k_norm_g")[:, None, :], (DEPTH, 128, 64)))
    shared["sink"] = np.ascontiguousarray(np.broadcast_to(g("attn_sink")[:, None, :], (DEPTH, 128, 8)))
    for k in ("w_branch", "w_out", "w_router", "w_exp_gate", "w_exp_up", "w_exp_down"):
        shared[k] = g(k)
    shared["rope"] = _rope_tables()
    shared["ctab"] = _const_table()
    maps = []
    for b in range(B):
        m = dict(shared)
        m["x"] = x[b]
        m["ctxin"] = ctx[b]
        cT = np.stack([c[b].reshape(8, 128).T, c_ctx.reshape(8, 128).T], axis=-1)
        m["cT"] = np.ascontiguousarray(cT.reshape(128, 16))
        maps.append(m)
    return maps


_NC_CACHE = {}


def kernel(**inputs):
    maps = _host_inputs(inputs)
    if "nc" not in _NC_CACHE:
        _NC_CACHE["nc"] = build_program()
    nc = _NC_CACHE["nc"]
    n = len(maps)
    res = run_bass_kernel_spmd(nc, maps, core_ids=list(range(n)))
    return np.stack([np.asarray(r["out"], dtype=np.float32) for r in res.results], axis=0)
```
